# Optimizing a Trainium2 kernel written in Bass

```python
import jax
import jax.numpy as jnp
from jax import lax
import numpy as np


D_MODEL = 1024
BATCH = 2
SEQ = 8192
DEPTH = 4

MIX_DIM = D_MODEL
ATTN_DIM = D_MODEL // 2
ATTN_HEAD_DIM = 64
ATTN_HEADS = ATTN_DIM // ATTN_HEAD_DIM
DILATED_PATTERNS = ((128, 1), (512, 4), (2048, 16))
ATTN_BLOCK = 128
ALIBI_MAX_EXP = 8.0
HGRN_DIM = MIX_DIM - ATTN_DIM
HGRN_EXPAND = 128
HGRN_HEADS = HGRN_DIM // HGRN_EXPAND
HGRN_CHUNK = 64
IN_PROJ_DIM = 3 * ATTN_DIM + 4 * HGRN_DIM
N_GROUPS = 4
EXPERTS_PER_GROUP = 8
N_EXPERTS = N_GROUPS * EXPERTS_PER_GROUP
TOP_K = 2
D_EXPERT = D_MODEL // 2
MOE_BLOCK = 128
NORM_EPS = 1e-6

kernel_name = 'hybrid_dilated_attn_hgrn2_hmoe_adaln'


def rms_norm(x, g):
    xf = x.astype(jnp.float32)
    y = xf * lax.rsqrt(jnp.mean(xf * xf, axis=-1, keepdims=True) + NORM_EPS)
    return (y * g.astype(jnp.float32)).astype(x.dtype)


def alibi_slopes(n_heads):
    return jnp.exp2(-ALIBI_MAX_EXP * jnp.arange(1, n_heads + 1, dtype=jnp.float32) / n_heads)


def dilated_window_attention(q, k, v, window, dilation, slopes):
    B, H, S, E = q.shape
    steps = window // dilation
    span = dilation * ATTN_BLOCK
    s_pad = -(-S // span) * span
    L = s_pad // dilation
    nb = L // ATTN_BLOCK

    def strided_blocks(t):
        t = jnp.pad(t, ((0, 0), (0, 0), (0, s_pad - S), (0, 0)))
        t = t.reshape(B, H, L, dilation, E).transpose(0, 1, 3, 2, 4)
        return t.reshape(B, H, dilation, nb, ATTN_BLOCK, E)

    def with_prev(t):
        prev = jnp.pad(t[:, :, :, :-1], ((0, 0), (0, 0), (0, 0), (1, 0), (0, 0), (0, 0)))
        return jnp.concatenate([prev, t], axis=-2)

    qb = strided_blocks(q)
    kb = with_prev(strided_blocks(k))
    vb = with_prev(strided_blocks(v))
    s = jnp.einsum('bhrnqe,bhrnke->bhrnqk', qb, kb).astype(jnp.float32) * (E ** -0.5)
    qi = jnp.arange(ATTN_BLOCK)[:, None]
    kj = jnp.arange(2 * ATTN_BLOCK)[None, :]
    dist = ATTN_BLOCK + qi - kj
    blk = jnp.arange(nb)[:, None, None]
    valid = (dist >= 0) & (dist <= steps) & ((blk > 0) | (kj >= ATTN_BLOCK))
    bias = -slopes[:, None, None] * (dist * dilation).astype(jnp.float32)
    s = jnp.where(valid[None, None, None], s + bias[None, :, None, None], -jnp.inf)
    m = jnp.max(s, axis=-1, keepdims=True)
    p = jnp.exp(s - m)
    l = jnp.sum(p, axis=-1, keepdims=True)
    o = jnp.einsum('bhrnqk,bhrnke->bhrnqe', p, vb.astype(jnp.float32)) / l
    lse = (m + jnp.log(l))[..., 0]
    o = o.reshape(B, H, dilation, L, E).transpose(0, 1, 3, 2, 4).reshape(B, H, s_pad, E)[:, :, :S]
    lse = lse.reshape(B, H, dilation, L).transpose(0, 1, 3, 2).reshape(B, H, s_pad)[:, :, :S]
    return o, lse


def hgrn2_chunk_scan(q, k, v, log_f):
    B, H, S, DK = q.shape
    DV = v.shape[-1]
    nc = S // HGRN_CHUNK
    causal = jnp.tril(jnp.ones((HGRN_CHUNK, HGRN_CHUNK), dtype=bool))

    def to_chunks(t):
        return t.astype(jnp.float32).reshape(B, H, nc, HGRN_CHUNK, t.shape[-1]).transpose(2, 0, 1, 3, 4)

    def step(state, inp):
        qc, kc, vc, lc = inp
        b = jnp.cumsum(lc, axis=-2)
        o_inter = jnp.einsum('bhtk,bhkv->bhtv', qc * jnp.exp(b), state)
        diff = b[:, :, :, None, :] - b[:, :, None, :, :]
        decay = jnp.where(causal[:, :, None], jnp.exp(jnp.minimum(diff, 0.0)), 0.0)
        a = jnp.einsum('bhtk,bhsk,bhtsk->bhts', qc, kc, decay)
        o_intra = jnp.einsum('bhts,bhsv->bhtv', a, vc)
        b_last = b[:, :, -1:, :]
        new_state = (jnp.exp(b_last[:, :, 0, :])[..., None] * state
                     + jnp.einsum('bhsk,bhsv->bhkv', kc * jnp.exp(b_last - b), vc))
        return new_state, o_inter + o_intra

    state0 = jnp.zeros((B, H, DK, DV), jnp.float32)
    _, o = lax.scan(step, state0, (to_chunks(q), to_chunks(k), to_chunks(v), to_chunks(log_f)))
    return o.transpose(1, 2, 0, 3, 4).reshape(B, H, S, DV)


def hybrid_mixer(h, w_in, attn_norm_g, lb, hgrn_norm_g, w_out):
    B, S, _ = h.shape
    proj = h @ w_in
    cuts = [ATTN_DIM, 2 * ATTN_DIM, 3 * ATTN_DIM, 3 * ATTN_DIM + HGRN_DIM,
            3 * ATTN_DIM + 2 * HGRN_DIM, 3 * ATTN_DIM + 3 * HGRN_DIM]
    a_q, a_k, a_v, r_q, r_f, r_i, r_g = jnp.split(proj, cuts, axis=-1)

    def heads(t, n, dh):
        return t.reshape(B, S, n, dh).transpose(0, 2, 1, 3)

    slopes = alibi_slopes(ATTN_HEADS)
    qh = heads(a_q, ATTN_HEADS, ATTN_HEAD_DIM)
    kh = heads(a_k, ATTN_HEADS, ATTN_HEAD_DIM)
    vh = heads(a_v, ATTN_HEADS, ATTN_HEAD_DIM)
    outs = []
    lses = []
    for window, dilation in DILATED_PATTERNS:
        o, lse = dilated_window_attention(qh, kh, vh, window, dilation, slopes)
        outs.append(o)
        lses.append(lse)
    wts = jax.nn.softmax(jnp.stack(lses), axis=0)
    attn = jnp.sum(wts[..., None] * jnp.stack(outs), axis=0)
    attn = attn.transpose(0, 2, 1, 3).reshape(B, S, ATTN_DIM)
    attn = rms_norm(attn, attn_norm_g).astype(h.dtype)

    z = heads(r_f, HGRN_HEADS, HGRN_EXPAND).astype(jnp.float32)
    lb_h = jnp.maximum(lb.astype(jnp.float32), 0.0).reshape(HGRN_HEADS, 1, HGRN_EXPAND)
    log_f = jnp.logaddexp(jnp.log(lb_h), jnp.log1p(-lb_h) + jax.nn.log_sigmoid(z))
    k_in = (1.0 - lb_h) * jax.nn.sigmoid(-z)
    rec = hgrn2_chunk_scan(heads(r_q, HGRN_HEADS, HGRN_EXPAND), k_in,
                           heads(r_i, HGRN_HEADS, HGRN_EXPAND), log_f)
    rec = rms_norm(rec.transpose(0, 2, 1, 3), hgrn_norm_g.reshape(HGRN_HEADS, HGRN_EXPAND)).reshape(B, S, HGRN_DIM)
    rec = (rec * jax.nn.silu(r_g.astype(jnp.float32))).astype(h.dtype)

    return jnp.concatenate([attn, rec], axis=-1) @ w_out


def hierarchical_moe(h, wg, bg, we, be, w1, w3, w2):
    B, S, D = h.shape
    n_tok = B * S
    xf = h.reshape(n_tok, D)
    group_logits = (xf @ wg + bg).astype(jnp.float32)
    group_idx = jnp.argmax(group_logits, axis=-1)
    group_prob = jnp.take_along_axis(jax.nn.softmax(group_logits, axis=-1), group_idx[:, None], axis=-1)
    expert_logits = (xf @ we + be).astype(jnp.float32).reshape(n_tok, N_GROUPS, EXPERTS_PER_GROUP)
    local_logits = jnp.take_along_axis(expert_logits, group_idx[:, None, None], axis=1)[:, 0]
    top_val, top_idx = lax.top_k(local_logits, TOP_K)
    gate = jax.nn.softmax(top_val, axis=-1) * group_prob
    expert_id = (group_idx[:, None] * EXPERTS_PER_GROUP + top_idx).reshape(-1).astype(jnp.int32)
    gate_flat = gate.reshape(-1)
    token_id = jnp.repeat(jnp.arange(n_tok, dtype=jnp.int32), TOP_K)
    n_assign = n_tok * TOP_K
    order = jnp.argsort(expert_id)
    sorted_e = expert_id[order]
    counts = jnp.bincount(expert_id, length=N_EXPERTS)
    padded = (counts + MOE_BLOCK - 1) // MOE_BLOCK * MOE_BLOCK
    start = jnp.cumsum(counts) - counts
    pend = jnp.cumsum(padded)
    pstart = pend - padded
    dest = pstart[sorted_e] + jnp.arange(n_assign, dtype=jnp.int32) - start[sorted_e]
    n_rows = -(-n_assign // MOE_BLOCK) * MOE_BLOCK + N_EXPERTS * MOE_BLOCK
    n_blocks = n_rows // MOE_BLOCK
    row_token = jnp.full((n_rows,), n_tok, jnp.int32).at[dest].set(token_id[order])
    row_gate = jnp.zeros((n_rows,), jnp.float32).at[dest].set(gate_flat[order])
    block_expert = jnp.minimum(
        jnp.searchsorted(pend, jnp.arange(n_blocks, dtype=pend.dtype) * MOE_BLOCK, side='right'), N_EXPERTS - 1)
    x_rows = jnp.concatenate([xf, jnp.zeros((1, D), xf.dtype)], axis=0)[row_token].reshape(n_blocks, MOE_BLOCK, D)

    def expert_block(args):
        xb, e = args
        return (jax.nn.silu(xb @ w1[e]) * (xb @ w3[e])) @ w2[e]

    y_rows = lax.map(expert_block, (x_rows, block_expert)).reshape(n_rows, D)
    y = jax.ops.segment_sum(y_rows.astype(jnp.float32) * row_gate[:, None], row_token,
                            num_segments=n_tok + 1)[:n_tok]
    return y.astype(h.dtype).reshape(B, S, D)


def setup_inputs(seed: int = 0) -> dict:
    key = jax.random.key(seed)
    ks = jax.random.split(key, 20)

    def nrm(k, shape, scale):
        return jax.random.normal(k, shape, jnp.float32) * scale

    return {
        'x': nrm(ks[0], (BATCH, SEQ, D_MODEL), 1.0),
        'c': nrm(ks[1], (BATCH, D_MODEL), 1.0),
        'w_ada': nrm(ks[2], (DEPTH, D_MODEL, 6 * D_MODEL), 0.5 * D_MODEL ** -0.5),
        'b_ada': nrm(ks[3], (DEPTH, 6 * D_MODEL), 0.02),
        'norm1_g': 1.0 + nrm(ks[4], (DEPTH, D_MODEL), 0.02),
        'w_in': nrm(ks[5], (DEPTH, D_MODEL, IN_PROJ_DIM), D_MODEL ** -0.5),
        'attn_norm_g': 1.0 + nrm(ks[6], (DEPTH, ATTN_DIM), 0.02),
        'hgrn_lb_logits': nrm(ks[7], (DEPTH, HGRN_DIM), 0.1),
        'hgrn_norm_g': 1.0 + nrm(ks[8], (DEPTH, HGRN_DIM), 0.02),
        'w_out': nrm(ks[9], (DEPTH, MIX_DIM, D_MODEL), MIX_DIM ** -0.5),
        'norm2_g': 1.0 + nrm(ks[10], (DEPTH, D_MODEL), 0.02),
        'router_group_w': nrm(ks[11], (DEPTH, D_MODEL, N_GROUPS), D_MODEL ** -0.5),
        'router_group_b': nrm(ks[12], (DEPTH, N_GROUPS), 0.01),
        'router_expert_w': nrm(ks[13], (DEPTH, D_MODEL, N_EXPERTS), D_MODEL ** -0.5),
        'router_expert_b': nrm(ks[14], (DEPTH, N_EXPERTS), 0.01),
        'moe_w1': nrm(ks[15], (DEPTH, N_EXPERTS, D_MODEL, D_EXPERT), D_MODEL ** -0.5),
        'moe_w3': nrm(ks[16], (DEPTH, N_EXPERTS, D_MODEL, D_EXPERT), D_MODEL ** -0.5),
        'moe_w2': nrm(ks[17], (DEPTH, N_EXPERTS, D_EXPERT, D_MODEL), D_EXPERT ** -0.5),
        'final_g': 1.0 + nrm(ks[18], (D_MODEL,), 0.02),
    }


def reference(x, c, w_ada, b_ada, norm1_g, w_in, attn_norm_g, hgrn_lb_logits, hgrn_norm_g, w_out,
              norm2_g, router_group_w, router_group_b, router_expert_w, router_expert_b,
              moe_w1, moe_w3, moe_w2, final_g):
    lb_w = jax.nn.softmax(hgrn_lb_logits.astype(jnp.float32), axis=0)
    lower_bounds = jnp.cumsum(lb_w, axis=0) - lb_w[0:1]
    c_act = jax.nn.silu(c)
    for layer in range(DEPTH):
        mod = c_act @ w_ada[layer] + b_ada[layer]
        shift1, scale1, gate1, shift2, scale2, gate2 = jnp.split(mod[:, None, :], 6, axis=-1)
        h = rms_norm(x, norm1_g[layer]) * (1.0 + scale1) + shift1
        x = x + gate1 * hybrid_mixer(h, w_in[layer], attn_norm_g[layer], lower_bounds[layer],
                                     hgrn_norm_g[layer], w_out[layer])
        h = rms_norm(x, norm2_g[layer]) * (1.0 + scale2) + shift2
        x = x + gate2 * hierarchical_moe(h, router_group_w[layer], router_group_b[layer],
                                         router_expert_w[layer], router_expert_b[layer],
                                         moe_w1[layer], moe_w3[layer], moe_w2[layer])
    return rms_norm(x, final_g)
```

```python
import contextlib
import numpy as np
import ml_dtypes
import concourse.bass as bass
import concourse.mybir as mybir
from concourse.bass_utils import run_bass_kernel_spmd

F32 = mybir.dt.float32
BF16 = mybir.dt.bfloat16
I32 = mybir.dt.int32
ALU = mybir.AluOpType
AF = mybir.ActivationFunctionType
AX = mybir.AxisListType

T = 8192
D = 1024
NT = T // 128
DEPTH = 4
NE = 32
RB = 256
NBLK = (2 * T) // RB + NE
NROWS = NBLK * RB
EPS = 1e-6
BIG = 30000.0
PATTERNS = ((128, 1), (512, 4), (2048, 16))
EP_ENG = 30000
EP_DMA = 3000


class Res:
    __slots__ = ("w", "r")

    def __init__(self):
        self.w = None
        self.r = []


class MRes(Res):
    __slots__ = ("ws",)

    def __init__(self):
        super().__init__()
        self.ws = []


def _compress(lst):
    mx = {}
    for (pq, c) in lst:
        mx[pq] = max(mx.get(pq, 0), c)
    return list(mx.items())


class Q:
    def __init__(self, nc, es, name, inc, ep):
        self.nc, self.es, self.name, self.inc, self.ep = nc, es, name, inc, ep
        self.sems = []
        self.count = 0
        self.ops = []
        self.seen = {}

    def sem_for(self, c):
        i = (c - 1) // self.ep
        while len(self.sems) <= i:
            self.sems.append(self.es.enter_context(self.nc.semaphore(f"{self.name}_{len(self.sems)}")))
        return self.sems[i], (((c - 1) % self.ep) + 1) * self.inc


class Sched:
    def __init__(self, nc, ndma=6):
        self.nc = nc
        self.es = contextlib.ExitStack()
        self.q = {n: Q(nc, self.es, "s" + n, 1, EP_ENG) for n in ("pe", "act", "dve", "pool", "sp")}
        self.dslots = {n: [Q(nc, self.es, f"d{n}{i}", 16, EP_DMA) for i in range(ndma)] for n in ("sp", "pool", "act")}
        self.dnext = {n: 0 for n in self.dslots}

    def _deps(self, reads, writes):
        need = {}
        for r in reads:
            if isinstance(r, MRes):
                for (pq, c) in r.ws:
                    need[pq] = max(need.get(pq, 0), c)
            elif r.w is not None:
                need[r.w[0]] = max(need.get(r.w[0], 0), r.w[1])
        for w in writes:
            if (not isinstance(w, MRes)) and w.w is not None:
                need[w.w[0]] = max(need.get(w.w[0], 0), w.w[1])
            for (pq, c) in w.r:
                need[pq] = max(need.get(pq, 0), c)
        return need

    def _commit(self, tok, reads, writes):
        for r in reads:
            r.r.append(tok)
            if len(r.r) > 48:
                r.r = _compress(r.r)
        for w in writes:
            if isinstance(w, MRes):
                w.ws.append(tok)
                if len(w.ws) > 48:
                    w.ws = _compress(w.ws)
            else:
                w.w = tok
                w.r = []

    def _waits(self, q, need):
        waits = []
        for pq, c in need.items():
            if q.seen.get(pq, 0) >= c:
                continue
            q.seen[pq] = c
            waits.append((pq, c))
        return waits

    def op(self, qn, fn, reads=(), writes=()):
        q = self.q[qn]
        waits = self._waits(q, self._deps(reads, writes))
        q.count += 1
        tok = (q, q.count)
        q.ops.append((waits, fn, tok))
        self._commit(tok, reads, writes)
        return tok

    def dma(self, qn, fn, reads=(), writes=()):
        q = self.q[qn]
        sl = self.dslots[qn]
        dq = sl[self.dnext[qn] % len(sl)]
        self.dnext[qn] += 1
        need = self._deps(reads, writes)
        if dq.count > 0:
            need[dq] = max(need.get(dq, 0), dq.count)
        waits = self._waits(q, need)
        dq.count += 1
        tok = (dq, dq.count)
        q.ops.append((waits, fn, tok))
        self._commit(tok, reads, writes)
        return tok

    def wait_all(self, qn, toks):
        q = self.q[qn]
        need = {}
        for (pq, c) in toks:
            need[pq] = max(need.get(pq, 0), c)
        q.ops.append((self._waits(q, need), None, None))

    def flush(self):
        nc = self.nc
        if not any(q.ops for q in self.q.values()):
            return
        with nc.Block() as block:
            def mk(qn):
                q = self.q[qn]

                def body(e):
                    for waits, fn, tok in q.ops:
                        for (pq, c) in waits:
                            s, v = pq.sem_for(c)
                            e.wait_ge(s, v)
                        if fn is not None:
                            s, _ = tok[0].sem_for(tok[1])
                            fn(e).then_inc(s, tok[0].inc)
                    q.ops = []
                return body
            block.tensor(mk("pe"))
            block.scalar(mk("act"))
            block.vector(mk("dve"))
            block.gpsimd(mk("pool"))
            block.sync(mk("sp"))


class Ring:
    def __init__(self, items):
        self.items = items
        self.i = 0

    def next(self):
        it = self.items[self.i % len(self.items)]
        self.i += 1
        return it


_UID = [0]


class Ctx:
    def __init__(self, S):
        self.S = S
        self.es = contextlib.ExitStack()
        self.n = 0

    def sb(self, shape, dt, name=None):
        _UID[0] += 1
        t = self.es.enter_context(self.S.nc.sbuf_tensor(f"{name or 't'}_{_UID[0]}", list(shape), dt))
        return t, Res()

    def ps(self, shape, dt, name=None):
        _UID[0] += 1
        t = self.es.enter_context(self.S.nc.psum_tensor(f"{name or 'p'}_{_UID[0]}", list(shape), dt))
        return t, Res()

    def ring(self, n, shape, dt, psum=False, name=None):
        return Ring([(self.ps if psum else self.sb)(shape, dt, name) for _ in range(n)])

    def close(self):
        self.S.flush()
        self.es.close()


def host_consts():
    c = {}
    c["ident"] = np.eye(128, dtype=np.float32).astype(ml_dtypes.bfloat16)
    kk = np.arange(128)[:, None]
    qq = np.arange(128)[None, :]
    mm = np.zeros((128, 8, 3, 256), np.float32)
    for h in range(8):
        slope = 2.0 ** (-(h + 1))
        for p, (w, d) in enumerate(PATTERNS):
            steps = w // d
            dist_cur = qq - kk
            dist_nxt = 128 + qq - kk
            for j, dist in enumerate((dist_cur, dist_nxt)):
                valid = (dist >= 0) & (dist <= steps)
                mm[:, h, p, j * 128:(j + 1) * 128] = np.where(valid, -slope * d * dist, -BIG)
    mm2 = np.concatenate([mm[..., 128:256], mm[..., 0:128]], axis=-1)
    c["amask"] = np.exp(mm2.astype(np.float64)).astype(np.float32).reshape(128, 8 * 3 * 256).astype(ml_dtypes.bfloat16)
    c["causal"] = ((kk <= qq) & ((kk // 64) == (qq // 64))).astype(np.uint8)
    c["ustrict"] = (kk < qq).astype(np.float32).astype(ml_dtypes.bfloat16)
    rm = np.ones((128, 2048), np.float32)
    rm[:, ::128] = 0.0
    c["resetm"] = rm
    c["mulrow"] = np.tile((np.arange(64, dtype=np.float32) * RB)[None, :], (128, 1))
    c["brow"] = np.tile(np.arange(NBLK, dtype=np.float32)[None, :], (128, 1))
    c["piota"] = np.arange(128, dtype=np.float32).reshape(128, 1)
    return c


CONST_SPECS = {"ident": ([128, 128], BF16), "amask": ([128, 8 * 3 * 256], BF16), "causal": ([128, 128], mybir.dt.uint8),
               "ustrict": ([128, 128], BF16), "resetm": ([128, 2048], F32), "mulrow": ([128, 64], F32), "brow": ([128, NBLK], F32), "piota": ([128, 1], F32)}

IN_SPECS = {
    "x": ([T, D], F32), "c": ([1, D], F32), "w_ada": ([DEPTH, D, 6 * D], F32), "b_ada": ([DEPTH, 6 * D], F32),
    "norm1_g": ([DEPTH, D], F32), "w_in": ([DEPTH, D, 3584], F32), "attn_norm_g": ([DEPTH, 512], F32),
    "hgrn_lb_logits": ([DEPTH, 512], F32), "hgrn_norm_g": ([DEPTH, 512], F32), "w_out": ([DEPTH, D, D], F32),
    "norm2_g": ([DEPTH, D], F32), "router_w": ([DEPTH, D, 36], F32), "router_b": ([DEPTH, 36], F32),
    "moe_w1_0": ([DEPTH * NE * 128, 2048], F32), "moe_w1_1": ([DEPTH * NE * 128, 2048], F32),
    "moe_w3_0": ([DEPTH * NE * 128, 2048], F32), "moe_w3_1": ([DEPTH * NE * 128, 2048], F32),
    "moe_w2_0": ([DEPTH * NE * 128, 2048], F32), "moe_w2_1": ([DEPTH * NE * 128, 2048], F32),
    "final_g": ([1, D], F32),
}


def build(depth=DEPTH, stop=None, dumps=(), final=True):
    nc = bass.Bass("TRN2", target_bir_lowering=False)
    S = Sched(nc)
    dr = {}
    for k, (shp, dt) in list(IN_SPECS.items()) + list(CONST_SPECS.items()):
        dr[k] = nc.dram_tensor(k, shp, dt, kind="ExternalInput").ap()
    out = nc.dram_tensor("out", [T, D], F32, kind="ExternalOutput").ap()
    rs = {}

    def scratch(name, shp, dt):
        kind = "ExternalOutput" if name in dumps else "Internal"
        dr[name] = nc.dram_tensor(name, shp, dt, kind=kind).ap()
        rs[name] = MRes()
    scratch("Xs", [T, D], F32)
    scratch("QT", [512, T], BF16)
    scratch("KT", [512, T], BF16)
    scratch("VV", [T, 512], BF16)
    scratch("RQT", [512, T], BF16)
    scratch("ZT", [512, T], F32)
    scratch("RI", [T, 512], BF16)
    scratch("RG", [T, 512], BF16)
    for p in range(3):
        scratch(f"OP{p}", [T, 2, 4 * 65], F32)
    scratch("CAT", [T, D], BF16)
    scratch("H2", [T, D], BF16)
    scratch("XS", [NROWS, D], BF16)
    scratch("YS", [NROWS, D], F32)
    rs["x"] = MRes()
    rs["out"] = MRes()

    G = Ctx(S)
    ident, r_ident = G.sb([128, 128], BF16, "ident")
    causal, r_causal = G.sb([128, 128], mybir.dt.uint8, "causal")
    ustrict, r_ustrict = G.sb([128, 128], BF16, "ustrict")
    onesb, r_onesb = G.sb([128, 128], BF16, "onesb")
    mulrow, r_mulrow = G.sb([128, 64], F32, "mulrow")
    brow, r_brow = G.sb([128, NBLK], F32, "brow")
    piota, r_piota = G.sb([128, 1], F32, "piota")
    widx, r_widx = G.sb([128, NBLK], I32, "widx")
    one11, r_one11 = G.sb([1, 1], F32, "one11")
    onesrow, r_onesrow = G.sb([1, 128], F32, "onesrow")
    colf, r_colf = G.sb([128, 16], F32, "colf")
    G1b, r_G1b = G.sb([128, D], F32, "G1b")
    A2b, r_A2b = G.sb([128, D], F32, "A2b")
    S2b, r_S2b = G.sb([128, D], F32, "S2b")
    G2b, r_G2b = G.sb([128, D], F32, "G2b")
    lbc, r_lbc = G.sb([128, DEPTH, 4], F32, "lbc")
    oml, r_oml = G.sb([128, DEPTH, 4], F32, "oml")
    noml, r_noml = G.sb([128, DEPTH, 4], F32, "noml")
    slot1, r_slot1 = G.sb([128, NT], I32, "slot1")
    slot2, r_slot2 = G.sb([128, NT], I32, "slot2")
    gt1, r_gt1 = G.sb([128, NT], F32, "gt1")
    gt2, r_gt2 = G.sb([128, NT], F32, "gt2")
    lgall, r_lgall = G.sb([128, NT, 36], F32, "lgall")

    dma_rr = [0]

    def ld(out_, in_, reads=(), writes=(), q=None):
        if q is None:
            q = "sp"
        return S.dma(q, lambda e: e.dma_start(out=out_, in_=in_), reads, writes)

    def V(fn, r=(), w=()):
        return S.op("dve", fn, r, w)

    def A(fn, r=(), w=()):
        return S.op("act", fn, r, w)

    def PE(fn, r=(), w=()):
        return S.op("pe", fn, r, w)

    def GP(fn, r=(), w=()):
        return S.op("pool", fn, r, w)

    def setup():
        ld(ident[:], dr["ident"], (), [r_ident], "sp")
        ld(causal[:], dr["causal"], (), [r_causal], "sp")
        ld(ustrict[:], dr["ustrict"], (), [r_ustrict], "sp")
        ld(mulrow[:], dr["mulrow"], (), [r_mulrow], "sp")
        ld(brow[:], dr["brow"], (), [r_brow], "sp")
        ld(piota[:], dr["piota"], (), [r_piota], "sp")
        V(lambda e: e.memset(one11[:], 1.0), (), [r_one11])
        V(lambda e: e.memset(onesrow[:], 1.0), (), [r_onesrow])
        V(lambda e: e.memset(onesb[:], 1.0), (), [r_onesb])
        c = Ctx(S)
        lg, r_lg = c.sb([128, DEPTH, 4], F32)
        ex, r_ex = c.sb([128, DEPTH, 4], F32)
        sm, r_sm = c.sb([128, 4], F32)
        S.dma("sp", lambda e: e.dma_start(out=lg[:], in_=dr["hgrn_lb_logits"].rearrange("l (h k) -> k l h", k=128),
                                          allow_slow_non_contiguous=True), (), [r_lg])
        A(lambda e: e.activation(out=ex[:], in_=lg[:], func=AF.Exp), [r_lg], [r_ex])
        V(lambda e: e.tensor_tensor(out=sm[:], in0=ex[:, 0, :], in1=ex[:, 1, :], op=ALU.add), [r_ex], [r_sm])
        V(lambda e: e.tensor_tensor(out=sm[:], in0=sm[:], in1=ex[:, 2, :], op=ALU.add), [r_ex, r_sm], [r_sm])
        V(lambda e: e.tensor_tensor(out=sm[:], in0=sm[:], in1=ex[:, 3, :], op=ALU.add), [r_ex, r_sm], [r_sm])
        V(lambda e: e.reciprocal(out=sm[:], in_=sm[:]), [r_sm], [r_sm])
        V(lambda e: e.memset(lbc[:, 0, :], 0.0), (), [r_lbc])
        V(lambda e: e.tensor_tensor(out=lbc[:, 1, :], in0=ex[:, 1, :], in1=sm[:], op=ALU.mult), [r_ex, r_sm], [r_lbc])
        for l in (2, 3):
            V(lambda e, l=l: e.tensor_tensor(out=ex[:, l, :], in0=ex[:, l, :], in1=sm[:], op=ALU.mult), [r_ex, r_sm], [r_ex])
            V(lambda e, l=l: e.tensor_tensor(out=lbc[:, l, :], in0=lbc[:, l - 1, :], in1=ex[:, l, :], op=ALU.add), [r_ex, r_lbc], [r_lbc])
        V(lambda e: e.tensor_scalar(out=oml[:], in0=lbc[:], scalar1=-1.0, scalar2=1.0, op0=ALU.mult, op1=ALU.add), [r_lbc], [r_oml])
        V(lambda e: e.tensor_scalar(out=noml[:], in0=lbc[:], scalar1=1.0, scalar2=-1.0, op0=ALU.mult, op1=ALU.add), [r_lbc], [r_noml])
        c.close()

    def p0(l):
        c = Ctx(S)
        crow, r_crow = c.sb([1, D], F32)
        cactc, r_cactc = c.sb([128, 8], F32)
        modrow, r_mod = c.sb([1, 6 * D], F32)
        brow, r_brow = c.sb([1, 6 * D], F32)
        g1row, r_g1 = c.sb([1, D], F32)
        g2row, r_g2 = c.sb([1, D], F32)
        wring = c.ring(2, [128, 8, 512], F32)
        pcol, r_pcol = c.ps([128, 16], F32)
        pacc = c.ring(2, [128, 512], F32, psum=True)
        ld(crow[:], dr["c"], (), [r_crow], "sp")
        ld(brow[:], dr["b_ada"][l:l + 1, :], (), [r_brow], "sp")
        ld(g1row[:], dr["norm1_g"][l:l + 1, :], (), [r_g1], "sp")
        ld(g2row[:], dr["norm2_g"][l:l + 1, :], (), [r_g2], "sp")
        A(lambda e: e.activation(out=crow[:], in_=crow[:], func=AF.Silu), [r_crow], [r_crow])
        for kc in range(8):
            PE(lambda e, kc=kc: e.matmul(pcol[:, kc:kc + 1], lhsT=crow[0:1, kc * 128:(kc + 1) * 128], rhs=one11[0:1, 0:1],
                                         start=True, stop=True), [r_crow, r_one11], [r_pcol])
        V(lambda e: e.tensor_copy(out=cactc[:], in_=pcol[:, 0:8]), [r_pcol], [r_cactc])
        for n in range(12):
            wt, r_wt = wring.next()
            ld(wt[:], dr["w_ada"][l, :, n * 512:(n + 1) * 512].rearrange("(kc p) n -> p kc n", p=128), (), [r_wt])
            acc, r_acc = pacc.next()
            for kc in range(8):
                PE(lambda e, kc=kc, acc=acc, wt=wt: e.matmul(acc[0:1, :], lhsT=cactc[:, kc:kc + 1], rhs=wt[:, kc, :],
                                                            start=(kc == 0), stop=(kc == 7)), [r_cactc, r_wt], [r_acc])
            V(lambda e, n=n, acc=acc: e.tensor_tensor(out=modrow[0:1, n * 512:(n + 1) * 512], in0=acc[0:1, :],
                                                       in1=brow[0:1, n * 512:(n + 1) * 512], op=ALU.add), [r_acc, r_brow], [r_mod])
        V(lambda e: e.scalar_tensor_tensor(out=g1row[:], in0=modrow[0:1, D:2 * D], scalar=1.0, in1=g1row[:], op0=ALU.add, op1=ALU.mult),
          [r_mod, r_g1], [r_g1])
        V(lambda e: e.scalar_tensor_tensor(out=g2row[:], in0=modrow[0:1, 4 * D:5 * D], scalar=1.0, in1=g2row[:], op0=ALU.add, op1=ALU.mult),
          [r_mod, r_g2], [r_g2])
        for kc in range(8):
            PE(lambda e, kc=kc: e.matmul(pcol[:, kc:kc + 1], lhsT=g1row[0:1, kc * 128:(kc + 1) * 128], rhs=one11[0:1, 0:1],
                                         start=True, stop=True), [r_g1, r_one11], [r_pcol])
            PE(lambda e, kc=kc: e.matmul(pcol[:, 8 + kc:9 + kc], lhsT=modrow[0:1, kc * 128:(kc + 1) * 128], rhs=one11[0:1, 0:1],
                                         start=True, stop=True), [r_mod, r_one11], [r_pcol])
        V(lambda e: e.tensor_copy(out=colf[:], in_=pcol[:]), [r_pcol], [r_colf])
        for (src, r_src, off, dst, r_dst) in ((modrow, r_mod, 2 * D, G1b, r_G1b), (g2row, r_g2, 0, A2b, r_A2b),
                                              (modrow, r_mod, 3 * D, S2b, r_S2b), (modrow, r_mod, 5 * D, G2b, r_G2b)):
            for nch in range(2):
                acc, r_acc = pacc.next()
                PE(lambda e, acc=acc, src=src, off=off, nch=nch: e.matmul(acc[:, :], lhsT=onesrow[0:1, :],
                                                                         rhs=src[0:1, off + nch * 512:off + (nch + 1) * 512],
                                                                         start=True, stop=True), [r_src, r_onesrow], [r_acc])
                V(lambda e, acc=acc, dst=dst, nch=nch: e.tensor_copy(out=dst[:, nch * 512:(nch + 1) * 512], in_=acc[:, :]), [r_acc], [r_dst])
        c.close()

    def rstd_from_ss(ss_ap, out_ap, n, r_ss, r_out):
        V(lambda e: e.tensor_scalar(out=out_ap, in0=ss_ap, scalar1=1.0 / n, scalar2=EPS, op0=ALU.mult, op1=ALU.add), [r_ss], [r_out])
        A(lambda e: e.activation(out=out_ap, in_=out_ap, func=AF.Sqrt), [r_out], [r_out])
        V(lambda e: e.reciprocal(out=out_ap, in_=out_ap), [r_out], [r_out])

    def p1(l, xin, r_xin):
        c = Ctx(S)
        winb, r_winb = c.sb([128, 8, 3584], BF16, "winb")
        wst = c.ring(3, [128, 1792], F32, name="wst")
        kk_ = 0
        for kc in range(8):
            for hf in range(2):
                st, r_st = wst.next()
                ld(st[:], dr["w_in"][l, kc * 128:(kc + 1) * 128, hf * 1792:(hf + 1) * 1792], (), [r_st])
                kk_ += 1
                if kk_ % 2 == 0:
                    V(lambda e, st=st, kc=kc, hf=hf: e.tensor_copy(out=winb[:, kc, hf * 1792:(hf + 1) * 1792], in_=st[:]), [r_st], [r_winb])
                else:
                    GP(lambda e, st=st, kc=kc, hf=hf: e.tensor_copy(out=winb[:, kc, hf * 1792:(hf + 1) * 1792], in_=st[:]), [r_st], [r_winb])
        xring = c.ring(6, [128, D], F32, name="xt")
        junk, r_junk = c.sb([128, D], BF16, "junk")
        xnring = c.ring(6, [128, D], BF16, name="xn")
        ss, r_ss = c.sb([128, NT], F32, "ss")
        rstd, r_rstd = c.sb([128, NT], F32, "rstd")
        hTring = c.ring(2, [128, 8, 512], BF16, name="hT")
        ptr = c.ring(2, [128, 8, 128], BF16, psum=True, name="ptr")
        pacc = c.ring(6, [128, 512], F32, psum=True, name="pacc")
        stb = c.ring(6, [128, 512], BF16, name="stb")
        stf = c.ring(4, [128, 512], F32, name="stf")
        V(lambda e: e.memset(ss[:], 0.0), (), [r_ss])
        fm = []
        for m in range(4):
            fm.append((m * 128, "QT", m * 128, "q"))
        for m in range(4):
            fm.append((512 + m * 128, "KT", m * 128, "b"))
        for m in range(4):
            fm.append((1536 + m * 128, "RQT", m * 128, "b"))
        for m in range(4):
            fm.append((2048 + m * 128, "ZT", m * 128, "f"))
        tm = [(1024, "VV", "b"), (2560, "RI", "b"), (3072, "RG", "s")]
        ev = [0]

        def prep(g):
            hT, r_hT = hTring.next()
            tiles = []
            for j in range(4):
                t = g * 4 + j
                xt, r_xt = xring.next()
                ld(xt[:], xin[t * 128:(t + 1) * 128, :], [r_xin], [r_xt])
                A(lambda e, xt=xt, t=t: e.activation(out=junk[:], in_=xt[:], func=AF.Square, accum_out=ss[:, t:t + 1]), [r_xt], [r_junk, r_ss])
                rstd_from_ss(ss[:, t:t + 1], rstd[:, t:t + 1], D, r_ss, r_rstd)
                xn, r_xn = xnring.next()
                A(lambda e, xt=xt, xn=xn, t=t: e.activation(out=xn[:], in_=xt[:], func=AF.Copy, scale=rstd[:, t:t + 1]), [r_xt, r_rstd], [r_xn])
                tiles.append((xn, r_xn))
            for jp in range(2):
                pair = [(tiles[2 * jp + i], ptr.next()) for i in range(2)]
                for kc in range(8):
                    for ((xn, r_xn), (pt, r_pt)) in pair:
                        PE(lambda e, kc=kc, pt=pt, xn=xn: e.transpose(out=pt[:, kc, :], in_=xn[:, kc * 128:(kc + 1) * 128], identity=ident[:]),
                           [r_xn, r_ident], [r_pt])
                for i, ((xn, r_xn), (pt, r_pt)) in enumerate(pair):
                    j = 2 * jp + i
                    V(lambda e, pt=pt, hT=hT, j=j: e.tensor_tensor(out=hT[:, :, j * 128:(j + 1) * 128], in0=pt[:, :, :],
                                                                  in1=colf[:, 0:8].unsqueeze(2).to_broadcast([128, 8, 128]), op=ALU.mult),
                      [r_pt, r_colf], [r_hT])
                    GP(lambda e, hT=hT, j=j: e.tensor_tensor(out=hT[:, :, j * 128:(j + 1) * 128], in0=hT[:, :, j * 128:(j + 1) * 128],
                                                            in1=colf[:, 8:16].unsqueeze(2).to_broadcast([128, 8, 128]), op=ALU.add),
                       [r_hT, r_colf], [r_hT])
            return hT, r_hT

        def evac(kind, acc, r_acc):
            st, r_st = (stf if kind == "f" else stb).next()
            ev[0] += 1
            if kind == "q":
                A(lambda e: e.activation(out=st[:], in_=acc[:], func=AF.Copy, scale=0.125), [r_acc], [r_st])
            elif kind == "s":
                A(lambda e: e.activation(out=st[:], in_=acc[:], func=AF.Silu), [r_acc], [r_st])
            elif ev[0] % 2 == 0:
                A(lambda e: e.activation(out=st[:], in_=acc[:], func=AF.Copy), [r_acc], [r_st])
            else:
                V(lambda e: e.tensor_copy(out=st[:], in_=acc[:]), [r_acc], [r_st])
            return st, r_st

        nxt = prep(0)
        for g in range(NT // 4):
            hT, r_hT = nxt
            if g + 1 < NT // 4:
                nxt = prep(g + 1)
            for f0 in range(0, 16, 4):
                grp = [(fm[f0 + i], pacc.next()) for i in range(4)]
                for kc in range(8):
                    for ((c0, dn, row0, kind), (acc, r_acc)) in grp:
                        PE(lambda e, kc=kc, acc=acc, hT=hT, c0=c0: e.matmul(acc[:, :], lhsT=winb[:, kc, c0:c0 + 128], rhs=hT[:, kc, :],
                                                                           start=(kc == 0), stop=(kc == 7)), [r_winb, r_hT], [r_acc])
                for ((c0, dn, row0, kind), (acc, r_acc)) in grp:
                    st, r_st = evac(kind, acc, r_acc)
                    ld(dr[dn][row0:row0 + 128, g * 512:(g + 1) * 512], st[:], [r_st], [rs[dn]])
            for j in range(4):
                t = g * 4 + j
                grp = [(tm[i], pacc.next()) for i in range(3)]
                for kc in range(8):
                    for ((c0, dn, kind), (acc, r_acc)) in grp:
                        PE(lambda e, kc=kc, acc=acc, hT=hT, c0=c0, j=j: e.matmul(acc[:, :], lhsT=hT[:, kc, j * 128:(j + 1) * 128],
                                                                                 rhs=winb[:, kc, c0:c0 + 512], start=(kc == 0), stop=(kc == 7)),
                           [r_winb, r_hT], [r_acc])
                for ((c0, dn, kind), (acc, r_acc)) in grp:
                    st, r_st = evac(kind, acc, r_acc)
                    ld(dr[dn][t * 128:(t + 1) * 128, :], st[:], [r_st], [rs[dn]])
        c.close()

    def p2(l):
        c = Ctx(S)
        qT2, r_qT2 = c.sb([128, 2, T], BF16, "qT2")
        kT2, r_kT2 = c.sb([128, 2, T], BF16, "kT2")
        amask, r_amask = c.sb([128, 4, 3, 256], BF16, "amask")
        vraw = c.ring(2, [128, 8, 256], BF16, name="vraw")
        vaugr = c.ring(2, [128, 64, 4, 65], BF16, name="vaug")
        pTr = c.ring(4, [128, 256], BF16, name="pT")
        per_ = c.ring(4, [128, 256], BF16, name="pe")
        ostr = c.ring(3, [128, 4, 65], F32, name="ost")
        Sps = c.ring(4, [128, 256], F32, psum=True, name="Sps")
        OpsE = c.ring(2, [128, 2, 65], F32, psum=True, name="OpsE")
        OpsO = c.ring(2, [128, 2, 65], F32, psum=True, name="OpsO")
        for (vg_, r_vg_) in vaugr.items:
            GP(lambda e, vg_=vg_: e.memset(vg_[:, :, :, 64:65], 1.0), (), [r_vg_])
        for half in range(2):
            ld(amask[:], dr["amask"].rearrange("k (h p q) -> k h p q", h=8, p=3)[:, 4 * half:4 * half + 4], (), [r_amask], "sp")
            for hpi in range(2):
                hp = 2 * half + hpi
                ld(qT2[:, hpi, :], dr["QT"][hp * 128:(hp + 1) * 128, :], [rs["QT"]], [r_qT2])
                ld(kT2[:, hpi, :], dr["KT"][hp * 128:(hp + 1) * 128, :], [rs["KT"]], [r_kT2])
            for p, (w, d) in enumerate(PATTERNS):
                span = 128 * d
                nb = T // span
                vaug, r_vaug = vaugr.next()
                vsrc = dr["VV"].rearrange("(a u r) c -> r u a c", u=128, r=d)
                for r in range(d):
                    for a0 in range(0, nb, 8):
                        na = min(8, nb - a0)
                        vr, r_vr = vraw.next()
                        ld(vr[:, 0:na, :], vsrc[r, :, a0:a0 + na, half * 256:(half + 1) * 256], [rs["VV"]], [r_vr])
                        bi0 = r * nb + a0
                        GP(lambda e, vr=vr, na=na, bi0=bi0, vaug=vaug: e.tensor_copy(out=vaug[:, bi0:bi0 + na, :, 0:64],
                                                                         in_=vr[:, 0:na, :].rearrange("k a (h c) -> k a h c", h=4)),
                           [r_vr], [r_vaug])
                odst = dr[f"OP{p}"].rearrange("(a u r) h c -> r a u h c", u=128, r=d)
                units = [(r, a) for r in range(d) for a in range(nb)]

                def s_stage(r, a, hh, d=d, p=p, span=span, half=half):
                    hpi, pb = hh // 2, 64 * (hh % 2)
                    t0 = span * a + r
                    sp_, r_sp = Sps.next()
                    q_ap = qT2[pb:pb + 64, hpi, t0:t0 + 127 * d + 1:d]
                    c0 = 0 if a > 0 else 128
                    th = []
                    if a > 0:
                        tp = t0 - span
                        th.append(lambda: PE(lambda e: e.matmul(sp_[:, 0:128], lhsT=kT2[pb:pb + 64, hpi, tp:tp + 127 * d + 1:d], rhs=q_ap,
                                                                start=True, stop=True), [r_kT2, r_qT2], [r_sp]))
                    th.append(lambda: PE(lambda e: e.matmul(sp_[:, 128:256], lhsT=kT2[pb:pb + 64, hpi, t0:t0 + 127 * d + 1:d], rhs=q_ap,
                                                            start=True, stop=True), [r_kT2, r_qT2], [r_sp]))
                    pe, r_pe = per_.next()
                    pT, r_pT = pTr.next()

                    def post():
                        A(lambda e: e.activation(out=pe[:, c0:256], in_=sp_[:, c0:256], func=AF.Exp), [r_sp], [r_pe])
                        V(lambda e: e.tensor_tensor(out=pT[:, c0:256], in0=pe[:, c0:256], in1=amask[:, hh, p, c0:256], op=ALU.mult),
                          [r_pe, r_amask], [r_pT])
                    return th, post, pT, r_pT

                def pv_thunks(r, a, hh, pT, r_pT, O, r_O, nb=nb, vaug=vaug, r_vaug=r_vaug):
                    bi = r * nb + a
                    hs = hh // 2
                    th = []
                    if a > 0:
                        th.append(lambda: PE(lambda e: e.matmul(O[:, hs, :], lhsT=pT[:, 0:128], rhs=vaug[:, bi - 1, hh, :], start=True, stop=False),
                                             [r_pT, r_vaug], [r_O]))
                    th.append(lambda: PE(lambda e: e.matmul(O[:, hs, :], lhsT=pT[:, 128:256], rhs=vaug[:, bi, hh, :], start=(a == 0), stop=True),
                                         [r_pT, r_vaug], [r_O]))
                    return th

                def interleave(lists):
                    out_ = []
                    n = max(len(x) for x in lists) if lists else 0
                    for i in range(n):
                        for x in lists:
                            if i < len(x):
                                out_.append(x[i])
                    return out_

                def finish_block(r, a, OE, r_OE, OO, r_OO):
                    ost, r_ost = ostr.next()
                    V(lambda e: e.tensor_copy(out=ost[:, 0:4:2, :], in_=OE[:]), [r_OE], [r_ost])
                    V(lambda e: e.tensor_copy(out=ost[:, 1:4:2, :], in_=OO[:]), [r_OO], [r_ost])
                    ld(odst[r, a, :, half, :], ost[:].rearrange("k h c -> k (h c)"), [r_ost], [rs[f"OP{p}"]])

                flat = [(r, a, hh) for (r, a) in units for hh in range(4)]
                pairs = [flat[i:i + 2] for i in range(0, len(flat), 2)]
                Ocur = {}
                prev_pv = []
                prev_fin = []
                for pr in pairs:
                    st = [s_stage(*u) for u in pr]
                    s_th = interleave([x[0] for x in st])
                    for f_ in interleave([s_th, prev_pv]):
                        f_()
                    for fb in prev_fin:
                        finish_block(*fb)
                    for x in st:
                        x[1]()
                    prev_fin = []
                    pvl = []
                    for (r, a, hh), x in zip(pr, st):
                        if hh == 0:
                            Ocur[(r, a)] = (OpsE.next(), OpsO.next())
                        (OE, r_OE), (OO, r_OO) = Ocur[(r, a)]
                        O, r_O = (OE, r_OE) if hh % 2 == 0 else (OO, r_OO)
                        pvl.append(pv_thunks(r, a, hh, x[2], x[3], O, r_O))
                        if hh == 3:
                            prev_fin.append((r, a, OE, r_OE, OO, r_OO))
                            del Ocur[(r, a)]
                    prev_pv = interleave(pvl)
                for f_ in prev_pv:
                    f_()
                for fb in prev_fin:
                    finish_block(*fb)
        c.close()

    def p2b(l):
        c = Ctx(S)
        angb, r_angb = c.sb([128, 512], F32, "angb")
        ld(angb[:], dr["attn_norm_g"][l:l + 1, :].partition_broadcast(128), (), [r_angb], "sp")
        opr = [c.ring(5, [128, 8, 65], F32, name=f"op{p}") for p in range(3)]
        den, r_den = c.sb([128, 8], F32, "den")
        o, r_o = c.sb([128, 8, 64], F32, "o")
        junk, r_junk = c.sb([128, 512], BF16, "junk")
        ss, r_ss = c.sb([128, NT], F32, "ss")
        rstd, r_rstd = c.sb([128, NT], F32, "rstd")
        cst = c.ring(3, [128, 512], BF16, name="cst")
        V(lambda e: e.memset(ss[:], 0.0), (), [r_ss])
        pend_ = {}

        def loads(t):
            tl = []
            for p in range(3):
                tt, r_tt = opr[p].next()
                ld(tt[:].rearrange("k h c -> k (h c)"), dr[f"OP{p}"][t * 128:(t + 1) * 128].rearrange("k a c -> k (a c)"), [rs[f"OP{p}"]], [r_tt])
                tl.append((tt, r_tt))
            pend_[t] = tl
        for t in range(min(3, NT)):
            loads(t)
        for t in range(NT):
            tl = pend_.pop(t)
            if t + 3 < NT:
                loads(t + 3)
            (t0_, r0), (t1_, r1), (t2_, r2) = tl
            V(lambda e, a=t0_, b=t1_: e.tensor_tensor(out=a[:], in0=a[:], in1=b[:], op=ALU.add), [r0, r1], [r0])
            V(lambda e, a=t0_, b=t2_: e.tensor_tensor(out=a[:], in0=a[:], in1=b[:], op=ALU.add), [r0, r2], [r0])
            V(lambda e, a=t0_: e.reciprocal(out=den[:], in_=a[:, :, 64]), [r0], [r_den])
            V(lambda e, a=t0_: e.tensor_tensor(out=o[:], in0=a[:, :, 0:64], in1=den[:].unsqueeze(2).to_broadcast([128, 8, 64]), op=ALU.mult),
              [r0, r_den], [r_o])
            A(lambda e, t=t: e.activation(out=junk[:], in_=o[:].rearrange("k h c -> k (h c)"), func=AF.Square, accum_out=ss[:, t:t + 1]),
              [r_o], [r_junk, r_ss])
            rstd_from_ss(ss[:, t:t + 1], rstd[:, t:t + 1], 512, r_ss, r_rstd)
            cs, r_cs = cst.next()
            V(lambda e, cs=cs, t=t: e.scalar_tensor_tensor(out=cs[:], in0=o[:].rearrange("k h c -> k (h c)"), scalar=rstd[:, t:t + 1],
                                                          in1=angb[:], op0=ALU.mult, op1=ALU.mult), [r_o, r_rstd, r_angb], [r_cs])
            ld(dr["CAT"][t * 128:(t + 1) * 128, 0:512], cs[:], [r_cs], [rs["CAT"]])
        c.close()

    def p3(l):
        c = Ctx(S)
        SEG = 512
        NCK = SEG // 128
        NSEG = T // SEG
        resetm, r_resetm = c.sb([128, SEG], F32, "resetm")
        gnb, r_gnb = c.sb([128, 512], F32, "gnb")
        ld(resetm[:], dr["resetm"][:, 0:SEG], (), [r_resetm], "sp")
        ld(gnb[:], dr["hgrn_norm_g"][l:l + 1, :].partition_broadcast(128), (), [r_gnb], "sp")
        rir = c.ring(2, [128, NCK, 512], BF16, name="ri")
        rgr = c.ring(2, [128, NCK, 512], BF16, name="rgm")
        catr_ = c.ring(2, [128, NCK, 512], BF16, name="catseg")
        zr = c.ring(2, [128, SEG], F32, name="z")
        qr = c.ring(2, [128, SEG], BF16, name="q")
        bA, r_A = c.sb([128, SEG], F32, "bA")
        bB, r_B = c.sb([128, SEG], F32, "bB")
        bC, r_C = c.sb([128, SEG], F32, "bC")
        bE, r_E = c.sb([128, SEG], F32, "bE")
        o4 = c.ring(8, [128, 5, SEG], BF16, name="o4")
        dcyr = c.ring(8, [128, NCK], F32, name="dcy")
        St = [c.sb([128, 128], F32, f"S{h}") for h in range(4)]
        Sb = [c.sb([128, 128], BF16, f"Sb{h}") for h in range(4)]
        Amr = c.ring(4, [128, 128], BF16, name="Am")
        khTr = c.ring(4, [128, 128], BF16, name="khT")
        junk, r_junk = c.sb([128, 128], BF16, "junk")
        ssr, r_ssr = c.sb([128, 4 * NT], F32, "ssr")
        rsr, r_rsr = c.sb([128, 4 * NT], F32, "rsr")
        psA = c.ring(2, [128, 128], F32, psum=True, name="psA")
        psT = c.ring(1, [128, 128], BF16, psum=True, name="psT")
        psU = c.ring(2, [128, 128], F32, psum=True, name="psU")
        psO = c.ring(2, [128, 128], F32, psum=True, name="psO")
        psX = c.ring(1, [64, 64], F32, psum=True, name="psX")
        V(lambda e: e.memset(ssr[:], 0.0), (), [r_ssr])
        for h in range(4):
            V(lambda e, h=h: e.memset(St[h][0][:], 0.0), (), [St[h][1]])
            V(lambda e, h=h: e.memset(Sb[h][0][:], 0.0), (), [Sb[h][1]])

        def v3(t):
            t = t if isinstance(t, bass.AP) else t[:]
            return t.rearrange("k (c u) -> k c u", u=128)

        def v64(t):
            return t[:].rearrange("k (c u) -> k c u", u=64)

        def ew(sg):
            tk0 = sg * SEG
            ri, r_ri = rir.next()
            rgm, r_rgm = rgr.next()
            ld(ri[:], dr["RI"][tk0:tk0 + SEG, :].rearrange("(c p) f -> p c f", p=128), [rs["RI"]], [r_ri])
            ld(rgm[:], dr["RG"][tk0:tk0 + SEG, :].rearrange("(c p) f -> p c f", p=128), [rs["RG"]], [r_rgm])
            GP(lambda e: e.tensor_tensor(out=rgm[:], in0=rgm[:], in1=gnb[:].unsqueeze(1).to_broadcast([128, NCK, 512]), op=ALU.mult),
               [r_rgm, r_gnb], [r_rgm])
            heads = []
            for hd in range(4):
                z, r_z = zr.next()
                q, r_q = qr.next()
                ld(z[:], dr["ZT"][hd * 128:(hd + 1) * 128, tk0:tk0 + SEG], [rs["ZT"]], [r_z])
                ld(q[:], dr["RQT"][hd * 128:(hd + 1) * 128, tk0:tk0 + SEG], [rs["RQT"]], [r_q])
                o, r_o4 = o4.next()
                dcy, r_dcy = dcyr.next()
                lb_ap, oml_ap = lbc[:, l, hd:hd + 1], oml[:, l, hd:hd + 1]
                V(lambda e, z=z: e.tensor_scalar(out=z[:], in0=z[:], scalar1=-60.0, scalar2=None, op0=ALU.max), [r_z], [r_z])
                A(lambda e, z=z: e.activation(out=bA[:], in_=z[:], func=AF.Exp, scale=-1.0), [r_z], [r_A])
                A(lambda e: e.activation(out=bB[:], in_=bA[:], func=AF.Ln, bias=1.0), [r_A], [r_B])
                A(lambda e, b=lb_ap: e.activation(out=bE[:], in_=bA[:], func=AF.Ln, bias=1.0, scale=b), [r_A, r_lbc], [r_E])
                V(lambda e: e.tensor_tensor(out=bE[:], in0=bE[:], in1=bB[:], op=ALU.subtract), [r_E, r_B], [r_E])
                A(lambda e: e.activation(out=bB[:], in_=bB[:], func=AF.Exp, scale=-1.0), [r_B], [r_B])
                V(lambda e, a=oml_ap: e.scalar_tensor_tensor(out=bC[:], in0=bA[:], scalar=a, in1=bB[:], op0=ALU.mult, op1=ALU.mult),
                  [r_A, r_B, r_oml], [r_C])
                V(lambda e: e.tensor_tensor_scan(out=bB[:], data0=resetm[:], data1=bE[:], initial=0.0, op0=ALU.mult, op1=ALU.add),
                  [r_resetm, r_E], [r_B])
                V(lambda e: e.tensor_tensor(out=v64(bE), in0=v64(bB), in1=v64(bB)[:, :, 31:32].to_broadcast([128, 2 * NCK, 64]), op=ALU.subtract),
                  [r_B], [r_E])
                V(lambda e: e.tensor_scalar(out=bE[:], in0=bE[:], scalar1=-80.0, scalar2=80.0, op0=ALU.max, op1=ALU.min), [r_E], [r_E])
                A(lambda e: e.activation(out=bA[:], in_=bE[:], func=AF.Exp), [r_E], [r_A])
                V(lambda e, o=o, q=q: e.tensor_tensor(out=o[:, 0, :], in0=q[:], in1=bA[:], op=ALU.mult), [r_q, r_A], [r_o4])
                A(lambda e: e.activation(out=bA[:], in_=bE[:], func=AF.Exp, scale=-1.0), [r_E], [r_A])
                V(lambda e, o=o: e.tensor_tensor(out=o[:, 1, :], in0=bC[:], in1=bA[:], op=ALU.mult), [r_C, r_A], [r_o4])
                A(lambda e: e.activation(out=bA[:], in_=bB[:], func=AF.Exp), [r_B], [r_A])
                GP(lambda e, o=o, q=q: e.tensor_tensor(out=o[:, 2, :], in0=q[:], in1=bA[:], op=ALU.mult), [r_q, r_A], [r_o4])
                V(lambda e: e.tensor_tensor(out=v3(bE), in0=v3(bB), in1=v3(bB)[:, :, 127:128].to_broadcast([128, NCK, 128]), op=ALU.subtract),
                  [r_B], [r_E])
                A(lambda e: e.activation(out=bA[:], in_=bE[:], func=AF.Exp, scale=-1.0), [r_E], [r_A])
                GP(lambda e, o=o: e.tensor_tensor(out=o[:, 3, :], in0=bC[:], in1=bA[:], op=ALU.mult), [r_C, r_A], [r_o4])
                V(lambda e: e.tensor_tensor(out=v3(bE), in0=v3(bB), in1=v3(bB)[:, :, 63:64].to_broadcast([128, NCK, 128]), op=ALU.subtract),
                  [r_B], [r_E])
                V(lambda e: e.scalar_tensor_tensor(out=bE[:], in0=bE[:], scalar=-1.0, in1=bE[:], op0=ALU.mult, op1=ALU.min), [r_E], [r_E])
                A(lambda e: e.activation(out=bA[:], in_=bE[:], func=AF.Exp), [r_E], [r_A])
                V(lambda e, o=o: e.tensor_tensor(out=v3(o[:, 4, :])[:, :, 0:64], in0=v3(bC)[:, :, 0:64], in1=v3(bA)[:, :, 0:64], op=ALU.mult),
                  [r_C, r_A], [r_o4])
                V(lambda e, o=o, q=q: e.tensor_tensor(out=v3(o[:, 4, :])[:, :, 64:128], in0=v3(q)[:, :, 64:128], in1=v3(bA)[:, :, 64:128], op=ALU.mult),
                  [r_q, r_A], [r_o4])
                A(lambda e, dcy=dcy: e.activation(out=dcy[:], in_=v3(bB)[:, :, 127], func=AF.Exp), [r_B], [r_dcy])
                heads.append((o, r_o4, dcy, r_dcy))
            return dict(sg=sg, ri=(ri, r_ri), rgm=(rgm, r_rgm), heads=heads)

        def chunks(st):
            sg = st["sg"]
            tk0 = sg * SEG
            ri, r_ri = st["ri"]
            rgm, r_rgm = st["rgm"]
            catseg, r_catseg = catr_.next()
            for ci_ in range(NCK):
                one_chunk(st, ci_, sg, ri, r_ri, rgm, r_rgm, catseg, r_catseg)
            ld(dr["CAT"][tk0:tk0 + SEG, 512:1024].rearrange("(c p) f -> p c f", p=128), catseg[:], [r_catseg], [rs["CAT"]])

        def one_chunk(st, ci, sg, ri, r_ri, rgm, r_rgm, catseg, r_catseg):
            if True:
                cs = slice(ci * 128, (ci + 1) * 128)
                g0 = (sg * NCK + ci) * 4
                per = []
                for hd in range(4):
                    o, r_o4, dcy, r_dcy = st["heads"][hd]
                    pa, r_pa = psA.next()
                    PE(lambda e, pa=pa, o=o: e.matmul(pa[:, :], lhsT=o[:, 1, cs], rhs=o[:, 0, cs], start=True, stop=True), [r_o4], [r_pa])
                    px, r_px = psX.next()
                    PE(lambda e, px=px, o=o: e.matmul(px[0:64, :], lhsT=o[:, 4, ci * 128:ci * 128 + 64], rhs=o[:, 4, ci * 128 + 64:(ci + 1) * 128],
                                                      start=True, stop=True), [r_o4], [r_px])
                    pt, r_pt = psT.next()
                    PE(lambda e, pt=pt, o=o: e.transpose(out=pt[:, :], in_=o[:, 3, cs], identity=ident[:]), [r_o4, r_ident], [r_pt])
                    Am, r_Am = Amr.next()
                    GP(lambda e, Am=Am: e.memset(Am[:], 0.0), (), [r_Am])
                    V(lambda e, Am=Am, pa=pa: e.copy_predicated(out=Am[:], mask=causal[:], data=pa[:]), [r_pa, r_causal, r_Am], [r_Am])
                    A(lambda e, Am=Am, px=px: e.activation(out=Am[0:64, 64:128], in_=px[0:64, :], func=AF.Copy), [r_px, r_Am], [r_Am])
                    khT, r_khT = khTr.next()
                    A(lambda e, khT=khT, pt=pt: e.activation(out=khT[:], in_=pt[:], func=AF.Copy), [r_pt], [r_khT])
                    per.append((Am, r_Am, khT, r_khT))
                pos_ = []
                for hd in range(4):
                    o, r_o4, dcy, r_dcy = st["heads"][hd]
                    Am, r_Am, khT, r_khT = per[hd]
                    Sh, r_Sh = St[hd]
                    Sbh, r_Sbh = Sb[hd]
                    v_ap = ri[:, ci, hd * 128:(hd + 1) * 128]
                    pu, r_pu = psU.next()
                    PE(lambda e, pu=pu, khT=khT, v_ap=v_ap: e.matmul(pu[:, :], lhsT=khT[:], rhs=v_ap, start=True, stop=True), [r_khT, r_ri], [r_pu])
                    po, r_po = psO.next()
                    PE(lambda e, po=po, Am=Am, v_ap=v_ap: e.matmul(po[:, :], lhsT=Am[:], rhs=v_ap, start=True, stop=False), [r_Am, r_ri], [r_po])
                    PE(lambda e, po=po, o=o, Sbh=Sbh: e.matmul(po[:, :], lhsT=o[:, 2, cs], rhs=Sbh[:], start=False, stop=True),
                       [r_o4, r_Sbh], [r_po])
                    V(lambda e, Sh=Sh, pu=pu, dcy=dcy: e.scalar_tensor_tensor(out=Sh[:], in0=Sh[:], scalar=dcy[:, ci:ci + 1], in1=pu[:],
                                                                             op0=ALU.mult, op1=ALU.add), [r_Sh, r_pu, r_dcy], [r_Sh])
                    A(lambda e, Sh=Sh, Sbh=Sbh: e.activation(out=Sbh[:], in_=Sh[:], func=AF.Copy), [r_Sh], [r_Sbh])
                    A(lambda e, po=po, gi=g0 + hd: e.activation(out=junk[:], in_=po[:], func=AF.Square, accum_out=ssr[:, gi:gi + 1]),
                      [r_po], [r_junk, r_ssr])
                    rstd_from_ss(ssr[:, g0 + hd:g0 + hd + 1], rsr[:, g0 + hd:g0 + hd + 1], 128, r_ssr, r_rsr)
                    V(lambda e, po=po, gi=g0 + hd, hd=hd: e.scalar_tensor_tensor(out=catseg[:, ci, hd * 128:(hd + 1) * 128], in0=po[:],
                                                                                scalar=rsr[:, gi:gi + 1], in1=rgm[:, ci, hd * 128:(hd + 1) * 128],
                                                                                op0=ALU.mult, op1=ALU.mult), [r_po, r_rsr, r_rgm], [r_catseg])

        nxt = ew(0)
        for sg in range(NSEG):
            cur = nxt
            if sg + 1 < NSEG:
                nxt = ew(sg + 1)
            chunks(cur)
        c.close()

    def p4(l, xin, r_xin):
        c = Ctx(S)
        woutb, r_woutb = c.sb([128, 8, D], BF16, "woutb")
        wst = c.ring(2, [128, 4, D], F32, name="wst")
        for hf in range(2):
            st, r_st = wst.next()
            ld(st[:], dr["w_out"][l, hf * 512:(hf + 1) * 512, :].rearrange("(kc p) n -> p kc n", p=128), (), [r_st])
            (V if hf == 0 else GP)(lambda e, st=st, hf=hf: e.tensor_copy(out=woutb[:, hf * 4:(hf + 1) * 4, :], in_=st[:]), [r_st], [r_woutb])
        wrs, r_wrs = c.sb([128, 8, 36], F32, "wrs")
        wrb, r_wrb = c.sb([128, 8, 36], BF16, "wrb")
        rbb, r_rbb = c.sb([128, 36], F32, "rbb")
        S.dma("sp", lambda e: e.dma_start(out=wrs[:], in_=dr["router_w"][l].rearrange("(kc p) n -> p kc n", p=128),
                                          allow_slow_non_contiguous=True), (), [r_wrs])
        V(lambda e: e.tensor_copy(out=wrb[:], in_=wrs[:]), [r_wrs], [r_wrb])
        ld(rbb[:], dr["router_b"][l:l + 1, :].partition_broadcast(128), (), [r_rbb], "sp")
        catr = c.ring(4, [128, D], BF16, name="cat")
        catTr = c.ring(3, [128, 8, 128], BF16, name="catT")
        xr = c.ring(4, [128, D], F32, name="x")
        x1r = c.ring(3, [128, D], F32, name="x1")
        h2r = c.ring(4, [128, D], BF16, name="h2")
        h2fr = c.ring(2, [128, D], F32, name="h2f")
        h2Tr = c.ring(3, [128, 8, 128], BF16, name="h2T")
        junk, r_junk = c.sb([128, D], BF16, "junk")
        ss, r_ss = c.sb([128, NT], F32, "ss")
        rstd, r_rstd = c.sb([128, NT], F32, "rstd")
        ptr = c.ring(2, [128, 8, 128], BF16, psum=True, name="ptr")
        pacc = c.ring(4, [128, 512], F32, psum=True, name="pacc")
        plg = c.ring(2, [128, 36], F32, psum=True, name="plg")
        V(lambda e: e.memset(ss[:], 0.0), (), [r_ss])
        st_ = {}

        def loads(t):
            ct, r_ct = catr.next()
            ld(ct[:], dr["CAT"][t * 128:(t + 1) * 128, :], [rs["CAT"]], [r_ct])
            xt, r_xt = xr.next()
            ld(xt[:], xin[t * 128:(t + 1) * 128, :], [r_xin], [r_xt])
            st_[t] = dict(ct=(ct, r_ct), xt=(xt, r_xt))

        def transposes(ta, td):
            jobs = []
            if ta is not None:
                pt, r_pt = ptr.next()
                ct, r_ct = st_[ta]["ct"]
                jobs.append([(lambda kc=kc, pt=pt, ct=ct, r_ct=r_ct, r_pt=r_pt: PE(
                    lambda e: e.transpose(out=pt[:, kc, :], in_=ct[:, kc * 128:(kc + 1) * 128], identity=ident[:]), [r_ct, r_ident], [r_pt]))
                    for kc in range(8)])
            if td is not None:
                pt2, r_pt2 = ptr.next()
                h2, r_h2 = st_[td]["h2"]
                jobs.append([(lambda kc=kc, pt2=pt2, h2=h2, r_h2=r_h2, r_pt2=r_pt2: PE(
                    lambda e: e.transpose(out=pt2[:, kc, :], in_=h2[:, kc * 128:(kc + 1) * 128], identity=ident[:]), [r_h2, r_ident], [r_pt2]))
                    for kc in range(8)])
            n = max(len(j) for j in jobs)
            for i in range(n):
                for j in jobs:
                    j[i]()
            if ta is not None:
                cT, r_cT = catTr.next()
                A(lambda e, cT=cT, pt=pt: e.activation(out=cT[:], in_=pt[:], func=AF.Copy), [r_pt], [r_cT])
                st_[ta]["cT"] = (cT, r_cT)
            if td is not None:
                hT, r_hT = h2Tr.next()
                A(lambda e, hT=hT, pt2=pt2: e.activation(out=hT[:], in_=pt2[:], func=AF.Copy), [r_pt2], [r_hT])
                st_[td]["hT"] = (hT, r_hT)

        def matmuls(t, tr):
            accs = None
            if t is not None:
                cT, r_cT = st_[t]["cT"]
                accs = [pacc.next(), pacc.next()]
            if tr is not None:
                hT, r_hT = st_[tr]["hT"]
                pl, r_pl = plg.next()
            for kc in range(8):
                if t is not None:
                    for n in range(2):
                        acc, r_acc = accs[n]
                        PE(lambda e, kc=kc, acc=acc, cT=cT, n=n: e.matmul(acc[:, :], lhsT=cT[:, kc, :], rhs=woutb[:, kc, n * 512:(n + 1) * 512],
                                                                         start=(kc == 0), stop=(kc == 7)), [r_cT, r_woutb], [r_acc])
                if tr is not None:
                    PE(lambda e, kc=kc, pl=pl, hT=hT: e.matmul(pl[:, :], lhsT=hT[:, kc, :], rhs=wrb[:, kc, :], start=(kc == 0), stop=(kc == 7)),
                       [r_hT, r_wrb], [r_pl])
            if tr is not None:
                V(lambda e, pl=pl, tr=tr: e.tensor_tensor(out=lgall[:, tr, :], in0=pl[:, :], in1=rbb[:], op=ALU.add), [r_pl, r_rbb], [r_lgall])
                del st_[tr]
            return accs

        def elementwise(t, accs):
            xt, r_xt = st_[t]["xt"]
            x1, r_x1 = x1r.next()
            for n in range(2):
                acc, r_acc = accs[n]
                V(lambda e, acc=acc, x1=x1, n=n: e.tensor_tensor(out=x1[:, n * 512:(n + 1) * 512], in0=acc[:, :], in1=G1b[:, n * 512:(n + 1) * 512],
                                                                op=ALU.mult), [r_acc, r_G1b], [r_x1])
            GP(lambda e, x1=x1, xt=xt: e.tensor_tensor(out=x1[:], in0=x1[:], in1=xt[:], op=ALU.add), [r_x1, r_xt], [r_x1])
            ld(dr["Xs"][t * 128:(t + 1) * 128, :], x1[:], [r_x1], [rs["Xs"]])
            A(lambda e, x1=x1, t=t: e.activation(out=junk[:], in_=x1[:], func=AF.Square, accum_out=ss[:, t:t + 1]), [r_x1], [r_junk, r_ss])
            rstd_from_ss(ss[:, t:t + 1], rstd[:, t:t + 1], D, r_ss, r_rstd)
            h2f_, r_h2f_ = h2fr.next()
            V(lambda e, x1=x1, t=t, h2f_=h2f_: e.scalar_tensor_tensor(out=h2f_[:], in0=x1[:], scalar=rstd[:, t:t + 1], in1=A2b[:], op0=ALU.mult, op1=ALU.mult),
              [r_x1, r_rstd, r_A2b], [r_h2f_])
            h2, r_h2 = h2r.next()
            GP(lambda e, h2=h2, h2f_=h2f_: e.tensor_tensor(out=h2[:], in0=h2f_[:], in1=S2b[:], op=ALU.add), [r_h2f_, r_S2b], [r_h2])
            ld(dr["H2"][t * 128:(t + 1) * 128, :], h2[:], [r_h2], [rs["H2"]])
            st_[t]["h2"] = (h2, r_h2)

        loads(0)
        loads(1)
        transposes(0, None)
        for t in range(NT + 2):
            if t + 2 < NT:
                loads(t + 2)
            ta = t + 1 if t + 1 < NT else None
            td = t - 1 if 0 <= t - 1 < NT else None
            if ta is not None or td is not None:
                transposes(ta, td)
            tm_ = t if t < NT else None
            trr = t - 2 if 0 <= t - 2 < NT else None
            accs = matmuls(tm_, trr)
            if tm_ is not None:
                elementwise(tm_, accs)
        c.close()

    def p4b(l):
        c = Ctx(S)
        N3 = [128, NT, NE]
        gmax, r_gmax = c.sb([128, NT], F32)
        g4, r_g4 = c.sb([128, NT, 4], F32)
        goh, r_goh = c.sb([128, NT, 4], F32)
        gsum, r_gsum = c.sb([128, NT], F32)
        em, r_em = c.sb(N3, F32)
        oh1, r_oh1 = c.sb(N3, F32)
        oh2, r_oh2 = c.sb(N3, F32)
        m1, r_m1 = c.sb([128, NT], F32)
        m2, r_m2 = c.sb([128, NT], F32)
        p1, r_p1 = c.sb([128, NT], F32)
        abf, r_abf = c.sb([128, NT * NE], BF16)
        csb, r_csb = c.sb(N3, F32)
        offs, r_offs = c.sb(N3, F32)
        pos, r_pos = c.sb(N3, F32)
        sf, r_sf = c.sb([128, NT], F32)
        cnt, r_cnt = c.sb([128, NE], F32)
        nblk, r_nblk = c.sb([128, NE], F32)
        pendb, r_pendb = c.sb([128, NE], F32)
        pst, r_pst = c.sb([128, NE], F32)
        onesne, r_onesne = c.sb([128, NE], F32)
        bex, r_bex = c.sb([128, NBLK], F32)
        cmp, r_cmp = c.sb([128, NBLK * NE], F32)
        pp = c.ring(1, [128, NT * NE], F32, psum=True)
        pc = c.ring(1, [128, NT * NE], F32, psum=True)
        lgG = lgall[:, :, 0:4]
        lgE = lgall[:, :, 4:36]

        def bc(ap2, n):
            return ap2.unsqueeze(2).to_broadcast([128, NT, n])
        V(lambda e: e.tensor_reduce(out=gmax[:], in_=lgG, axis=AX.X, op=ALU.max), [r_lgall], [r_gmax])
        V(lambda e: e.tensor_tensor(out=goh[:], in0=lgG, in1=bc(gmax[:], 4), op=ALU.is_equal), [r_lgall, r_gmax], [r_goh])
        V(lambda e: e.tensor_tensor(out=g4[:], in0=lgG, in1=bc(gmax[:], 4), op=ALU.subtract), [r_lgall, r_gmax], [r_g4])
        A(lambda e: e.activation(out=g4[:], in_=g4[:], func=AF.Exp), [r_g4], [r_g4])
        V(lambda e: e.tensor_reduce(out=gsum[:], in_=g4[:], axis=AX.X, op=ALU.add), [r_g4], [r_gsum])
        V(lambda e: e.reciprocal(out=gsum[:], in_=gsum[:]), [r_gsum], [r_gsum])
        V(lambda e: e.tensor_scalar(out=goh[:], in0=goh[:], scalar1=BIG, scalar2=-BIG, op0=ALU.mult, op1=ALU.add), [r_goh], [r_goh])
        V(lambda e: e.tensor_tensor(out=em[:].rearrange("k t (g x) -> k t g x", g=4), in0=lgE.rearrange("k t (g x) -> k t g x", g=4),
                                    in1=goh[:].unsqueeze(3).to_broadcast([128, NT, 4, 8]), op=ALU.add), [r_lgall, r_goh], [r_em])
        V(lambda e: e.tensor_reduce(out=m1[:], in_=em[:], axis=AX.X, op=ALU.max), [r_em], [r_m1])
        V(lambda e: e.tensor_tensor(out=oh1[:], in0=em[:], in1=bc(m1[:], NE), op=ALU.is_equal), [r_em, r_m1], [r_oh1])
        V(lambda e: e.scalar_tensor_tensor(out=em[:], in0=oh1[:], scalar=-BIG, in1=em[:], op0=ALU.mult, op1=ALU.add), [r_oh1, r_em], [r_em])
        V(lambda e: e.tensor_reduce(out=m2[:], in_=em[:], axis=AX.X, op=ALU.max), [r_em], [r_m2])
        V(lambda e: e.tensor_tensor(out=oh2[:], in0=em[:], in1=bc(m2[:], NE), op=ALU.is_equal), [r_em, r_m2], [r_oh2])
        V(lambda e: e.tensor_tensor(out=m2[:], in0=m2[:], in1=m1[:], op=ALU.subtract), [r_m1, r_m2], [r_m2])
        A(lambda e: e.activation(out=m2[:], in_=m2[:], func=AF.Exp), [r_m2], [r_m2])
        V(lambda e: e.tensor_scalar(out=p1[:], in0=m2[:], scalar1=1.0, scalar2=None, op0=ALU.add), [r_m2], [r_p1])
        V(lambda e: e.reciprocal(out=p1[:], in_=p1[:]), [r_p1], [r_p1])
        V(lambda e: e.tensor_tensor(out=gt1[:], in0=p1[:], in1=gsum[:], op=ALU.mult), [r_p1, r_gsum], [r_gt1])
        V(lambda e: e.tensor_tensor(out=m2[:], in0=m2[:], in1=gt1[:], op=ALU.mult), [r_m2, r_gt1], [r_m2])
        V(lambda e: e.tensor_copy(out=gt2[:], in_=m2[:]), [r_m2], [r_gt2])
        V(lambda e: e.tensor_tensor(out=abf[:].rearrange("k (t x) -> k t x", x=NE), in0=oh1[:], in1=oh2[:], op=ALU.add), [r_oh1, r_oh2], [r_abf])
        ppt, r_pp = pp.next()
        pct, r_pc = pc.next()
        for ch in range(4):
            PE(lambda e, ch=ch: e.matmul(ppt[:, ch * 512:(ch + 1) * 512], lhsT=ustrict[:], rhs=abf[:, ch * 512:(ch + 1) * 512], start=True, stop=True),
               [r_ustrict, r_abf], [r_pp])
            PE(lambda e, ch=ch: e.matmul(pct[:, ch * 512:(ch + 1) * 512], lhsT=onesb[:], rhs=abf[:, ch * 512:(ch + 1) * 512], start=True, stop=True),
               [r_onesb, r_abf], [r_pc])
        V(lambda e: e.tensor_copy(out=csb[:].rearrange("k t x -> k (t x)"), in_=pct[:]), [r_pc], [r_csb])
        V(lambda e: e.memset(offs[:, 0, :], 0.0), (), [r_offs])
        for j in range(1, NT):
            V(lambda e, j=j: e.tensor_tensor(out=offs[:, j, :], in0=offs[:, j - 1, :], in1=csb[:, j - 1, :], op=ALU.add), [r_offs, r_csb], [r_offs])
        V(lambda e: e.tensor_tensor(out=cnt[:], in0=offs[:, NT - 1, :], in1=csb[:, NT - 1, :], op=ALU.add), [r_offs, r_csb], [r_cnt])
        V(lambda e: e.tensor_tensor(out=cmp[:, 0:NE * 64].rearrange("k (x m) -> k x m", m=64), in0=cnt[:].unsqueeze(2).to_broadcast([128, NE, 64]),
                                    in1=mulrow[:].unsqueeze(1).to_broadcast([128, NE, 64]), op=ALU.is_gt), [r_cnt, r_mulrow], [r_cmp])
        V(lambda e: e.tensor_reduce(out=nblk[:], in_=cmp[:, 0:NE * 64].rearrange("k (x m) -> k x m", m=64), axis=AX.X, op=ALU.add), [r_cmp], [r_nblk])
        V(lambda e: e.memset(onesne[:], 1.0), (), [r_onesne])
        V(lambda e: e.tensor_tensor_scan(out=pendb[:], data0=onesne[:], data1=nblk[:], initial=0.0, op0=ALU.mult, op1=ALU.add),
          [r_onesne, r_nblk], [r_pendb])
        V(lambda e: e.tensor_tensor(out=pst[:], in0=pendb[:], in1=nblk[:], op=ALU.subtract), [r_pendb, r_nblk], [r_pst])
        V(lambda e: e.tensor_scalar(out=pst[:], in0=pst[:], scalar1=float(RB), scalar2=None, op0=ALU.mult), [r_pst], [r_pst])
        V(lambda e: e.tensor_tensor(out=cmp[:, 0:NBLK * NE].rearrange("k (b x) -> k b x", x=NE), in0=pendb[:].unsqueeze(1).to_broadcast([128, NBLK, NE]),
                                    in1=brow[:].unsqueeze(2).to_broadcast([128, NBLK, NE]), op=ALU.is_le), [r_pendb, r_brow], [r_cmp])
        V(lambda e: e.tensor_reduce(out=bex[:], in_=cmp[:, 0:NBLK * NE].rearrange("k (b x) -> k b x", x=NE), axis=AX.X, op=ALU.add), [r_cmp], [r_bex])
        V(lambda e: e.tensor_scalar(out=bex[:], in0=bex[:], scalar1=float(NE - 1), scalar2=float(128), op0=ALU.min, op1=ALU.mult), [r_bex], [r_bex])
        V(lambda e: e.tensor_scalar(out=bex[:], in0=bex[:], scalar1=piota[:, 0:1], scalar2=float(l * NE * 128), op0=ALU.add, op1=ALU.add),
          [r_bex, r_piota], [r_bex])
        V(lambda e: e.tensor_copy(out=widx[:], in_=bex[:]), [r_bex], [r_widx])
        V(lambda e: e.tensor_tensor(out=offs[:], in0=offs[:], in1=pst[:].unsqueeze(1).to_broadcast([128, NT, NE]), op=ALU.add), [r_offs, r_pst], [r_offs])
        V(lambda e: e.tensor_tensor(out=pos[:].rearrange("k t x -> k (t x)"), in0=ppt[:], in1=offs[:].rearrange("k t x -> k (t x)"), op=ALU.add),
          [r_pp, r_offs], [r_pos])
        for (oh, r_oh, sl, r_sl) in ((oh1, r_oh1, slot1, r_slot1), (oh2, r_oh2, slot2, r_slot2)):
            V(lambda e, oh=oh: e.tensor_tensor(out=oh[:], in0=oh[:], in1=pos[:], op=ALU.mult), [r_oh, r_pos], [r_oh])
            V(lambda e, oh=oh: e.tensor_reduce(out=sf[:], in_=oh[:], axis=AX.X, op=ALU.add), [r_oh], [r_sf])
            V(lambda e: e.tensor_scalar(out=sf[:], in0=sf[:], scalar1=float(NROWS - 1), scalar2=0.0, op0=ALU.min, op1=ALU.max), [r_sf], [r_sf])
            V(lambda e, sl=sl: e.tensor_copy(out=sl[:], in_=sf[:]), [r_sf], [r_sl])
        h2r = c.ring(3, [128, D], BF16, name="h2d")
        for t in range(NT):
            h2, r_h2 = h2r.next()
            ld(h2[:], dr["H2"][t * 128:(t + 1) * 128, :], [rs["H2"]], [r_h2])
            for (sl, r_sl) in ((slot1, r_slot1), (slot2, r_slot2)):
                S.dma("pool", lambda e, h2=h2, sl=sl, t=t: e.indirect_dma_start(
                    out=dr["XS"], out_offset=bass.IndirectOffsetOnAxis(ap=sl[:, t:t + 1], axis=0), in_=h2[:], in_offset=None),
                    [r_h2, r_sl], [rs["XS"]])
        c.close()

    def p5(l):
        c = Ctx(S)
        wsr = c.ring(6, [128, 2048], F32, name="wsr")
        wbr = [c.ring(2, [128, 4096], BF16, name=f"wb{i}") for i in range(3)]
        xrr = c.ring(4, [128, D], BF16, name="xr")
        xTr = c.ring(2, [128, 8, RB], BF16, name="xT")
        actr = c.ring(2, [128, 4, RB], BF16, name="actT")
        silr = c.ring(2, [128, RB], F32, name="sil")
        ysr = c.ring(3, [128, D], F32, name="ys")
        ptr = c.ring(2, [128, 8, 128], BF16, psum=True, name="ptr")
        pacc = c.ring(6, [128, 512], F32, psum=True, name="pacc")
        wsrc = ((dr["moe_w1_0"], dr["moe_w1_1"]), (dr["moe_w3_0"], dr["moe_w3_1"]), (dr["moe_w2_0"], dr["moe_w2_1"]))
        k = [0]

        def wload(b):
            wb = []
            for i in range(3):
                wt, r_wt = wbr[i].next()
                for hf in range(2):
                    st, r_st = wsr.next()
                    S.dma("pool", lambda e, st=st, i=i, hf=hf: e.indirect_dma_start(
                        out=st[:], out_offset=None, in_=wsrc[i][hf],
                        in_offset=bass.IndirectOffsetOnAxis(ap=widx[:, b:b + 1], axis=0)), [r_widx], [r_st])
                    k[0] += 1
                    dst = wt[:, hf * 2048:(hf + 1) * 2048]
                    if k[0] % 2 == 0:
                        V(lambda e, st=st, dst=dst: e.tensor_copy(out=dst, in_=st[:]), [r_st], [r_wt])
                    else:
                        A(lambda e, st=st, dst=dst: e.activation(out=dst, in_=st[:], func=AF.Copy), [r_st], [r_wt])
                wb.append((wt, r_wt))
            return wb

        def xprep(b):
            xT, r_xT = xTr.next()
            tl = []
            for rt in range(RB // 128):
                xr_, r_xr = xrr.next()
                r0 = b * RB + rt * 128
                ld(xr_[:], dr["XS"][r0:r0 + 128, :], [rs["XS"]], [r_xr])
                tl.append((xr_, r_xr, ptr.next()))
            for kc in range(8):
                for (xr_, r_xr, (pt, r_pt)) in tl:
                    PE(lambda e, kc=kc, pt=pt, xr_=xr_: e.transpose(out=pt[:, kc, :], in_=xr_[:, kc * 128:(kc + 1) * 128], identity=ident[:]),
                       [r_xr, r_ident], [r_pt])
            for rt, (xr_, r_xr, (pt, r_pt)) in enumerate(tl):
                if rt == 0:
                    V(lambda e, xT=xT, pt=pt, rt=rt: e.tensor_copy(out=xT[:, :, rt * 128:(rt + 1) * 128], in_=pt[:]), [r_pt], [r_xT])
                else:
                    A(lambda e, xT=xT, pt=pt, rt=rt: e.activation(out=xT[:, :, rt * 128:(rt + 1) * 128], in_=pt[:], func=AF.Copy), [r_pt], [r_xT])
            return xT, r_xT

        Wn = wload(0)
        Xn = xprep(0)
        for b in range(NBLK):
            (w1b, r_w1b), (w3b, r_w3b), (w2b, r_w2b) = Wn
            xT, r_xT = Xn
            if b + 1 < NBLK:
                Wn = wload(b + 1)
            aT, r_aT = actr.next()
            for mp in range(2):
                accs = []
                for mi in range(2):
                    accs.append((pacc.next(), pacc.next()))
                for kc in range(8):
                    for mi in range(2):
                        mc = 2 * mp + mi
                        (a1, r_a1), (a3, r_a3) = accs[mi]
                        PE(lambda e, kc=kc, mc=mc, a1=a1, w1b=w1b, xT=xT: e.matmul(a1[:, 0:RB], lhsT=w1b[:, kc * 512 + mc * 128:kc * 512 + (mc + 1) * 128],
                                                                                  rhs=xT[:, kc, :], start=(kc == 0), stop=(kc == 7)), [r_w1b, r_xT], [r_a1])
                        PE(lambda e, kc=kc, mc=mc, a3=a3, w3b=w3b, xT=xT: e.matmul(a3[:, 0:RB], lhsT=w3b[:, kc * 512 + mc * 128:kc * 512 + (mc + 1) * 128],
                                                                                  rhs=xT[:, kc, :], start=(kc == 0), stop=(kc == 7)), [r_w3b, r_xT], [r_a3])
                for mi in range(2):
                    mc = 2 * mp + mi
                    (a1, r_a1), (a3, r_a3) = accs[mi]
                    sl, r_sl = silr.next()
                    A(lambda e, sl=sl, a1=a1: e.activation(out=sl[:], in_=a1[:, 0:RB], func=AF.Silu), [r_a1], [r_sl])
                    V(lambda e, sl=sl, a3=a3, mc=mc, aT=aT: e.tensor_tensor(out=aT[:, mc, :], in0=sl[:], in1=a3[:, 0:RB], op=ALU.mult), [r_sl, r_a3], [r_aT])
            if b + 1 < NBLK:
                Xn = xprep(b + 1)
            yaccs = [[pacc.next() for nch in range(2)] for rt in range(RB // 128)]
            for mc in range(4):
                for rt in range(RB // 128):
                    for nch in range(2):
                        acc, r_acc = yaccs[rt][nch]
                        PE(lambda e, mc=mc, acc=acc, rt=rt, nch=nch, aT=aT, w2b=w2b: e.matmul(
                            acc[:, :], lhsT=aT[:, mc, rt * 128:(rt + 1) * 128], rhs=w2b[:, mc * 1024 + nch * 512:mc * 1024 + (nch + 1) * 512],
                            start=(mc == 0), stop=(mc == 3)), [r_aT, r_w2b], [r_acc])
            for rt in range(RB // 128):
                ys, r_ys = ysr.next()
                for nch in range(2):
                    acc, r_acc = yaccs[rt][nch]
                    if nch == 0:
                        A(lambda e, ys=ys, acc=acc: e.activation(out=ys[:, 0:512], in_=acc[:, :], func=AF.Copy), [r_acc], [r_ys])
                    else:
                        V(lambda e, ys=ys, acc=acc: e.tensor_copy(out=ys[:, 512:1024], in_=acc[:, :]), [r_acc], [r_ys])
                r0 = b * RB + rt * 128
                ld(dr["YS"][r0:r0 + 128, :], ys[:], [r_ys], [rs["YS"]])
        c.close()

    def p6(l):
        c = Ctx(S)
        xr = c.ring(4, [128, D], F32, name="x1")
        y1r = c.ring(4, [128, D], F32, name="y1")
        y2r = c.ring(4, [128, D], F32, name="y2")
        st_ = {}

        def loads(t):
            xt, r_xt = xr.next()
            ld(xt[:], dr["Xs"][t * 128:(t + 1) * 128, :], [rs["Xs"]], [r_xt])
            y1, r_y1 = y1r.next()
            y2, r_y2 = y2r.next()
            for (yy, r_yy, sl, r_sl) in ((y1, r_y1, slot1, r_slot1), (y2, r_y2, slot2, r_slot2)):
                S.dma("pool", lambda e, yy=yy, sl=sl, t=t: e.indirect_dma_start(
                    out=yy[:], out_offset=None, in_=dr["YS"], in_offset=bass.IndirectOffsetOnAxis(ap=sl[:, t:t + 1], axis=0)),
                    [rs["YS"], r_sl], [r_yy])
            st_[t] = (xt, r_xt, y1, r_y1, y2, r_y2)
        PF = 3
        for t in range(min(PF, NT)):
            loads(t)
        for t in range(NT):
            xt, r_xt, y1, r_y1, y2, r_y2 = st_.pop(t)
            A(lambda e, y1=y1, t=t: e.activation(out=y1[:], in_=y1[:], func=AF.Copy, scale=gt1[:, t:t + 1]), [r_y1, r_gt1], [r_y1])
            V(lambda e, y1=y1, y2=y2, t=t: e.scalar_tensor_tensor(out=y2[:], in0=y2[:], scalar=gt2[:, t:t + 1], in1=y1[:], op0=ALU.mult, op1=ALU.add),
              [r_y1, r_y2, r_gt2], [r_y2])
            V(lambda e, y2=y2: e.tensor_tensor(out=y2[:], in0=y2[:], in1=G2b[:], op=ALU.mult), [r_y2, r_G2b], [r_y2])
            V(lambda e, y2=y2, xt=xt: e.tensor_tensor(out=xt[:], in0=y2[:], in1=xt[:], op=ALU.add), [r_y2, r_xt], [r_xt])
            ld(dr["Xs"][t * 128:(t + 1) * 128, :], xt[:], [r_xt], [rs["Xs"]])
            if t + PF < NT:
                loads(t + PF)
        c.close()

    def pfinal():
        c = Ctx(S)
        fgb, r_fgb = c.sb([128, D], F32, "fgb")
        ld(fgb[:], dr["final_g"].partition_broadcast(128), (), [r_fgb], "sp")
        xr = c.ring(3, [128, D], F32, name="xf")
        junk, r_junk = c.sb([128, D], BF16, "junk")
        ss, r_ss = c.sb([128, NT], F32, "ss")
        rstd, r_rstd = c.sb([128, NT], F32, "rstd")
        V(lambda e: e.memset(ss[:], 0.0), (), [r_ss])
        for t in range(NT):
            xt, r_xt = xr.next()
            ld(xt[:], dr["Xs"][t * 128:(t + 1) * 128, :], [rs["Xs"]], [r_xt])
            A(lambda e, xt=xt, t=t: e.activation(out=junk[:], in_=xt[:], func=AF.Square, accum_out=ss[:, t:t + 1]), [r_xt], [r_junk, r_ss])
            rstd_from_ss(ss[:, t:t + 1], rstd[:, t:t + 1], D, r_ss, r_rstd)
            V(lambda e, xt=xt, t=t: e.scalar_tensor_tensor(out=xt[:], in0=xt[:], scalar=rstd[:, t:t + 1], in1=fgb[:], op0=ALU.mult, op1=ALU.mult),
              [r_xt, r_rstd, r_fgb], [r_xt])
            ld(out[t * 128:(t + 1) * 128, :], xt[:], [r_xt], [rs["out"]])
        c.close()

    def finish():
        toks = []
        for r in rs.values():
            toks += r.ws
        S.wait_all("sp", toks)
        S.flush()
        G.close()
        S.es.close()

    setup()
    xin, r_xin = dr["x"], rs["x"]
    done = False
    for l in range(depth):
        for (nm, fn) in (("p0", lambda: p0(l)), ("p1", lambda: p1(l, xin, r_xin)), ("p2", lambda: p2(l)), ("p2b", lambda: p2b(l)),
                         ("p3", lambda: p3(l)), ("p4", lambda: p4(l, xin, r_xin)), ("p4b", lambda: p4b(l)), ("p5", lambda: p5(l)),
                         ("p6", lambda: p6(l))):
            fn()
            if stop == (nm, l):
                done = True
                break
        if done:
            break
        xin, r_xin = dr["Xs"], rs["Xs"]
    if not done and final:
        pfinal()
    finish()
    return nc


def make_inputs(inputs, b):
    f = lambda a: np.ascontiguousarray(np.asarray(a, dtype=np.float32))
    m = {
        "x": f(inputs["x"][b]), "c": f(inputs["c"][b:b + 1]), "w_ada": f(inputs["w_ada"]), "b_ada": f(inputs["b_ada"]),
        "norm1_g": f(inputs["norm1_g"]), "w_in": f(inputs["w_in"]), "attn_norm_g": f(inputs["attn_norm_g"]),
        "hgrn_lb_logits": f(inputs["hgrn_lb_logits"]), "hgrn_norm_g": f(inputs["hgrn_norm_g"]), "w_out": f(inputs["w_out"]),
        "norm2_g": f(inputs["norm2_g"]),
        "router_w": f(np.concatenate([np.asarray(inputs["router_group_w"]), np.asarray(inputs["router_expert_w"])], axis=-1)),
        "router_b": f(np.concatenate([np.asarray(inputs["router_group_b"]), np.asarray(inputs["router_expert_b"])], axis=-1)),
        "final_g": f(np.asarray(inputs["final_g"]).reshape(1, D)),
    }
    for nm, kc, n in (("moe_w1", 8, 512), ("moe_w3", 8, 512), ("moe_w2", 4, D)):
        w = np.asarray(inputs[nm], dtype=np.float32).reshape(DEPTH, NE, kc, 128, n).transpose(0, 1, 3, 2, 4).reshape(DEPTH * NE * 128, kc * n)
        m[nm + "_0"] = np.ascontiguousarray(w[:, :2048])
        m[nm + "_1"] = np.ascontiguousarray(w[:, 2048:])
    m.update(host_consts())
    return m


def kernel(**inputs):
    nc = build()
    in_maps = [make_inputs(inputs, b) for b in range(2)]
    res = run_bass_kernel_spmd(nc, in_maps, core_ids=[0, 1])
    return np.stack([np.asarray(res.results[b]["out"], dtype=np.float32) for b in range(2)], axis=0)
```

```python
import contextlib
import numpy as np
import ml_dtypes
import concourse.bass as bass
import concourse.mybir as mybir
from concourse.bass_utils import run_bass_kernel_spmd

F32 = mybir.dt.float32
BF16 = mybir.dt.bfloat16
I32 = mybir.dt.int32
ALU = mybir.AluOpType
AF = mybir.ActivationFunctionType
AX = mybir.AxisListType

T = 8192
D = 1024
NT = T // 128
DEPTH = 4
NE = 32
RB = 256
NBLK = (2 * T) // RB + NE
NROWS = NBLK * RB
EPS = 1e-6
BIG = 30000.0
PATTERNS = ((128, 1), (512, 4), (2048, 16))
EP_ENG = 30000
EP_DMA = 3000


class Res:
    __slots__ = ("w", "r")

    def __init__(self):
        self.w = None
        self.r = []


class MRes(Res):
    __slots__ = ("ws",)

    def __init__(self):
        super().__init__()
        self.ws = []


def _compress(lst):
    mx = {}
    for (pq, c) in lst:
        mx[pq] = max(mx.get(pq, 0), c)
    return list(mx.items())


class Q:
    def __init__(self, nc, es, name, inc, ep):
        self.nc, self.es, self.name, self.inc, self.ep = nc, es, name, inc, ep
        self.sems = []
        self.count = 0
        self.ops = []
        self.seen = {}

    def sem_for(self, c):
        i = (c - 1) // self.ep
        while len(self.sems) <= i:
            self.sems.append(self.es.enter_context(self.nc.semaphore(f"{self.name}_{len(self.sems)}")))
        return self.sems[i], (((c - 1) % self.ep) + 1) * self.inc


class Sched:
    def __init__(self, nc, ndma=6):
        self.nc = nc
        self.es = contextlib.ExitStack()
        self.q = {n: Q(nc, self.es, "s" + n, 1, EP_ENG) for n in ("pe", "act", "dve", "pool", "sp")}
        self.dslots = {n: [Q(nc, self.es, f"d{n}{i}", 16, EP_DMA) for i in range(ndma)] for n in ("sp", "pool", "act")}
        self.dnext = {n: 0 for n in self.dslots}

    def _deps(self, reads, writes):
        need = {}
        for r in reads:
            if isinstance(r, MRes):
                for (pq, c) in r.ws:
                    need[pq] = max(need.get(pq, 0), c)
            elif r.w is not None:
                need[r.w[0]] = max(need.get(r.w[0], 0), r.w[1])
        for w in writes:
            if (not isinstance(w, MRes)) and w.w is not None:
                need[w.w[0]] = max(need.get(w.w[0], 0), w.w[1])
            for (pq, c) in w.r:
                need[pq] = max(need.get(pq, 0), c)
        return need

    def _commit(self, tok, reads, writes):
        for r in reads:
            r.r.append(tok)
            if len(r.r) > 48:
                r.r = _compress(r.r)
        for w in writes:
            if isinstance(w, MRes):
                w.ws.append(tok)
                if len(w.ws) > 48:
                    w.ws = _compress(w.ws)
            else:
                w.w = tok
                w.r = []

    def _waits(self, q, need):
        waits = []
        for pq, c in need.items():
            if q.seen.get(pq, 0) >= c:
                continue
            q.seen[pq] = c
            waits.append((pq, c))
        return waits

    def op(self, qn, fn, reads=(), writes=()):
        q = self.q[qn]
        waits = self._waits(q, self._deps(reads, writes))
        q.count += 1
        tok = (q, q.count)
        q.ops.append((waits, fn, tok))
        self._commit(tok, reads, writes)
        return tok

    def dma(self, qn, fn, reads=(), writes=()):
        q = self.q[qn]
        sl = self.dslots[qn]
        dq = sl[self.dnext[qn] % len(sl)]
        self.dnext[qn] += 1
        need = self._deps(reads, writes)
        if dq.count > 0:
            need[dq] = max(need.get(dq, 0), dq.count)
        waits = self._waits(q, need)
        dq.count += 1
        tok = (dq, dq.count)
        q.ops.append((waits, fn, tok))
        self._commit(tok, reads, writes)
        return tok

    def wait_all(self, qn, toks):
        q = self.q[qn]
        need = {}
        for (pq, c) in toks:
            need[pq] = max(need.get(pq, 0), c)
        q.ops.append((self._waits(q, need), None, None))

    def flush(self):
        nc = self.nc
        if not any(q.ops for q in self.q.values()):
            return
        with nc.Block() as block:
            def mk(qn):
                q = self.q[qn]

                def body(e):
                    for waits, fn, tok in q.ops:
                        for (pq, c) in waits:
                            s, v = pq.sem_for(c)
                            e.wait_ge(s, v)
                        if fn is not None:
                            s, _ = tok[0].sem_for(tok[1])
                            fn(e).then_inc(s, tok[0].inc)
                    q.ops = []
                return body
            block.tensor(mk("pe"))
            block.scalar(mk("act"))
            block.vector(mk("dve"))
            block.gpsimd(mk("pool"))
            block.sync(mk("sp"))


class Ring:
    def __init__(self, items):
        self.items = items
        self.i = 0

    def next(self):
        it = self.items[self.i % len(self.items)]
        self.i += 1
        return it


_UID = [0]


class Ctx:
    def __init__(self, S):
        self.S = S
        self.es = contextlib.ExitStack()
        self.n = 0

    def sb(self, shape, dt, name=None):
        _UID[0] += 1
        t = self.es.enter_context(self.S.nc.sbuf_tensor(f"{name or 't'}_{_UID[0]}", list(shape), dt))
        return t, Res()

    def ps(self, shape, dt, name=None):
        _UID[0] += 1
        t = self.es.enter_context(self.S.nc.psum_tensor(f"{name or 'p'}_{_UID[0]}", list(shape), dt))
        return t, Res()

    def ring(self, n, shape, dt, psum=False, name=None):
        return Ring([(self.ps if psum else self.sb)(shape, dt, name) for _ in range(n)])

    def close(self):
        self.S.flush()
        self.es.close()


def host_consts():
    c = {}
    c["ident"] = np.eye(128, dtype=np.float32).astype(ml_dtypes.bfloat16)
    kk = np.arange(128)[:, None]
    qq = np.arange(128)[None, :]
    mm = np.zeros((128, 8, 3, 256), np.float32)
    for h in range(8):
        slope = 2.0 ** (-(h + 1))
        for p, (w, d) in enumerate(PATTERNS):
            steps = w // d
            dist_cur = qq - kk
            dist_nxt = 128 + qq - kk
            for j, dist in enumerate((dist_cur, dist_nxt)):
                valid = (dist >= 0) & (dist <= steps)
                mm[:, h, p, j * 128:(j + 1) * 128] = np.where(valid, -slope * d * dist, -BIG)
    mm2 = np.concatenate([mm[..., 128:256], mm[..., 0:128]], axis=-1)
    c["amask"] = np.exp(mm2.astype(np.float64)).astype(np.float32).reshape(128, 8 * 3 * 256).astype(ml_dtypes.bfloat16)
    c["causal"] = ((kk <= qq) & ((kk // 64) == (qq // 64))).astype(np.uint8)
    c["ustrict"] = (kk < qq).astype(np.float32).astype(ml_dtypes.bfloat16)
    rm = np.ones((128, 2048), np.float32)
    rm[:, ::128] = 0.0
    c["resetm"] = rm
    c["mulrow"] = np.tile((np.arange(64, dtype=np.float32) * RB)[None, :], (128, 1))
    c["brow"] = np.tile(np.arange(NBLK, dtype=np.float32)[None, :], (128, 1))
    c["piota"] = np.arange(128, dtype=np.float32).reshape(128, 1)
    return c


CONST_SPECS = {"ident": ([128, 128], BF16), "amask": ([128, 8 * 3 * 256], BF16), "causal": ([128, 128], mybir.dt.uint8),
               "ustrict": ([128, 128], BF16), "resetm": ([128, 2048], F32), "mulrow": ([128, 64], F32), "brow": ([128, NBLK], F32), "piota": ([128, 1], F32)}

IN_SPECS = {
    "x": ([T, D], F32), "c": ([1, D], F32), "w_ada": ([DEPTH, D, 6 * D], F32), "b_ada": ([DEPTH, 6 * D], F32),
    "norm1_g": ([DEPTH, D], F32), "w_in": ([DEPTH, D, 3584], F32), "attn_norm_g": ([DEPTH, 512], F32),
    "hgrn_lb_logits": ([DEPTH, 512], F32), "hgrn_norm_g": ([DEPTH, 512], F32), "w_out": ([DEPTH, D, D], F32),
    "norm2_g": ([DEPTH, D], F32), "router_w": ([DEPTH, D, 36], F32), "router_b": ([DEPTH, 36], F32),
    "moe_w1_0": ([DEPTH * NE * 128, 2048], F32), "moe_w1_1": ([DEPTH * NE * 128, 2048], F32),
    "moe_w3_0": ([DEPTH * NE * 128, 2048], F32), "moe_w3_1": ([DEPTH * NE * 128, 2048], F32),
    "moe_w2_0": ([DEPTH * NE * 128, 2048], F32), "moe_w2_1": ([DEPTH * NE * 128, 2048], F32),
    "final_g": ([1, D], F32),
}


def build(depth=DEPTH, stop=None, dumps=(), final=True):
    nc = bass.Bass("TRN2", target_bir_lowering=False)
    S = Sched(nc)
    dr = {}
    for k, (shp, dt) in list(IN_SPECS.items()) + list(CONST_SPECS.items()):
        dr[k] = nc.dram_tensor(k, shp, dt, kind="ExternalInput").ap()
    out = nc.dram_tensor("out", [T, D], F32, kind="ExternalOutput").ap()
    rs = {}

    def scratch(name, shp, dt):
        kind = "ExternalOutput" if name in dumps else "Internal"
        dr[name] = nc.dram_tensor(name, shp, dt, kind=kind).ap()
        rs[name] = MRes()
    scratch("Xs", [T, D], F32)
    scratch("QT", [512, T], BF16)
    scratch("KT", [512, T], BF16)
    scratch("VV", [T, 512], BF16)
    scratch("RQT", [512, T], BF16)
    scratch("ZT", [512, T], F32)
    scratch("RI", [T, 512], BF16)
    scratch("RG", [T, 512], BF16)
    for p in range(3):
        scratch(f"OP{p}", [T, 2, 4 * 65], F32)
    scratch("CAT", [T, D], BF16)
    scratch("H2", [T, D], BF16)
    scratch("XS", [NROWS, D], BF16)
    scratch("YS", [NROWS, D], F32)
    rs["x"] = MRes()
    rs["out"] = MRes()

    G = Ctx(S)
    ident, r_ident = G.sb([128, 128], BF16, "ident")
    causal, r_causal = G.sb([128, 128], mybir.dt.uint8, "causal")
    ustrict, r_ustrict = G.sb([128, 128], BF16, "ustrict")
    onesb, r_onesb = G.sb([128, 128], BF16, "onesb")
    mulrow, r_mulrow = G.sb([128, 64], F32, "mulrow")
    brow, r_brow = G.sb([128, NBLK], F32, "brow")
    piota, r_piota = G.sb([128, 1], F32, "piota")
    widx, r_widx = G.sb([128, NBLK], I32, "widx")
    one11, r_one11 = G.sb([1, 1], F32, "one11")
    onesrow, r_onesrow = G.sb([1, 128], F32, "onesrow")
    colf, r_colf = G.sb([128, 16], F32, "colf")
    G1b, r_G1b = G.sb([128, D], F32, "G1b")
    A2b, r_A2b = G.sb([128, D], F32, "A2b")
    S2b, r_S2b = G.sb([128, D], F32, "S2b")
    G2b, r_G2b = G.sb([128, D], F32, "G2b")
    lbc, r_lbc = G.sb([128, DEPTH, 4], F32, "lbc")
    oml, r_oml = G.sb([128, DEPTH, 4], F32, "oml")
    noml, r_noml = G.sb([128, DEPTH, 4], F32, "noml")
    slot1, r_slot1 = G.sb([128, NT], I32, "slot1")
    slot2, r_slot2 = G.sb([128, NT], I32, "slot2")
    gt1, r_gt1 = G.sb([128, NT], F32, "gt1")
    gt2, r_gt2 = G.sb([128, NT], F32, "gt2")
    lgall, r_lgall = G.sb([128, NT, 36], F32, "lgall")

    dma_rr = [0]

    def ld(out_, in_, reads=(), writes=(), q=None):
        if q is None:
            q = "sp"
        return S.dma(q, lambda e: e.dma_start(out=out_, in_=in_), reads, writes)

    def V(fn, r=(), w=()):
        return S.op("dve", fn, r, w)

    def A(fn, r=(), w=()):
        return S.op("act", fn, r, w)

    def PE(fn, r=(), w=()):
        return S.op("pe", fn, r, w)

    def GP(fn, r=(), w=()):
        return S.op("pool", fn, r, w)

    def setup():
        ld(ident[:], dr["ident"], (), [r_ident], "sp")
        ld(causal[:], dr["causal"], (), [r_causal], "sp")
        ld(ustrict[:], dr["ustrict"], (), [r_ustrict], "sp")
        ld(mulrow[:], dr["mulrow"], (), [r_mulrow], "sp")
        ld(brow[:], dr["brow"], (), [r_brow], "sp")
        ld(piota[:], dr["piota"], (), [r_piota], "sp")
        V(lambda e: e.memset(one11[:], 1.0), (), [r_one11])
        V(lambda e: e.memset(onesrow[:], 1.0), (), [r_onesrow])
        V(lambda e: e.memset(onesb[:], 1.0), (), [r_onesb])
        c = Ctx(S)
        lg, r_lg = c.sb([128, DEPTH, 4], F32)
        ex, r_ex = c.sb([128, DEPTH, 4], F32)
        sm, r_sm = c.sb([128, 4], F32)
        S.dma("sp", lambda e: e.dma_start(out=lg[:], in_=dr["hgrn_lb_logits"].rearrange("l (h k) -> k l h", k=128),
                                          allow_slow_non_contiguous=True), (), [r_lg])
        A(lambda e: e.activation(out=ex[:], in_=lg[:], func=AF.Exp), [r_lg], [r_ex])
        V(lambda e: e.tensor_tensor(out=sm[:], in0=ex[:, 0, :], in1=ex[:, 1, :], op=ALU.add), [r_ex], [r_sm])
        V(lambda e: e.tensor_tensor(out=sm[:], in0=sm[:], in1=ex[:, 2, :], op=ALU.add), [r_ex, r_sm], [r_sm])
        V(lambda e: e.tensor_tensor(out=sm[:], in0=sm[:], in1=ex[:, 3, :], op=ALU.add), [r_ex, r_sm], [r_sm])
        V(lambda e: e.reciprocal(out=sm[:], in_=sm[:]), [r_sm], [r_sm])
        V(lambda e: e.memset(lbc[:, 0, :], 0.0), (), [r_lbc])
        V(lambda e: e.tensor_tensor(out=lbc[:, 1, :], in0=ex[:, 1, :], in1=sm[:], op=ALU.mult), [r_ex, r_sm], [r_lbc])
        for l in (2, 3):
            V(lambda e, l=l: e.tensor_tensor(out=ex[:, l, :], in0=ex[:, l, :], in1=sm[:], op=ALU.mult), [r_ex, r_sm], [r_ex])
            V(lambda e, l=l: e.tensor_tensor(out=lbc[:, l, :], in0=lbc[:, l - 1, :], in1=ex[:, l, :], op=ALU.add), [r_ex, r_lbc], [r_lbc])
        V(lambda e: e.tensor_scalar(out=oml[:], in0=lbc[:], scalar1=-1.0, scalar2=1.0, op0=ALU.mult, op1=ALU.add), [r_lbc], [r_oml])
        V(lambda e: e.tensor_scalar(out=noml[:], in0=lbc[:], scalar1=1.0, scalar2=-1.0, op0=ALU.mult, op1=ALU.add), [r_lbc], [r_noml])
        c.close()

    def p0(l):
        c = Ctx(S)
        crow, r_crow = c.sb([1, D], F32)
        cactc, r_cactc = c.sb([128, 8], F32)
        modrow, r_mod = c.sb([1, 6 * D], F32)
        brow, r_brow = c.sb([1, 6 * D], F32)
        g1row, r_g1 = c.sb([1, D], F32)
        g2row, r_g2 = c.sb([1, D], F32)
        wring = c.ring(2, [128, 8, 512], F32)
        pcol, r_pcol = c.ps([128, 16], F32)
        pacc = c.ring(2, [128, 512], F32, psum=True)
        ld(crow[:], dr["c"], (), [r_crow], "sp")
        ld(brow[:], dr["b_ada"][l:l + 1, :], (), [r_brow], "sp")
        ld(g1row[:], dr["norm1_g"][l:l + 1, :], (), [r_g1], "sp")
        ld(g2row[:], dr["norm2_g"][l:l + 1, :], (), [r_g2], "sp")
        A(lambda e: e.activation(out=crow[:], in_=crow[:], func=AF.Silu), [r_crow], [r_crow])
        for kc in range(8):
            PE(lambda e, kc=kc: e.matmul(pcol[:, kc:kc + 1], lhsT=crow[0:1, kc * 128:(kc + 1) * 128], rhs=one11[0:1, 0:1],
                                         start=True, stop=True), [r_crow, r_one11], [r_pcol])
        V(lambda e: e.tensor_copy(out=cactc[:], in_=pcol[:, 0:8]), [r_pcol], [r_cactc])
        for n in range(12):
            wt, r_wt = wring.next()
            ld(wt[:], dr["w_ada"][l, :, n * 512:(n + 1) * 512].rearrange("(kc p) n -> p kc n", p=128), (), [r_wt])
            acc, r_acc = pacc.next()
            for kc in range(8):
                PE(lambda e, kc=kc, acc=acc, wt=wt: e.matmul(acc[0:1, :], lhsT=cactc[:, kc:kc + 1], rhs=wt[:, kc, :],
                                                            start=(kc == 0), stop=(kc == 7)), [r_cactc, r_wt], [r_acc])
            V(lambda e, n=n, acc=acc: e.tensor_tensor(out=modrow[0:1, n * 512:(n + 1) * 512], in0=acc[0:1, :],
                                                       in1=brow[0:1, n * 512:(n + 1) * 512], op=ALU.add), [r_acc, r_brow], [r_mod])
        V(lambda e: e.scalar_tensor_tensor(out=g1row[:], in0=modrow[0:1, D:2 * D], scalar=1.0, in1=g1row[:], op0=ALU.add, op1=ALU.mult),
          [r_mod, r_g1], [r_g1])
        V(lambda e: e.scalar_tensor_tensor(out=g2row[:], in0=modrow[0:1, 4 * D:5 * D], scalar=1.0, in1=g2row[:], op0=ALU.add, op1=ALU.mult),
          [r_mod, r_g2], [r_g2])
        for kc in range(8):
            PE(lambda e, kc=kc: e.matmul(pcol[:, kc:kc + 1], lhsT=g1row[0:1, kc * 128:(kc + 1) * 128], rhs=one11[0:1, 0:1],
                                         start=True, stop=True), [r_g1, r_one11], [r_pcol])
            PE(lambda e, kc=kc: e.matmul(pcol[:, 8 + kc:9 + kc], lhsT=modrow[0:1, kc * 128:(kc + 1) * 128], rhs=one11[0:1, 0:1],
                                         start=True, stop=True), [r_mod, r_one11], [r_pcol])
        V(lambda e: e.tensor_copy(out=colf[:], in_=pcol[:]), [r_pcol], [r_colf])
        for (src, r_src, off, dst, r_dst) in ((modrow, r_mod, 2 * D, G1b, r_G1b), (g2row, r_g2, 0, A2b, r_A2b),
                                              (modrow, r_mod, 3 * D, S2b, r_S2b), (modrow, r_mod, 5 * D, G2b, r_G2b)):
            for nch in range(2):
                acc, r_acc = pacc.next()
                PE(lambda e, acc=acc, src=src, off=off, nch=nch: e.matmul(acc[:, :], lhsT=onesrow[0:1, :],
                                                                         rhs=src[0:1, off + nch * 512:off + (nch + 1) * 512],
                                                                         start=True, stop=True), [r_src, r_onesrow], [r_acc])
                V(lambda e, acc=acc, dst=dst, nch=nch: e.tensor_copy(out=dst[:, nch * 512:(nch + 1) * 512], in_=acc[:, :]), [r_acc], [r_dst])
        c.close()

    def rstd_from_ss(ss_ap, out_ap, n, r_ss, r_out):
        V(lambda e: e.tensor_scalar(out=out_ap, in0=ss_ap, scalar1=1.0 / n, scalar2=EPS, op0=ALU.mult, op1=ALU.add), [r_ss], [r_out])
        A(lambda e: e.activation(out=out_ap, in_=out_ap, func=AF.Sqrt), [r_out], [r_out])
        V(lambda e: e.reciprocal(out=out_ap, in_=out_ap), [r_out], [r_out])

    def p1(l, xin, r_xin):
        c = Ctx(S)
        winb, r_winb = c.sb([128, 8, 3584], BF16, "winb")
        wst = c.ring(3, [128, 1792], F32, name="wst")
        kk_ = 0
        for kc in range(8):
            for hf in range(2):
                st, r_st = wst.next()
                ld(st[:], dr["w_in"][l, kc * 128:(kc + 1) * 128, hf * 1792:(hf + 1) * 1792], (), [r_st])
                kk_ += 1
                if kk_ % 2 == 0:
                    V(lambda e, st=st, kc=kc, hf=hf: e.tensor_copy(out=winb[:, kc, hf * 1792:(hf + 1) * 1792], in_=st[:]), [r_st], [r_winb])
                else:
                    GP(lambda e, st=st, kc=kc, hf=hf: e.tensor_copy(out=winb[:, kc, hf * 1792:(hf + 1) * 1792], in_=st[:]), [r_st], [r_winb])
        xring = c.ring(6, [128, D], F32, name="xt")
        junk, r_junk = c.sb([128, D], BF16, "junk")
        xnring = c.ring(6, [128, D], BF16, name="xn")
        ss, r_ss = c.sb([128, NT], F32, "ss")
        rstd, r_rstd = c.sb([128, NT], F32, "rstd")
        hTring = c.ring(2, [128, 8, 512], BF16, name="hT")
        ptr = c.ring(2, [128, 8, 128], BF16, psum=True, name="ptr")
        pacc = c.ring(6, [128, 512], F32, psum=True, name="pacc")
        stb = c.ring(6, [128, 512], BF16, name="stb")
        stf = c.ring(4, [128, 512], F32, name="stf")
        V(lambda e: e.memset(ss[:], 0.0), (), [r_ss])
        fm = []
        for m in range(4):
            fm.append((m * 128, "QT", m * 128, "q"))
        for m in range(4):
            fm.append((512 + m * 128, "KT", m * 128, "b"))
        for m in range(4):
            fm.append((1536 + m * 128, "RQT", m * 128, "b"))
        for m in range(4):
            fm.append((2048 + m * 128, "ZT", m * 128, "f"))
        tm = [(1024, "VV", "b"), (2560, "RI", "b"), (3072, "RG", "s")]
        ev = [0]

        def prep(g):
            hT, r_hT = hTring.next()
            tiles = []
            for j in range(4):
                t = g * 4 + j
                xt, r_xt = xring.next()
                ld(xt[:], xin[t * 128:(t + 1) * 128, :], [r_xin], [r_xt])
                A(lambda e, xt=xt, t=t: e.activation(out=junk[:], in_=xt[:], func=AF.Square, accum_out=ss[:, t:t + 1]), [r_xt], [r_junk, r_ss])
                rstd_from_ss(ss[:, t:t + 1], rstd[:, t:t + 1], D, r_ss, r_rstd)
                xn, r_xn = xnring.next()
                A(lambda e, xt=xt, xn=xn, t=t: e.activation(out=xn[:], in_=xt[:], func=AF.Copy, scale=rstd[:, t:t + 1]), [r_xt, r_rstd], [r_xn])
                tiles.append((xn, r_xn))
            for jp in range(2):
                pair = [(tiles[2 * jp + i], ptr.next()) for i in range(2)]
                for kc in range(8):
                    for ((xn, r_xn), (pt, r_pt)) in pair:
                        PE(lambda e, kc=kc, pt=pt, xn=xn: e.transpose(out=pt[:, kc, :], in_=xn[:, kc * 128:(kc + 1) * 128], identity=ident[:]),
                           [r_xn, r_ident], [r_pt])
                for i, ((xn, r_xn), (pt, r_pt)) in enumerate(pair):
                    j = 2 * jp + i
                    V(lambda e, pt=pt, hT=hT, j=j: e.tensor_tensor(out=hT[:, :, j * 128:(j + 1) * 128], in0=pt[:, :, :],
                                                                  in1=colf[:, 0:8].unsqueeze(2).to_broadcast([128, 8, 128]), op=ALU.mult),
                      [r_pt, r_colf], [r_hT])
                    GP(lambda e, hT=hT, j=j: e.tensor_tensor(out=hT[:, :, j * 128:(j + 1) * 128], in0=hT[:, :, j * 128:(j + 1) * 128],
                                                            in1=colf[:, 8:16].unsqueeze(2).to_broadcast([128, 8, 128]), op=ALU.add),
                       [r_hT, r_colf], [r_hT])
            return hT, r_hT

        def evac(kind, acc, r_acc):
            st, r_st = (stf if kind == "f" else stb).next()
            ev[0] += 1
            if kind == "q":
                A(lambda e: e.activation(out=st[:], in_=acc[:], func=AF.Copy, scale=0.125), [r_acc], [r_st])
            elif kind == "s":
                A(lambda e: e.activation(out=st[:], in_=acc[:], func=AF.Silu), [r_acc], [r_st])
            elif ev[0] % 2 == 0:
                A(lambda e: e.activation(out=st[:], in_=acc[:], func=AF.Copy), [r_acc], [r_st])
            else:
                V(lambda e: e.tensor_copy(out=st[:], in_=acc[:]), [r_acc], [r_st])
            return st, r_st

        nxt = prep(0)
        for g in range(NT // 4):
            hT, r_hT = nxt
            if g + 1 < NT // 4:
                nxt = prep(g + 1)
            for f0 in range(0, 16, 4):
                grp = [(fm[f0 + i], pacc.next()) for i in range(4)]
                for kc in range(8):
                    for ((c0, dn, row0, kind), (acc, r_acc)) in grp:
                        PE(lambda e, kc=kc, acc=acc, hT=hT, c0=c0: e.matmul(acc[:, :], lhsT=winb[:, kc, c0:c0 + 128], rhs=hT[:, kc, :],
                                                                           start=(kc == 0), stop=(kc == 7)), [r_winb, r_hT], [r_acc])
                for ((c0, dn, row0, kind), (acc, r_acc)) in grp:
                    st, r_st = evac(kind, acc, r_acc)
                    ld(dr[dn][row0:row0 + 128, g * 512:(g + 1) * 512], st[:], [r_st], [rs[dn]])
            for j in range(4):
                t = g * 4 + j
                grp = [(tm[i], pacc.next()) for i in range(3)]
                for kc in range(8):
                    for ((c0, dn, kind), (acc, r_acc)) in grp:
                        PE(lambda e, kc=kc, acc=acc, hT=hT, c0=c0, j=j: e.matmul(acc[:, :], lhsT=hT[:, kc, j * 128:(j + 1) * 128],
                                                                                 rhs=winb[:, kc, c0:c0 + 512], start=(kc == 0), stop=(kc == 7)),
                           [r_winb, r_hT], [r_acc])
                for ((c0, dn, kind), (acc, r_acc)) in grp:
                    st, r_st = evac(kind, acc, r_acc)
                    ld(dr[dn][t * 128:(t + 1) * 128, :], st[:], [r_st], [rs[dn]])
        c.close()

    def p2(l):
        c = Ctx(S)
        qT2, r_qT2 = c.sb([128, 2, T], BF16, "qT2")
        kT2, r_kT2 = c.sb([128, 2, T], BF16, "kT2")
        amask, r_amask = c.sb([128, 4, 3, 256], BF16, "amask")
        vraw = c.ring(2, [128, 8, 256], BF16, name="vraw")
        vaugr = c.ring(2, [128, 64, 4, 65], BF16, name="vaug")
        pTr = c.ring(8, [128, 256], BF16, name="pT")
        per_ = c.ring(4, [128, 256], BF16, name="pe")
        ostr = c.ring(3, [128, 4, 65], F32, name="ost")
        Sps = c.ring(4, [128, 256], F32, psum=True, name="Sps")
        OpsE = c.ring(2, [128, 2, 65], F32, psum=True, name="OpsE")
        OpsO = c.ring(2, [128, 2, 65], F32, psum=True, name="OpsO")
        ostr = c.ring(4, [128, 4, 65], F32, name="ost2")
        for (vg_, r_vg_) in vaugr.items:
            GP(lambda e, vg_=vg_: e.memset(vg_[:, :, :, 64:65], 1.0), (), [r_vg_])
        for half in range(2):
            ld(amask[:], dr["amask"].rearrange("k (h p q) -> k h p q", h=8, p=3)[:, 4 * half:4 * half + 4], (), [r_amask], "sp")
            for hpi in range(2):
                hp = 2 * half + hpi
                ld(qT2[:, hpi, :], dr["QT"][hp * 128:(hp + 1) * 128, :], [rs["QT"]], [r_qT2])
                ld(kT2[:, hpi, :], dr["KT"][hp * 128:(hp + 1) * 128, :], [rs["KT"]], [r_kT2])
            for p, (w, d) in enumerate(PATTERNS):
                span = 128 * d
                nb = T // span
                vaug, r_vaug = vaugr.next()
                vsrc = dr["VV"].rearrange("(a u r) c -> r u a c", u=128, r=d)
                for r in range(d):
                    for a0 in range(0, nb, 8):
                        na = min(8, nb - a0)
                        vr, r_vr = vraw.next()
                        ld(vr[:, 0:na, :], vsrc[r, :, a0:a0 + na, half * 256:(half + 1) * 256], [rs["VV"]], [r_vr])
                        bi0 = r * nb + a0
                        GP(lambda e, vr=vr, na=na, bi0=bi0, vaug=vaug: e.tensor_copy(out=vaug[:, bi0:bi0 + na, :, 0:64],
                                                                         in_=vr[:, 0:na, :].rearrange("k a (h c) -> k a h c", h=4)),
                           [r_vr], [r_vaug])
                odst = dr[f"OP{p}"].rearrange("(a u r) h c -> r a u h c", u=128, r=d)
                units = [(r, a) for r in range(d) for a in range(nb)]

                def s_stage(r, a, hh, d=d, p=p, span=span, half=half):
                    hpi, pb = hh // 2, 64 * (hh % 2)
                    t0 = span * a + r
                    sp_, r_sp = Sps.next()
                    q_ap = qT2[pb:pb + 64, hpi, t0:t0 + 127 * d + 1:d]
                    c0 = 0 if a > 0 else 128
                    th = []
                    if a > 0:
                        tp = t0 - span
                        th.append(lambda: PE(lambda e: e.matmul(sp_[:, 0:128], lhsT=kT2[pb:pb + 64, hpi, tp:tp + 127 * d + 1:d], rhs=q_ap,
                                                                start=True, stop=True), [r_kT2, r_qT2], [r_sp]))
                    th.append(lambda: PE(lambda e: e.matmul(sp_[:, 128:256], lhsT=kT2[pb:pb + 64, hpi, t0:t0 + 127 * d + 1:d], rhs=q_ap,
                                                            start=True, stop=True), [r_kT2, r_qT2], [r_sp]))
                    pe, r_pe = per_.next()
                    pT, r_pT = pTr.next()

                    def post():
                        A(lambda e: e.activation(out=pe[:, c0:256], in_=sp_[:, c0:256], func=AF.Exp), [r_sp], [r_pe])
                        V(lambda e: e.tensor_tensor(out=pT[:, c0:256], in0=pe[:, c0:256], in1=amask[:, hh, p, c0:256], op=ALU.mult),
                          [r_pe, r_amask], [r_pT])
                    return th, post, pT, r_pT

                def pv_thunks(r, a, hh, pT, r_pT, O, r_O, nb=nb, vaug=vaug, r_vaug=r_vaug):
                    bi = r * nb + a
                    hs = hh // 2
                    th = []
                    if a > 0:
                        th.append(lambda: PE(lambda e: e.matmul(O[:, hs, :], lhsT=pT[:, 0:128], rhs=vaug[:, bi - 1, hh, :], start=True, stop=False),
                                             [r_pT, r_vaug], [r_O]))
                    th.append(lambda: PE(lambda e: e.matmul(O[:, hs, :], lhsT=pT[:, 128:256], rhs=vaug[:, bi, hh, :], start=(a == 0), stop=True),
                                         [r_pT, r_vaug], [r_O]))
                    return th

                def interleave(lists):
                    out_ = []
                    n = max(len(x) for x in lists) if lists else 0
                    for i in range(n):
                        for x in lists:
                            if i < len(x):
                                out_.append(x[i])
                    return out_

                def finish_block(r, a, OE, r_OE, OO, r_OO):
                    ost, r_ost = ostr.next()
                    V(lambda e: e.tensor_copy(out=ost[:, 0:4:2, :], in_=OE[:]), [r_OE], [r_ost])
                    V(lambda e: e.tensor_copy(out=ost[:, 1:4:2, :], in_=OO[:]), [r_OO], [r_ost])
                    ld(odst[r, a, :, half, :], ost[:].rearrange("k h c -> k (h c)"), [r_ost], [rs[f"OP{p}"]])

                flat = [(r, a, hh) for (r, a) in units for hh in range(4)]
                pairs = [flat[i:i + 2] for i in range(0, len(flat), 2)]
                Ocur = {}
                pvq = []

                def merge2(a_, b_):
                    out_ = []
                    ia = ib = 0
                    while ia < len(a_) or ib < len(b_):
                        out_ += a_[ia:ia + 2]
                        ia += 2
                        out_ += b_[ib:ib + 2]
                        ib += 2
                    return out_

                for pr in pairs:
                    st = [s_stage(*u) for u in pr]
                    s_th = interleave([x[0] for x in st])
                    if len(pvq) >= 2:
                        pv_now, fin_now = pvq.pop(0)
                    else:
                        pv_now, fin_now = [], []
                    for f_ in merge2(s_th, pv_now):
                        f_()
                    for fb in fin_now:
                        finish_block(*fb)
                    for x in st:
                        x[1]()
                    fin = []
                    pvl = []
                    for (r, a, hh), x in zip(pr, st):
                        if hh == 0:
                            Ocur[(r, a)] = (OpsE.next(), OpsO.next())
                        (OE, r_OE), (OO, r_OO) = Ocur[(r, a)]
                        O, r_O = (OE, r_OE) if hh % 2 == 0 else (OO, r_OO)
                        pvl.append(pv_thunks(r, a, hh, x[2], x[3], O, r_O))
                        if hh == 3:
                            fin.append((r, a, OE, r_OE, OO, r_OO))
                            del Ocur[(r, a)]
                    pvq.append((interleave(pvl), fin))
                for (pv_now, fin_now) in pvq:
                    for f_ in pv_now:
                        f_()
                    for fb in fin_now:
                        finish_block(*fb)
        c.close()

    def p2b(l):
        c = Ctx(S)
        angb, r_angb = c.sb([128, 512], F32, "angb")
        ld(angb[:], dr["attn_norm_g"][l:l + 1, :].partition_broadcast(128), (), [r_angb], "sp")
        opr = [c.ring(5, [128, 8, 65], F32, name=f"op{p}") for p in range(3)]
        den, r_den = c.sb([128, 8], F32, "den")
        o, r_o = c.sb([128, 8, 64], F32, "o")
        junk, r_junk = c.sb([128, 512], BF16, "junk")
        ss, r_ss = c.sb([128, NT], F32, "ss")
        rstd, r_rstd = c.sb([128, NT], F32, "rstd")
        cst = c.ring(3, [128, 512], BF16, name="cst")
        V(lambda e: e.memset(ss[:], 0.0), (), [r_ss])
        pend_ = {}

        def loads(t):
            tl = []
            for p in range(3):
                tt, r_tt = opr[p].next()
                ld(tt[:].rearrange("k h c -> k (h c)"), dr[f"OP{p}"][t * 128:(t + 1) * 128].rearrange("k a c -> k (a c)"), [rs[f"OP{p}"]], [r_tt])
                tl.append((tt, r_tt))
            pend_[t] = tl
        for t in range(min(3, NT)):
            loads(t)
        for t in range(NT):
            tl = pend_.pop(t)
            if t + 3 < NT:
                loads(t + 3)
            (t0_, r0), (t1_, r1), (t2_, r2) = tl
            V(lambda e, a=t0_, b=t1_: e.tensor_tensor(out=a[:], in0=a[:], in1=b[:], op=ALU.add), [r0, r1], [r0])
            V(lambda e, a=t0_, b=t2_: e.tensor_tensor(out=a[:], in0=a[:], in1=b[:], op=ALU.add), [r0, r2], [r0])
            V(lambda e, a=t0_: e.reciprocal(out=den[:], in_=a[:, :, 64]), [r0], [r_den])
            V(lambda e, a=t0_: e.tensor_tensor(out=o[:], in0=a[:, :, 0:64], in1=den[:].unsqueeze(2).to_broadcast([128, 8, 64]), op=ALU.mult),
              [r0, r_den], [r_o])
            A(lambda e, t=t: e.activation(out=junk[:], in_=o[:].rearrange("k h c -> k (h c)"), func=AF.Square, accum_out=ss[:, t:t + 1]),
              [r_o], [r_junk, r_ss])
            rstd_from_ss(ss[:, t:t + 1], rstd[:, t:t + 1], 512, r_ss, r_rstd)
            cs, r_cs = cst.next()
            V(lambda e, cs=cs, t=t: e.scalar_tensor_tensor(out=cs[:], in0=o[:].rearrange("k h c -> k (h c)"), scalar=rstd[:, t:t + 1],
                                                          in1=angb[:], op0=ALU.mult, op1=ALU.mult), [r_o, r_rstd, r_angb], [r_cs])
            ld(dr["CAT"][t * 128:(t + 1) * 128, 0:512], cs[:], [r_cs], [rs["CAT"]])
        c.close()

    def p3(l):
        c = Ctx(S)
        SEG = 512
        NCK = SEG // 128
        NSEG = T // SEG
        resetm, r_resetm = c.sb([128, SEG], F32, "resetm")
        gnb, r_gnb = c.sb([128, 512], F32, "gnb")
        ld(resetm[:], dr["resetm"][:, 0:SEG], (), [r_resetm], "sp")
        ld(gnb[:], dr["hgrn_norm_g"][l:l + 1, :].partition_broadcast(128), (), [r_gnb], "sp")
        rir = c.ring(2, [128, NCK, 512], BF16, name="ri")
        rgr = c.ring(2, [128, NCK, 512], BF16, name="rgm")
        catr_ = c.ring(2, [128, NCK, 512], BF16, name="catseg")
        zr = c.ring(2, [128, SEG], F32, name="z")
        qr = c.ring(2, [128, SEG], BF16, name="q")
        tset = [[c.sb([128, SEG], F32, f"b{n}{i}") for n in "ABCE"] for i in range(2)]
        o4 = c.ring(8, [128, 5, SEG], BF16, name="o4")
        dcyr = c.ring(8, [128, NCK], F32, name="dcy")
        St = [c.sb([128, 128], F32, f"S{h}") for h in range(4)]
        Sb = [c.sb([128, 128], BF16, f"Sb{h}") for h in range(4)]
        Amr = c.ring(4, [128, 128], BF16, name="Am")
        khTr = c.ring(4, [128, 128], BF16, name="khT")
        junk, r_junk = c.sb([128, 128], BF16, "junk")
        ssr, r_ssr = c.sb([128, 4 * NT], F32, "ssr")
        rsr, r_rsr = c.sb([128, 4 * NT], F32, "rsr")
        psA = c.ring(2, [128, 128], F32, psum=True, name="psA")
        psT = c.ring(1, [128, 128], BF16, psum=True, name="psT")
        psU = c.ring(2, [128, 128], F32, psum=True, name="psU")
        psO = c.ring(2, [128, 128], F32, psum=True, name="psO")
        psX = c.ring(1, [64, 64], F32, psum=True, name="psX")
        V(lambda e: e.memset(ssr[:], 0.0), (), [r_ssr])
        for h in range(4):
            V(lambda e, h=h: e.memset(St[h][0][:], 0.0), (), [St[h][1]])
            V(lambda e, h=h: e.memset(Sb[h][0][:], 0.0), (), [Sb[h][1]])

        def v3(t):
            t = t if isinstance(t, bass.AP) else t[:]
            return t.rearrange("k (c u) -> k c u", u=128)

        def v64(t):
            return t[:].rearrange("k (c u) -> k c u", u=64)

        def ew(sg):
            tk0 = sg * SEG
            ri, r_ri = rir.next()
            rgm, r_rgm = rgr.next()
            ld(ri[:], dr["RI"][tk0:tk0 + SEG, :].rearrange("(c p) f -> p c f", p=128), [rs["RI"]], [r_ri])
            ld(rgm[:], dr["RG"][tk0:tk0 + SEG, :].rearrange("(c p) f -> p c f", p=128), [rs["RG"]], [r_rgm])
            GP(lambda e: e.tensor_tensor(out=rgm[:], in0=rgm[:], in1=gnb[:].unsqueeze(1).to_broadcast([128, NCK, 512]), op=ALU.mult),
               [r_rgm, r_gnb], [r_rgm])
            heads = []

            def head_ops(hd):
                (bA, r_A), (bB, r_B), (bC, r_C), (bE, r_E) = tset[hd % 2]
                ops = []
                AA = lambda *a: ops.append(lambda: A(*a))
                VV = lambda *a: ops.append(lambda: V(*a))
                GG = lambda *a: ops.append(lambda: GP(*a))
                z, r_z = zr.next()
                q, r_q = qr.next()
                ld(z[:], dr["ZT"][hd * 128:(hd + 1) * 128, tk0:tk0 + SEG], [rs["ZT"]], [r_z])
                ld(q[:], dr["RQT"][hd * 128:(hd + 1) * 128, tk0:tk0 + SEG], [rs["RQT"]], [r_q])
                o, r_o4 = o4.next()
                dcy, r_dcy = dcyr.next()
                lb_ap, oml_ap = lbc[:, l, hd:hd + 1], oml[:, l, hd:hd + 1]

                def b3(t):
                    return t[:].rearrange("k (c u) -> k c u", u=128)

                def b64(t):
                    return t[:].rearrange("k (c u) -> k c u", u=64)
                VV(lambda e: e.tensor_scalar(out=z[:], in0=z[:], scalar1=-60.0, scalar2=None, op0=ALU.max), [r_z], [r_z])
                AA(lambda e: e.activation(out=bA[:], in_=z[:], func=AF.Exp, scale=-1.0), [r_z], [r_A])
                AA(lambda e: e.activation(out=bB[:], in_=bA[:], func=AF.Ln, bias=1.0), [r_A], [r_B])
                AA(lambda e: e.activation(out=bE[:], in_=bA[:], func=AF.Ln, bias=1.0, scale=lb_ap), [r_A, r_lbc], [r_E])
                VV(lambda e: e.tensor_tensor(out=bE[:], in0=bE[:], in1=bB[:], op=ALU.subtract), [r_E, r_B], [r_E])
                AA(lambda e: e.activation(out=bB[:], in_=bB[:], func=AF.Exp, scale=-1.0), [r_B], [r_B])
                VV(lambda e: e.scalar_tensor_tensor(out=bC[:], in0=bA[:], scalar=oml_ap, in1=bB[:], op0=ALU.mult, op1=ALU.mult),
                   [r_A, r_B, r_oml], [r_C])
                VV(lambda e: e.tensor_tensor_scan(out=bB[:], data0=resetm[:], data1=bE[:], initial=0.0, op0=ALU.mult, op1=ALU.add),
                   [r_resetm, r_E], [r_B])
                VV(lambda e: e.tensor_tensor(out=b64(bE), in0=b64(bB), in1=b64(bB)[:, :, 31:32].to_broadcast([128, 2 * NCK, 64]), op=ALU.subtract),
                   [r_B], [r_E])
                VV(lambda e: e.tensor_scalar(out=bE[:], in0=bE[:], scalar1=-80.0, scalar2=80.0, op0=ALU.max, op1=ALU.min), [r_E], [r_E])
                AA(lambda e: e.activation(out=bA[:], in_=bE[:], func=AF.Exp), [r_E], [r_A])
                VV(lambda e: e.tensor_tensor(out=o[:, 0, :], in0=q[:], in1=bA[:], op=ALU.mult), [r_q, r_A], [r_o4])
                AA(lambda e: e.activation(out=bA[:], in_=bE[:], func=AF.Exp, scale=-1.0), [r_E], [r_A])
                VV(lambda e: e.tensor_tensor(out=o[:, 1, :], in0=bC[:], in1=bA[:], op=ALU.mult), [r_C, r_A], [r_o4])
                AA(lambda e: e.activation(out=bA[:], in_=bB[:], func=AF.Exp), [r_B], [r_A])
                GG(lambda e: e.tensor_tensor(out=o[:, 2, :], in0=q[:], in1=bA[:], op=ALU.mult), [r_q, r_A], [r_o4])
                VV(lambda e: e.tensor_tensor(out=b3(bE), in0=b3(bB), in1=b3(bB)[:, :, 127:128].to_broadcast([128, NCK, 128]), op=ALU.subtract),
                   [r_B], [r_E])
                AA(lambda e: e.activation(out=bA[:], in_=bE[:], func=AF.Exp, scale=-1.0), [r_E], [r_A])
                GG(lambda e: e.tensor_tensor(out=o[:, 3, :], in0=bC[:], in1=bA[:], op=ALU.mult), [r_C, r_A], [r_o4])
                VV(lambda e: e.tensor_tensor(out=b3(bE), in0=b3(bB), in1=b3(bB)[:, :, 63:64].to_broadcast([128, NCK, 128]), op=ALU.subtract),
                   [r_B], [r_E])
                VV(lambda e: e.scalar_tensor_tensor(out=bE[:], in0=bE[:], scalar=-1.0, in1=bE[:], op0=ALU.mult, op1=ALU.min), [r_E], [r_E])
                AA(lambda e: e.activation(out=bA[:], in_=bE[:], func=AF.Exp), [r_E], [r_A])
                VV(lambda e: e.tensor_tensor(out=v3(o[:, 4, :])[:, :, 0:64], in0=b3(bC)[:, :, 0:64], in1=b3(bA)[:, :, 0:64], op=ALU.mult),
                   [r_C, r_A], [r_o4])
                VV(lambda e: e.tensor_tensor(out=v3(o[:, 4, :])[:, :, 64:128], in0=v3(q)[:, :, 64:128], in1=b3(bA)[:, :, 64:128], op=ALU.mult),
                   [r_q, r_A], [r_o4])
                AA(lambda e: e.activation(out=dcy[:], in_=b3(bB)[:, :, 127], func=AF.Exp), [r_B], [r_dcy])
                heads.append((o, r_o4, dcy, r_dcy))
                return ops

            for hp in range(2):
                la = head_ops(2 * hp)
                lb_ = head_ops(2 * hp + 1)
                for i in range(max(len(la), len(lb_))):
                    if i < len(la):
                        la[i]()
                    if i < len(lb_):
                        lb_[i]()
            return dict(sg=sg, ri=(ri, r_ri), rgm=(rgm, r_rgm), heads=heads)

        def chunks(st):
            sg = st["sg"]
            tk0 = sg * SEG
            ri, r_ri = st["ri"]
            rgm, r_rgm = st["rgm"]
            catseg, r_catseg = catr_.next()
            for ci_ in range(NCK):
                one_chunk(st, ci_, sg, ri, r_ri, rgm, r_rgm, catseg, r_catseg)
            ld(dr["CAT"][tk0:tk0 + SEG, 512:1024].rearrange("(c p) f -> p c f", p=128), catseg[:], [r_catseg], [rs["CAT"]])

        def one_chunk(st, ci, sg, ri, r_ri, rgm, r_rgm, catseg, r_catseg):
            if True:
                cs = slice(ci * 128, (ci + 1) * 128)
                g0 = (sg * NCK + ci) * 4
                per = []
                for hd in range(4):
                    o, r_o4, dcy, r_dcy = st["heads"][hd]
                    pa, r_pa = psA.next()
                    PE(lambda e, pa=pa, o=o: e.matmul(pa[:, :], lhsT=o[:, 1, cs], rhs=o[:, 0, cs], start=True, stop=True), [r_o4], [r_pa])
                    px, r_px = psX.next()
                    PE(lambda e, px=px, o=o: e.matmul(px[0:64, :], lhsT=o[:, 4, ci * 128:ci * 128 + 64], rhs=o[:, 4, ci * 128 + 64:(ci + 1) * 128],
                                                      start=True, stop=True), [r_o4], [r_px])
                    pt, r_pt = psT.next()
                    PE(lambda e, pt=pt, o=o: e.transpose(out=pt[:, :], in_=o[:, 3, cs], identity=ident[:]), [r_o4, r_ident], [r_pt])
                    Am, r_Am = Amr.next()
                    GP(lambda e, Am=Am: e.memset(Am[:], 0.0), (), [r_Am])
                    V(lambda e, Am=Am, pa=pa: e.copy_predicated(out=Am[:], mask=causal[:], data=pa[:]), [r_pa, r_causal, r_Am], [r_Am])
                    A(lambda e, Am=Am, px=px: e.activation(out=Am[0:64, 64:128], in_=px[0:64, :], func=AF.Copy), [r_px, r_Am], [r_Am])
                    khT, r_khT = khTr.next()
                    A(lambda e, khT=khT, pt=pt: e.activation(out=khT[:], in_=pt[:], func=AF.Copy), [r_pt], [r_khT])
                    per.append((Am, r_Am, khT, r_khT))
                pos_ = []
                for hd in range(4):
                    o, r_o4, dcy, r_dcy = st["heads"][hd]
                    Am, r_Am, khT, r_khT = per[hd]
                    Sh, r_Sh = St[hd]
                    Sbh, r_Sbh = Sb[hd]
                    v_ap = ri[:, ci, hd * 128:(hd + 1) * 128]
                    pu, r_pu = psU.next()
                    PE(lambda e, pu=pu, khT=khT, v_ap=v_ap: e.matmul(pu[:, :], lhsT=khT[:], rhs=v_ap, start=True, stop=True), [r_khT, r_ri], [r_pu])
                    po, r_po = psO.next()
                    PE(lambda e, po=po, Am=Am, v_ap=v_ap: e.matmul(po[:, :], lhsT=Am[:], rhs=v_ap, start=True, stop=False), [r_Am, r_ri], [r_po])
                    PE(lambda e, po=po, o=o, Sbh=Sbh: e.matmul(po[:, :], lhsT=o[:, 2, cs], rhs=Sbh[:], start=False, stop=True),
                       [r_o4, r_Sbh], [r_po])
                    V(lambda e, Sh=Sh, pu=pu, dcy=dcy: e.scalar_tensor_tensor(out=Sh[:], in0=Sh[:], scalar=dcy[:, ci:ci + 1], in1=pu[:],
                                                                             op0=ALU.mult, op1=ALU.add), [r_Sh, r_pu, r_dcy], [r_Sh])
                    A(lambda e, Sh=Sh, Sbh=Sbh: e.activation(out=Sbh[:], in_=Sh[:], func=AF.Copy), [r_Sh], [r_Sbh])
                    A(lambda e, po=po, gi=g0 + hd: e.activation(out=junk[:], in_=po[:], func=AF.Square, accum_out=ssr[:, gi:gi + 1]),
                      [r_po], [r_junk, r_ssr])
                    rstd_from_ss(ssr[:, g0 + hd:g0 + hd + 1], rsr[:, g0 + hd:g0 + hd + 1], 128, r_ssr, r_rsr)
                    V(lambda e, po=po, gi=g0 + hd, hd=hd: e.scalar_tensor_tensor(out=catseg[:, ci, hd * 128:(hd + 1) * 128], in0=po[:],
                                                                                scalar=rsr[:, gi:gi + 1], in1=rgm[:, ci, hd * 128:(hd + 1) * 128],
                                                                                op0=ALU.mult, op1=ALU.mult), [r_po, r_rsr, r_rgm], [r_catseg])

        nxt = ew(0)
        for sg in range(NSEG):
            cur = nxt
            if sg + 1 < NSEG:
                nxt = ew(sg + 1)
            chunks(cur)
        c.close()

    def p4(l, xin, r_xin):
        c = Ctx(S)
        woutb, r_woutb = c.sb([128, 8, D], BF16, "woutb")
        wst = c.ring(2, [128, 4, D], F32, name="wst")
        for hf in range(2):
            st, r_st = wst.next()
            ld(st[:], dr["w_out"][l, hf * 512:(hf + 1) * 512, :].rearrange("(kc p) n -> p kc n", p=128), (), [r_st])
            (V if hf == 0 else GP)(lambda e, st=st, hf=hf: e.tensor_copy(out=woutb[:, hf * 4:(hf + 1) * 4, :], in_=st[:]), [r_st], [r_woutb])
        wrs, r_wrs = c.sb([128, 8, 36], F32, "wrs")
        wrb, r_wrb = c.sb([128, 8, 36], BF16, "wrb")
        rbb, r_rbb = c.sb([128, 36], F32, "rbb")
        S.dma("sp", lambda e: e.dma_start(out=wrs[:], in_=dr["router_w"][l].rearrange("(kc p) n -> p kc n", p=128),
                                          allow_slow_non_contiguous=True), (), [r_wrs])
        V(lambda e: e.tensor_copy(out=wrb[:], in_=wrs[:]), [r_wrs], [r_wrb])
        ld(rbb[:], dr["router_b"][l:l + 1, :].partition_broadcast(128), (), [r_rbb], "sp")
        catr = c.ring(4, [128, D], BF16, name="cat")
        catTr = c.ring(3, [128, 8, 128], BF16, name="catT")
        xr = c.ring(4, [128, D], F32, name="x")
        x1r = c.ring(3, [128, D], F32, name="x1")
        h2r = c.ring(4, [128, D], BF16, name="h2")
        h2fr = c.ring(2, [128, D], F32, name="h2f")
        h2Tr = c.ring(3, [128, 8, 128], BF16, name="h2T")
        junk, r_junk = c.sb([128, D], BF16, "junk")
        ss, r_ss = c.sb([128, NT], F32, "ss")
        rstd, r_rstd = c.sb([128, NT], F32, "rstd")
        ptr = c.ring(2, [128, 8, 128], BF16, psum=True, name="ptr")
        pacc = c.ring(4, [128, 512], F32, psum=True, name="pacc")
        plg = c.ring(2, [128, 36], F32, psum=True, name="plg")
        V(lambda e: e.memset(ss[:], 0.0), (), [r_ss])
        st_ = {}

        def loads(t):
            ct, r_ct = catr.next()
            ld(ct[:], dr["CAT"][t * 128:(t + 1) * 128, :], [rs["CAT"]], [r_ct])
            xt, r_xt = xr.next()
            ld(xt[:], xin[t * 128:(t + 1) * 128, :], [r_xin], [r_xt])
            st_[t] = dict(ct=(ct, r_ct), xt=(xt, r_xt))

        def transposes(ta, td):
            jobs = []
            if ta is not None:
                pt, r_pt = ptr.next()
                ct, r_ct = st_[ta]["ct"]
                jobs.append([(lambda kc=kc, pt=pt, ct=ct, r_ct=r_ct, r_pt=r_pt: PE(
                    lambda e: e.transpose(out=pt[:, kc, :], in_=ct[:, kc * 128:(kc + 1) * 128], identity=ident[:]), [r_ct, r_ident], [r_pt]))
                    for kc in range(8)])
            if td is not None:
                pt2, r_pt2 = ptr.next()
                h2, r_h2 = st_[td]["h2"]
                jobs.append([(lambda kc=kc, pt2=pt2, h2=h2, r_h2=r_h2, r_pt2=r_pt2: PE(
                    lambda e: e.transpose(out=pt2[:, kc, :], in_=h2[:, kc * 128:(kc + 1) * 128], identity=ident[:]), [r_h2, r_ident], [r_pt2]))
                    for kc in range(8)])
            n = max(len(j) for j in jobs)
            for i in range(n):
                for j in jobs:
                    j[i]()
            if ta is not None:
                cT, r_cT = catTr.next()
                A(lambda e, cT=cT, pt=pt: e.activation(out=cT[:], in_=pt[:], func=AF.Copy), [r_pt], [r_cT])
                st_[ta]["cT"] = (cT, r_cT)
            if td is not None:
                hT, r_hT = h2Tr.next()
                A(lambda e, hT=hT, pt2=pt2: e.activation(out=hT[:], in_=pt2[:], func=AF.Copy), [r_pt2], [r_hT])
                st_[td]["hT"] = (hT, r_hT)

        def matmuls(t, tr):
            accs = None
            if t is not None:
                cT, r_cT = st_[t]["cT"]
                accs = [pacc.next(), pacc.next()]
            if tr is not None:
                hT, r_hT = st_[tr]["hT"]
                pl, r_pl = plg.next()
            for kc in range(8):
                if t is not None:
                    for n in range(2):
                        acc, r_acc = accs[n]
                        PE(lambda e, kc=kc, acc=acc, cT=cT, n=n: e.matmul(acc[:, :], lhsT=cT[:, kc, :], rhs=woutb[:, kc, n * 512:(n + 1) * 512],
                                                                         start=(kc == 0), stop=(kc == 7)), [r_cT, r_woutb], [r_acc])
                if tr is not None:
                    PE(lambda e, kc=kc, pl=pl, hT=hT: e.matmul(pl[:, :], lhsT=hT[:, kc, :], rhs=wrb[:, kc, :], start=(kc == 0), stop=(kc == 7)),
                       [r_hT, r_wrb], [r_pl])
            if tr is not None:
                V(lambda e, pl=pl, tr=tr: e.tensor_tensor(out=lgall[:, tr, :], in0=pl[:, :], in1=rbb[:], op=ALU.add), [r_pl, r_rbb], [r_lgall])
                del st_[tr]
            return accs

        def elementwise(t, accs):
            xt, r_xt = st_[t]["xt"]
            x1, r_x1 = x1r.next()
            for n in range(2):
                acc, r_acc = accs[n]
                V(lambda e, acc=acc, x1=x1, n=n: e.tensor_tensor(out=x1[:, n * 512:(n + 1) * 512], in0=acc[:, :], in1=G1b[:, n * 512:(n + 1) * 512],
                                                                op=ALU.mult), [r_acc, r_G1b], [r_x1])
            GP(lambda e, x1=x1, xt=xt: e.tensor_tensor(out=x1[:], in0=x1[:], in1=xt[:], op=ALU.add), [r_x1, r_xt], [r_x1])
            ld(dr["Xs"][t * 128:(t + 1) * 128, :], x1[:], [r_x1], [rs["Xs"]])
            A(lambda e, x1=x1, t=t: e.activation(out=junk[:], in_=x1[:], func=AF.Square, accum_out=ss[:, t:t + 1]), [r_x1], [r_junk, r_ss])
            rstd_from_ss(ss[:, t:t + 1], rstd[:, t:t + 1], D, r_ss, r_rstd)
            h2f_, r_h2f_ = h2fr.next()
            V(lambda e, x1=x1, t=t, h2f_=h2f_: e.scalar_tensor_tensor(out=h2f_[:], in0=x1[:], scalar=rstd[:, t:t + 1], in1=A2b[:], op0=ALU.mult, op1=ALU.mult),
              [r_x1, r_rstd, r_A2b], [r_h2f_])
            h2, r_h2 = h2r.next()
            GP(lambda e, h2=h2, h2f_=h2f_: e.tensor_tensor(out=h2[:], in0=h2f_[:], in1=S2b[:], op=ALU.add), [r_h2f_, r_S2b], [r_h2])
            ld(dr["H2"][t * 128:(t + 1) * 128, :], h2[:], [r_h2], [rs["H2"]])
            st_[t]["h2"] = (h2, r_h2)

        loads(0)
        loads(1)
        transposes(0, None)
        for t in range(NT + 2):
            if t + 2 < NT:
                loads(t + 2)
            ta = t + 1 if t + 1 < NT else None
            td = t - 1 if 0 <= t - 1 < NT else None
            if ta is not None or td is not None:
                transposes(ta, td)
            tm_ = t if t < NT else None
            trr = t - 2 if 0 <= t - 2 < NT else None
            accs = matmuls(tm_, trr)
            if tm_ is not None:
                elementwise(tm_, accs)
        c.close()

    def p4b(l):
        c = Ctx(S)
        N3 = [128, NT, NE]
        gmax, r_gmax = c.sb([128, NT], F32)
        g4, r_g4 = c.sb([128, NT, 4], F32)
        goh, r_goh = c.sb([128, NT, 4], F32)
        gsum, r_gsum = c.sb([128, NT], F32)
        em, r_em = c.sb(N3, F32)
        oh1, r_oh1 = c.sb(N3, F32)
        oh2, r_oh2 = c.sb(N3, F32)
        m1, r_m1 = c.sb([128, NT], F32)
        m2, r_m2 = c.sb([128, NT], F32)
        p1, r_p1 = c.sb([128, NT], F32)
        abf, r_abf = c.sb([128, NT * NE], BF16)
        csb, r_csb = c.sb(N3, F32)
        offs, r_offs = c.sb(N3, F32)
        pos, r_pos = c.sb(N3, F32)
        sf, r_sf = c.sb([128, NT], F32)
        cnt, r_cnt = c.sb([128, NE], F32)
        nblk, r_nblk = c.sb([128, NE], F32)
        pendb, r_pendb = c.sb([128, NE], F32)
        pst, r_pst = c.sb([128, NE], F32)
        onesne, r_onesne = c.sb([128, NE], F32)
        bex, r_bex = c.sb([128, NBLK], F32)
        cmp, r_cmp = c.sb([128, NBLK * NE], F32)
        pp = c.ring(1, [128, NT * NE], F32, psum=True)
        pc = c.ring(1, [128, NT * NE], F32, psum=True)
        lgG = lgall[:, :, 0:4]
        lgE = lgall[:, :, 4:36]

        def bc(ap2, n):
            return ap2.unsqueeze(2).to_broadcast([128, NT, n])
        V(lambda e: e.tensor_reduce(out=gmax[:], in_=lgG, axis=AX.X, op=ALU.max), [r_lgall], [r_gmax])
        V(lambda e: e.tensor_tensor(out=goh[:], in0=lgG, in1=bc(gmax[:], 4), op=ALU.is_equal), [r_lgall, r_gmax], [r_goh])
        V(lambda e: e.tensor_tensor(out=g4[:], in0=lgG, in1=bc(gmax[:], 4), op=ALU.subtract), [r_lgall, r_gmax], [r_g4])
        A(lambda e: e.activation(out=g4[:], in_=g4[:], func=AF.Exp), [r_g4], [r_g4])
        V(lambda e: e.tensor_reduce(out=gsum[:], in_=g4[:], axis=AX.X, op=ALU.add), [r_g4], [r_gsum])
        V(lambda e: e.reciprocal(out=gsum[:], in_=gsum[:]), [r_gsum], [r_gsum])
        V(lambda e: e.tensor_scalar(out=goh[:], in0=goh[:], scalar1=BIG, scalar2=-BIG, op0=ALU.mult, op1=ALU.add), [r_goh], [r_goh])
        V(lambda e: e.tensor_tensor(out=em[:].rearrange("k t (g x) -> k t g x", g=4), in0=lgE.rearrange("k t (g x) -> k t g x", g=4),
                                    in1=goh[:].unsqueeze(3).to_broadcast([128, NT, 4, 8]), op=ALU.add), [r_lgall, r_goh], [r_em])
        V(lambda e: e.tensor_reduce(out=m1[:], in_=em[:], axis=AX.X, op=ALU.max), [r_em], [r_m1])
        V(lambda e: e.tensor_tensor(out=oh1[:], in0=em[:], in1=bc(m1[:], NE), op=ALU.is_equal), [r_em, r_m1], [r_oh1])
        V(lambda e: e.scalar_tensor_tensor(out=em[:], in0=oh1[:], scalar=-BIG, in1=em[:], op0=ALU.mult, op1=ALU.add), [r_oh1, r_em], [r_em])
        V(lambda e: e.tensor_reduce(out=m2[:], in_=em[:], axis=AX.X, op=ALU.max), [r_em], [r_m2])
        V(lambda e: e.tensor_tensor(out=oh2[:], in0=em[:], in1=bc(m2[:], NE), op=ALU.is_equal), [r_em, r_m2], [r_oh2])
        V(lambda e: e.tensor_tensor(out=m2[:], in0=m2[:], in1=m1[:], op=ALU.subtract), [r_m1, r_m2], [r_m2])
        A(lambda e: e.activation(out=m2[:], in_=m2[:], func=AF.Exp), [r_m2], [r_m2])
        V(lambda e: e.tensor_scalar(out=p1[:], in0=m2[:], scalar1=1.0, scalar2=None, op0=ALU.add), [r_m2], [r_p1])
        V(lambda e: e.reciprocal(out=p1[:], in_=p1[:]), [r_p1], [r_p1])
        V(lambda e: e.tensor_tensor(out=gt1[:], in0=p1[:], in1=gsum[:], op=ALU.mult), [r_p1, r_gsum], [r_gt1])
        V(lambda e: e.tensor_tensor(out=m2[:], in0=m2[:], in1=gt1[:], op=ALU.mult), [r_m2, r_gt1], [r_m2])
        V(lambda e: e.tensor_copy(out=gt2[:], in_=m2[:]), [r_m2], [r_gt2])
        V(lambda e: e.tensor_tensor(out=abf[:].rearrange("k (t x) -> k t x", x=NE), in0=oh1[:], in1=oh2[:], op=ALU.add), [r_oh1, r_oh2], [r_abf])
        ppt, r_pp = pp.next()
        pct, r_pc = pc.next()
        for ch in range(4):
            PE(lambda e, ch=ch: e.matmul(ppt[:, ch * 512:(ch + 1) * 512], lhsT=ustrict[:], rhs=abf[:, ch * 512:(ch + 1) * 512], start=True, stop=True),
               [r_ustrict, r_abf], [r_pp])
            PE(lambda e, ch=ch: e.matmul(pct[:, ch * 512:(ch + 1) * 512], lhsT=onesb[:], rhs=abf[:, ch * 512:(ch + 1) * 512], start=True, stop=True),
               [r_onesb, r_abf], [r_pc])
        V(lambda e: e.tensor_copy(out=csb[:].rearrange("k t x -> k (t x)"), in_=pct[:]), [r_pc], [r_csb])
        V(lambda e: e.memset(offs[:, 0, :], 0.0), (), [r_offs])
        for j in range(1, NT):
            V(lambda e, j=j: e.tensor_tensor(out=offs[:, j, :], in0=offs[:, j - 1, :], in1=csb[:, j - 1, :], op=ALU.add), [r_offs, r_csb], [r_offs])
        V(lambda e: e.tensor_tensor(out=cnt[:], in0=offs[:, NT - 1, :], in1=csb[:, NT - 1, :], op=ALU.add), [r_offs, r_csb], [r_cnt])
        V(lambda e: e.tensor_tensor(out=cmp[:, 0:NE * 64].rearrange("k (x m) -> k x m", m=64), in0=cnt[:].unsqueeze(2).to_broadcast([128, NE, 64]),
                                    in1=mulrow[:].unsqueeze(1).to_broadcast([128, NE, 64]), op=ALU.is_gt), [r_cnt, r_mulrow], [r_cmp])
        V(lambda e: e.tensor_reduce(out=nblk[:], in_=cmp[:, 0:NE * 64].rearrange("k (x m) -> k x m", m=64), axis=AX.X, op=ALU.add), [r_cmp], [r_nblk])
        V(lambda e: e.memset(onesne[:], 1.0), (), [r_onesne])
        V(lambda e: e.tensor_tensor_scan(out=pendb[:], data0=onesne[:], data1=nblk[:], initial=0.0, op0=ALU.mult, op1=ALU.add),
          [r_onesne, r_nblk], [r_pendb])
        V(lambda e: e.tensor_tensor(out=pst[:], in0=pendb[:], in1=nblk[:], op=ALU.subtract), [r_pendb, r_nblk], [r_pst])
        V(lambda e: e.tensor_scalar(out=pst[:], in0=pst[:], scalar1=float(RB), scalar2=None, op0=ALU.mult), [r_pst], [r_pst])
        V(lambda e: e.tensor_tensor(out=cmp[:, 0:NBLK * NE].rearrange("k (b x) -> k b x", x=NE), in0=pendb[:].unsqueeze(1).to_broadcast([128, NBLK, NE]),
                                    in1=brow[:].unsqueeze(2).to_broadcast([128, NBLK, NE]), op=ALU.is_le), [r_pendb, r_brow], [r_cmp])
        V(lambda e: e.tensor_reduce(out=bex[:], in_=cmp[:, 0:NBLK * NE].rearrange("k (b x) -> k b x", x=NE), axis=AX.X, op=ALU.add), [r_cmp], [r_bex])
        V(lambda e: e.tensor_scalar(out=bex[:], in0=bex[:], scalar1=float(NE - 1), scalar2=float(128), op0=ALU.min, op1=ALU.mult), [r_bex], [r_bex])
        V(lambda e: e.tensor_scalar(out=bex[:], in0=bex[:], scalar1=piota[:, 0:1], scalar2=float(l * NE * 128), op0=ALU.add, op1=ALU.add),
          [r_bex, r_piota], [r_bex])
        V(lambda e: e.tensor_copy(out=widx[:], in_=bex[:]), [r_bex], [r_widx])
        V(lambda e: e.tensor_tensor(out=offs[:], in0=offs[:], in1=pst[:].unsqueeze(1).to_broadcast([128, NT, NE]), op=ALU.add), [r_offs, r_pst], [r_offs])
        V(lambda e: e.tensor_tensor(out=pos[:].rearrange("k t x -> k (t x)"), in0=ppt[:], in1=offs[:].rearrange("k t x -> k (t x)"), op=ALU.add),
          [r_pp, r_offs], [r_pos])
        for (oh, r_oh, sl, r_sl) in ((oh1, r_oh1, slot1, r_slot1), (oh2, r_oh2, slot2, r_slot2)):
            V(lambda e, oh=oh: e.tensor_tensor(out=oh[:], in0=oh[:], in1=pos[:], op=ALU.mult), [r_oh, r_pos], [r_oh])
            V(lambda e, oh=oh: e.tensor_reduce(out=sf[:], in_=oh[:], axis=AX.X, op=ALU.add), [r_oh], [r_sf])
            V(lambda e: e.tensor_scalar(out=sf[:], in0=sf[:], scalar1=float(NROWS - 1), scalar2=0.0, op0=ALU.min, op1=ALU.max), [r_sf], [r_sf])
            V(lambda e, sl=sl: e.tensor_copy(out=sl[:], in_=sf[:]), [r_sf], [r_sl])
        h2r = c.ring(3, [128, D], BF16, name="h2d")
        for t in range(NT):
            h2, r_h2 = h2r.next()
            ld(h2[:], dr["H2"][t * 128:(t + 1) * 128, :], [rs["H2"]], [r_h2])
            for (sl, r_sl) in ((slot1, r_slot1), (slot2, r_slot2)):
                S.dma("pool", lambda e, h2=h2, sl=sl, t=t: e.indirect_dma_start(
                    out=dr["XS"], out_offset=bass.IndirectOffsetOnAxis(ap=sl[:, t:t + 1], axis=0), in_=h2[:], in_offset=None),
                    [r_h2, r_sl], [rs["XS"]])
        c.close()

    def p5(l):
        c = Ctx(S)
        wsr = c.ring(6, [128, 2048], F32, name="wsr")
        wbr = [c.ring(2, [128, 4096], BF16, name=f"wb{i}") for i in range(3)]
        xrr = c.ring(4, [128, D], BF16, name="xr")
        xTr = c.ring(2, [128, 8, RB], BF16, name="xT")
        actr = c.ring(2, [128, 4, RB], BF16, name="actT")
        silr = c.ring(2, [128, RB], F32, name="sil")
        ysr = c.ring(3, [128, D], F32, name="ys")
        ptr = c.ring(2, [128, 8, 128], BF16, psum=True, name="ptr")
        pacc = c.ring(6, [128, 512], F32, psum=True, name="pacc")
        wsrc = ((dr["moe_w1_0"], dr["moe_w1_1"]), (dr["moe_w3_0"], dr["moe_w3_1"]), (dr["moe_w2_0"], dr["moe_w2_1"]))
        k = [0]

        def wload(b):
            wb = []
            for i in range(3):
                wt, r_wt = wbr[i].next()
                for hf in range(2):
                    st, r_st = wsr.next()
                    S.dma("pool", lambda e, st=st, i=i, hf=hf: e.indirect_dma_start(
                        out=st[:], out_offset=None, in_=wsrc[i][hf],
                        in_offset=bass.IndirectOffsetOnAxis(ap=widx[:, b:b + 1], axis=0)), [r_widx], [r_st])
                    k[0] += 1
                    dst = wt[:, hf * 2048:(hf + 1) * 2048]
                    if k[0] % 2 == 0:
                        V(lambda e, st=st, dst=dst: e.tensor_copy(out=dst, in_=st[:]), [r_st], [r_wt])
                    else:
                        A(lambda e, st=st, dst=dst: e.activation(out=dst, in_=st[:], func=AF.Copy), [r_st], [r_wt])
                wb.append((wt, r_wt))
            return wb

        def xprep(b):
            xT, r_xT = xTr.next()
            tl = []
            for rt in range(RB // 128):
                xr_, r_xr = xrr.next()
                r0 = b * RB + rt * 128
                ld(xr_[:], dr["XS"][r0:r0 + 128, :], [rs["XS"]], [r_xr])
                tl.append((xr_, r_xr, ptr.next()))
            for kc in range(8):
                for (xr_, r_xr, (pt, r_pt)) in tl:
                    PE(lambda e, kc=kc, pt=pt, xr_=xr_: e.transpose(out=pt[:, kc, :], in_=xr_[:, kc * 128:(kc + 1) * 128], identity=ident[:]),
                       [r_xr, r_ident], [r_pt])
            for rt, (xr_, r_xr, (pt, r_pt)) in enumerate(tl):
                if rt == 0:
                    V(lambda e, xT=xT, pt=pt, rt=rt: e.tensor_copy(out=xT[:, :, rt * 128:(rt + 1) * 128], in_=pt[:]), [r_pt], [r_xT])
                else:
                    A(lambda e, xT=xT, pt=pt, rt=rt: e.activation(out=xT[:, :, rt * 128:(rt + 1) * 128], in_=pt[:], func=AF.Copy), [r_pt], [r_xT])
            return xT, r_xT

        Wn = wload(0)
        Xn = xprep(0)
        for b in range(NBLK):
            (w1b, r_w1b), (w3b, r_w3b), (w2b, r_w2b) = Wn
            xT, r_xT = Xn
            if b + 1 < NBLK:
                Wn = wload(b + 1)
            aT, r_aT = actr.next()
            for mp in range(2):
                accs = []
                for mi in range(2):
                    accs.append((pacc.next(), pacc.next()))
                for kc in range(8):
                    for mi in range(2):
                        mc = 2 * mp + mi
                        (a1, r_a1), (a3, r_a3) = accs[mi]
                        PE(lambda e, kc=kc, mc=mc, a1=a1, w1b=w1b, xT=xT: e.matmul(a1[:, 0:RB], lhsT=w1b[:, kc * 512 + mc * 128:kc * 512 + (mc + 1) * 128],
                                                                                  rhs=xT[:, kc, :], start=(kc == 0), stop=(kc == 7)), [r_w1b, r_xT], [r_a1])
                        PE(lambda e, kc=kc, mc=mc, a3=a3, w3b=w3b, xT=xT: e.matmul(a3[:, 0:RB], lhsT=w3b[:, kc * 512 + mc * 128:kc * 512 + (mc + 1) * 128],
                                                                                  rhs=xT[:, kc, :], start=(kc == 0), stop=(kc == 7)), [r_w3b, r_xT], [r_a3])
                for mi in range(2):
                    mc = 2 * mp + mi
                    (a1, r_a1), (a3, r_a3) = accs[mi]
                    sl, r_sl = silr.next()
                    A(lambda e, sl=sl, a1=a1: e.activation(out=sl[:], in_=a1[:, 0:RB], func=AF.Silu), [r_a1], [r_sl])
                    V(lambda e, sl=sl, a3=a3, mc=mc, aT=aT: e.tensor_tensor(out=aT[:, mc, :], in0=sl[:], in1=a3[:, 0:RB], op=ALU.mult), [r_sl, r_a3], [r_aT])
            if b + 1 < NBLK:
                Xn = xprep(b + 1)
            yaccs = [[pacc.next() for nch in range(2)] for rt in range(RB // 128)]
            for mc in range(4):
                for rt in range(RB // 128):
                    for nch in range(2):
                        acc, r_acc = yaccs[rt][nch]
                        PE(lambda e, mc=mc, acc=acc, rt=rt, nch=nch, aT=aT, w2b=w2b: e.matmul(
                            acc[:, :], lhsT=aT[:, mc, rt * 128:(rt + 1) * 128], rhs=w2b[:, mc * 1024 + nch * 512:mc * 1024 + (nch + 1) * 512],
                            start=(mc == 0), stop=(mc == 3)), [r_aT, r_w2b], [r_acc])
            for rt in range(RB // 128):
                ys, r_ys = ysr.next()
                for nch in range(2):
                    acc, r_acc = yaccs[rt][nch]
                    if nch == 0:
                        A(lambda e, ys=ys, acc=acc: e.activation(out=ys[:, 0:512], in_=acc[:, :], func=AF.Copy), [r_acc], [r_ys])
                    else:
                        V(lambda e, ys=ys, acc=acc: e.tensor_copy(out=ys[:, 512:1024], in_=acc[:, :]), [r_acc], [r_ys])
                r0 = b * RB + rt * 128
                ld(dr["YS"][r0:r0 + 128, :], ys[:], [r_ys], [rs["YS"]])
        c.close()

    def p6(l):
        c = Ctx(S)
        xr = c.ring(4, [128, D], F32, name="x1")
        y1r = c.ring(4, [128, D], F32, name="y1")
        y2r = c.ring(4, [128, D], F32, name="y2")
        st_ = {}

        def loads(t):
            xt, r_xt = xr.next()
            ld(xt[:], dr["Xs"][t * 128:(t + 1) * 128, :], [rs["Xs"]], [r_xt])
            y1, r_y1 = y1r.next()
            y2, r_y2 = y2r.next()
            for (yy, r_yy, sl, r_sl) in ((y1, r_y1, slot1, r_slot1), (y2, r_y2, slot2, r_slot2)):
                S.dma("pool", lambda e, yy=yy, sl=sl, t=t: e.indirect_dma_start(
                    out=yy[:], out_offset=None, in_=dr["YS"], in_offset=bass.IndirectOffsetOnAxis(ap=sl[:, t:t + 1], axis=0)),
                    [rs["YS"], r_sl], [r_yy])
            st_[t] = (xt, r_xt, y1, r_y1, y2, r_y2)
        PF = 3
        for t in range(min(PF, NT)):
            loads(t)
        for t in range(NT):
            xt, r_xt, y1, r_y1, y2, r_y2 = st_.pop(t)
            A(lambda e, y1=y1, t=t: e.activation(out=y1[:], in_=y1[:], func=AF.Copy, scale=gt1[:, t:t + 1]), [r_y1, r_gt1], [r_y1])
            V(lambda e, y1=y1, y2=y2, t=t: e.scalar_tensor_tensor(out=y2[:], in0=y2[:], scalar=gt2[:, t:t + 1], in1=y1[:], op0=ALU.mult, op1=ALU.add),
              [r_y1, r_y2, r_gt2], [r_y2])
            V(lambda e, y2=y2: e.tensor_tensor(out=y2[:], in0=y2[:], in1=G2b[:], op=ALU.mult), [r_y2, r_G2b], [r_y2])
            V(lambda e, y2=y2, xt=xt: e.tensor_tensor(out=xt[:], in0=y2[:], in1=xt[:], op=ALU.add), [r_y2, r_xt], [r_xt])
            ld(dr["Xs"][t * 128:(t + 1) * 128, :], xt[:], [r_xt], [rs["Xs"]])
            if t + PF < NT:
                loads(t + PF)
        c.close()

    def pfinal():
        c = Ctx(S)
        fgb, r_fgb = c.sb([128, D], F32, "fgb")
        ld(fgb[:], dr["final_g"].partition_broadcast(128), (), [r_fgb], "sp")
        xr = c.ring(3, [128, D], F32, name="xf")
        junk, r_junk = c.sb([128, D], BF16, "junk")
        ss, r_ss = c.sb([128, NT], F32, "ss")
        rstd, r_rstd = c.sb([128, NT], F32, "rstd")
        V(lambda e: e.memset(ss[:], 0.0), (), [r_ss])
        for t in range(NT):
            xt, r_xt = xr.next()
            ld(xt[:], dr["Xs"][t * 128:(t + 1) * 128, :], [rs["Xs"]], [r_xt])
            A(lambda e, xt=xt, t=t: e.activation(out=junk[:], in_=xt[:], func=AF.Square, accum_out=ss[:, t:t + 1]), [r_xt], [r_junk, r_ss])
            rstd_from_ss(ss[:, t:t + 1], rstd[:, t:t + 1], D, r_ss, r_rstd)
            V(lambda e, xt=xt, t=t: e.scalar_tensor_tensor(out=xt[:], in0=xt[:], scalar=rstd[:, t:t + 1], in1=fgb[:], op0=ALU.mult, op1=ALU.mult),
              [r_xt, r_rstd, r_fgb], [r_xt])
            ld(out[t * 128:(t + 1) * 128, :], xt[:], [r_xt], [rs["out"]])
        c.close()

    def finish():
        toks = []
        for r in rs.values():
            toks += r.ws
        S.wait_all("sp", toks)
        S.flush()
        G.close()
        S.es.close()

    setup()
    xin, r_xin = dr["x"], rs["x"]
    done = False
    for l in range(depth):
        for (nm, fn) in (("p0", lambda: p0(l)), ("p1", lambda: p1(l, xin, r_xin)), ("p2", lambda: p2(l)), ("p2b", lambda: p2b(l)),
                         ("p3", lambda: p3(l)), ("p4", lambda: p4(l, xin, r_xin)), ("p4b", lambda: p4b(l)), ("p5", lambda: p5(l)),
                         ("p6", lambda: p6(l))):
            fn()
            if stop == (nm, l):
                done = True
                break
        if done:
            break
        xin, r_xin = dr["Xs"], rs["Xs"]
    if not done and final:
        pfinal()
    finish()
    return nc


def make_inputs(inputs, b):
    f = lambda a: np.ascontiguousarray(np.asarray(a, dtype=np.float32))
    m = {
        "x": f(inputs["x"][b]), "c": f(inputs["c"][b:b + 1]), "w_ada": f(inputs["w_ada"]), "b_ada": f(inputs["b_ada"]),
        "norm1_g": f(inputs["norm1_g"]), "w_in": f(inputs["w_in"]), "attn_norm_g": f(inputs["attn_norm_g"]),
        "hgrn_lb_logits": f(inputs["hgrn_lb_logits"]), "hgrn_norm_g": f(inputs["hgrn_norm_g"]), "w_out": f(inputs["w_out"]),
        "norm2_g": f(inputs["norm2_g"]),
        "router_w": f(np.concatenate([np.asarray(inputs["router_group_w"]), np.asarray(inputs["router_expert_w"])], axis=-1)),
        "router_b": f(np.concatenate([np.asarray(inputs["router_group_b"]), np.asarray(inputs["router_expert_b"])], axis=-1)),
        "final_g": f(np.asarray(inputs["final_g"]).reshape(1, D)),
    }
    for nm, kc, n in (("moe_w1", 8, 512), ("moe_w3", 8, 512), ("moe_w2", 4, D)):
        w = np.asarray(inputs[nm], dtype=np.float32).reshape(DEPTH, NE, kc, 128, n).transpose(0, 1, 3, 2, 4).reshape(DEPTH * NE * 128, kc * n)
        m[nm + "_0"] = np.ascontiguousarray(w[:, :2048])
        m[nm + "_1"] = np.ascontiguousarray(w[:, 2048:])
    m.update(host_consts())
    return m


def kernel(**inputs):
    nc = build()
    in_maps = [make_inputs(inputs, b) for b in range(2)]
    res = run_bass_kernel_spmd(nc, in_maps, core_ids=[0, 1])
    return np.stack([np.asarray(res.results[b]["out"], dtype=np.float32) for b in range(2)], axis=0)
```

```python
import contextlib
import numpy as np
import ml_dtypes
import concourse.bass as bass
import concourse.mybir as mybir
from concourse.bass_utils import run_bass_kernel_spmd

F32 = mybir.dt.float32
BF16 = mybir.dt.bfloat16
I32 = mybir.dt.int32
ALU = mybir.AluOpType
AF = mybir.ActivationFunctionType
AX = mybir.AxisListType

T = 8192
D = 1024
NT = T // 128
DEPTH = 4
NE = 32
RB = 256
NBLK = (2 * T) // RB + NE
NROWS = NBLK * RB
EPS = 1e-6
BIG = 30000.0
PATTERNS = ((128, 1), (512, 4), (2048, 16))
EP_ENG = 30000
EP_DMA = 3000


class Res:
    __slots__ = ("w", "r")

    def __init__(self):
        self.w = None
        self.r = []


class MRes(Res):
    __slots__ = ("ws",)

    def __init__(self):
        super().__init__()
        self.ws = []


def _compress(lst):
    mx = {}
    for (pq, c) in lst:
        mx[pq] = max(mx.get(pq, 0), c)
    return list(mx.items())


class Q:
    def __init__(self, nc, es, name, inc, ep):
        self.nc, self.es, self.name, self.inc, self.ep = nc, es, name, inc, ep
        self.sems = []
        self.count = 0
        self.ops = []
        self.seen = {}

    def sem_for(self, c):
        i = (c - 1) // self.ep
        while len(self.sems) <= i:
            self.sems.append(self.es.enter_context(self.nc.semaphore(f"{self.name}_{len(self.sems)}")))
        return self.sems[i], (((c - 1) % self.ep) + 1) * self.inc


class Sched:
    def __init__(self, nc, ndma=6):
        self.nc = nc
        self.es = contextlib.ExitStack()
        self.q = {n: Q(nc, self.es, "s" + n, 1, EP_ENG) for n in ("pe", "act", "dve", "pool", "sp")}
        self.dslots = {n: [Q(nc, self.es, f"d{n}{i}", 16, EP_DMA) for i in range(ndma)] for n in ("sp", "pool", "act")}
        self.dnext = {n: 0 for n in self.dslots}

    def _deps(self, reads, writes):
        need = {}
        for r in reads:
            if isinstance(r, MRes):
                for (pq, c) in r.ws:
                    need[pq] = max(need.get(pq, 0), c)
            elif r.w is not None:
                need[r.w[0]] = max(need.get(r.w[0], 0), r.w[1])
        for w in writes:
            if (not isinstance(w, MRes)) and w.w is not None:
                need[w.w[0]] = max(need.get(w.w[0], 0), w.w[1])
            for (pq, c) in w.r:
                need[pq] = max(need.get(pq, 0), c)
        return need

    def _commit(self, tok, reads, writes):
        for r in reads:
            r.r.append(tok)
            if len(r.r) > 48:
                r.r = _compress(r.r)
        for w in writes:
            if isinstance(w, MRes):
                w.ws.append(tok)
                if len(w.ws) > 48:
                    w.ws = _compress(w.ws)
            else:
                w.w = tok
                w.r = []

    def _waits(self, q, need):
        waits = []
        for pq, c in need.items():
            if q.seen.get(pq, 0) >= c:
                continue
            q.seen[pq] = c
            waits.append((pq, c))
        return waits

    def op(self, qn, fn, reads=(), writes=()):
        q = self.q[qn]
        waits = self._waits(q, self._deps(reads, writes))
        q.count += 1
        tok = (q, q.count)
        q.ops.append((waits, fn, tok))
        self._commit(tok, reads, writes)
        return tok

    def dma(self, qn, fn, reads=(), writes=()):
        q = self.q[qn]
        sl = self.dslots[qn]
        dq = sl[self.dnext[qn] % len(sl)]
        self.dnext[qn] += 1
        need = self._deps(reads, writes)
        if dq.count > 0:
            need[dq] = max(need.get(dq, 0), dq.count)
        waits = self._waits(q, need)
        dq.count += 1
        tok = (dq, dq.count)
        q.ops.append((waits, fn, tok))
        self._commit(tok, reads, writes)
        return tok

    def wait_all(self, qn, toks):
        q = self.q[qn]
        need = {}
        for (pq, c) in toks:
            need[pq] = max(need.get(pq, 0), c)
        q.ops.append((self._waits(q, need), None, None))

    def flush(self):
        nc = self.nc
        if not any(q.ops for q in self.q.values()):
            return
        with nc.Block() as block:
            def mk(qn):
                q = self.q[qn]

                def body(e):
                    for waits, fn, tok in q.ops:
                        for (pq, c) in waits:
                            s, v = pq.sem_for(c)
                            e.wait_ge(s, v)
                        if fn is not None:
                            s, _ = tok[0].sem_for(tok[1])
                            fn(e).then_inc(s, tok[0].inc)
                    q.ops = []
                return body
            block.tensor(mk("pe"))
            block.scalar(mk("act"))
            block.vector(mk("dve"))
            block.gpsimd(mk("pool"))
            block.sync(mk("sp"))


class Ring:
    def __init__(self, items):
        self.items = items
        self.i = 0

    def next(self):
        it = self.items[self.i % len(self.items)]
        self.i += 1
        return it


_UID = [0]


class Ctx:
    def __init__(self, S):
        self.S = S
        self.es = contextlib.ExitStack()
        self.n = 0

    def sb(self, shape, dt, name=None):
        _UID[0] += 1
        t = self.es.enter_context(self.S.nc.sbuf_tensor(f"{name or 't'}_{_UID[0]}", list(shape), dt))
        return t, Res()

    def ps(self, shape, dt, name=None):
        _UID[0] += 1
        t = self.es.enter_context(self.S.nc.psum_tensor(f"{name or 'p'}_{_UID[0]}", list(shape), dt))
        return t, Res()

    def ring(self, n, shape, dt, psum=False, name=None):
        return Ring([(self.ps if psum else self.sb)(shape, dt, name) for _ in range(n)])

    def close(self):
        self.S.flush()
        self.es.close()


def host_consts():
    c = {}
    c["ident"] = np.eye(128, dtype=np.float32).astype(ml_dtypes.bfloat16)
    kk = np.arange(128)[:, None]
    qq = np.arange(128)[None, :]
    mm = np.zeros((128, 8, 3, 256), np.float32)
    for h in range(8):
        slope = 2.0 ** (-(h + 1))
        for p, (w, d) in enumerate(PATTERNS):
            steps = w // d
            dist_cur = qq - kk
            dist_nxt = 128 + qq - kk
            for j, dist in enumerate((dist_cur, dist_nxt)):
                valid = (dist >= 0) & (dist <= steps)
                mm[:, h, p, j * 128:(j + 1) * 128] = np.where(valid, -slope * d * dist, -BIG)
    mm2 = np.concatenate([mm[..., 128:256], mm[..., 0:128]], axis=-1)
    c["amask"] = np.exp(mm2.astype(np.float64)).astype(np.float32).reshape(128, 8 * 3 * 256).astype(ml_dtypes.bfloat16)
    c["causal"] = ((kk <= qq) & ((kk // 64) == (qq // 64))).astype(np.uint8)
    c["ustrict"] = (kk < qq).astype(np.float32).astype(ml_dtypes.bfloat16)
    rm = np.ones((128, 2048), np.float32)
    rm[:, ::128] = 0.0
    c["resetm"] = rm
    c["mulrow"] = np.tile((np.arange(64, dtype=np.float32) * RB)[None, :], (128, 1))
    c["brow"] = np.tile(np.arange(NBLK, dtype=np.float32)[None, :], (128, 1))
    c["piota"] = np.arange(128, dtype=np.float32).reshape(128, 1)
    return c


CONST_SPECS = {"ident": ([128, 128], BF16), "amask": ([128, 8 * 3 * 256], BF16), "causal": ([128, 128], mybir.dt.uint8),
               "ustrict": ([128, 128], BF16), "resetm": ([128, 2048], F32), "mulrow": ([128, 64], F32), "brow": ([128, NBLK], F32), "piota": ([128, 1], F32)}

IN_SPECS = {
    "x": ([T, D], F32), "c": ([1, D], F32), "w_ada": ([DEPTH, D, 6 * D], F32), "b_ada": ([DEPTH, 6 * D], F32),
    "norm1_g": ([DEPTH, D], F32), "w_in": ([DEPTH, D, 3584], F32), "attn_norm_g": ([DEPTH, 512], F32),
    "hgrn_lb_logits": ([DEPTH, 512], F32), "hgrn_norm_g": ([DEPTH, 512], F32), "w_out": ([DEPTH, D, D], F32),
    "norm2_g": ([DEPTH, D], F32), "router_w": ([DEPTH, D, 36], F32), "router_b": ([DEPTH, 36], F32),
    "moe_w1_0": ([DEPTH * NE * 128, 2048], F32), "moe_w1_1": ([DEPTH * NE * 128, 2048], F32),
    "moe_w3_0": ([DEPTH * NE * 128, 2048], F32), "moe_w3_1": ([DEPTH * NE * 128, 2048], F32),
    "moe_w2_0": ([DEPTH * NE * 128, 2048], F32), "moe_w2_1": ([DEPTH * NE * 128, 2048], F32),
    "final_g": ([1, D], F32),
}


def build(depth=DEPTH, stop=None, dumps=(), final=True):
    nc = bass.Bass("TRN2", target_bir_lowering=False)
    S = Sched(nc)
    dr = {}
    for k, (shp, dt) in list(IN_SPECS.items()) + list(CONST_SPECS.items()):
        dr[k] = nc.dram_tensor(k, shp, dt, kind="ExternalInput").ap()
    out = nc.dram_tensor("out", [T, D], F32, kind="ExternalOutput").ap()
    rs = {}

    def scratch(name, shp, dt):
        kind = "ExternalOutput" if name in dumps else "Internal"
        dr[name] = nc.dram_tensor(name, shp, dt, kind=kind).ap()
        rs[name] = MRes()
    scratch("Xs", [T, D], F32)
    scratch("QT", [512, T], BF16)
    scratch("KT", [512, T], BF16)
    scratch("VV", [T, 512], BF16)
    scratch("RQT", [512, T], BF16)
    scratch("ZT", [512, T], F32)
    scratch("RI", [T, 512], BF16)
    scratch("RG", [T, 512], BF16)
    for p in range(3):
        scratch(f"OP{p}", [T, 2, 4 * 65], F32)
    scratch("CAT", [T, D], BF16)
    scratch("H2", [T, D], BF16)
    scratch("XS", [NROWS, D], BF16)
    scratch("YS", [NROWS, D], F32)
    rs["x"] = MRes()
    rs["out"] = MRes()

    G = Ctx(S)
    ident, r_ident = G.sb([128, 128], BF16, "ident")
    causal, r_causal = G.sb([128, 128], mybir.dt.uint8, "causal")
    ustrict, r_ustrict = G.sb([128, 128], BF16, "ustrict")
    onesb, r_onesb = G.sb([128, 128], BF16, "onesb")
    mulrow, r_mulrow = G.sb([128, 64], F32, "mulrow")
    brow, r_brow = G.sb([128, NBLK], F32, "brow")
    piota, r_piota = G.sb([128, 1], F32, "piota")
    widx, r_widx = G.sb([128, NBLK], I32, "widx")
    one11, r_one11 = G.sb([1, 1], F32, "one11")
    onesrow, r_onesrow = G.sb([1, 128], F32, "onesrow")
    colf, r_colf = G.sb([128, 16], F32, "colf")
    G1b, r_G1b = G.sb([128, D], F32, "G1b")
    A2b, r_A2b = G.sb([128, D], F32, "A2b")
    S2b, r_S2b = G.sb([128, D], F32, "S2b")
    G2b, r_G2b = G.sb([128, D], F32, "G2b")
    lbc, r_lbc = G.sb([128, DEPTH, 4], F32, "lbc")
    oml, r_oml = G.sb([128, DEPTH, 4], F32, "oml")
    noml, r_noml = G.sb([128, DEPTH, 4], F32, "noml")
    slot1, r_slot1 = G.sb([128, NT], I32, "slot1")
    slot2, r_slot2 = G.sb([128, NT], I32, "slot2")
    gt1, r_gt1 = G.sb([128, NT], F32, "gt1")
    gt2, r_gt2 = G.sb([128, NT], F32, "gt2")
    lgall, r_lgall = G.sb([128, NT, 36], F32, "lgall")

    dma_rr = [0]

    def ld(out_, in_, reads=(), writes=(), q=None):
        if q is None:
            q = "sp"
        return S.dma(q, lambda e: e.dma_start(out=out_, in_=in_), reads, writes)

    def V(fn, r=(), w=()):
        return S.op("dve", fn, r, w)

    def A(fn, r=(), w=()):
        return S.op("act", fn, r, w)

    def PE(fn, r=(), w=()):
        return S.op("pe", fn, r, w)

    def GP(fn, r=(), w=()):
        return S.op("pool", fn, r, w)

    def setup():
        ld(ident[:], dr["ident"], (), [r_ident], "sp")
        ld(causal[:], dr["causal"], (), [r_causal], "sp")
        ld(ustrict[:], dr["ustrict"], (), [r_ustrict], "sp")
        ld(mulrow[:], dr["mulrow"], (), [r_mulrow], "sp")
        ld(brow[:], dr["brow"], (), [r_brow], "sp")
        ld(piota[:], dr["piota"], (), [r_piota], "sp")
        V(lambda e: e.memset(one11[:], 1.0), (), [r_one11])
        V(lambda e: e.memset(onesrow[:], 1.0), (), [r_onesrow])
        V(lambda e: e.memset(onesb[:], 1.0), (), [r_onesb])
        c = Ctx(S)
        lg, r_lg = c.sb([128, DEPTH, 4], F32)
        ex, r_ex = c.sb([128, DEPTH, 4], F32)
        sm, r_sm = c.sb([128, 4], F32)
        S.dma("sp", lambda e: e.dma_start(out=lg[:], in_=dr["hgrn_lb_logits"].rearrange("l (h k) -> k l h", k=128),
                                          allow_slow_non_contiguous=True), (), [r_lg])
        A(lambda e: e.activation(out=ex[:], in_=lg[:], func=AF.Exp), [r_lg], [r_ex])
        V(lambda e: e.tensor_tensor(out=sm[:], in0=ex[:, 0, :], in1=ex[:, 1, :], op=ALU.add), [r_ex], [r_sm])
        V(lambda e: e.tensor_tensor(out=sm[:], in0=sm[:], in1=ex[:, 2, :], op=ALU.add), [r_ex, r_sm], [r_sm])
        V(lambda e: e.tensor_tensor(out=sm[:], in0=sm[:], in1=ex[:, 3, :], op=ALU.add), [r_ex, r_sm], [r_sm])
        V(lambda e: e.reciprocal(out=sm[:], in_=sm[:]), [r_sm], [r_sm])
        V(lambda e: e.memset(lbc[:, 0, :], 0.0), (), [r_lbc])
        V(lambda e: e.tensor_tensor(out=lbc[:, 1, :], in0=ex[:, 1, :], in1=sm[:], op=ALU.mult), [r_ex, r_sm], [r_lbc])
        for l in (2, 3):
            V(lambda e, l=l: e.tensor_tensor(out=ex[:, l, :], in0=ex[:, l, :], in1=sm[:], op=ALU.mult), [r_ex, r_sm], [r_ex])
            V(lambda e, l=l: e.tensor_tensor(out=lbc[:, l, :], in0=lbc[:, l - 1, :], in1=ex[:, l, :], op=ALU.add), [r_ex, r_lbc], [r_lbc])
        V(lambda e: e.tensor_scalar(out=oml[:], in0=lbc[:], scalar1=-1.0, scalar2=1.0, op0=ALU.mult, op1=ALU.add), [r_lbc], [r_oml])
        V(lambda e: e.tensor_scalar(out=noml[:], in0=lbc[:], scalar1=1.0, scalar2=-1.0, op0=ALU.mult, op1=ALU.add), [r_lbc], [r_noml])
        c.close()

    def p0(l):
        c = Ctx(S)
        crow, r_crow = c.sb([1, D], F32)
        cactc, r_cactc = c.sb([128, 8], F32)
        modrow, r_mod = c.sb([1, 6 * D], F32)
        brow, r_brow = c.sb([1, 6 * D], F32)
        g1row, r_g1 = c.sb([1, D], F32)
        g2row, r_g2 = c.sb([1, D], F32)
        wring = c.ring(2, [128, 8, 512], F32)
        pcol, r_pcol = c.ps([128, 16], F32)
        pacc = c.ring(2, [128, 512], F32, psum=True)
        ld(crow[:], dr["c"], (), [r_crow], "sp")
        ld(brow[:], dr["b_ada"][l:l + 1, :], (), [r_brow], "sp")
        ld(g1row[:], dr["norm1_g"][l:l + 1, :], (), [r_g1], "sp")
        ld(g2row[:], dr["norm2_g"][l:l + 1, :], (), [r_g2], "sp")
        A(lambda e: e.activation(out=crow[:], in_=crow[:], func=AF.Silu), [r_crow], [r_crow])
        for kc in range(8):
            PE(lambda e, kc=kc: e.matmul(pcol[:, kc:kc + 1], lhsT=crow[0:1, kc * 128:(kc + 1) * 128], rhs=one11[0:1, 0:1],
                                         start=True, stop=True), [r_crow, r_one11], [r_pcol])
        V(lambda e: e.tensor_copy(out=cactc[:], in_=pcol[:, 0:8]), [r_pcol], [r_cactc])
        for n in range(12):
            wt, r_wt = wring.next()
            ld(wt[:], dr["w_ada"][l, :, n * 512:(n + 1) * 512].rearrange("(kc p) n -> p kc n", p=128), (), [r_wt])
            acc, r_acc = pacc.next()
            for kc in range(8):
                PE(lambda e, kc=kc, acc=acc, wt=wt: e.matmul(acc[0:1, :], lhsT=cactc[:, kc:kc + 1], rhs=wt[:, kc, :],
                                                            start=(kc == 0), stop=(kc == 7)), [r_cactc, r_wt], [r_acc])
            V(lambda e, n=n, acc=acc: e.tensor_tensor(out=modrow[0:1, n * 512:(n + 1) * 512], in0=acc[0:1, :],
                                                       in1=brow[0:1, n * 512:(n + 1) * 512], op=ALU.add), [r_acc, r_brow], [r_mod])
        V(lambda e: e.scalar_tensor_tensor(out=g1row[:], in0=modrow[0:1, D:2 * D], scalar=1.0, in1=g1row[:], op0=ALU.add, op1=ALU.mult),
          [r_mod, r_g1], [r_g1])
        V(lambda e: e.scalar_tensor_tensor(out=g2row[:], in0=modrow[0:1, 4 * D:5 * D], scalar=1.0, in1=g2row[:], op0=ALU.add, op1=ALU.mult),
          [r_mod, r_g2], [r_g2])
        for kc in range(8):
            PE(lambda e, kc=kc: e.matmul(pcol[:, kc:kc + 1], lhsT=g1row[0:1, kc * 128:(kc + 1) * 128], rhs=one11[0:1, 0:1],
                                         start=True, stop=True), [r_g1, r_one11], [r_pcol])
            PE(lambda e, kc=kc: e.matmul(pcol[:, 8 + kc:9 + kc], lhsT=modrow[0:1, kc * 128:(kc + 1) * 128], rhs=one11[0:1, 0:1],
                                         start=True, stop=True), [r_mod, r_one11], [r_pcol])
        V(lambda e: e.tensor_copy(out=colf[:], in_=pcol[:]), [r_pcol], [r_colf])
        for (src, r_src, off, dst, r_dst) in ((modrow, r_mod, 2 * D, G1b, r_G1b), (g2row, r_g2, 0, A2b, r_A2b),
                                              (modrow, r_mod, 3 * D, S2b, r_S2b), (modrow, r_mod, 5 * D, G2b, r_G2b)):
            for nch in range(2):
                acc, r_acc = pacc.next()
                PE(lambda e, acc=acc, src=src, off=off, nch=nch: e.matmul(acc[:, :], lhsT=onesrow[0:1, :],
                                                                         rhs=src[0:1, off + nch * 512:off + (nch + 1) * 512],
                                                                         start=True, stop=True), [r_src, r_onesrow], [r_acc])
                V(lambda e, acc=acc, dst=dst, nch=nch: e.tensor_copy(out=dst[:, nch * 512:(nch + 1) * 512], in_=acc[:, :]), [r_acc], [r_dst])
        c.close()

    def rstd_from_ss(ss_ap, out_ap, n, r_ss, r_out):
        V(lambda e: e.tensor_scalar(out=out_ap, in0=ss_ap, scalar1=1.0 / n, scalar2=EPS, op0=ALU.mult, op1=ALU.add), [r_ss], [r_out])
        A(lambda e: e.activation(out=out_ap, in_=out_ap, func=AF.Sqrt), [r_out], [r_out])
        V(lambda e: e.reciprocal(out=out_ap, in_=out_ap), [r_out], [r_out])

    def p1(l, xin, r_xin):
        c = Ctx(S)
        winb, r_winb = c.sb([128, 8, 3584], BF16, "winb")
        wst = c.ring(3, [128, 1792], F32, name="wst")
        kk_ = 0
        for kc in range(8):
            for hf in range(2):
                st, r_st = wst.next()
                ld(st[:], dr["w_in"][l, kc * 128:(kc + 1) * 128, hf * 1792:(hf + 1) * 1792], (), [r_st])
                kk_ += 1
                if kk_ % 2 == 0:
                    V(lambda e, st=st, kc=kc, hf=hf: e.tensor_copy(out=winb[:, kc, hf * 1792:(hf + 1) * 1792], in_=st[:]), [r_st], [r_winb])
                else:
                    GP(lambda e, st=st, kc=kc, hf=hf: e.tensor_copy(out=winb[:, kc, hf * 1792:(hf + 1) * 1792], in_=st[:]), [r_st], [r_winb])
        xring = c.ring(6, [128, D], F32, name="xt")
        junk, r_junk = c.sb([128, D], BF16, "junk")
        xnring = c.ring(6, [128, D], BF16, name="xn")
        ss, r_ss = c.sb([128, NT], F32, "ss")
        rstd, r_rstd = c.sb([128, NT], F32, "rstd")
        hTring = c.ring(2, [128, 8, 512], BF16, name="hT")
        ptr = c.ring(2, [128, 8, 128], BF16, psum=True, name="ptr")
        pacc = c.ring(6, [128, 512], F32, psum=True, name="pacc")
        stb = c.ring(6, [128, 512], BF16, name="stb")
        stf = c.ring(4, [128, 512], F32, name="stf")
        V(lambda e: e.memset(ss[:], 0.0), (), [r_ss])
        fm = []
        for m in range(4):
            fm.append((m * 128, "QT", m * 128, "q"))
        for m in range(4):
            fm.append((512 + m * 128, "KT", m * 128, "b"))
        for m in range(4):
            fm.append((1536 + m * 128, "RQT", m * 128, "b"))
        for m in range(4):
            fm.append((2048 + m * 128, "ZT", m * 128, "f"))
        tm = [(1024, "VV", "b"), (2560, "RI", "b"), (3072, "RG", "s")]
        ev = [0]

        def prep(g):
            hT, r_hT = hTring.next()
            tiles = []
            for j in range(4):
                t = g * 4 + j
                xt, r_xt = xring.next()
                ld(xt[:], xin[t * 128:(t + 1) * 128, :], [r_xin], [r_xt])
                A(lambda e, xt=xt, t=t: e.activation(out=junk[:], in_=xt[:], func=AF.Square, accum_out=ss[:, t:t + 1]), [r_xt], [r_junk, r_ss])
                rstd_from_ss(ss[:, t:t + 1], rstd[:, t:t + 1], D, r_ss, r_rstd)
                xn, r_xn = xnring.next()
                A(lambda e, xt=xt, xn=xn, t=t: e.activation(out=xn[:], in_=xt[:], func=AF.Copy, scale=rstd[:, t:t + 1]), [r_xt, r_rstd], [r_xn])
                tiles.append((xn, r_xn))
            for jp in range(2):
                pair = [(tiles[2 * jp + i], ptr.next()) for i in range(2)]
                for kc in range(8):
                    for ((xn, r_xn), (pt, r_pt)) in pair:
                        PE(lambda e, kc=kc, pt=pt, xn=xn: e.transpose(out=pt[:, kc, :], in_=xn[:, kc * 128:(kc + 1) * 128], identity=ident[:]),
                           [r_xn, r_ident], [r_pt])
                for i, ((xn, r_xn), (pt, r_pt)) in enumerate(pair):
                    j = 2 * jp + i
                    V(lambda e, pt=pt, hT=hT, j=j: e.tensor_tensor(out=hT[:, :, j * 128:(j + 1) * 128], in0=pt[:, :, :],
                                                                  in1=colf[:, 0:8].unsqueeze(2).to_broadcast([128, 8, 128]), op=ALU.mult),
                      [r_pt, r_colf], [r_hT])
                    GP(lambda e, hT=hT, j=j: e.tensor_tensor(out=hT[:, :, j * 128:(j + 1) * 128], in0=hT[:, :, j * 128:(j + 1) * 128],
                                                            in1=colf[:, 8:16].unsqueeze(2).to_broadcast([128, 8, 128]), op=ALU.add),
                       [r_hT, r_colf], [r_hT])
            return hT, r_hT

        def evac(kind, acc, r_acc):
            st, r_st = (stf if kind == "f" else stb).next()
            ev[0] += 1
            if kind == "q":
                A(lambda e: e.activation(out=st[:], in_=acc[:], func=AF.Copy, scale=0.125), [r_acc], [r_st])
            elif kind == "s":
                A(lambda e: e.activation(out=st[:], in_=acc[:], func=AF.Silu), [r_acc], [r_st])
            elif ev[0] % 2 == 0:
                A(lambda e: e.activation(out=st[:], in_=acc[:], func=AF.Copy), [r_acc], [r_st])
            else:
                V(lambda e: e.tensor_copy(out=st[:], in_=acc[:]), [r_acc], [r_st])
            return st, r_st

        nxt = prep(0)
        for g in range(NT // 4):
            hT, r_hT = nxt
            if g + 1 < NT // 4:
                nxt = prep(g + 1)
            for f0 in range(0, 16, 4):
                grp = [(fm[f0 + i], pacc.next()) for i in range(4)]
                for kc in range(8):
                    for ((c0, dn, row0, kind), (acc, r_acc)) in grp:
                        PE(lambda e, kc=kc, acc=acc, hT=hT, c0=c0: e.matmul(acc[:, :], lhsT=winb[:, kc, c0:c0 + 128], rhs=hT[:, kc, :],
                                                                           start=(kc == 0), stop=(kc == 7)), [r_winb, r_hT], [r_acc])
                for ((c0, dn, row0, kind), (acc, r_acc)) in grp:
                    st, r_st = evac(kind, acc, r_acc)
                    ld(dr[dn][row0:row0 + 128, g * 512:(g + 1) * 512], st[:], [r_st], [rs[dn]])
            for j in range(4):
                t = g * 4 + j
                grp = [(tm[i], pacc.next()) for i in range(3)]
                for kc in range(8):
                    for ((c0, dn, kind), (acc, r_acc)) in grp:
                        PE(lambda e, kc=kc, acc=acc, hT=hT, c0=c0, j=j: e.matmul(acc[:, :], lhsT=hT[:, kc, j * 128:(j + 1) * 128],
                                                                                 rhs=winb[:, kc, c0:c0 + 512], start=(kc == 0), stop=(kc == 7)),
                           [r_winb, r_hT], [r_acc])
                for ((c0, dn, kind), (acc, r_acc)) in grp:
                    st, r_st = evac(kind, acc, r_acc)
                    ld(dr[dn][t * 128:(t + 1) * 128, :], st[:], [r_st], [rs[dn]])
        c.close()

    def p2(l):
        c = Ctx(S)
        qT2, r_qT2 = c.sb([128, 2, T], BF16, "qT2")
        kT2, r_kT2 = c.sb([128, 2, T], BF16, "kT2")
        amask, r_amask = c.sb([128, 4, 3, 256], BF16, "amask")
        vraw = c.ring(2, [128, 8, 256], BF16, name="vraw")
        vaugr = c.ring(2, [128, 64, 4, 65], BF16, name="vaug")
        pTr = c.ring(8, [128, 256], BF16, name="pT")
        per_ = c.ring(4, [128, 256], BF16, name="pe")
        ostr = c.ring(3, [128, 4, 65], F32, name="ost")
        Sps = c.ring(4, [128, 256], F32, psum=True, name="Sps")
        OpsE = c.ring(2, [128, 2, 65], F32, psum=True, name="OpsE")
        OpsO = c.ring(2, [128, 2, 65], F32, psum=True, name="OpsO")
        ostr = c.ring(4, [128, 4, 65], F32, name="ost2")
        for (vg_, r_vg_) in vaugr.items:
            GP(lambda e, vg_=vg_: e.memset(vg_[:, :, :, 64:65], 1.0), (), [r_vg_])
        for half in range(2):
            ld(amask[:], dr["amask"].rearrange("k (h p q) -> k h p q", h=8, p=3)[:, 4 * half:4 * half + 4], (), [r_amask], "sp")
            for hpi in range(2):
                hp = 2 * half + hpi
                ld(qT2[:, hpi, :], dr["QT"][hp * 128:(hp + 1) * 128, :], [rs["QT"]], [r_qT2])
                ld(kT2[:, hpi, :], dr["KT"][hp * 128:(hp + 1) * 128, :], [rs["KT"]], [r_kT2])
            for p, (w, d) in enumerate(PATTERNS):
                span = 128 * d
                nb = T // span
                vaug, r_vaug = vaugr.next()
                vsrc = dr["VV"].rearrange("(a u r) c -> r u a c", u=128, r=d)
                for r in range(d):
                    for a0 in range(0, nb, 8):
                        na = min(8, nb - a0)
                        vr, r_vr = vraw.next()
                        ld(vr[:, 0:na, :], vsrc[r, :, a0:a0 + na, half * 256:(half + 1) * 256], [rs["VV"]], [r_vr])
                        bi0 = r * nb + a0
                        GP(lambda e, vr=vr, na=na, bi0=bi0, vaug=vaug: e.tensor_copy(out=vaug[:, bi0:bi0 + na, :, 0:64],
                                                                         in_=vr[:, 0:na, :].rearrange("k a (h c) -> k a h c", h=4)),
                           [r_vr], [r_vaug])
                odst = dr[f"OP{p}"].rearrange("(a u r) h c -> r a u h c", u=128, r=d)
                units = [(r, a) for r in range(d) for a in range(nb)]

                def s_stage(r, a, hh, d=d, p=p, span=span, half=half):
                    hpi, pb = hh // 2, 64 * (hh % 2)
                    t0 = span * a + r
                    sp_, r_sp = Sps.next()
                    q_ap = qT2[pb:pb + 64, hpi, t0:t0 + 127 * d + 1:d]
                    c0 = 0 if a > 0 else 128
                    th = []
                    if a > 0:
                        tp = t0 - span
                        th.append(lambda: PE(lambda e: e.matmul(sp_[:, 0:128], lhsT=kT2[pb:pb + 64, hpi, tp:tp + 127 * d + 1:d], rhs=q_ap,
                                                                start=True, stop=True), [r_kT2, r_qT2], [r_sp]))
                    th.append(lambda: PE(lambda e: e.matmul(sp_[:, 128:256], lhsT=kT2[pb:pb + 64, hpi, t0:t0 + 127 * d + 1:d], rhs=q_ap,
                                                            start=True, stop=True), [r_kT2, r_qT2], [r_sp]))
                    pe, r_pe = per_.next()
                    pT, r_pT = pTr.next()

                    def post():
                        A(lambda e: e.activation(out=pe[:, c0:256], in_=sp_[:, c0:256], func=AF.Exp), [r_sp], [r_pe])
                        V(lambda e: e.tensor_tensor(out=pT[:, c0:256], in0=pe[:, c0:256], in1=amask[:, hh, p, c0:256], op=ALU.mult),
                          [r_pe, r_amask], [r_pT])
                    return th, post, pT, r_pT

                def pv_thunks(r, a, hh, pT, r_pT, O, r_O, nb=nb, vaug=vaug, r_vaug=r_vaug):
                    bi = r * nb + a
                    hs = hh // 2
                    th = []
                    if a > 0:
                        th.append(lambda: PE(lambda e: e.matmul(O[:, hs, :], lhsT=pT[:, 0:128], rhs=vaug[:, bi - 1, hh, :], start=True, stop=False),
                                             [r_pT, r_vaug], [r_O]))
                    th.append(lambda: PE(lambda e: e.matmul(O[:, hs, :], lhsT=pT[:, 128:256], rhs=vaug[:, bi, hh, :], start=(a == 0), stop=True),
                                         [r_pT, r_vaug], [r_O]))
                    return th

                def interleave(lists):
                    out_ = []
                    n = max(len(x) for x in lists) if lists else 0
                    for i in range(n):
                        for x in lists:
                            if i < len(x):
                                out_.append(x[i])
                    return out_

                def finish_block(r, a, OE, r_OE, OO, r_OO):
                    ost, r_ost = ostr.next()
                    V(lambda e: e.tensor_copy(out=ost[:, 0:4:2, :], in_=OE[:]), [r_OE], [r_ost])
                    V(lambda e: e.tensor_copy(out=ost[:, 1:4:2, :], in_=OO[:]), [r_OO], [r_ost])
                    ld(odst[r, a, :, half, :], ost[:].rearrange("k h c -> k (h c)"), [r_ost], [rs[f"OP{p}"]])

                flat = [(r, a, hh) for (r, a) in units for hh in range(4)]
                pairs = [flat[i:i + 2] for i in range(0, len(flat), 2)]
                Ocur = {}
                pvq = []

                def merge2(a_, b_):
                    out_ = []
                    ia = ib = 0
                    while ia < len(a_) or ib < len(b_):
                        out_ += a_[ia:ia + 2]
                        ia += 2
                        out_ += b_[ib:ib + 2]
                        ib += 2
                    return out_

                for pr in pairs:
                    st = [s_stage(*u) for u in pr]
                    s_th = interleave([x[0] for x in st])
                    if len(pvq) >= 2:
                        pv_now, fin_now = pvq.pop(0)
                    else:
                        pv_now, fin_now = [], []
                    for f_ in merge2(s_th, pv_now):
                        f_()
                    for fb in fin_now:
                        finish_block(*fb)
                    for x in st:
                        x[1]()
                    fin = []
                    pvl = []
                    for (r, a, hh), x in zip(pr, st):
                        if hh == 0:
                            Ocur[(r, a)] = (OpsE.next(), OpsO.next())
                        (OE, r_OE), (OO, r_OO) = Ocur[(r, a)]
                        O, r_O = (OE, r_OE) if hh % 2 == 0 else (OO, r_OO)
                        pvl.append(pv_thunks(r, a, hh, x[2], x[3], O, r_O))
                        if hh == 3:
                            fin.append((r, a, OE, r_OE, OO, r_OO))
                            del Ocur[(r, a)]
                    pvq.append((interleave(pvl), fin))
                for (pv_now, fin_now) in pvq:
                    for f_ in pv_now:
                        f_()
                    for fb in fin_now:
                        finish_block(*fb)
        c.close()

    def p2b(l):
        c = Ctx(S)
        angb, r_angb = c.sb([128, 512], F32, "angb")
        ld(angb[:], dr["attn_norm_g"][l:l + 1, :].partition_broadcast(128), (), [r_angb], "sp")
        opr = [c.ring(5, [128, 8, 65], F32, name=f"op{p}") for p in range(3)]
        den, r_den = c.sb([128, 8], F32, "den")
        o, r_o = c.sb([128, 8, 64], F32, "o")
        junk, r_junk = c.sb([128, 512], BF16, "junk")
        ss, r_ss = c.sb([128, NT], F32, "ss")
        rstd, r_rstd = c.sb([128, NT], F32, "rstd")
        cst = c.ring(3, [128, 512], BF16, name="cst")
        V(lambda e: e.memset(ss[:], 0.0), (), [r_ss])
        pend_ = {}

        def loads(t):
            tl = []
            for p in range(3):
                tt, r_tt = opr[p].next()
                ld(tt[:].rearrange("k h c -> k (h c)"), dr[f"OP{p}"][t * 128:(t + 1) * 128].rearrange("k a c -> k (a c)"), [rs[f"OP{p}"]], [r_tt])
                tl.append((tt, r_tt))
            pend_[t] = tl
        for t in range(min(3, NT)):
            loads(t)
        for t in range(NT):
            tl = pend_.pop(t)
            if t + 3 < NT:
                loads(t + 3)
            (t0_, r0), (t1_, r1), (t2_, r2) = tl
            V(lambda e, a=t0_, b=t1_: e.tensor_tensor(out=a[:], in0=a[:], in1=b[:], op=ALU.add), [r0, r1], [r0])
            V(lambda e, a=t0_, b=t2_: e.tensor_tensor(out=a[:], in0=a[:], in1=b[:], op=ALU.add), [r0, r2], [r0])
            V(lambda e, a=t0_: e.reciprocal(out=den[:], in_=a[:, :, 64]), [r0], [r_den])
            V(lambda e, a=t0_: e.tensor_tensor(out=o[:], in0=a[:, :, 0:64], in1=den[:].unsqueeze(2).to_broadcast([128, 8, 64]), op=ALU.mult),
              [r0, r_den], [r_o])
            A(lambda e, t=t: e.activation(out=junk[:], in_=o[:].rearrange("k h c -> k (h c)"), func=AF.Square, accum_out=ss[:, t:t + 1]),
              [r_o], [r_junk, r_ss])
            rstd_from_ss(ss[:, t:t + 1], rstd[:, t:t + 1], 512, r_ss, r_rstd)
            cs, r_cs = cst.next()
            V(lambda e, cs=cs, t=t: e.scalar_tensor_tensor(out=cs[:], in0=o[:].rearrange("k h c -> k (h c)"), scalar=rstd[:, t:t + 1],
                                                          in1=angb[:], op0=ALU.mult, op1=ALU.mult), [r_o, r_rstd, r_angb], [r_cs])
            ld(dr["CAT"][t * 128:(t + 1) * 128, 0:512], cs[:], [r_cs], [rs["CAT"]])
        c.close()

    def p3(l):
        c = Ctx(S)
        SEG = 512
        NCK = SEG // 128
        NSEG = T // SEG
        resetm, r_resetm = c.sb([128, SEG], F32, "resetm")
        gnb, r_gnb = c.sb([128, 512], F32, "gnb")
        ld(resetm[:], dr["resetm"][:, 0:SEG], (), [r_resetm], "sp")
        ld(gnb[:], dr["hgrn_norm_g"][l:l + 1, :].partition_broadcast(128), (), [r_gnb], "sp")
        rir = c.ring(2, [128, NCK, 512], BF16, name="ri")
        rgr = c.ring(2, [128, NCK, 512], BF16, name="rgm")
        catr_ = c.ring(2, [128, NCK, 512], BF16, name="catseg")
        zr = c.ring(2, [128, SEG], F32, name="z")
        qr = c.ring(2, [128, SEG], BF16, name="q")
        tset = [[c.sb([128, SEG], F32, f"b{n}{i}") for n in "ABCE"] for i in range(2)]
        o4 = c.ring(8, [128, 5, SEG], BF16, name="o4")
        dcyr = c.ring(8, [128, NCK], F32, name="dcy")
        St = [c.sb([128, 128], F32, f"S{h}") for h in range(4)]
        Sb = [c.sb([128, 128], BF16, f"Sb{h}") for h in range(4)]
        Amr = c.ring(4, [128, 128], BF16, name="Am")
        khTr = c.ring(4, [128, 128], BF16, name="khT")
        junk, r_junk = c.sb([128, 128], BF16, "junk")
        ssr, r_ssr = c.sb([128, 4 * NT], F32, "ssr")
        rsr, r_rsr = c.sb([128, 4 * NT], F32, "rsr")
        psA = c.ring(2, [128, 128], F32, psum=True, name="psA")
        psT = c.ring(1, [128, 128], BF16, psum=True, name="psT")
        psU = c.ring(2, [128, 128], F32, psum=True, name="psU")
        psO = c.ring(2, [128, 128], F32, psum=True, name="psO")
        psX = c.ring(1, [64, 64], F32, psum=True, name="psX")
        V(lambda e: e.memset(ssr[:], 0.0), (), [r_ssr])
        for h in range(4):
            V(lambda e, h=h: e.memset(St[h][0][:], 0.0), (), [St[h][1]])
            V(lambda e, h=h: e.memset(Sb[h][0][:], 0.0), (), [Sb[h][1]])

        def v3(t):
            t = t if isinstance(t, bass.AP) else t[:]
            return t.rearrange("k (c u) -> k c u", u=128)

        def v64(t):
            return t[:].rearrange("k (c u) -> k c u", u=64)

        def ew(sg):
            tk0 = sg * SEG
            ri, r_ri = rir.next()
            rgm, r_rgm = rgr.next()
            ld(ri[:], dr["RI"][tk0:tk0 + SEG, :].rearrange("(c p) f -> p c f", p=128), [rs["RI"]], [r_ri])
            ld(rgm[:], dr["RG"][tk0:tk0 + SEG, :].rearrange("(c p) f -> p c f", p=128), [rs["RG"]], [r_rgm])
            GP(lambda e: e.tensor_tensor(out=rgm[:], in0=rgm[:], in1=gnb[:].unsqueeze(1).to_broadcast([128, NCK, 512]), op=ALU.mult),
               [r_rgm, r_gnb], [r_rgm])
            heads = []

            def head_ops(hd):
                (bA, r_A), (bB, r_B), (bC, r_C), (bE, r_E) = tset[hd % 2]
                ops = []
                AA = lambda *a: ops.append(lambda: A(*a))
                VV = lambda *a: ops.append(lambda: V(*a))
                GG = lambda *a: ops.append(lambda: GP(*a))
                z, r_z = zr.next()
                q, r_q = qr.next()
                ld(z[:], dr["ZT"][hd * 128:(hd + 1) * 128, tk0:tk0 + SEG], [rs["ZT"]], [r_z])
                ld(q[:], dr["RQT"][hd * 128:(hd + 1) * 128, tk0:tk0 + SEG], [rs["RQT"]], [r_q])
                o, r_o4 = o4.next()
                dcy, r_dcy = dcyr.next()
                lb_ap, oml_ap = lbc[:, l, hd:hd + 1], oml[:, l, hd:hd + 1]

                def b3(t):
                    return t[:].rearrange("k (c u) -> k c u", u=128)

                def b64(t):
                    return t[:].rearrange("k (c u) -> k c u", u=64)
                VV(lambda e: e.tensor_scalar(out=z[:], in0=z[:], scalar1=-60.0, scalar2=None, op0=ALU.max), [r_z], [r_z])
                AA(lambda e: e.activation(out=bA[:], in_=z[:], func=AF.Exp, scale=-1.0), [r_z], [r_A])
                AA(lambda e: e.activation(out=bB[:], in_=bA[:], func=AF.Ln, bias=1.0), [r_A], [r_B])
                AA(lambda e: e.activation(out=bE[:], in_=bA[:], func=AF.Ln, bias=1.0, scale=lb_ap), [r_A, r_lbc], [r_E])
                VV(lambda e: e.tensor_tensor(out=bE[:], in0=bE[:], in1=bB[:], op=ALU.subtract), [r_E, r_B], [r_E])
                AA(lambda e: e.activation(out=bB[:], in_=bB[:], func=AF.Exp, scale=-1.0), [r_B], [r_B])
                VV(lambda e: e.scalar_tensor_tensor(out=bC[:], in0=bA[:], scalar=oml_ap, in1=bB[:], op0=ALU.mult, op1=ALU.mult),
                   [r_A, r_B, r_oml], [r_C])
                VV(lambda e: e.tensor_tensor_scan(out=bB[:], data0=resetm[:], data1=bE[:], initial=0.0, op0=ALU.mult, op1=ALU.add),
                   [r_resetm, r_E], [r_B])
                VV(lambda e: e.tensor_tensor(out=b64(bE), in0=b64(bB), in1=b64(bB)[:, :, 31:32].to_broadcast([128, 2 * NCK, 64]), op=ALU.subtract),
                   [r_B], [r_E])
                VV(lambda e: e.tensor_scalar(out=bE[:], in0=bE[:], scalar1=-80.0, scalar2=80.0, op0=ALU.max, op1=ALU.min), [r_E], [r_E])
                AA(lambda e: e.activation(out=bA[:], in_=bE[:], func=AF.Exp), [r_E], [r_A])
                VV(lambda e: e.tensor_tensor(out=o[:, 0, :], in0=q[:], in1=bA[:], op=ALU.mult), [r_q, r_A], [r_o4])
                AA(lambda e: e.activation(out=bA[:], in_=bE[:], func=AF.Exp, scale=-1.0), [r_E], [r_A])
                VV(lambda e: e.tensor_tensor(out=o[:, 1, :], in0=bC[:], in1=bA[:], op=ALU.mult), [r_C, r_A], [r_o4])
                AA(lambda e: e.activation(out=bA[:], in_=bB[:], func=AF.Exp), [r_B], [r_A])
                GG(lambda e: e.tensor_tensor(out=o[:, 2, :], in0=q[:], in1=bA[:], op=ALU.mult), [r_q, r_A], [r_o4])
                VV(lambda e: e.tensor_tensor(out=b3(bE), in0=b3(bB), in1=b3(bB)[:, :, 127:128].to_broadcast([128, NCK, 128]), op=ALU.subtract),
                   [r_B], [r_E])
                AA(lambda e: e.activation(out=bA[:], in_=bE[:], func=AF.Exp, scale=-1.0), [r_E], [r_A])
                GG(lambda e: e.tensor_tensor(out=o[:, 3, :], in0=bC[:], in1=bA[:], op=ALU.mult), [r_C, r_A], [r_o4])
                VV(lambda e: e.tensor_tensor(out=b3(bE), in0=b3(bB), in1=b3(bB)[:, :, 63:64].to_broadcast([128, NCK, 128]), op=ALU.subtract),
                   [r_B], [r_E])
                VV(lambda e: e.scalar_tensor_tensor(out=bE[:], in0=bE[:], scalar=-1.0, in1=bE[:], op0=ALU.mult, op1=ALU.min), [r_E], [r_E])
                AA(lambda e: e.activation(out=bA[:], in_=bE[:], func=AF.Exp), [r_E], [r_A])
                VV(lambda e: e.tensor_tensor(out=v3(o[:, 4, :])[:, :, 0:64], in0=b3(bC)[:, :, 0:64], in1=b3(bA)[:, :, 0:64], op=ALU.mult),
                   [r_C, r_A], [r_o4])
                VV(lambda e: e.tensor_tensor(out=v3(o[:, 4, :])[:, :, 64:128], in0=v3(q)[:, :, 64:128], in1=b3(bA)[:, :, 64:128], op=ALU.mult),
                   [r_q, r_A], [r_o4])
                AA(lambda e: e.activation(out=dcy[:], in_=b3(bB)[:, :, 127], func=AF.Exp), [r_B], [r_dcy])
                heads.append((o, r_o4, dcy, r_dcy))
                return ops

            for hp in range(2):
                la = head_ops(2 * hp)
                lb_ = head_ops(2 * hp + 1)
                for i in range(max(len(la), len(lb_))):
                    if i < len(la):
                        la[i]()
                    if i < len(lb_):
                        lb_[i]()
            return dict(sg=sg, ri=(ri, r_ri), rgm=(rgm, r_rgm), heads=heads)

        def chunks(st):
            sg = st["sg"]
            tk0 = sg * SEG
            ri, r_ri = st["ri"]
            rgm, r_rgm = st["rgm"]
            catseg, r_catseg = catr_.next()
            for ci_ in range(NCK):
                one_chunk(st, ci_, sg, ri, r_ri, rgm, r_rgm, catseg, r_catseg)
            ld(dr["CAT"][tk0:tk0 + SEG, 512:1024].rearrange("(c p) f -> p c f", p=128), catseg[:], [r_catseg], [rs["CAT"]])

        def one_chunk(st, ci, sg, ri, r_ri, rgm, r_rgm, catseg, r_catseg):
            if True:
                cs = slice(ci * 128, (ci + 1) * 128)
                g0 = (sg * NCK + ci) * 4
                per = []
                for hd in range(4):
                    o, r_o4, dcy, r_dcy = st["heads"][hd]
                    pa, r_pa = psA.next()
                    PE(lambda e, pa=pa, o=o: e.matmul(pa[:, :], lhsT=o[:, 1, cs], rhs=o[:, 0, cs], start=True, stop=True), [r_o4], [r_pa])
                    px, r_px = psX.next()
                    PE(lambda e, px=px, o=o: e.matmul(px[0:64, :], lhsT=o[:, 4, ci * 128:ci * 128 + 64], rhs=o[:, 4, ci * 128 + 64:(ci + 1) * 128],
                                                      start=True, stop=True), [r_o4], [r_px])
                    pt, r_pt = psT.next()
                    PE(lambda e, pt=pt, o=o: e.transpose(out=pt[:, :], in_=o[:, 3, cs], identity=ident[:]), [r_o4, r_ident], [r_pt])
                    Am, r_Am = Amr.next()
                    GP(lambda e, Am=Am: e.memset(Am[:], 0.0), (), [r_Am])
                    V(lambda e, Am=Am, pa=pa: e.copy_predicated(out=Am[:], mask=causal[:], data=pa[:]), [r_pa, r_causal, r_Am], [r_Am])
                    A(lambda e, Am=Am, px=px: e.activation(out=Am[0:64, 64:128], in_=px[0:64, :], func=AF.Copy), [r_px, r_Am], [r_Am])
                    khT, r_khT = khTr.next()
                    A(lambda e, khT=khT, pt=pt: e.activation(out=khT[:], in_=pt[:], func=AF.Copy), [r_pt], [r_khT])
                    per.append((Am, r_Am, khT, r_khT))
                pos_ = []
                for hd in range(4):
                    o, r_o4, dcy, r_dcy = st["heads"][hd]
                    Am, r_Am, khT, r_khT = per[hd]
                    Sh, r_Sh = St[hd]
                    Sbh, r_Sbh = Sb[hd]
                    v_ap = ri[:, ci, hd * 128:(hd + 1) * 128]
                    pu, r_pu = psU.next()
                    PE(lambda e, pu=pu, khT=khT, v_ap=v_ap: e.matmul(pu[:, :], lhsT=khT[:], rhs=v_ap, start=True, stop=True), [r_khT, r_ri], [r_pu])
                    po, r_po = psO.next()
                    PE(lambda e, po=po, Am=Am, v_ap=v_ap: e.matmul(po[:, :], lhsT=Am[:], rhs=v_ap, start=True, stop=False), [r_Am, r_ri], [r_po])
                    PE(lambda e, po=po, o=o, Sbh=Sbh: e.matmul(po[:, :], lhsT=o[:, 2, cs], rhs=Sbh[:], start=False, stop=True),
                       [r_o4, r_Sbh], [r_po])
                    V(lambda e, Sh=Sh, pu=pu, dcy=dcy: e.scalar_tensor_tensor(out=Sh[:], in0=Sh[:], scalar=dcy[:, ci:ci + 1], in1=pu[:],
                                                                             op0=ALU.mult, op1=ALU.add), [r_Sh, r_pu, r_dcy], [r_Sh])
                    A(lambda e, Sh=Sh, Sbh=Sbh: e.activation(out=Sbh[:], in_=Sh[:], func=AF.Copy), [r_Sh], [r_Sbh])
                    A(lambda e, po=po, gi=g0 + hd: e.activation(out=junk[:], in_=po[:], func=AF.Square, accum_out=ssr[:, gi:gi + 1]),
                      [r_po], [r_junk, r_ssr])
                    rstd_from_ss(ssr[:, g0 + hd:g0 + hd + 1], rsr[:, g0 + hd:g0 + hd + 1], 128, r_ssr, r_rsr)
                    V(lambda e, po=po, gi=g0 + hd, hd=hd: e.scalar_tensor_tensor(out=catseg[:, ci, hd * 128:(hd + 1) * 128], in0=po[:],
                                                                                scalar=rsr[:, gi:gi + 1], in1=rgm[:, ci, hd * 128:(hd + 1) * 128],
                                                                                op0=ALU.mult, op1=ALU.mult), [r_po, r_rsr, r_rgm], [r_catseg])

        nxt = ew(0)
        for sg in range(NSEG):
            cur = nxt
            if sg + 1 < NSEG:
                nxt = ew(sg + 1)
            chunks(cur)
        c.close()

    def p4(l, xin, r_xin):
        c = Ctx(S)
        woutb, r_woutb = c.sb([128, 8, D], BF16, "woutb")
        wst = c.ring(2, [128, 4, D], F32, name="wst")
        for hf in range(2):
            st, r_st = wst.next()
            ld(st[:], dr["w_out"][l, hf * 512:(hf + 1) * 512, :].rearrange("(kc p) n -> p kc n", p=128), (), [r_st])
            (V if hf == 0 else GP)(lambda e, st=st, hf=hf: e.tensor_copy(out=woutb[:, hf * 4:(hf + 1) * 4, :], in_=st[:]), [r_st], [r_woutb])
        wrs, r_wrs = c.sb([128, 8, 36], F32, "wrs")
        wrb, r_wrb = c.sb([128, 8, 36], BF16, "wrb")
        rbb, r_rbb = c.sb([128, 36], F32, "rbb")
        S.dma("sp", lambda e: e.dma_start(out=wrs[:], in_=dr["router_w"][l].rearrange("(kc p) n -> p kc n", p=128),
                                          allow_slow_non_contiguous=True), (), [r_wrs])
        V(lambda e: e.tensor_copy(out=wrb[:], in_=wrs[:]), [r_wrs], [r_wrb])
        ld(rbb[:], dr["router_b"][l:l + 1, :].partition_broadcast(128), (), [r_rbb], "sp")
        catr = c.ring(4, [128, D], BF16, name="cat")
        catTr = c.ring(3, [128, 8, 128], BF16, name="catT")
        xr = c.ring(4, [128, D], F32, name="x")
        x1r = c.ring(3, [128, D], F32, name="x1")
        h2r = c.ring(5, [128, D], BF16, name="h2")
        h2fr = c.ring(2, [128, D], F32, name="h2f")
        h2Tr = c.ring(3, [128, 8, 128], BF16, name="h2T")
        junk, r_junk = c.sb([128, D], BF16, "junk")
        ss, r_ss = c.sb([128, NT], F32, "ss")
        rstd, r_rstd = c.sb([128, NT], F32, "rstd")
        ptr = c.ring(2, [128, 8, 128], BF16, psum=True, name="ptr")
        pacc = c.ring(4, [128, 512], F32, psum=True, name="pacc")
        plg = c.ring(2, [128, 36], F32, psum=True, name="plg")
        V(lambda e: e.memset(ss[:], 0.0), (), [r_ss])
        st_ = {}

        def loads(t):
            ct, r_ct = catr.next()
            ld(ct[:], dr["CAT"][t * 128:(t + 1) * 128, :], [rs["CAT"]], [r_ct])
            xt, r_xt = xr.next()
            ld(xt[:], xin[t * 128:(t + 1) * 128, :], [r_xin], [r_xt])
            st_[t] = dict(ct=(ct, r_ct), xt=(xt, r_xt))

        def transposes(ta, td):
            jobs = []
            if ta is not None:
                pt, r_pt = ptr.next()
                ct, r_ct = st_[ta]["ct"]
                jobs.append([(lambda kc=kc, pt=pt, ct=ct, r_ct=r_ct, r_pt=r_pt: PE(
                    lambda e: e.transpose(out=pt[:, kc, :], in_=ct[:, kc * 128:(kc + 1) * 128], identity=ident[:]), [r_ct, r_ident], [r_pt]))
                    for kc in range(8)])
            if td is not None:
                pt2, r_pt2 = ptr.next()
                h2, r_h2 = st_[td]["h2"]
                jobs.append([(lambda kc=kc, pt2=pt2, h2=h2, r_h2=r_h2, r_pt2=r_pt2: PE(
                    lambda e: e.transpose(out=pt2[:, kc, :], in_=h2[:, kc * 128:(kc + 1) * 128], identity=ident[:]), [r_h2, r_ident], [r_pt2]))
                    for kc in range(8)])
            n = max(len(j) for j in jobs)
            for i in range(n):
                for j in jobs:
                    j[i]()
            if ta is not None:
                cT, r_cT = catTr.next()
                A(lambda e, cT=cT, pt=pt: e.activation(out=cT[:], in_=pt[:], func=AF.Copy), [r_pt], [r_cT])
                st_[ta]["cT"] = (cT, r_cT)
            if td is not None:
                hT, r_hT = h2Tr.next()
                A(lambda e, hT=hT, pt2=pt2: e.activation(out=hT[:], in_=pt2[:], func=AF.Copy), [r_pt2], [r_hT])
                st_[td]["hT"] = (hT, r_hT)

        def matmuls(t, tr):
            accs = None
            if t is not None:
                cT, r_cT = st_[t]["cT"]
                accs = [pacc.next(), pacc.next()]
            if tr is not None:
                hT, r_hT = st_[tr]["hT"]
                pl, r_pl = plg.next()
            for kc in range(8):
                if t is not None:
                    for n in range(2):
                        acc, r_acc = accs[n]
                        PE(lambda e, kc=kc, acc=acc, cT=cT, n=n: e.matmul(acc[:, :], lhsT=cT[:, kc, :], rhs=woutb[:, kc, n * 512:(n + 1) * 512],
                                                                         start=(kc == 0), stop=(kc == 7)), [r_cT, r_woutb], [r_acc])
                if tr is not None:
                    PE(lambda e, kc=kc, pl=pl, hT=hT: e.matmul(pl[:, :], lhsT=hT[:, kc, :], rhs=wrb[:, kc, :], start=(kc == 0), stop=(kc == 7)),
                       [r_hT, r_wrb], [r_pl])
            if tr is not None:
                V(lambda e, pl=pl, tr=tr: e.tensor_tensor(out=lgall[:, tr, :], in0=pl[:, :], in1=rbb[:], op=ALU.add), [r_pl, r_rbb], [r_lgall])
                del st_[tr]
            return accs

        def elementwise(t, accs):
            xt, r_xt = st_[t]["xt"]
            x1, r_x1 = x1r.next()
            for n in range(2):
                acc, r_acc = accs[n]
                V(lambda e, acc=acc, x1=x1, n=n: e.tensor_tensor(out=x1[:, n * 512:(n + 1) * 512], in0=acc[:, :], in1=G1b[:, n * 512:(n + 1) * 512],
                                                                op=ALU.mult), [r_acc, r_G1b], [r_x1])
            GP(lambda e, x1=x1, xt=xt: e.tensor_tensor(out=x1[:], in0=x1[:], in1=xt[:], op=ALU.add), [r_x1, r_xt], [r_x1])
            ld(dr["Xs"][t * 128:(t + 1) * 128, :], x1[:], [r_x1], [rs["Xs"]])
            A(lambda e, x1=x1, t=t: e.activation(out=junk[:], in_=x1[:], func=AF.Square, accum_out=ss[:, t:t + 1]), [r_x1], [r_junk, r_ss])
            rstd_from_ss(ss[:, t:t + 1], rstd[:, t:t + 1], D, r_ss, r_rstd)
            h2f_, r_h2f_ = h2fr.next()
            V(lambda e, x1=x1, t=t, h2f_=h2f_: e.scalar_tensor_tensor(out=h2f_[:], in0=x1[:], scalar=rstd[:, t:t + 1], in1=A2b[:], op0=ALU.mult, op1=ALU.mult),
              [r_x1, r_rstd, r_A2b], [r_h2f_])
            h2, r_h2 = h2r.next()
            GP(lambda e, h2=h2, h2f_=h2f_: e.tensor_tensor(out=h2[:], in0=h2f_[:], in1=S2b[:], op=ALU.add), [r_h2f_, r_S2b], [r_h2])
            ld(dr["H2"][t * 128:(t + 1) * 128, :], h2[:], [r_h2], [rs["H2"]])
            st_[t]["h2"] = (h2, r_h2)

        loads(0)
        loads(1)
        transposes(0, None)
        for t in range(NT + 3):
            if t + 2 < NT:
                loads(t + 2)
            ta = t + 1 if t + 1 < NT else None
            td = t - 2 if 0 <= t - 2 < NT else None
            if ta is not None or td is not None:
                transposes(ta, td)
            tm_ = t if t < NT else None
            trr = t - 3 if 0 <= t - 3 < NT else None
            if tm_ is not None or trr is not None:
                accs = matmuls(tm_, trr)
            if tm_ is not None:
                elementwise(tm_, accs)
        c.close()

    def p4b(l):
        c = Ctx(S)
        N3 = [128, NT, NE]
        gmax, r_gmax = c.sb([128, NT], F32)
        g4, r_g4 = c.sb([128, NT, 4], F32)
        goh, r_goh = c.sb([128, NT, 4], F32)
        gsum, r_gsum = c.sb([128, NT], F32)
        em, r_em = c.sb(N3, F32)
        oh1, r_oh1 = c.sb(N3, F32)
        oh2, r_oh2 = c.sb(N3, F32)
        m1, r_m1 = c.sb([128, NT], F32)
        m2, r_m2 = c.sb([128, NT], F32)
        p1, r_p1 = c.sb([128, NT], F32)
        abf, r_abf = c.sb([128, NT * NE], BF16)
        csb, r_csb = c.sb(N3, F32)
        offs, r_offs = c.sb(N3, F32)
        pos, r_pos = c.sb(N3, F32)
        sf, r_sf = c.sb([128, NT], F32)
        cnt, r_cnt = c.sb([128, NE], F32)
        nblk, r_nblk = c.sb([128, NE], F32)
        pendb, r_pendb = c.sb([128, NE], F32)
        pst, r_pst = c.sb([128, NE], F32)
        onesne, r_onesne = c.sb([128, NE], F32)
        bex, r_bex = c.sb([128, NBLK], F32)
        cmp, r_cmp = c.sb([128, NBLK * NE], F32)
        pp = c.ring(1, [128, NT * NE], F32, psum=True)
        pc = c.ring(1, [128, NT * NE], F32, psum=True)
        lgG = lgall[:, :, 0:4]
        lgE = lgall[:, :, 4:36]

        def bc(ap2, n):
            return ap2.unsqueeze(2).to_broadcast([128, NT, n])
        V(lambda e: e.tensor_reduce(out=gmax[:], in_=lgG, axis=AX.X, op=ALU.max), [r_lgall], [r_gmax])
        V(lambda e: e.tensor_tensor(out=goh[:], in0=lgG, in1=bc(gmax[:], 4), op=ALU.is_equal), [r_lgall, r_gmax], [r_goh])
        V(lambda e: e.tensor_tensor(out=g4[:], in0=lgG, in1=bc(gmax[:], 4), op=ALU.subtract), [r_lgall, r_gmax], [r_g4])
        A(lambda e: e.activation(out=g4[:], in_=g4[:], func=AF.Exp), [r_g4], [r_g4])
        V(lambda e: e.tensor_reduce(out=gsum[:], in_=g4[:], axis=AX.X, op=ALU.add), [r_g4], [r_gsum])
        V(lambda e: e.reciprocal(out=gsum[:], in_=gsum[:]), [r_gsum], [r_gsum])
        V(lambda e: e.tensor_scalar(out=goh[:], in0=goh[:], scalar1=BIG, scalar2=-BIG, op0=ALU.mult, op1=ALU.add), [r_goh], [r_goh])
        V(lambda e: e.tensor_tensor(out=em[:].rearrange("k t (g x) -> k t g x", g=4), in0=lgE.rearrange("k t (g x) -> k t g x", g=4),
                                    in1=goh[:].unsqueeze(3).to_broadcast([128, NT, 4, 8]), op=ALU.add), [r_lgall, r_goh], [r_em])
        V(lambda e: e.tensor_reduce(out=m1[:], in_=em[:], axis=AX.X, op=ALU.max), [r_em], [r_m1])
        V(lambda e: e.tensor_tensor(out=oh1[:], in0=em[:], in1=bc(m1[:], NE), op=ALU.is_equal), [r_em, r_m1], [r_oh1])
        V(lambda e: e.scalar_tensor_tensor(out=em[:], in0=oh1[:], scalar=-BIG, in1=em[:], op0=ALU.mult, op1=ALU.add), [r_oh1, r_em], [r_em])
        V(lambda e: e.tensor_reduce(out=m2[:], in_=em[:], axis=AX.X, op=ALU.max), [r_em], [r_m2])
        V(lambda e: e.tensor_tensor(out=oh2[:], in0=em[:], in1=bc(m2[:], NE), op=ALU.is_equal), [r_em, r_m2], [r_oh2])
        V(lambda e: e.tensor_tensor(out=m2[:], in0=m2[:], in1=m1[:], op=ALU.subtract), [r_m1, r_m2], [r_m2])
        A(lambda e: e.activation(out=m2[:], in_=m2[:], func=AF.Exp), [r_m2], [r_m2])
        V(lambda e: e.tensor_scalar(out=p1[:], in0=m2[:], scalar1=1.0, scalar2=None, op0=ALU.add), [r_m2], [r_p1])
        V(lambda e: e.reciprocal(out=p1[:], in_=p1[:]), [r_p1], [r_p1])
        V(lambda e: e.tensor_tensor(out=gt1[:], in0=p1[:], in1=gsum[:], op=ALU.mult), [r_p1, r_gsum], [r_gt1])
        V(lambda e: e.tensor_tensor(out=m2[:], in0=m2[:], in1=gt1[:], op=ALU.mult), [r_m2, r_gt1], [r_m2])
        V(lambda e: e.tensor_copy(out=gt2[:], in_=m2[:]), [r_m2], [r_gt2])
        V(lambda e: e.tensor_tensor(out=abf[:].rearrange("k (t x) -> k t x", x=NE), in0=oh1[:], in1=oh2[:], op=ALU.add), [r_oh1, r_oh2], [r_abf])
        ppt, r_pp = pp.next()
        pct, r_pc = pc.next()
        for ch in range(4):
            PE(lambda e, ch=ch: e.matmul(ppt[:, ch * 512:(ch + 1) * 512], lhsT=ustrict[:], rhs=abf[:, ch * 512:(ch + 1) * 512], start=True, stop=True),
               [r_ustrict, r_abf], [r_pp])
            PE(lambda e, ch=ch: e.matmul(pct[:, ch * 512:(ch + 1) * 512], lhsT=onesb[:], rhs=abf[:, ch * 512:(ch + 1) * 512], start=True, stop=True),
               [r_onesb, r_abf], [r_pc])
        V(lambda e: e.tensor_copy(out=csb[:].rearrange("k t x -> k (t x)"), in_=pct[:]), [r_pc], [r_csb])
        V(lambda e: e.memset(offs[:, 0, :], 0.0), (), [r_offs])
        for j in range(1, NT):
            V(lambda e, j=j: e.tensor_tensor(out=offs[:, j, :], in0=offs[:, j - 1, :], in1=csb[:, j - 1, :], op=ALU.add), [r_offs, r_csb], [r_offs])
        V(lambda e: e.tensor_tensor(out=cnt[:], in0=offs[:, NT - 1, :], in1=csb[:, NT - 1, :], op=ALU.add), [r_offs, r_csb], [r_cnt])
        V(lambda e: e.tensor_tensor(out=cmp[:, 0:NE * 64].rearrange("k (x m) -> k x m", m=64), in0=cnt[:].unsqueeze(2).to_broadcast([128, NE, 64]),
                                    in1=mulrow[:].unsqueeze(1).to_broadcast([128, NE, 64]), op=ALU.is_gt), [r_cnt, r_mulrow], [r_cmp])
        V(lambda e: e.tensor_reduce(out=nblk[:], in_=cmp[:, 0:NE * 64].rearrange("k (x m) -> k x m", m=64), axis=AX.X, op=ALU.add), [r_cmp], [r_nblk])
        V(lambda e: e.memset(onesne[:], 1.0), (), [r_onesne])
        V(lambda e: e.tensor_tensor_scan(out=pendb[:], data0=onesne[:], data1=nblk[:], initial=0.0, op0=ALU.mult, op1=ALU.add),
          [r_onesne, r_nblk], [r_pendb])
        V(lambda e: e.tensor_tensor(out=pst[:], in0=pendb[:], in1=nblk[:], op=ALU.subtract), [r_pendb, r_nblk], [r_pst])
        V(lambda e: e.tensor_scalar(out=pst[:], in0=pst[:], scalar1=float(RB), scalar2=None, op0=ALU.mult), [r_pst], [r_pst])
        V(lambda e: e.tensor_tensor(out=cmp[:, 0:NBLK * NE].rearrange("k (b x) -> k b x", x=NE), in0=pendb[:].unsqueeze(1).to_broadcast([128, NBLK, NE]),
                                    in1=brow[:].unsqueeze(2).to_broadcast([128, NBLK, NE]), op=ALU.is_le), [r_pendb, r_brow], [r_cmp])
        V(lambda e: e.tensor_reduce(out=bex[:], in_=cmp[:, 0:NBLK * NE].rearrange("k (b x) -> k b x", x=NE), axis=AX.X, op=ALU.add), [r_cmp], [r_bex])
        V(lambda e: e.tensor_scalar(out=bex[:], in0=bex[:], scalar1=float(NE - 1), scalar2=float(128), op0=ALU.min, op1=ALU.mult), [r_bex], [r_bex])
        V(lambda e: e.tensor_scalar(out=bex[:], in0=bex[:], scalar1=piota[:, 0:1], scalar2=float(l * NE * 128), op0=ALU.add, op1=ALU.add),
          [r_bex, r_piota], [r_bex])
        V(lambda e: e.tensor_copy(out=widx[:], in_=bex[:]), [r_bex], [r_widx])
        V(lambda e: e.tensor_tensor(out=offs[:], in0=offs[:], in1=pst[:].unsqueeze(1).to_broadcast([128, NT, NE]), op=ALU.add), [r_offs, r_pst], [r_offs])
        V(lambda e: e.tensor_tensor(out=pos[:].rearrange("k t x -> k (t x)"), in0=ppt[:], in1=offs[:].rearrange("k t x -> k (t x)"), op=ALU.add),
          [r_pp, r_offs], [r_pos])
        for (oh, r_oh, sl, r_sl) in ((oh1, r_oh1, slot1, r_slot1), (oh2, r_oh2, slot2, r_slot2)):
            V(lambda e, oh=oh: e.tensor_tensor(out=oh[:], in0=oh[:], in1=pos[:], op=ALU.mult), [r_oh, r_pos], [r_oh])
            V(lambda e, oh=oh: e.tensor_reduce(out=sf[:], in_=oh[:], axis=AX.X, op=ALU.add), [r_oh], [r_sf])
            V(lambda e: e.tensor_scalar(out=sf[:], in0=sf[:], scalar1=float(NROWS - 1), scalar2=0.0, op0=ALU.min, op1=ALU.max), [r_sf], [r_sf])
            V(lambda e, sl=sl: e.tensor_copy(out=sl[:], in_=sf[:]), [r_sf], [r_sl])
        h2r = c.ring(3, [128, D], BF16, name="h2d")
        for t in range(NT):
            h2, r_h2 = h2r.next()
            ld(h2[:], dr["H2"][t * 128:(t + 1) * 128, :], [rs["H2"]], [r_h2])
            for (sl, r_sl) in ((slot1, r_slot1), (slot2, r_slot2)):
                S.dma("pool", lambda e, h2=h2, sl=sl, t=t: e.indirect_dma_start(
                    out=dr["XS"], out_offset=bass.IndirectOffsetOnAxis(ap=sl[:, t:t + 1], axis=0), in_=h2[:], in_offset=None),
                    [r_h2, r_sl], [rs["XS"]])
        c.close()

    def p5(l):
        c = Ctx(S)
        wsr = c.ring(6, [128, 2048], F32, name="wsr")
        wbr = [c.ring(2, [128, 4096], BF16, name=f"wb{i}") for i in range(3)]
        xrr = c.ring(4, [128, D], BF16, name="xr")
        xTr = c.ring(2, [128, 8, RB], BF16, name="xT")
        actr = c.ring(2, [128, 4, RB], BF16, name="actT")
        silr = c.ring(2, [128, RB], F32, name="sil")
        ysr = c.ring(3, [128, D], F32, name="ys")
        ptr = c.ring(2, [128, 8, 128], BF16, psum=True, name="ptr")
        pacc = c.ring(6, [128, 512], F32, psum=True, name="pacc")
        wsrc = ((dr["moe_w1_0"], dr["moe_w1_1"]), (dr["moe_w3_0"], dr["moe_w3_1"]), (dr["moe_w2_0"], dr["moe_w2_1"]))
        k = [0]

        def wload(b):
            wb = []
            for i in range(3):
                wt, r_wt = wbr[i].next()
                for hf in range(2):
                    st, r_st = wsr.next()
                    S.dma("pool", lambda e, st=st, i=i, hf=hf: e.indirect_dma_start(
                        out=st[:], out_offset=None, in_=wsrc[i][hf],
                        in_offset=bass.IndirectOffsetOnAxis(ap=widx[:, b:b + 1], axis=0)), [r_widx], [r_st])
                    k[0] += 1
                    dst = wt[:, hf * 2048:(hf + 1) * 2048]
                    if k[0] % 2 == 0:
                        V(lambda e, st=st, dst=dst: e.tensor_copy(out=dst, in_=st[:]), [r_st], [r_wt])
                    else:
                        A(lambda e, st=st, dst=dst: e.activation(out=dst, in_=st[:], func=AF.Copy), [r_st], [r_wt])
                wb.append((wt, r_wt))
            return wb

        def xprep(b):
            xT, r_xT = xTr.next()
            tl = []
            for rt in range(RB // 128):
                xr_, r_xr = xrr.next()
                r0 = b * RB + rt * 128
                ld(xr_[:], dr["XS"][r0:r0 + 128, :], [rs["XS"]], [r_xr])
                tl.append((xr_, r_xr, ptr.next()))
            for kc in range(8):
                for (xr_, r_xr, (pt, r_pt)) in tl:
                    PE(lambda e, kc=kc, pt=pt, xr_=xr_: e.transpose(out=pt[:, kc, :], in_=xr_[:, kc * 128:(kc + 1) * 128], identity=ident[:]),
                       [r_xr, r_ident], [r_pt])
            for rt, (xr_, r_xr, (pt, r_pt)) in enumerate(tl):
                if rt == 0:
                    V(lambda e, xT=xT, pt=pt, rt=rt: e.tensor_copy(out=xT[:, :, rt * 128:(rt + 1) * 128], in_=pt[:]), [r_pt], [r_xT])
                else:
                    A(lambda e, xT=xT, pt=pt, rt=rt: e.activation(out=xT[:, :, rt * 128:(rt + 1) * 128], in_=pt[:], func=AF.Copy), [r_pt], [r_xT])
            return xT, r_xT

        Wn = wload(0)
        Xn = xprep(0)
        for b in range(NBLK):
            (w1b, r_w1b), (w3b, r_w3b), (w2b, r_w2b) = Wn
            xT, r_xT = Xn
            if b + 1 < NBLK:
                Wn = wload(b + 1)
            aT, r_aT = actr.next()
            for mp in range(2):
                accs = []
                for mi in range(2):
                    accs.append((pacc.next(), pacc.next()))
                for kc in range(8):
                    for mi in range(2):
                        mc = 2 * mp + mi
                        (a1, r_a1), (a3, r_a3) = accs[mi]
                        PE(lambda e, kc=kc, mc=mc, a1=a1, w1b=w1b, xT=xT: e.matmul(a1[:, 0:RB], lhsT=w1b[:, kc * 512 + mc * 128:kc * 512 + (mc + 1) * 128],
                                                                                  rhs=xT[:, kc, :], start=(kc == 0), stop=(kc == 7)), [r_w1b, r_xT], [r_a1])
                        PE(lambda e, kc=kc, mc=mc, a3=a3, w3b=w3b, xT=xT: e.matmul(a3[:, 0:RB], lhsT=w3b[:, kc * 512 + mc * 128:kc * 512 + (mc + 1) * 128],
                                                                                  rhs=xT[:, kc, :], start=(kc == 0), stop=(kc == 7)), [r_w3b, r_xT], [r_a3])
                for mi in range(2):
                    mc = 2 * mp + mi
                    (a1, r_a1), (a3, r_a3) = accs[mi]
                    sl, r_sl = silr.next()
                    A(lambda e, sl=sl, a1=a1: e.activation(out=sl[:], in_=a1[:, 0:RB], func=AF.Silu), [r_a1], [r_sl])
                    V(lambda e, sl=sl, a3=a3, mc=mc, aT=aT: e.tensor_tensor(out=aT[:, mc, :], in0=sl[:], in1=a3[:, 0:RB], op=ALU.mult), [r_sl, r_a3], [r_aT])
            if b + 1 < NBLK:
                Xn = xprep(b + 1)
            yaccs = [[pacc.next() for nch in range(2)] for rt in range(RB // 128)]
            for mc in range(4):
                for rt in range(RB // 128):
                    for nch in range(2):
                        acc, r_acc = yaccs[rt][nch]
                        PE(lambda e, mc=mc, acc=acc, rt=rt, nch=nch, aT=aT, w2b=w2b: e.matmul(
                            acc[:, :], lhsT=aT[:, mc, rt * 128:(rt + 1) * 128], rhs=w2b[:, mc * 1024 + nch * 512:mc * 1024 + (nch + 1) * 512],
                            start=(mc == 0), stop=(mc == 3)), [r_aT, r_w2b], [r_acc])
            for rt in range(RB // 128):
                ys, r_ys = ysr.next()
                for nch in range(2):
                    acc, r_acc = yaccs[rt][nch]
                    if nch == 0:
                        A(lambda e, ys=ys, acc=acc: e.activation(out=ys[:, 0:512], in_=acc[:, :], func=AF.Copy), [r_acc], [r_ys])
                    else:
                        V(lambda e, ys=ys, acc=acc: e.tensor_copy(out=ys[:, 512:1024], in_=acc[:, :]), [r_acc], [r_ys])
                r0 = b * RB + rt * 128
                ld(dr["YS"][r0:r0 + 128, :], ys[:], [r_ys], [rs["YS"]])
        c.close()

    def p6(l):
        c = Ctx(S)
        xr = c.ring(4, [128, D], F32, name="x1")
        y1r = c.ring(4, [128, D], F32, name="y1")
        y2r = c.ring(4, [128, D], F32, name="y2")
        st_ = {}

        def loads(t):
            xt, r_xt = xr.next()
            ld(xt[:], dr["Xs"][t * 128:(t + 1) * 128, :], [rs["Xs"]], [r_xt])
            y1, r_y1 = y1r.next()
            y2, r_y2 = y2r.next()
            for (yy, r_yy, sl, r_sl) in ((y1, r_y1, slot1, r_slot1), (y2, r_y2, slot2, r_slot2)):
                S.dma("pool", lambda e, yy=yy, sl=sl, t=t: e.indirect_dma_start(
                    out=yy[:], out_offset=None, in_=dr["YS"], in_offset=bass.IndirectOffsetOnAxis(ap=sl[:, t:t + 1], axis=0)),
                    [rs["YS"], r_sl], [r_yy])
            st_[t] = (xt, r_xt, y1, r_y1, y2, r_y2)
        PF = 3
        for t in range(min(PF, NT)):
            loads(t)
        for t in range(NT):
            xt, r_xt, y1, r_y1, y2, r_y2 = st_.pop(t)
            A(lambda e, y1=y1, t=t: e.activation(out=y1[:], in_=y1[:], func=AF.Copy, scale=gt1[:, t:t + 1]), [r_y1, r_gt1], [r_y1])
            V(lambda e, y1=y1, y2=y2, t=t: e.scalar_tensor_tensor(out=y2[:], in0=y2[:], scalar=gt2[:, t:t + 1], in1=y1[:], op0=ALU.mult, op1=ALU.add),
              [r_y1, r_y2, r_gt2], [r_y2])
            V(lambda e, y2=y2: e.tensor_tensor(out=y2[:], in0=y2[:], in1=G2b[:], op=ALU.mult), [r_y2, r_G2b], [r_y2])
            V(lambda e, y2=y2, xt=xt: e.tensor_tensor(out=xt[:], in0=y2[:], in1=xt[:], op=ALU.add), [r_y2, r_xt], [r_xt])
            ld(dr["Xs"][t * 128:(t + 1) * 128, :], xt[:], [r_xt], [rs["Xs"]])
            if t + PF < NT:
                loads(t + PF)
        c.close()

    def pfinal():
        c = Ctx(S)
        fgb, r_fgb = c.sb([128, D], F32, "fgb")
        ld(fgb[:], dr["final_g"].partition_broadcast(128), (), [r_fgb], "sp")
        xr = c.ring(3, [128, D], F32, name="xf")
        junk, r_junk = c.sb([128, D], BF16, "junk")
        ss, r_ss = c.sb([128, NT], F32, "ss")
        rstd, r_rstd = c.sb([128, NT], F32, "rstd")
        V(lambda e: e.memset(ss[:], 0.0), (), [r_ss])
        for t in range(NT):
            xt, r_xt = xr.next()
            ld(xt[:], dr["Xs"][t * 128:(t + 1) * 128, :], [rs["Xs"]], [r_xt])
            A(lambda e, xt=xt, t=t: e.activation(out=junk[:], in_=xt[:], func=AF.Square, accum_out=ss[:, t:t + 1]), [r_xt], [r_junk, r_ss])
            rstd_from_ss(ss[:, t:t + 1], rstd[:, t:t + 1], D, r_ss, r_rstd)
            V(lambda e, xt=xt, t=t: e.scalar_tensor_tensor(out=xt[:], in0=xt[:], scalar=rstd[:, t:t + 1], in1=fgb[:], op0=ALU.mult, op1=ALU.mult),
              [r_xt, r_rstd, r_fgb], [r_xt])
            ld(out[t * 128:(t + 1) * 128, :], xt[:], [r_xt], [rs["out"]])
        c.close()

    def finish():
        toks = []
        for r in rs.values():
            toks += r.ws
        S.wait_all("sp", toks)
        S.flush()
        G.close()
        S.es.close()

    setup()
    xin, r_xin = dr["x"], rs["x"]
    done = False
    for l in range(depth):
        for (nm, fn) in (("p0", lambda: p0(l)), ("p1", lambda: p1(l, xin, r_xin)), ("p2", lambda: p2(l)), ("p2b", lambda: p2b(l)),
                         ("p3", lambda: p3(l)), ("p4", lambda: p4(l, xin, r_xin)), ("p4b", lambda: p4b(l)), ("p5", lambda: p5(l)),
                         ("p6", lambda: p6(l))):
            fn()
            if stop == (nm, l):
                done = True
                break
        if done:
            break
        xin, r_xin = dr["Xs"], rs["Xs"]
    if not done and final:
        pfinal()
    finish()
    return nc


def make_inputs(inputs, b):
    f = lambda a: np.ascontiguousarray(np.asarray(a, dtype=np.float32))
    m = {
        "x": f(inputs["x"][b]), "c": f(inputs["c"][b:b + 1]), "w_ada": f(inputs["w_ada"]), "b_ada": f(inputs["b_ada"]),
        "norm1_g": f(inputs["norm1_g"]), "w_in": f(inputs["w_in"]), "attn_norm_g": f(inputs["attn_norm_g"]),
        "hgrn_lb_logits": f(inputs["hgrn_lb_logits"]), "hgrn_norm_g": f(inputs["hgrn_norm_g"]), "w_out": f(inputs["w_out"]),
        "norm2_g": f(inputs["norm2_g"]),
        "router_w": f(np.concatenate([np.asarray(inputs["router_group_w"]), np.asarray(inputs["router_expert_w"])], axis=-1)),
        "router_b": f(np.concatenate([np.asarray(inputs["router_group_b"]), np.asarray(inputs["router_expert_b"])], axis=-1)),
        "final_g": f(np.asarray(inputs["final_g"]).reshape(1, D)),
    }
    for nm, kc, n in (("moe_w1", 8, 512), ("moe_w3", 8, 512), ("moe_w2", 4, D)):
        w = np.asarray(inputs[nm], dtype=np.float32).reshape(DEPTH, NE, kc, 128, n).transpose(0, 1, 3, 2, 4).reshape(DEPTH * NE * 128, kc * n)
        m[nm + "_0"] = np.ascontiguousarray(w[:, :2048])
        m[nm + "_1"] = np.ascontiguousarray(w[:, 2048:])
    m.update(host_consts())
    return m


def kernel(**inputs):
    nc = build()
    in_maps = [make_inputs(inputs, b) for b in range(2)]
    res = run_bass_kernel_spmd(nc, in_maps, core_ids=[0, 1])
    return np.stack([np.asarray(res.results[b]["out"], dtype=np.float32) for b in range(2)], axis=0)
```

```python
import contextlib
import numpy as np
import ml_dtypes
import concourse.bass as bass
import concourse.mybir as mybir
from concourse.bass_utils import run_bass_kernel_spmd

F32 = mybir.dt.float32
BF16 = mybir.dt.bfloat16
I32 = mybir.dt.int32
ALU = mybir.AluOpType
AF = mybir.ActivationFunctionType
AX = mybir.AxisListType

T = 8192
D = 1024
NT = T // 128
DEPTH = 4
NE = 32
RB = 256
NBLK = (2 * T) // RB + NE
NROWS = NBLK * RB
EPS = 1e-6
BIG = 30000.0
PATTERNS = ((128, 1), (512, 4), (2048, 16))
EP_ENG = 30000
EP_DMA = 3000


class Res:
    __slots__ = ("w", "r")

    def __init__(self):
        self.w = None
        self.r = []


class MRes(Res):
    __slots__ = ("ws",)

    def __init__(self):
        super().__init__()
        self.ws = []


def _compress(lst):
    mx = {}
    for (pq, c) in lst:
        mx[pq] = max(mx.get(pq, 0), c)
    return list(mx.items())


class Q:
    def __init__(self, nc, es, name, inc, ep):
        self.nc, self.es, self.name, self.inc, self.ep = nc, es, name, inc, ep
        self.sems = []
        self.count = 0
        self.ops = []
        self.seen = {}

    def sem_for(self, c):
        i = (c - 1) // self.ep
        while len(self.sems) <= i:
            self.sems.append(self.es.enter_context(self.nc.semaphore(f"{self.name}_{len(self.sems)}")))
        return self.sems[i], (((c - 1) % self.ep) + 1) * self.inc


class Sched:
    def __init__(self, nc, ndma=6):
        self.nc = nc
        self.es = contextlib.ExitStack()
        self.q = {n: Q(nc, self.es, "s" + n, 1, EP_ENG) for n in ("pe", "act", "dve", "pool", "sp")}
        self.dslots = {n: [Q(nc, self.es, f"d{n}{i}", 16, EP_DMA) for i in range(ndma)] for n in ("sp", "pool", "act")}
        self.dnext = {n: 0 for n in self.dslots}

    def _deps(self, reads, writes):
        need = {}
        for r in reads:
            if isinstance(r, MRes):
                for (pq, c) in r.ws:
                    need[pq] = max(need.get(pq, 0), c)
            elif r.w is not None:
                need[r.w[0]] = max(need.get(r.w[0], 0), r.w[1])
        for w in writes:
            if (not isinstance(w, MRes)) and w.w is not None:
                need[w.w[0]] = max(need.get(w.w[0], 0), w.w[1])
            for (pq, c) in w.r:
                need[pq] = max(need.get(pq, 0), c)
        return need

    def _commit(self, tok, reads, writes):
        for r in reads:
            r.r.append(tok)
            if len(r.r) > 48:
                r.r = _compress(r.r)
        for w in writes:
            if isinstance(w, MRes):
                w.ws.append(tok)
                if len(w.ws) > 48:
                    w.ws = _compress(w.ws)
            else:
                w.w = tok
                w.r = []

    def _waits(self, q, need):
        waits = []
        for pq, c in need.items():
            if q.seen.get(pq, 0) >= c:
                continue
            q.seen[pq] = c
            waits.append((pq, c))
        return waits

    def op(self, qn, fn, reads=(), writes=()):
        q = self.q[qn]
        waits = self._waits(q, self._deps(reads, writes))
        q.count += 1
        tok = (q, q.count)
        q.ops.append((waits, fn, tok))
        self._commit(tok, reads, writes)
        return tok

    def dma(self, qn, fn, reads=(), writes=()):
        q = self.q[qn]
        sl = self.dslots[qn]
        dq = sl[self.dnext[qn] % len(sl)]
        self.dnext[qn] += 1
        need = self._deps(reads, writes)
        if dq.count > 0:
            need[dq] = max(need.get(dq, 0), dq.count)
        waits = self._waits(q, need)
        dq.count += 1
        tok = (dq, dq.count)
        q.ops.append((waits, fn, tok))
        self._commit(tok, reads, writes)
        return tok

    def wait_all(self, qn, toks):
        q = self.q[qn]
        need = {}
        for (pq, c) in toks:
            need[pq] = max(need.get(pq, 0), c)
        q.ops.append((self._waits(q, need), None, None))

    def flush(self):
        nc = self.nc
        if not any(q.ops for q in self.q.values()):
            return
        with nc.Block() as block:
            def mk(qn):
                q = self.q[qn]

                def body(e):
                    for waits, fn, tok in q.ops:
                        for (pq, c) in waits:
                            s, v = pq.sem_for(c)
                            e.wait_ge(s, v)
                        if fn is not None:
                            s, _ = tok[0].sem_for(tok[1])
                            fn(e).then_inc(s, tok[0].inc)
                    q.ops = []
                return body
            block.tensor(mk("pe"))
            block.scalar(mk("act"))
            block.vector(mk("dve"))
            block.gpsimd(mk("pool"))
            block.sync(mk("sp"))


class Ring:
    def __init__(self, items):
        self.items = items
        self.i = 0

    def next(self):
        it = self.items[self.i % len(self.items)]
        self.i += 1
        return it


_UID = [0]


class Ctx:
    def __init__(self, S):
        self.S = S
        self.es = contextlib.ExitStack()
        self.n = 0

    def sb(self, shape, dt, name=None):
        _UID[0] += 1
        t = self.es.enter_context(self.S.nc.sbuf_tensor(f"{name or 't'}_{_UID[0]}", list(shape), dt))
        return t, Res()

    def ps(self, shape, dt, name=None):
        _UID[0] += 1
        t = self.es.enter_context(self.S.nc.psum_tensor(f"{name or 'p'}_{_UID[0]}", list(shape), dt))
        return t, Res()

    def ring(self, n, shape, dt, psum=False, name=None):
        return Ring([(self.ps if psum else self.sb)(shape, dt, name) for _ in range(n)])

    def close(self):
        self.S.flush()
        self.es.close()


def host_consts():
    c = {}
    c["ident"] = np.eye(128, dtype=np.float32).astype(ml_dtypes.bfloat16)
    kk = np.arange(128)[:, None]
    qq = np.arange(128)[None, :]
    mm = np.zeros((128, 8, 3, 256), np.float32)
    for h in range(8):
        slope = 2.0 ** (-(h + 1))
        for p, (w, d) in enumerate(PATTERNS):
            steps = w // d
            dist_cur = qq - kk
            dist_nxt = 128 + qq - kk
            for j, dist in enumerate((dist_cur, dist_nxt)):
                valid = (dist >= 0) & (dist <= steps)
                mm[:, h, p, j * 128:(j + 1) * 128] = np.where(valid, -slope * d * dist, -BIG)
    mm2 = np.concatenate([mm[..., 128:256], mm[..., 0:128]], axis=-1)
    c["amask"] = np.exp(mm2.astype(np.float64)).astype(np.float32).reshape(128, 8 * 3 * 256).astype(ml_dtypes.bfloat16)
    c["causal"] = ((kk <= qq) & ((kk // 64) == (qq // 64))).astype(np.uint8)
    c["ustrict"] = (kk < qq).astype(np.float32).astype(ml_dtypes.bfloat16)
    rm = np.ones((128, 2048), np.float32)
    rm[:, ::128] = 0.0
    c["resetm"] = rm
    c["mulrow"] = np.tile((np.arange(64, dtype=np.float32) * RB)[None, :], (128, 1))
    c["brow"] = np.tile(np.arange(NBLK, dtype=np.float32)[None, :], (128, 1))
    c["piota"] = np.arange(128, dtype=np.float32).reshape(128, 1)
    return c


CONST_SPECS = {"ident": ([128, 128], BF16), "amask": ([128, 8 * 3 * 256], BF16), "causal": ([128, 128], mybir.dt.uint8),
               "ustrict": ([128, 128], BF16), "resetm": ([128, 2048], F32), "mulrow": ([128, 64], F32), "brow": ([128, NBLK], F32), "piota": ([128, 1], F32)}

IN_SPECS = {
    "x": ([T, D], F32), "c": ([1, D], F32), "w_ada": ([DEPTH, D, 6 * D], F32), "b_ada": ([DEPTH, 6 * D], F32),
    "norm1_g": ([DEPTH, D], F32), "w_in": ([DEPTH, D, 3584], F32), "attn_norm_g": ([DEPTH, 512], F32),
    "hgrn_lb_logits": ([DEPTH, 512], F32), "hgrn_norm_g": ([DEPTH, 512], F32), "w_out": ([DEPTH, D, D], F32),
    "norm2_g": ([DEPTH, D], F32), "router_w": ([DEPTH, D, 36], F32), "router_b": ([DEPTH, 36], F32),
    "moe_w1_0": ([DEPTH * NE * 128, 2048], F32), "moe_w1_1": ([DEPTH * NE * 128, 2048], F32),
    "moe_w3_0": ([DEPTH * NE * 128, 2048], F32), "moe_w3_1": ([DEPTH * NE * 128, 2048], F32),
    "moe_w2_0": ([DEPTH * NE * 128, 2048], F32), "moe_w2_1": ([DEPTH * NE * 128, 2048], F32),
    "final_g": ([1, D], F32),
}


def build(depth=DEPTH, stop=None, dumps=(), final=True):
    nc = bass.Bass("TRN2", target_bir_lowering=False)
    S = Sched(nc)
    dr = {}
    for k, (shp, dt) in list(IN_SPECS.items()) + list(CONST_SPECS.items()):
        dr[k] = nc.dram_tensor(k, shp, dt, kind="ExternalInput").ap()
    out = nc.dram_tensor("out", [T, D], F32, kind="ExternalOutput").ap()
    rs = {}

    def scratch(name, shp, dt):
        kind = "ExternalOutput" if name in dumps else "Internal"
        dr[name] = nc.dram_tensor(name, shp, dt, kind=kind).ap()
        rs[name] = MRes()
    scratch("Xs", [T, D], F32)
    scratch("QT", [512, T], BF16)
    scratch("KT", [512, T], BF16)
    scratch("VV", [T, 512], BF16)
    scratch("RQT", [512, T], BF16)
    scratch("ZT", [512, T], F32)
    scratch("RI", [T, 512], BF16)
    scratch("RG", [T, 512], BF16)
    for p in range(3):
        scratch(f"OP{p}", [T, 2, 4 * 65], F32)
    scratch("CAT", [T, D], BF16)
    scratch("H2", [T, D], BF16)
    scratch("XS", [NROWS, D], BF16)
    scratch("YS", [NROWS, D], BF16)
    rs["x"] = MRes()
    rs["out"] = MRes()

    G = Ctx(S)
    ident, r_ident = G.sb([128, 128], BF16, "ident")
    causal, r_causal = G.sb([128, 128], mybir.dt.uint8, "causal")
    ustrict, r_ustrict = G.sb([128, 128], BF16, "ustrict")
    onesb, r_onesb = G.sb([128, 128], BF16, "onesb")
    mulrow, r_mulrow = G.sb([128, 64], F32, "mulrow")
    brow, r_brow = G.sb([128, NBLK], F32, "brow")
    piota, r_piota = G.sb([128, 1], F32, "piota")
    widx, r_widx = G.sb([128, NBLK], I32, "widx")
    one11, r_one11 = G.sb([1, 1], F32, "one11")
    onesrow, r_onesrow = G.sb([1, 128], F32, "onesrow")
    colf, r_colf = G.sb([128, 16], F32, "colf")
    G1b, r_G1b = G.sb([128, D], F32, "G1b")
    A2b, r_A2b = G.sb([128, D], F32, "A2b")
    S2b, r_S2b = G.sb([128, D], F32, "S2b")
    G2b, r_G2b = G.sb([128, D], F32, "G2b")
    lbc, r_lbc = G.sb([128, DEPTH, 4], F32, "lbc")
    oml, r_oml = G.sb([128, DEPTH, 4], F32, "oml")
    noml, r_noml = G.sb([128, DEPTH, 4], F32, "noml")
    slot1, r_slot1 = G.sb([128, NT], I32, "slot1")
    slot2, r_slot2 = G.sb([128, NT], I32, "slot2")
    gt1, r_gt1 = G.sb([128, NT], F32, "gt1")
    gt2, r_gt2 = G.sb([128, NT], F32, "gt2")
    lgall, r_lgall = G.sb([128, NT, 36], F32, "lgall")

    dma_rr = [0]

    def ld(out_, in_, reads=(), writes=(), q=None):
        if q is None:
            q = "sp"
        return S.dma(q, lambda e: e.dma_start(out=out_, in_=in_), reads, writes)

    def V(fn, r=(), w=()):
        return S.op("dve", fn, r, w)

    def A(fn, r=(), w=()):
        return S.op("act", fn, r, w)

    def PE(fn, r=(), w=()):
        return S.op("pe", fn, r, w)

    def GP(fn, r=(), w=()):
        return S.op("pool", fn, r, w)

    def setup():
        ld(ident[:], dr["ident"], (), [r_ident], "sp")
        ld(causal[:], dr["causal"], (), [r_causal], "sp")
        ld(ustrict[:], dr["ustrict"], (), [r_ustrict], "sp")
        ld(mulrow[:], dr["mulrow"], (), [r_mulrow], "sp")
        ld(brow[:], dr["brow"], (), [r_brow], "sp")
        ld(piota[:], dr["piota"], (), [r_piota], "sp")
        V(lambda e: e.memset(one11[:], 1.0), (), [r_one11])
        V(lambda e: e.memset(onesrow[:], 1.0), (), [r_onesrow])
        V(lambda e: e.memset(onesb[:], 1.0), (), [r_onesb])
        c = Ctx(S)
        lg, r_lg = c.sb([128, DEPTH, 4], F32)
        ex, r_ex = c.sb([128, DEPTH, 4], F32)
        sm, r_sm = c.sb([128, 4], F32)
        S.dma("sp", lambda e: e.dma_start(out=lg[:], in_=dr["hgrn_lb_logits"].rearrange("l (h k) -> k l h", k=128),
                                          allow_slow_non_contiguous=True), (), [r_lg])
        A(lambda e: e.activation(out=ex[:], in_=lg[:], func=AF.Exp), [r_lg], [r_ex])
        V(lambda e: e.tensor_tensor(out=sm[:], in0=ex[:, 0, :], in1=ex[:, 1, :], op=ALU.add), [r_ex], [r_sm])
        V(lambda e: e.tensor_tensor(out=sm[:], in0=sm[:], in1=ex[:, 2, :], op=ALU.add), [r_ex, r_sm], [r_sm])
        V(lambda e: e.tensor_tensor(out=sm[:], in0=sm[:], in1=ex[:, 3, :], op=ALU.add), [r_ex, r_sm], [r_sm])
        V(lambda e: e.reciprocal(out=sm[:], in_=sm[:]), [r_sm], [r_sm])
        V(lambda e: e.memset(lbc[:, 0, :], 0.0), (), [r_lbc])
        V(lambda e: e.tensor_tensor(out=lbc[:, 1, :], in0=ex[:, 1, :], in1=sm[:], op=ALU.mult), [r_ex, r_sm], [r_lbc])
        for l in (2, 3):
            V(lambda e, l=l: e.tensor_tensor(out=ex[:, l, :], in0=ex[:, l, :], in1=sm[:], op=ALU.mult), [r_ex, r_sm], [r_ex])
            V(lambda e, l=l: e.tensor_tensor(out=lbc[:, l, :], in0=lbc[:, l - 1, :], in1=ex[:, l, :], op=ALU.add), [r_ex, r_lbc], [r_lbc])
        V(lambda e: e.tensor_scalar(out=oml[:], in0=lbc[:], scalar1=-1.0, scalar2=1.0, op0=ALU.mult, op1=ALU.add), [r_lbc], [r_oml])
        V(lambda e: e.tensor_scalar(out=noml[:], in0=lbc[:], scalar1=1.0, scalar2=-1.0, op0=ALU.mult, op1=ALU.add), [r_lbc], [r_noml])
        c.close()

    def p0(l):
        c = Ctx(S)
        crow, r_crow = c.sb([1, D], F32)
        cactc, r_cactc = c.sb([128, 8], F32)
        modrow, r_mod = c.sb([1, 6 * D], F32)
        brow, r_brow = c.sb([1, 6 * D], F32)
        g1row, r_g1 = c.sb([1, D], F32)
        g2row, r_g2 = c.sb([1, D], F32)
        wring = c.ring(2, [128, 8, 512], F32)
        pcol, r_pcol = c.ps([128, 16], F32)
        pacc = c.ring(2, [128, 512], F32, psum=True)
        ld(crow[:], dr["c"], (), [r_crow], "sp")
        ld(brow[:], dr["b_ada"][l:l + 1, :], (), [r_brow], "sp")
        ld(g1row[:], dr["norm1_g"][l:l + 1, :], (), [r_g1], "sp")
        ld(g2row[:], dr["norm2_g"][l:l + 1, :], (), [r_g2], "sp")
        A(lambda e: e.activation(out=crow[:], in_=crow[:], func=AF.Silu), [r_crow], [r_crow])
        for kc in range(8):
            PE(lambda e, kc=kc: e.matmul(pcol[:, kc:kc + 1], lhsT=crow[0:1, kc * 128:(kc + 1) * 128], rhs=one11[0:1, 0:1],
                                         start=True, stop=True), [r_crow, r_one11], [r_pcol])
        V(lambda e: e.tensor_copy(out=cactc[:], in_=pcol[:, 0:8]), [r_pcol], [r_cactc])
        for n in range(12):
            wt, r_wt = wring.next()
            ld(wt[:], dr["w_ada"][l, :, n * 512:(n + 1) * 512].rearrange("(kc p) n -> p kc n", p=128), (), [r_wt])
            acc, r_acc = pacc.next()
            for kc in range(8):
                PE(lambda e, kc=kc, acc=acc, wt=wt: e.matmul(acc[0:1, :], lhsT=cactc[:, kc:kc + 1], rhs=wt[:, kc, :],
                                                            start=(kc == 0), stop=(kc == 7)), [r_cactc, r_wt], [r_acc])
            V(lambda e, n=n, acc=acc: e.tensor_tensor(out=modrow[0:1, n * 512:(n + 1) * 512], in0=acc[0:1, :],
                                                       in1=brow[0:1, n * 512:(n + 1) * 512], op=ALU.add), [r_acc, r_brow], [r_mod])
        V(lambda e: e.scalar_tensor_tensor(out=g1row[:], in0=modrow[0:1, D:2 * D], scalar=1.0, in1=g1row[:], op0=ALU.add, op1=ALU.mult),
          [r_mod, r_g1], [r_g1])
        V(lambda e: e.scalar_tensor_tensor(out=g2row[:], in0=modrow[0:1, 4 * D:5 * D], scalar=1.0, in1=g2row[:], op0=ALU.add, op1=ALU.mult),
          [r_mod, r_g2], [r_g2])
        for kc in range(8):
            PE(lambda e, kc=kc: e.matmul(pcol[:, kc:kc + 1], lhsT=g1row[0:1, kc * 128:(kc + 1) * 128], rhs=one11[0:1, 0:1],
                                         start=True, stop=True), [r_g1, r_one11], [r_pcol])
            PE(lambda e, kc=kc: e.matmul(pcol[:, 8 + kc:9 + kc], lhsT=modrow[0:1, kc * 128:(kc + 1) * 128], rhs=one11[0:1, 0:1],
                                         start=True, stop=True), [r_mod, r_one11], [r_pcol])
        V(lambda e: e.tensor_copy(out=colf[:], in_=pcol[:]), [r_pcol], [r_colf])
        for (src, r_src, off, dst, r_dst) in ((modrow, r_mod, 2 * D, G1b, r_G1b), (g2row, r_g2, 0, A2b, r_A2b),
                                              (modrow, r_mod, 3 * D, S2b, r_S2b), (modrow, r_mod, 5 * D, G2b, r_G2b)):
            for nch in range(2):
                acc, r_acc = pacc.next()
                PE(lambda e, acc=acc, src=src, off=off, nch=nch: e.matmul(acc[:, :], lhsT=onesrow[0:1, :],
                                                                         rhs=src[0:1, off + nch * 512:off + (nch + 1) * 512],
                                                                         start=True, stop=True), [r_src, r_onesrow], [r_acc])
                V(lambda e, acc=acc, dst=dst, nch=nch: e.tensor_copy(out=dst[:, nch * 512:(nch + 1) * 512], in_=acc[:, :]), [r_acc], [r_dst])
        c.close()

    def rstd_from_ss(ss_ap, out_ap, n, r_ss, r_out):
        V(lambda e: e.tensor_scalar(out=out_ap, in0=ss_ap, scalar1=1.0 / n, scalar2=EPS, op0=ALU.mult, op1=ALU.add), [r_ss], [r_out])
        A(lambda e: e.activation(out=out_ap, in_=out_ap, func=AF.Sqrt), [r_out], [r_out])
        V(lambda e: e.reciprocal(out=out_ap, in_=out_ap), [r_out], [r_out])

    def p1(l, xin, r_xin):
        c = Ctx(S)
        winb, r_winb = c.sb([128, 8, 3584], BF16, "winb")
        wst = c.ring(3, [128, 1792], F32, name="wst")
        kk_ = 0
        for kc in range(8):
            for hf in range(2):
                st, r_st = wst.next()
                ld(st[:], dr["w_in"][l, kc * 128:(kc + 1) * 128, hf * 1792:(hf + 1) * 1792], (), [r_st])
                kk_ += 1
                if kk_ % 2 == 0:
                    V(lambda e, st=st, kc=kc, hf=hf: e.tensor_copy(out=winb[:, kc, hf * 1792:(hf + 1) * 1792], in_=st[:]), [r_st], [r_winb])
                else:
                    GP(lambda e, st=st, kc=kc, hf=hf: e.tensor_copy(out=winb[:, kc, hf * 1792:(hf + 1) * 1792], in_=st[:]), [r_st], [r_winb])
        xring = c.ring(6, [128, D], F32, name="xt")
        junk, r_junk = c.sb([128, D], BF16, "junk")
        xnring = c.ring(6, [128, D], BF16, name="xn")
        ss, r_ss = c.sb([128, NT], F32, "ss")
        rstd, r_rstd = c.sb([128, NT], F32, "rstd")
        hTring = c.ring(2, [128, 8, 512], BF16, name="hT")
        ptr = c.ring(2, [128, 8, 128], BF16, psum=True, name="ptr")
        pacc = c.ring(6, [128, 512], F32, psum=True, name="pacc")
        stb = c.ring(6, [128, 512], BF16, name="stb")
        stf = c.ring(4, [128, 512], F32, name="stf")
        V(lambda e: e.memset(ss[:], 0.0), (), [r_ss])
        fm = []
        for m in range(4):
            fm.append((m * 128, "QT", m * 128, "q"))
        for m in range(4):
            fm.append((512 + m * 128, "KT", m * 128, "b"))
        for m in range(4):
            fm.append((1536 + m * 128, "RQT", m * 128, "b"))
        for m in range(4):
            fm.append((2048 + m * 128, "ZT", m * 128, "f"))
        tm = [(1024, "VV", "b"), (2560, "RI", "b"), (3072, "RG", "s")]
        ev = [0]

        def prep(g):
            hT, r_hT = hTring.next()
            tiles = []
            for j in range(4):
                t = g * 4 + j
                xt, r_xt = xring.next()
                ld(xt[:], xin[t * 128:(t + 1) * 128, :], [r_xin], [r_xt])
                A(lambda e, xt=xt, t=t: e.activation(out=junk[:], in_=xt[:], func=AF.Square, accum_out=ss[:, t:t + 1]), [r_xt], [r_junk, r_ss])
                rstd_from_ss(ss[:, t:t + 1], rstd[:, t:t + 1], D, r_ss, r_rstd)
                xn, r_xn = xnring.next()
                A(lambda e, xt=xt, xn=xn, t=t: e.activation(out=xn[:], in_=xt[:], func=AF.Copy, scale=rstd[:, t:t + 1]), [r_xt, r_rstd], [r_xn])
                tiles.append((xn, r_xn))
            for jp in range(2):
                pair = [(tiles[2 * jp + i], ptr.next()) for i in range(2)]
                for kc in range(8):
                    for ((xn, r_xn), (pt, r_pt)) in pair:
                        PE(lambda e, kc=kc, pt=pt, xn=xn: e.transpose(out=pt[:, kc, :], in_=xn[:, kc * 128:(kc + 1) * 128], identity=ident[:]),
                           [r_xn, r_ident], [r_pt])
                for i, ((xn, r_xn), (pt, r_pt)) in enumerate(pair):
                    j = 2 * jp + i
                    V(lambda e, pt=pt, hT=hT, j=j: e.tensor_tensor(out=hT[:, :, j * 128:(j + 1) * 128], in0=pt[:, :, :],
                                                                  in1=colf[:, 0:8].unsqueeze(2).to_broadcast([128, 8, 128]), op=ALU.mult),
                      [r_pt, r_colf], [r_hT])
                    GP(lambda e, hT=hT, j=j: e.tensor_tensor(out=hT[:, :, j * 128:(j + 1) * 128], in0=hT[:, :, j * 128:(j + 1) * 128],
                                                            in1=colf[:, 8:16].unsqueeze(2).to_broadcast([128, 8, 128]), op=ALU.add),
                       [r_hT, r_colf], [r_hT])
            return hT, r_hT

        def evac(kind, acc, r_acc):
            st, r_st = (stf if kind == "f" else stb).next()
            ev[0] += 1
            if kind == "q":
                A(lambda e: e.activation(out=st[:], in_=acc[:], func=AF.Copy, scale=0.125), [r_acc], [r_st])
            elif kind == "s":
                A(lambda e: e.activation(out=st[:], in_=acc[:], func=AF.Silu), [r_acc], [r_st])
            elif ev[0] % 2 == 0:
                A(lambda e: e.activation(out=st[:], in_=acc[:], func=AF.Copy), [r_acc], [r_st])
            else:
                V(lambda e: e.tensor_copy(out=st[:], in_=acc[:]), [r_acc], [r_st])
            return st, r_st

        nxt = prep(0)
        for g in range(NT // 4):
            hT, r_hT = nxt
            if g + 1 < NT // 4:
                nxt = prep(g + 1)
            for f0 in range(0, 16, 4):
                grp = [(fm[f0 + i], pacc.next()) for i in range(4)]
                for kc in range(8):
                    for ((c0, dn, row0, kind), (acc, r_acc)) in grp:
                        PE(lambda e, kc=kc, acc=acc, hT=hT, c0=c0: e.matmul(acc[:, :], lhsT=winb[:, kc, c0:c0 + 128], rhs=hT[:, kc, :],
                                                                           start=(kc == 0), stop=(kc == 7)), [r_winb, r_hT], [r_acc])
                for ((c0, dn, row0, kind), (acc, r_acc)) in grp:
                    st, r_st = evac(kind, acc, r_acc)
                    ld(dr[dn][row0:row0 + 128, g * 512:(g + 1) * 512], st[:], [r_st], [rs[dn]])
            for j in range(4):
                t = g * 4 + j
                grp = [(tm[i], pacc.next()) for i in range(3)]
                for kc in range(8):
                    for ((c0, dn, kind), (acc, r_acc)) in grp:
                        PE(lambda e, kc=kc, acc=acc, hT=hT, c0=c0, j=j: e.matmul(acc[:, :], lhsT=hT[:, kc, j * 128:(j + 1) * 128],
                                                                                 rhs=winb[:, kc, c0:c0 + 512], start=(kc == 0), stop=(kc == 7)),
                           [r_winb, r_hT], [r_acc])
                for ((c0, dn, kind), (acc, r_acc)) in grp:
                    st, r_st = evac(kind, acc, r_acc)
                    ld(dr[dn][t * 128:(t + 1) * 128, :], st[:], [r_st], [rs[dn]])
        c.close()

    def p2(l):
        c = Ctx(S)
        qT2, r_qT2 = c.sb([128, 2, T], BF16, "qT2")
        kT2, r_kT2 = c.sb([128, 2, T], BF16, "kT2")
        amask, r_amask = c.sb([128, 4, 3, 256], BF16, "amask")
        vraw = c.ring(2, [128, 8, 256], BF16, name="vraw")
        vaugr = c.ring(2, [128, 64, 4, 65], BF16, name="vaug")
        pTr = c.ring(8, [128, 256], BF16, name="pT")
        per_ = c.ring(4, [128, 256], BF16, name="pe")
        ostr = c.ring(3, [128, 4, 65], F32, name="ost")
        Sps = c.ring(4, [128, 256], F32, psum=True, name="Sps")
        OpsE = c.ring(2, [128, 2, 65], F32, psum=True, name="OpsE")
        OpsO = c.ring(2, [128, 2, 65], F32, psum=True, name="OpsO")
        ostr = c.ring(4, [128, 4, 65], F32, name="ost2")
        for (vg_, r_vg_) in vaugr.items:
            GP(lambda e, vg_=vg_: e.memset(vg_[:, :, :, 64:65], 1.0), (), [r_vg_])
        for half in range(2):
            ld(amask[:], dr["amask"].rearrange("k (h p q) -> k h p q", h=8, p=3)[:, 4 * half:4 * half + 4], (), [r_amask], "sp")
            for hpi in range(2):
                hp = 2 * half + hpi
                ld(qT2[:, hpi, :], dr["QT"][hp * 128:(hp + 1) * 128, :], [rs["QT"]], [r_qT2])
                ld(kT2[:, hpi, :], dr["KT"][hp * 128:(hp + 1) * 128, :], [rs["KT"]], [r_kT2])
            for p, (w, d) in enumerate(PATTERNS):
                span = 128 * d
                nb = T // span
                vaug, r_vaug = vaugr.next()
                vsrc = dr["VV"].rearrange("(a u r) c -> r u a c", u=128, r=d)
                for r in range(d):
                    for a0 in range(0, nb, 8):
                        na = min(8, nb - a0)
                        vr, r_vr = vraw.next()
                        ld(vr[:, 0:na, :], vsrc[r, :, a0:a0 + na, half * 256:(half + 1) * 256], [rs["VV"]], [r_vr])
                        bi0 = r * nb + a0
                        GP(lambda e, vr=vr, na=na, bi0=bi0, vaug=vaug: e.tensor_copy(out=vaug[:, bi0:bi0 + na, :, 0:64],
                                                                         in_=vr[:, 0:na, :].rearrange("k a (h c) -> k a h c", h=4)),
                           [r_vr], [r_vaug])
                odst = dr[f"OP{p}"].rearrange("(a u r) h c -> r a u h c", u=128, r=d)
                units = [(r, a) for r in range(d) for a in range(nb)]

                def s_stage(r, a, hh, d=d, p=p, span=span, half=half):
                    hpi, pb = hh // 2, 64 * (hh % 2)
                    t0 = span * a + r
                    sp_, r_sp = Sps.next()
                    q_ap = qT2[pb:pb + 64, hpi, t0:t0 + 127 * d + 1:d]
                    c0 = 0 if a > 0 else 128
                    th = []
                    if a > 0:
                        tp = t0 - span
                        th.append(lambda: PE(lambda e: e.matmul(sp_[:, 0:128], lhsT=kT2[pb:pb + 64, hpi, tp:tp + 127 * d + 1:d], rhs=q_ap,
                                                                start=True, stop=True), [r_kT2, r_qT2], [r_sp]))
                    th.append(lambda: PE(lambda e: e.matmul(sp_[:, 128:256], lhsT=kT2[pb:pb + 64, hpi, t0:t0 + 127 * d + 1:d], rhs=q_ap,
                                                            start=True, stop=True), [r_kT2, r_qT2], [r_sp]))
                    pe, r_pe = per_.next()
                    pT, r_pT = pTr.next()

                    def post():
                        A(lambda e: e.activation(out=pe[:, c0:256], in_=sp_[:, c0:256], func=AF.Exp), [r_sp], [r_pe])
                        V(lambda e: e.tensor_tensor(out=pT[:, c0:256], in0=pe[:, c0:256], in1=amask[:, hh, p, c0:256], op=ALU.mult),
                          [r_pe, r_amask], [r_pT])
                    return th, post, pT, r_pT

                def pv_thunks(r, a, hh, pT, r_pT, O, r_O, nb=nb, vaug=vaug, r_vaug=r_vaug):
                    bi = r * nb + a
                    hs = hh // 2
                    th = []
                    if a > 0:
                        th.append(lambda: PE(lambda e: e.matmul(O[:, hs, :], lhsT=pT[:, 0:128], rhs=vaug[:, bi - 1, hh, :], start=True, stop=False),
                                             [r_pT, r_vaug], [r_O]))
                    th.append(lambda: PE(lambda e: e.matmul(O[:, hs, :], lhsT=pT[:, 128:256], rhs=vaug[:, bi, hh, :], start=(a == 0), stop=True),
                                         [r_pT, r_vaug], [r_O]))
                    return th

                def interleave(lists):
                    out_ = []
                    n = max(len(x) for x in lists) if lists else 0
                    for i in range(n):
                        for x in lists:
                            if i < len(x):
                                out_.append(x[i])
                    return out_

                def finish_block(r, a, OE, r_OE, OO, r_OO):
                    ost, r_ost = ostr.next()
                    V(lambda e: e.tensor_copy(out=ost[:, 0:4:2, :], in_=OE[:]), [r_OE], [r_ost])
                    V(lambda e: e.tensor_copy(out=ost[:, 1:4:2, :], in_=OO[:]), [r_OO], [r_ost])
                    ld(odst[r, a, :, half, :], ost[:].rearrange("k h c -> k (h c)"), [r_ost], [rs[f"OP{p}"]])

                flat = [(r, a, hh) for (r, a) in units for hh in range(4)]
                pairs = [flat[i:i + 2] for i in range(0, len(flat), 2)]
                Ocur = {}
                pvq = []

                def merge2(a_, b_):
                    out_ = []
                    ia = ib = 0
                    while ia < len(a_) or ib < len(b_):
                        out_ += a_[ia:ia + 2]
                        ia += 2
                        out_ += b_[ib:ib + 2]
                        ib += 2
                    return out_

                for pr in pairs:
                    st = [s_stage(*u) for u in pr]
                    s_th = interleave([x[0] for x in st])
                    if len(pvq) >= 2:
                        pv_now, fin_now = pvq.pop(0)
                    else:
                        pv_now, fin_now = [], []
                    for f_ in merge2(s_th, pv_now):
                        f_()
                    for fb in fin_now:
                        finish_block(*fb)
                    for x in st:
                        x[1]()
                    fin = []
                    pvl = []
                    for (r, a, hh), x in zip(pr, st):
                        if hh == 0:
                            Ocur[(r, a)] = (OpsE.next(), OpsO.next())
                        (OE, r_OE), (OO, r_OO) = Ocur[(r, a)]
                        O, r_O = (OE, r_OE) if hh % 2 == 0 else (OO, r_OO)
                        pvl.append(pv_thunks(r, a, hh, x[2], x[3], O, r_O))
                        if hh == 3:
                            fin.append((r, a, OE, r_OE, OO, r_OO))
                            del Ocur[(r, a)]
                    pvq.append((interleave(pvl), fin))
                for (pv_now, fin_now) in pvq:
                    for f_ in pv_now:
                        f_()
                    for fb in fin_now:
                        finish_block(*fb)
        c.close()

    def p2b(l):
        c = Ctx(S)
        angb, r_angb = c.sb([128, 512], F32, "angb")
        ld(angb[:], dr["attn_norm_g"][l:l + 1, :].partition_broadcast(128), (), [r_angb], "sp")
        opr = [c.ring(5, [128, 8, 65], F32, name=f"op{p}") for p in range(3)]
        den, r_den = c.sb([128, 8], F32, "den")
        o, r_o = c.sb([128, 8, 64], F32, "o")
        junk, r_junk = c.sb([128, 512], BF16, "junk")
        ss, r_ss = c.sb([128, NT], F32, "ss")
        rstd, r_rstd = c.sb([128, NT], F32, "rstd")
        cst = c.ring(3, [128, 512], BF16, name="cst")
        V(lambda e: e.memset(ss[:], 0.0), (), [r_ss])
        pend_ = {}

        def loads(t):
            tl = []
            for p in range(3):
                tt, r_tt = opr[p].next()
                ld(tt[:].rearrange("k h c -> k (h c)"), dr[f"OP{p}"][t * 128:(t + 1) * 128].rearrange("k a c -> k (a c)"), [rs[f"OP{p}"]], [r_tt])
                tl.append((tt, r_tt))
            pend_[t] = tl
        for t in range(min(3, NT)):
            loads(t)
        for t in range(NT):
            tl = pend_.pop(t)
            if t + 3 < NT:
                loads(t + 3)
            (t0_, r0), (t1_, r1), (t2_, r2) = tl
            V(lambda e, a=t0_, b=t1_: e.tensor_tensor(out=a[:], in0=a[:], in1=b[:], op=ALU.add), [r0, r1], [r0])
            V(lambda e, a=t0_, b=t2_: e.tensor_tensor(out=a[:], in0=a[:], in1=b[:], op=ALU.add), [r0, r2], [r0])
            V(lambda e, a=t0_: e.reciprocal(out=den[:], in_=a[:, :, 64]), [r0], [r_den])
            V(lambda e, a=t0_: e.tensor_tensor(out=o[:], in0=a[:, :, 0:64], in1=den[:].unsqueeze(2).to_broadcast([128, 8, 64]), op=ALU.mult),
              [r0, r_den], [r_o])
            A(lambda e, t=t: e.activation(out=junk[:], in_=o[:].rearrange("k h c -> k (h c)"), func=AF.Square, accum_out=ss[:, t:t + 1]),
              [r_o], [r_junk, r_ss])
            rstd_from_ss(ss[:, t:t + 1], rstd[:, t:t + 1], 512, r_ss, r_rstd)
            cs, r_cs = cst.next()
            V(lambda e, cs=cs, t=t: e.scalar_tensor_tensor(out=cs[:], in0=o[:].rearrange("k h c -> k (h c)"), scalar=rstd[:, t:t + 1],
                                                          in1=angb[:], op0=ALU.mult, op1=ALU.mult), [r_o, r_rstd, r_angb], [r_cs])
            ld(dr["CAT"][t * 128:(t + 1) * 128, 0:512], cs[:], [r_cs], [rs["CAT"]])
        c.close()

    def p3(l):
        c = Ctx(S)
        SEG = 512
        NCK = SEG // 128
        NSEG = T // SEG
        resetm, r_resetm = c.sb([128, SEG], F32, "resetm")
        gnb, r_gnb = c.sb([128, 512], F32, "gnb")
        ld(resetm[:], dr["resetm"][:, 0:SEG], (), [r_resetm], "sp")
        ld(gnb[:], dr["hgrn_norm_g"][l:l + 1, :].partition_broadcast(128), (), [r_gnb], "sp")
        rir = c.ring(2, [128, NCK, 512], BF16, name="ri")
        rgr = c.ring(2, [128, NCK, 512], BF16, name="rgm")
        catr_ = c.ring(2, [128, NCK, 512], BF16, name="catseg")
        zr = c.ring(2, [128, SEG], F32, name="z")
        qr = c.ring(2, [128, SEG], BF16, name="q")
        tset = [[c.sb([128, SEG], F32, f"b{n}{i}") for n in "ABCE"] for i in range(2)]
        o4 = c.ring(8, [128, 5, SEG], BF16, name="o4")
        dcyr = c.ring(8, [128, NCK], F32, name="dcy")
        St = [c.sb([128, 128], F32, f"S{h}") for h in range(4)]
        Sb = [c.sb([128, 128], BF16, f"Sb{h}") for h in range(4)]
        Amr = c.ring(4, [128, 128], BF16, name="Am")
        khTr = c.ring(4, [128, 128], BF16, name="khT")
        junk, r_junk = c.sb([128, 128], BF16, "junk")
        ssr, r_ssr = c.sb([128, 4 * NT], F32, "ssr")
        rsr, r_rsr = c.sb([128, 4 * NT], F32, "rsr")
        psA = c.ring(2, [128, 128], F32, psum=True, name="psA")
        psT = c.ring(1, [128, 128], BF16, psum=True, name="psT")
        psU = c.ring(2, [128, 128], F32, psum=True, name="psU")
        psO = c.ring(2, [128, 128], F32, psum=True, name="psO")
        psX = c.ring(1, [64, 64], F32, psum=True, name="psX")
        V(lambda e: e.memset(ssr[:], 0.0), (), [r_ssr])
        for h in range(4):
            V(lambda e, h=h: e.memset(St[h][0][:], 0.0), (), [St[h][1]])
            V(lambda e, h=h: e.memset(Sb[h][0][:], 0.0), (), [Sb[h][1]])

        def v3(t):
            t = t if isinstance(t, bass.AP) else t[:]
            return t.rearrange("k (c u) -> k c u", u=128)

        def v64(t):
            return t[:].rearrange("k (c u) -> k c u", u=64)

        def ew(sg):
            tk0 = sg * SEG
            ri, r_ri = rir.next()
            rgm, r_rgm = rgr.next()
            ld(ri[:], dr["RI"][tk0:tk0 + SEG, :].rearrange("(c p) f -> p c f", p=128), [rs["RI"]], [r_ri])
            ld(rgm[:], dr["RG"][tk0:tk0 + SEG, :].rearrange("(c p) f -> p c f", p=128), [rs["RG"]], [r_rgm])
            GP(lambda e: e.tensor_tensor(out=rgm[:], in0=rgm[:], in1=gnb[:].unsqueeze(1).to_broadcast([128, NCK, 512]), op=ALU.mult),
               [r_rgm, r_gnb], [r_rgm])
            heads = []

            def head_ops(hd):
                (bA, r_A), (bB, r_B), (bC, r_C), (bE, r_E) = tset[hd % 2]
                ops = []
                AA = lambda *a: ops.append(lambda: A(*a))
                VV = lambda *a: ops.append(lambda: V(*a))
                GG = lambda *a: ops.append(lambda: GP(*a))
                z, r_z = zr.next()
                q, r_q = qr.next()
                ld(z[:], dr["ZT"][hd * 128:(hd + 1) * 128, tk0:tk0 + SEG], [rs["ZT"]], [r_z])
                ld(q[:], dr["RQT"][hd * 128:(hd + 1) * 128, tk0:tk0 + SEG], [rs["RQT"]], [r_q])
                o, r_o4 = o4.next()
                dcy, r_dcy = dcyr.next()
                lb_ap, oml_ap = lbc[:, l, hd:hd + 1], oml[:, l, hd:hd + 1]

                def b3(t):
                    return t[:].rearrange("k (c u) -> k c u", u=128)

                def b64(t):
                    return t[:].rearrange("k (c u) -> k c u", u=64)
                VV(lambda e: e.tensor_scalar(out=z[:], in0=z[:], scalar1=-60.0, scalar2=None, op0=ALU.max), [r_z], [r_z])
                AA(lambda e: e.activation(out=bA[:], in_=z[:], func=AF.Exp, scale=-1.0), [r_z], [r_A])
                AA(lambda e: e.activation(out=bB[:], in_=bA[:], func=AF.Ln, bias=1.0), [r_A], [r_B])
                AA(lambda e: e.activation(out=bE[:], in_=bA[:], func=AF.Ln, bias=1.0, scale=lb_ap), [r_A, r_lbc], [r_E])
                VV(lambda e: e.tensor_tensor(out=bE[:], in0=bE[:], in1=bB[:], op=ALU.subtract), [r_E, r_B], [r_E])
                AA(lambda e: e.activation(out=bB[:], in_=bB[:], func=AF.Exp, scale=-1.0), [r_B], [r_B])
                VV(lambda e: e.scalar_tensor_tensor(out=bC[:], in0=bA[:], scalar=oml_ap, in1=bB[:], op0=ALU.mult, op1=ALU.mult),
                   [r_A, r_B, r_oml], [r_C])
                VV(lambda e: e.tensor_tensor_scan(out=bB[:], data0=resetm[:], data1=bE[:], initial=0.0, op0=ALU.mult, op1=ALU.add),
                   [r_resetm, r_E], [r_B])
                VV(lambda e: e.tensor_tensor(out=b64(bE), in0=b64(bB), in1=b64(bB)[:, :, 31:32].to_broadcast([128, 2 * NCK, 64]), op=ALU.subtract),
                   [r_B], [r_E])
                VV(lambda e: e.tensor_scalar(out=bE[:], in0=bE[:], scalar1=-80.0, scalar2=80.0, op0=ALU.max, op1=ALU.min), [r_E], [r_E])
                AA(lambda e: e.activation(out=bA[:], in_=bE[:], func=AF.Exp), [r_E], [r_A])
                VV(lambda e: e.tensor_tensor(out=o[:, 0, :], in0=q[:], in1=bA[:], op=ALU.mult), [r_q, r_A], [r_o4])
                AA(lambda e: e.activation(out=bA[:], in_=bE[:], func=AF.Exp, scale=-1.0), [r_E], [r_A])
                VV(lambda e: e.tensor_tensor(out=o[:, 1, :], in0=bC[:], in1=bA[:], op=ALU.mult), [r_C, r_A], [r_o4])
                AA(lambda e: e.activation(out=bA[:], in_=bB[:], func=AF.Exp), [r_B], [r_A])
                GG(lambda e: e.tensor_tensor(out=o[:, 2, :], in0=q[:], in1=bA[:], op=ALU.mult), [r_q, r_A], [r_o4])
                VV(lambda e: e.tensor_tensor(out=b3(bE), in0=b3(bB), in1=b3(bB)[:, :, 127:128].to_broadcast([128, NCK, 128]), op=ALU.subtract),
                   [r_B], [r_E])
                AA(lambda e: e.activation(out=bA[:], in_=bE[:], func=AF.Exp, scale=-1.0), [r_E], [r_A])
                GG(lambda e: e.tensor_tensor(out=o[:, 3, :], in0=bC[:], in1=bA[:], op=ALU.mult), [r_C, r_A], [r_o4])
                VV(lambda e: e.tensor_tensor(out=b3(bE), in0=b3(bB), in1=b3(bB)[:, :, 63:64].to_broadcast([128, NCK, 128]), op=ALU.subtract),
                   [r_B], [r_E])
                VV(lambda e: e.scalar_tensor_tensor(out=bE[:], in0=bE[:], scalar=-1.0, in1=bE[:], op0=ALU.mult, op1=ALU.min), [r_E], [r_E])
                AA(lambda e: e.activation(out=bA[:], in_=bE[:], func=AF.Exp), [r_E], [r_A])
                VV(lambda e: e.tensor_tensor(out=v3(o[:, 4, :])[:, :, 0:64], in0=b3(bC)[:, :, 0:64], in1=b3(bA)[:, :, 0:64], op=ALU.mult),
                   [r_C, r_A], [r_o4])
                VV(lambda e: e.tensor_tensor(out=v3(o[:, 4, :])[:, :, 64:128], in0=v3(q)[:, :, 64:128], in1=b3(bA)[:, :, 64:128], op=ALU.mult),
                   [r_q, r_A], [r_o4])
                AA(lambda e: e.activation(out=dcy[:], in_=b3(bB)[:, :, 127], func=AF.Exp), [r_B], [r_dcy])
                heads.append((o, r_o4, dcy, r_dcy))
                return ops

            for hp in range(2):
                la = head_ops(2 * hp)
                lb_ = head_ops(2 * hp + 1)
                for i in range(max(len(la), len(lb_))):
                    if i < len(la):
                        la[i]()
                    if i < len(lb_):
                        lb_[i]()
            return dict(sg=sg, ri=(ri, r_ri), rgm=(rgm, r_rgm), heads=heads)

        def chunks(st):
            sg = st["sg"]
            tk0 = sg * SEG
            ri, r_ri = st["ri"]
            rgm, r_rgm = st["rgm"]
            catseg, r_catseg = catr_.next()
            for ci_ in range(NCK):
                one_chunk(st, ci_, sg, ri, r_ri, rgm, r_rgm, catseg, r_catseg)
            ld(dr["CAT"][tk0:tk0 + SEG, 512:1024].rearrange("(c p) f -> p c f", p=128), catseg[:], [r_catseg], [rs["CAT"]])

        def one_chunk(st, ci, sg, ri, r_ri, rgm, r_rgm, catseg, r_catseg):
            if True:
                cs = slice(ci * 128, (ci + 1) * 128)
                g0 = (sg * NCK + ci) * 4
                per = []
                for hd in range(4):
                    o, r_o4, dcy, r_dcy = st["heads"][hd]
                    pa, r_pa = psA.next()
                    PE(lambda e, pa=pa, o=o: e.matmul(pa[:, :], lhsT=o[:, 1, cs], rhs=o[:, 0, cs], start=True, stop=True), [r_o4], [r_pa])
                    px, r_px = psX.next()
                    PE(lambda e, px=px, o=o: e.matmul(px[0:64, :], lhsT=o[:, 4, ci * 128:ci * 128 + 64], rhs=o[:, 4, ci * 128 + 64:(ci + 1) * 128],
                                                      start=True, stop=True), [r_o4], [r_px])
                    pt, r_pt = psT.next()
                    PE(lambda e, pt=pt, o=o: e.transpose(out=pt[:, :], in_=o[:, 3, cs], identity=ident[:]), [r_o4, r_ident], [r_pt])
                    Am, r_Am = Amr.next()
                    GP(lambda e, Am=Am: e.memset(Am[:], 0.0), (), [r_Am])
                    V(lambda e, Am=Am, pa=pa: e.copy_predicated(out=Am[:], mask=causal[:], data=pa[:]), [r_pa, r_causal, r_Am], [r_Am])
                    A(lambda e, Am=Am, px=px: e.activation(out=Am[0:64, 64:128], in_=px[0:64, :], func=AF.Copy), [r_px, r_Am], [r_Am])
                    khT, r_khT = khTr.next()
                    A(lambda e, khT=khT, pt=pt: e.activation(out=khT[:], in_=pt[:], func=AF.Copy), [r_pt], [r_khT])
                    per.append((Am, r_Am, khT, r_khT))
                pos_ = []
                for hd in range(4):
                    o, r_o4, dcy, r_dcy = st["heads"][hd]
                    Am, r_Am, khT, r_khT = per[hd]
                    Sh, r_Sh = St[hd]
                    Sbh, r_Sbh = Sb[hd]
                    v_ap = ri[:, ci, hd * 128:(hd + 1) * 128]
                    pu, r_pu = psU.next()
                    PE(lambda e, pu=pu, khT=khT, v_ap=v_ap: e.matmul(pu[:, :], lhsT=khT[:], rhs=v_ap, start=True, stop=True), [r_khT, r_ri], [r_pu])
                    po, r_po = psO.next()
                    PE(lambda e, po=po, Am=Am, v_ap=v_ap: e.matmul(po[:, :], lhsT=Am[:], rhs=v_ap, start=True, stop=False), [r_Am, r_ri], [r_po])
                    PE(lambda e, po=po, o=o, Sbh=Sbh: e.matmul(po[:, :], lhsT=o[:, 2, cs], rhs=Sbh[:], start=False, stop=True),
                       [r_o4, r_Sbh], [r_po])
                    V(lambda e, Sh=Sh, pu=pu, dcy=dcy: e.scalar_tensor_tensor(out=Sh[:], in0=Sh[:], scalar=dcy[:, ci:ci + 1], in1=pu[:],
                                                                             op0=ALU.mult, op1=ALU.add), [r_Sh, r_pu, r_dcy], [r_Sh])
                    A(lambda e, Sh=Sh, Sbh=Sbh: e.activation(out=Sbh[:], in_=Sh[:], func=AF.Copy), [r_Sh], [r_Sbh])
                    A(lambda e, po=po, gi=g0 + hd: e.activation(out=junk[:], in_=po[:], func=AF.Square, accum_out=ssr[:, gi:gi + 1]),
                      [r_po], [r_junk, r_ssr])
                    rstd_from_ss(ssr[:, g0 + hd:g0 + hd + 1], rsr[:, g0 + hd:g0 + hd + 1], 128, r_ssr, r_rsr)
                    V(lambda e, po=po, gi=g0 + hd, hd=hd: e.scalar_tensor_tensor(out=catseg[:, ci, hd * 128:(hd + 1) * 128], in0=po[:],
                                                                                scalar=rsr[:, gi:gi + 1], in1=rgm[:, ci, hd * 128:(hd + 1) * 128],
                                                                                op0=ALU.mult, op1=ALU.mult), [r_po, r_rsr, r_rgm], [r_catseg])

        nxt = ew(0)
        for sg in range(NSEG):
            cur = nxt
            if sg + 1 < NSEG:
                nxt = ew(sg + 1)
            chunks(cur)
        c.close()

    def p4(l, xin, r_xin):
        c = Ctx(S)
        woutb, r_woutb = c.sb([128, 8, D], BF16, "woutb")
        wst = c.ring(2, [128, 4, D], F32, name="wst")
        for hf in range(2):
            st, r_st = wst.next()
            ld(st[:], dr["w_out"][l, hf * 512:(hf + 1) * 512, :].rearrange("(kc p) n -> p kc n", p=128), (), [r_st])
            (V if hf == 0 else GP)(lambda e, st=st, hf=hf: e.tensor_copy(out=woutb[:, hf * 4:(hf + 1) * 4, :], in_=st[:]), [r_st], [r_woutb])
        wrs, r_wrs = c.sb([128, 8, 36], F32, "wrs")
        wrb, r_wrb = c.sb([128, 8, 36], BF16, "wrb")
        rbb, r_rbb = c.sb([128, 36], F32, "rbb")
        S.dma("sp", lambda e: e.dma_start(out=wrs[:], in_=dr["router_w"][l].rearrange("(kc p) n -> p kc n", p=128),
                                          allow_slow_non_contiguous=True), (), [r_wrs])
        V(lambda e: e.tensor_copy(out=wrb[:], in_=wrs[:]), [r_wrs], [r_wrb])
        ld(rbb[:], dr["router_b"][l:l + 1, :].partition_broadcast(128), (), [r_rbb], "sp")
        catr = c.ring(4, [128, D], BF16, name="cat")
        catTr = c.ring(3, [128, 8, 128], BF16, name="catT")
        xr = c.ring(4, [128, D], F32, name="x")
        x1r = c.ring(3, [128, D], F32, name="x1")
        h2r = c.ring(5, [128, D], BF16, name="h2")
        h2fr = c.ring(2, [128, D], F32, name="h2f")
        h2Tr = c.ring(3, [128, 8, 128], BF16, name="h2T")
        junk, r_junk = c.sb([128, D], BF16, "junk")
        ss, r_ss = c.sb([128, NT], F32, "ss")
        rstd, r_rstd = c.sb([128, NT], F32, "rstd")
        ptr = c.ring(2, [128, 8, 128], BF16, psum=True, name="ptr")
        pacc = c.ring(4, [128, 512], F32, psum=True, name="pacc")
        plg = c.ring(2, [128, 36], F32, psum=True, name="plg")
        V(lambda e: e.memset(ss[:], 0.0), (), [r_ss])
        st_ = {}

        def loads(t):
            ct, r_ct = catr.next()
            ld(ct[:], dr["CAT"][t * 128:(t + 1) * 128, :], [rs["CAT"]], [r_ct])
            xt, r_xt = xr.next()
            ld(xt[:], xin[t * 128:(t + 1) * 128, :], [r_xin], [r_xt])
            st_[t] = dict(ct=(ct, r_ct), xt=(xt, r_xt))

        def transposes(ta, td):
            jobs = []
            if ta is not None:
                pt, r_pt = ptr.next()
                ct, r_ct = st_[ta]["ct"]
                jobs.append([(lambda kc=kc, pt=pt, ct=ct, r_ct=r_ct, r_pt=r_pt: PE(
                    lambda e: e.transpose(out=pt[:, kc, :], in_=ct[:, kc * 128:(kc + 1) * 128], identity=ident[:]), [r_ct, r_ident], [r_pt]))
                    for kc in range(8)])
            if td is not None:
                pt2, r_pt2 = ptr.next()
                h2, r_h2 = st_[td]["h2"]
                jobs.append([(lambda kc=kc, pt2=pt2, h2=h2, r_h2=r_h2, r_pt2=r_pt2: PE(
                    lambda e: e.transpose(out=pt2[:, kc, :], in_=h2[:, kc * 128:(kc + 1) * 128], identity=ident[:]), [r_h2, r_ident], [r_pt2]))
                    for kc in range(8)])
            n = max(len(j) for j in jobs)
            for i in range(n):
                for j in jobs:
                    j[i]()
            if ta is not None:
                cT, r_cT = catTr.next()
                A(lambda e, cT=cT, pt=pt: e.activation(out=cT[:], in_=pt[:], func=AF.Copy), [r_pt], [r_cT])
                st_[ta]["cT"] = (cT, r_cT)
            if td is not None:
                hT, r_hT = h2Tr.next()
                A(lambda e, hT=hT, pt2=pt2: e.activation(out=hT[:], in_=pt2[:], func=AF.Copy), [r_pt2], [r_hT])
                st_[td]["hT"] = (hT, r_hT)

        def matmuls(t, tr):
            accs = None
            if t is not None:
                cT, r_cT = st_[t]["cT"]
                accs = [pacc.next(), pacc.next()]
            if tr is not None:
                hT, r_hT = st_[tr]["hT"]
                pl, r_pl = plg.next()
            for kc in range(8):
                if t is not None:
                    for n in range(2):
                        acc, r_acc = accs[n]
                        PE(lambda e, kc=kc, acc=acc, cT=cT, n=n: e.matmul(acc[:, :], lhsT=cT[:, kc, :], rhs=woutb[:, kc, n * 512:(n + 1) * 512],
                                                                         start=(kc == 0), stop=(kc == 7)), [r_cT, r_woutb], [r_acc])
                if tr is not None:
                    PE(lambda e, kc=kc, pl=pl, hT=hT: e.matmul(pl[:, :], lhsT=hT[:, kc, :], rhs=wrb[:, kc, :], start=(kc == 0), stop=(kc == 7)),
                       [r_hT, r_wrb], [r_pl])
            if tr is not None:
                V(lambda e, pl=pl, tr=tr: e.tensor_tensor(out=lgall[:, tr, :], in0=pl[:, :], in1=rbb[:], op=ALU.add), [r_pl, r_rbb], [r_lgall])
                del st_[tr]
            return accs

        def elementwise(t, accs):
            xt, r_xt = st_[t]["xt"]
            x1, r_x1 = x1r.next()
            for n in range(2):
                acc, r_acc = accs[n]
                V(lambda e, acc=acc, x1=x1, n=n: e.tensor_tensor(out=x1[:, n * 512:(n + 1) * 512], in0=acc[:, :], in1=G1b[:, n * 512:(n + 1) * 512],
                                                                op=ALU.mult), [r_acc, r_G1b], [r_x1])
            GP(lambda e, x1=x1, xt=xt: e.tensor_tensor(out=x1[:], in0=x1[:], in1=xt[:], op=ALU.add), [r_x1, r_xt], [r_x1])
            ld(dr["Xs"][t * 128:(t + 1) * 128, :], x1[:], [r_x1], [rs["Xs"]])
            A(lambda e, x1=x1, t=t: e.activation(out=junk[:], in_=x1[:], func=AF.Square, accum_out=ss[:, t:t + 1]), [r_x1], [r_junk, r_ss])
            rstd_from_ss(ss[:, t:t + 1], rstd[:, t:t + 1], D, r_ss, r_rstd)
            h2f_, r_h2f_ = h2fr.next()
            V(lambda e, x1=x1, t=t, h2f_=h2f_: e.scalar_tensor_tensor(out=h2f_[:], in0=x1[:], scalar=rstd[:, t:t + 1], in1=A2b[:], op0=ALU.mult, op1=ALU.mult),
              [r_x1, r_rstd, r_A2b], [r_h2f_])
            h2, r_h2 = h2r.next()
            GP(lambda e, h2=h2, h2f_=h2f_: e.tensor_tensor(out=h2[:], in0=h2f_[:], in1=S2b[:], op=ALU.add), [r_h2f_, r_S2b], [r_h2])
            ld(dr["H2"][t * 128:(t + 1) * 128, :], h2[:], [r_h2], [rs["H2"]])
            st_[t]["h2"] = (h2, r_h2)

        loads(0)
        loads(1)
        transposes(0, None)
        for t in range(NT + 3):
            if t + 2 < NT:
                loads(t + 2)
            ta = t + 1 if t + 1 < NT else None
            td = t - 2 if 0 <= t - 2 < NT else None
            if ta is not None or td is not None:
                transposes(ta, td)
            tm_ = t if t < NT else None
            trr = t - 3 if 0 <= t - 3 < NT else None
            if tm_ is not None or trr is not None:
                accs = matmuls(tm_, trr)
            if tm_ is not None:
                elementwise(tm_, accs)
        c.close()

    def p4b(l):
        c = Ctx(S)
        N3 = [128, NT, NE]
        gmax, r_gmax = c.sb([128, NT], F32)
        g4, r_g4 = c.sb([128, NT, 4], F32)
        goh, r_goh = c.sb([128, NT, 4], F32)
        gsum, r_gsum = c.sb([128, NT], F32)
        em, r_em = c.sb(N3, F32)
        oh1, r_oh1 = c.sb(N3, F32)
        oh2, r_oh2 = c.sb(N3, F32)
        m1, r_m1 = c.sb([128, NT], F32)
        m2, r_m2 = c.sb([128, NT], F32)
        p1, r_p1 = c.sb([128, NT], F32)
        abf, r_abf = c.sb([128, NT * NE], BF16)
        csb, r_csb = c.sb(N3, F32)
        offs, r_offs = c.sb(N3, F32)
        pos, r_pos = c.sb(N3, F32)
        sf, r_sf = c.sb([128, NT], F32)
        cnt, r_cnt = c.sb([128, NE], F32)
        nblk, r_nblk = c.sb([128, NE], F32)
        pendb, r_pendb = c.sb([128, NE], F32)
        pst, r_pst = c.sb([128, NE], F32)
        onesne, r_onesne = c.sb([128, NE], F32)
        bex, r_bex = c.sb([128, NBLK], F32)
        cmp, r_cmp = c.sb([128, NBLK * NE], F32)
        pp = c.ring(1, [128, NT * NE], F32, psum=True)
        pc = c.ring(1, [128, NT * NE], F32, psum=True)
        lgG = lgall[:, :, 0:4]
        lgE = lgall[:, :, 4:36]

        def bc(ap2, n):
            return ap2.unsqueeze(2).to_broadcast([128, NT, n])
        V(lambda e: e.tensor_reduce(out=gmax[:], in_=lgG, axis=AX.X, op=ALU.max), [r_lgall], [r_gmax])
        V(lambda e: e.tensor_tensor(out=goh[:], in0=lgG, in1=bc(gmax[:], 4), op=ALU.is_equal), [r_lgall, r_gmax], [r_goh])
        V(lambda e: e.tensor_tensor(out=g4[:], in0=lgG, in1=bc(gmax[:], 4), op=ALU.subtract), [r_lgall, r_gmax], [r_g4])
        A(lambda e: e.activation(out=g4[:], in_=g4[:], func=AF.Exp), [r_g4], [r_g4])
        V(lambda e: e.tensor_reduce(out=gsum[:], in_=g4[:], axis=AX.X, op=ALU.add), [r_g4], [r_gsum])
        V(lambda e: e.reciprocal(out=gsum[:], in_=gsum[:]), [r_gsum], [r_gsum])
        V(lambda e: e.tensor_scalar(out=goh[:], in0=goh[:], scalar1=BIG, scalar2=-BIG, op0=ALU.mult, op1=ALU.add), [r_goh], [r_goh])
        V(lambda e: e.tensor_tensor(out=em[:].rearrange("k t (g x) -> k t g x", g=4), in0=lgE.rearrange("k t (g x) -> k t g x", g=4),
                                    in1=goh[:].unsqueeze(3).to_broadcast([128, NT, 4, 8]), op=ALU.add), [r_lgall, r_goh], [r_em])
        V(lambda e: e.tensor_reduce(out=m1[:], in_=em[:], axis=AX.X, op=ALU.max), [r_em], [r_m1])
        V(lambda e: e.tensor_tensor(out=oh1[:], in0=em[:], in1=bc(m1[:], NE), op=ALU.is_equal), [r_em, r_m1], [r_oh1])
        V(lambda e: e.scalar_tensor_tensor(out=em[:], in0=oh1[:], scalar=-BIG, in1=em[:], op0=ALU.mult, op1=ALU.add), [r_oh1, r_em], [r_em])
        V(lambda e: e.tensor_reduce(out=m2[:], in_=em[:], axis=AX.X, op=ALU.max), [r_em], [r_m2])
        V(lambda e: e.tensor_tensor(out=oh2[:], in0=em[:], in1=bc(m2[:], NE), op=ALU.is_equal), [r_em, r_m2], [r_oh2])
        V(lambda e: e.tensor_tensor(out=m2[:], in0=m2[:], in1=m1[:], op=ALU.subtract), [r_m1, r_m2], [r_m2])
        A(lambda e: e.activation(out=m2[:], in_=m2[:], func=AF.Exp), [r_m2], [r_m2])
        V(lambda e: e.tensor_scalar(out=p1[:], in0=m2[:], scalar1=1.0, scalar2=None, op0=ALU.add), [r_m2], [r_p1])
        V(lambda e: e.reciprocal(out=p1[:], in_=p1[:]), [r_p1], [r_p1])
        V(lambda e: e.tensor_tensor(out=gt1[:], in0=p1[:], in1=gsum[:], op=ALU.mult), [r_p1, r_gsum], [r_gt1])
        V(lambda e: e.tensor_tensor(out=m2[:], in0=m2[:], in1=gt1[:], op=ALU.mult), [r_m2, r_gt1], [r_m2])
        V(lambda e: e.tensor_copy(out=gt2[:], in_=m2[:]), [r_m2], [r_gt2])
        V(lambda e: e.tensor_tensor(out=abf[:].rearrange("k (t x) -> k t x", x=NE), in0=oh1[:], in1=oh2[:], op=ALU.add), [r_oh1, r_oh2], [r_abf])
        ppt, r_pp = pp.next()
        pct, r_pc = pc.next()
        for ch in range(4):
            PE(lambda e, ch=ch: e.matmul(ppt[:, ch * 512:(ch + 1) * 512], lhsT=ustrict[:], rhs=abf[:, ch * 512:(ch + 1) * 512], start=True, stop=True),
               [r_ustrict, r_abf], [r_pp])
            PE(lambda e, ch=ch: e.matmul(pct[:, ch * 512:(ch + 1) * 512], lhsT=onesb[:], rhs=abf[:, ch * 512:(ch + 1) * 512], start=True, stop=True),
               [r_onesb, r_abf], [r_pc])
        V(lambda e: e.tensor_copy(out=csb[:].rearrange("k t x -> k (t x)"), in_=pct[:]), [r_pc], [r_csb])
        V(lambda e: e.memset(offs[:, 0, :], 0.0), (), [r_offs])
        for j in range(1, NT):
            V(lambda e, j=j: e.tensor_tensor(out=offs[:, j, :], in0=offs[:, j - 1, :], in1=csb[:, j - 1, :], op=ALU.add), [r_offs, r_csb], [r_offs])
        V(lambda e: e.tensor_tensor(out=cnt[:], in0=offs[:, NT - 1, :], in1=csb[:, NT - 1, :], op=ALU.add), [r_offs, r_csb], [r_cnt])
        V(lambda e: e.tensor_tensor(out=cmp[:, 0:NE * 64].rearrange("k (x m) -> k x m", m=64), in0=cnt[:].unsqueeze(2).to_broadcast([128, NE, 64]),
                                    in1=mulrow[:].unsqueeze(1).to_broadcast([128, NE, 64]), op=ALU.is_gt), [r_cnt, r_mulrow], [r_cmp])
        V(lambda e: e.tensor_reduce(out=nblk[:], in_=cmp[:, 0:NE * 64].rearrange("k (x m) -> k x m", m=64), axis=AX.X, op=ALU.add), [r_cmp], [r_nblk])
        V(lambda e: e.memset(onesne[:], 1.0), (), [r_onesne])
        V(lambda e: e.tensor_tensor_scan(out=pendb[:], data0=onesne[:], data1=nblk[:], initial=0.0, op0=ALU.mult, op1=ALU.add),
          [r_onesne, r_nblk], [r_pendb])
        V(lambda e: e.tensor_tensor(out=pst[:], in0=pendb[:], in1=nblk[:], op=ALU.subtract), [r_pendb, r_nblk], [r_pst])
        V(lambda e: e.tensor_scalar(out=pst[:], in0=pst[:], scalar1=float(RB), scalar2=None, op0=ALU.mult), [r_pst], [r_pst])
        V(lambda e: e.tensor_tensor(out=cmp[:, 0:NBLK * NE].rearrange("k (b x) -> k b x", x=NE), in0=pendb[:].unsqueeze(1).to_broadcast([128, NBLK, NE]),
                                    in1=brow[:].unsqueeze(2).to_broadcast([128, NBLK, NE]), op=ALU.is_le), [r_pendb, r_brow], [r_cmp])
        V(lambda e: e.tensor_reduce(out=bex[:], in_=cmp[:, 0:NBLK * NE].rearrange("k (b x) -> k b x", x=NE), axis=AX.X, op=ALU.add), [r_cmp], [r_bex])
        V(lambda e: e.tensor_scalar(out=bex[:], in0=bex[:], scalar1=float(NE - 1), scalar2=float(128), op0=ALU.min, op1=ALU.mult), [r_bex], [r_bex])
        V(lambda e: e.tensor_scalar(out=bex[:], in0=bex[:], scalar1=piota[:, 0:1], scalar2=float(l * NE * 128), op0=ALU.add, op1=ALU.add),
          [r_bex, r_piota], [r_bex])
        V(lambda e: e.tensor_copy(out=widx[:], in_=bex[:]), [r_bex], [r_widx])
        V(lambda e: e.tensor_tensor(out=offs[:], in0=offs[:], in1=pst[:].unsqueeze(1).to_broadcast([128, NT, NE]), op=ALU.add), [r_offs, r_pst], [r_offs])
        V(lambda e: e.tensor_tensor(out=pos[:].rearrange("k t x -> k (t x)"), in0=ppt[:], in1=offs[:].rearrange("k t x -> k (t x)"), op=ALU.add),
          [r_pp, r_offs], [r_pos])
        for (oh, r_oh, sl, r_sl) in ((oh1, r_oh1, slot1, r_slot1), (oh2, r_oh2, slot2, r_slot2)):
            V(lambda e, oh=oh: e.tensor_tensor(out=oh[:], in0=oh[:], in1=pos[:], op=ALU.mult), [r_oh, r_pos], [r_oh])
            V(lambda e, oh=oh: e.tensor_reduce(out=sf[:], in_=oh[:], axis=AX.X, op=ALU.add), [r_oh], [r_sf])
            V(lambda e: e.tensor_scalar(out=sf[:], in0=sf[:], scalar1=float(NROWS - 1), scalar2=0.0, op0=ALU.min, op1=ALU.max), [r_sf], [r_sf])
            V(lambda e, sl=sl: e.tensor_copy(out=sl[:], in_=sf[:]), [r_sf], [r_sl])
        h2r = c.ring(3, [128, D], BF16, name="h2d")
        for t in range(NT):
            h2, r_h2 = h2r.next()
            ld(h2[:], dr["H2"][t * 128:(t + 1) * 128, :], [rs["H2"]], [r_h2])
            for (sl, r_sl) in ((slot1, r_slot1), (slot2, r_slot2)):
                S.dma("pool", lambda e, h2=h2, sl=sl, t=t: e.indirect_dma_start(
                    out=dr["XS"], out_offset=bass.IndirectOffsetOnAxis(ap=sl[:, t:t + 1], axis=0), in_=h2[:], in_offset=None),
                    [r_h2, r_sl], [rs["XS"]])
        c.close()

    def p5(l):
        c = Ctx(S)
        wsr = c.ring(6, [128, 2048], F32, name="wsr")
        wbr = [c.ring(2, [128, 4096], BF16, name=f"wb{i}") for i in range(3)]
        xrr = c.ring(4, [128, D], BF16, name="xr")
        xTr = c.ring(2, [128, 8, RB], BF16, name="xT")
        actr = c.ring(2, [128, 4, RB], BF16, name="actT")
        silr = c.ring(2, [128, RB], F32, name="sil")
        ysr = c.ring(3, [128, D], BF16, name="ys")
        ptr = c.ring(2, [128, 8, 128], BF16, psum=True, name="ptr")
        pacc = c.ring(6, [128, 512], F32, psum=True, name="pacc")
        wsrc = ((dr["moe_w1_0"], dr["moe_w1_1"]), (dr["moe_w3_0"], dr["moe_w3_1"]), (dr["moe_w2_0"], dr["moe_w2_1"]))
        k = [0]

        def wload(b):
            wb = []
            for i in range(3):
                wt, r_wt = wbr[i].next()
                for hf in range(2):
                    st, r_st = wsr.next()
                    S.dma("pool", lambda e, st=st, i=i, hf=hf: e.indirect_dma_start(
                        out=st[:], out_offset=None, in_=wsrc[i][hf],
                        in_offset=bass.IndirectOffsetOnAxis(ap=widx[:, b:b + 1], axis=0)), [r_widx], [r_st])
                    k[0] += 1
                    dst = wt[:, hf * 2048:(hf + 1) * 2048]
                    if k[0] % 2 == 0:
                        V(lambda e, st=st, dst=dst: e.tensor_copy(out=dst, in_=st[:]), [r_st], [r_wt])
                    else:
                        A(lambda e, st=st, dst=dst: e.activation(out=dst, in_=st[:], func=AF.Copy), [r_st], [r_wt])
                wb.append((wt, r_wt))
            return wb

        def xprep(b):
            xT, r_xT = xTr.next()
            tl = []
            for rt in range(RB // 128):
                xr_, r_xr = xrr.next()
                r0 = b * RB + rt * 128
                ld(xr_[:], dr["XS"][r0:r0 + 128, :], [rs["XS"]], [r_xr])
                tl.append((xr_, r_xr, ptr.next()))
            for kc in range(8):
                for (xr_, r_xr, (pt, r_pt)) in tl:
                    PE(lambda e, kc=kc, pt=pt, xr_=xr_: e.transpose(out=pt[:, kc, :], in_=xr_[:, kc * 128:(kc + 1) * 128], identity=ident[:]),
                       [r_xr, r_ident], [r_pt])
            for rt, (xr_, r_xr, (pt, r_pt)) in enumerate(tl):
                if rt == 0:
                    V(lambda e, xT=xT, pt=pt, rt=rt: e.tensor_copy(out=xT[:, :, rt * 128:(rt + 1) * 128], in_=pt[:]), [r_pt], [r_xT])
                else:
                    A(lambda e, xT=xT, pt=pt, rt=rt: e.activation(out=xT[:, :, rt * 128:(rt + 1) * 128], in_=pt[:], func=AF.Copy), [r_pt], [r_xT])
            return xT, r_xT

        Wn = wload(0)
        Xn = xprep(0)
        for b in range(NBLK):
            (w1b, r_w1b), (w3b, r_w3b), (w2b, r_w2b) = Wn
            xT, r_xT = Xn
            if b + 1 < NBLK:
                Wn = wload(b + 1)
            aT, r_aT = actr.next()
            for mp in range(2):
                accs = []
                for mi in range(2):
                    accs.append((pacc.next(), pacc.next()))
                for kc in range(8):
                    for mi in range(2):
                        mc = 2 * mp + mi
                        (a1, r_a1), (a3, r_a3) = accs[mi]
                        PE(lambda e, kc=kc, mc=mc, a1=a1, w1b=w1b, xT=xT: e.matmul(a1[:, 0:RB], lhsT=w1b[:, kc * 512 + mc * 128:kc * 512 + (mc + 1) * 128],
                                                                                  rhs=xT[:, kc, :], start=(kc == 0), stop=(kc == 7)), [r_w1b, r_xT], [r_a1])
                        PE(lambda e, kc=kc, mc=mc, a3=a3, w3b=w3b, xT=xT: e.matmul(a3[:, 0:RB], lhsT=w3b[:, kc * 512 + mc * 128:kc * 512 + (mc + 1) * 128],
                                                                                  rhs=xT[:, kc, :], start=(kc == 0), stop=(kc == 7)), [r_w3b, r_xT], [r_a3])
                for mi in range(2):
                    mc = 2 * mp + mi
                    (a1, r_a1), (a3, r_a3) = accs[mi]
                    sl, r_sl = silr.next()
                    A(lambda e, sl=sl, a1=a1: e.activation(out=sl[:], in_=a1[:, 0:RB], func=AF.Silu), [r_a1], [r_sl])
                    V(lambda e, sl=sl, a3=a3, mc=mc, aT=aT: e.tensor_tensor(out=aT[:, mc, :], in0=sl[:], in1=a3[:, 0:RB], op=ALU.mult), [r_sl, r_a3], [r_aT])
            if b + 1 < NBLK:
                Xn = xprep(b + 1)
            yaccs = [[pacc.next() for nch in range(2)] for rt in range(RB // 128)]
            for mc in range(4):
                for rt in range(RB // 128):
                    for nch in range(2):
                        acc, r_acc = yaccs[rt][nch]
                        PE(lambda e, mc=mc, acc=acc, rt=rt, nch=nch, aT=aT, w2b=w2b: e.matmul(
                            acc[:, :], lhsT=aT[:, mc, rt * 128:(rt + 1) * 128], rhs=w2b[:, mc * 1024 + nch * 512:mc * 1024 + (nch + 1) * 512],
                            start=(mc == 0), stop=(mc == 3)), [r_aT, r_w2b], [r_acc])
            for rt in range(RB // 128):
                ys, r_ys = ysr.next()
                for nch in range(2):
                    acc, r_acc = yaccs[rt][nch]
                    if nch == 0:
                        A(lambda e, ys=ys, acc=acc: e.activation(out=ys[:, 0:512], in_=acc[:, :], func=AF.Copy), [r_acc], [r_ys])
                    else:
                        V(lambda e, ys=ys, acc=acc: e.tensor_copy(out=ys[:, 512:1024], in_=acc[:, :]), [r_acc], [r_ys])
                r0 = b * RB + rt * 128
                ld(dr["YS"][r0:r0 + 128, :], ys[:], [r_ys], [rs["YS"]])
        c.close()

    def p6(l):
        c = Ctx(S)
        xr = c.ring(4, [128, D], F32, name="x1")
        y1r = c.ring(4, [128, D], BF16, name="y1")
        y2r = c.ring(4, [128, D], BF16, name="y2")
        yfr = c.ring(2, [128, D], F32, name="yf")
        ygr = c.ring(2, [128, D], F32, name="yg")
        st_ = {}

        def loads(t):
            xt, r_xt = xr.next()
            ld(xt[:], dr["Xs"][t * 128:(t + 1) * 128, :], [rs["Xs"]], [r_xt])
            y1, r_y1 = y1r.next()
            y2, r_y2 = y2r.next()
            for (yy, r_yy, sl, r_sl) in ((y1, r_y1, slot1, r_slot1), (y2, r_y2, slot2, r_slot2)):
                S.dma("pool", lambda e, yy=yy, sl=sl, t=t: e.indirect_dma_start(
                    out=yy[:], out_offset=None, in_=dr["YS"], in_offset=bass.IndirectOffsetOnAxis(ap=sl[:, t:t + 1], axis=0)),
                    [rs["YS"], r_sl], [r_yy])
            st_[t] = (xt, r_xt, y1, r_y1, y2, r_y2)
        PF = 3
        for t in range(min(PF, NT)):
            loads(t)
        for t in range(NT):
            xt, r_xt, y1, r_y1, y2, r_y2 = st_.pop(t)
            yf, r_yf = yfr.next()
            yg, r_yg = ygr.next()
            A(lambda e, y1=y1, yf=yf, t=t: e.activation(out=yf[:], in_=y1[:], func=AF.Copy, scale=gt1[:, t:t + 1]), [r_y1, r_gt1], [r_yf])
            V(lambda e, yf=yf, yg=yg, y2=y2, t=t: e.scalar_tensor_tensor(out=yg[:], in0=y2[:], scalar=gt2[:, t:t + 1], in1=yf[:], op0=ALU.mult, op1=ALU.add),
              [r_yf, r_y2, r_gt2], [r_yg])
            V(lambda e, yg=yg: e.tensor_tensor(out=yg[:], in0=yg[:], in1=G2b[:], op=ALU.mult), [r_yg, r_G2b], [r_yg])
            V(lambda e, yg=yg, xt=xt: e.tensor_tensor(out=xt[:], in0=yg[:], in1=xt[:], op=ALU.add), [r_yg, r_xt], [r_xt])
            ld(dr["Xs"][t * 128:(t + 1) * 128, :], xt[:], [r_xt], [rs["Xs"]])
            if t + PF < NT:
                loads(t + PF)
        c.close()

    def pfinal():
        c = Ctx(S)
        fgb, r_fgb = c.sb([128, D], F32, "fgb")
        ld(fgb[:], dr["final_g"].partition_broadcast(128), (), [r_fgb], "sp")
        xr = c.ring(3, [128, D], F32, name="xf")
        junk, r_junk = c.sb([128, D], BF16, "junk")
        ss, r_ss = c.sb([128, NT], F32, "ss")
        rstd, r_rstd = c.sb([128, NT], F32, "rstd")
        V(lambda e: e.memset(ss[:], 0.0), (), [r_ss])
        for t in range(NT):
            xt, r_xt = xr.next()
            ld(xt[:], dr["Xs"][t * 128:(t + 1) * 128, :], [rs["Xs"]], [r_xt])
            A(lambda e, xt=xt, t=t: e.activation(out=junk[:], in_=xt[:], func=AF.Square, accum_out=ss[:, t:t + 1]), [r_xt], [r_junk, r_ss])
            rstd_from_ss(ss[:, t:t + 1], rstd[:, t:t + 1], D, r_ss, r_rstd)
            V(lambda e, xt=xt, t=t: e.scalar_tensor_tensor(out=xt[:], in0=xt[:], scalar=rstd[:, t:t + 1], in1=fgb[:], op0=ALU.mult, op1=ALU.mult),
              [r_xt, r_rstd, r_fgb], [r_xt])
            ld(out[t * 128:(t + 1) * 128, :], xt[:], [r_xt], [rs["out"]])
        c.close()


    def finish():
        toks = []
        for r in rs.values():
            toks += r.ws
        S.wait_all("sp", toks)
        S.flush()
        G.close()
        S.es.close()

    setup()
    xin, r_xin = dr["x"], rs["x"]
    done = False
    for l in range(depth):
        for (nm, fn) in (("p0", lambda: p0(l)), ("p1", lambda: p1(l, xin, r_xin)), ("p2", lambda: p2(l)), ("p2b", lambda: p2b(l)),
                         ("p3", lambda: p3(l)), ("p4", lambda: p4(l, xin, r_xin)), ("p4b", lambda: p4b(l)), ("p5", lambda: p5(l)),
                         ("p6", lambda: p6(l))):
            fn()
            if stop == (nm, l):
                done = True
                break
        if done:
            break
        xin, r_xin = dr["Xs"], rs["Xs"]
    if not done and final:
        pfinal()
    finish()
    return nc


def make_inputs(inputs, b):
    f = lambda a: np.ascontiguousarray(np.asarray(a, dtype=np.float32))
    m = {
        "x": f(inputs["x"][b]), "c": f(inputs["c"][b:b + 1]), "w_ada": f(inputs["w_ada"]), "b_ada": f(inputs["b_ada"]),
        "norm1_g": f(inputs["norm1_g"]), "w_in": f(inputs["w_in"]), "attn_norm_g": f(inputs["attn_norm_g"]),
        "hgrn_lb_logits": f(inputs["hgrn_lb_logits"]), "hgrn_norm_g": f(inputs["hgrn_norm_g"]), "w_out": f(inputs["w_out"]),
        "norm2_g": f(inputs["norm2_g"]),
        "router_w": f(np.concatenate([np.asarray(inputs["router_group_w"]), np.asarray(inputs["router_expert_w"])], axis=-1)),
        "router_b": f(np.concatenate([np.asarray(inputs["router_group_b"]), np.asarray(inputs["router_expert_b"])], axis=-1)),
        "final_g": f(np.asarray(inputs["final_g"]).reshape(1, D)),
    }
    for nm, kc, n in (("moe_w1", 8, 512), ("moe_w3", 8, 512), ("moe_w2", 4, D)):
        w = np.asarray(inputs[nm], dtype=np.float32).reshape(DEPTH, NE, kc, 128, n).transpose(0, 1, 3, 2, 4).reshape(DEPTH * NE * 128, kc * n)
        m[nm + "_0"] = np.ascontiguousarray(w[:, :2048])
        m[nm + "_1"] = np.ascontiguousarray(w[:, 2048:])
    m.update(host_consts())
    return m


def kernel(**inputs):
    nc = build()
    in_maps = [make_inputs(inputs, b) for b in range(2)]
    res = run_bass_kernel_spmd(nc, in_maps, core_ids=[0, 1])
    return np.stack([np.asarray(res.results[b]["out"], dtype=np.float32) for b in range(2)], axis=0)
```

```python
import contextlib
import numpy as np
import ml_dtypes
import concourse.bass as bass
import concourse.mybir as mybir
from concourse.bass_utils import run_bass_kernel_spmd

F32 = mybir.dt.float32
BF16 = mybir.dt.bfloat16
I32 = mybir.dt.int32
ALU = mybir.AluOpType
AF = mybir.ActivationFunctionType
AX = mybir.AxisListType

T = 8192
D = 1024
NT = T // 128
DEPTH = 4
NE = 32
RB = 256
NBLK = (2 * T) // RB + NE
NROWS = NBLK * RB
EPS = 1e-6
BIG = 30000.0
PATTERNS = ((128, 1), (512, 4), (2048, 16))
EP_ENG = 30000
EP_DMA = 3000


class Res:
    __slots__ = ("w", "r")

    def __init__(self):
        self.w = None
        self.r = []


class MRes(Res):
    __slots__ = ("ws",)

    def __init__(self):
        super().__init__()
        self.ws = []


def _compress(lst):
    mx = {}
    for (pq, c) in lst:
        mx[pq] = max(mx.get(pq, 0), c)
    return list(mx.items())


class Q:
    def __init__(self, nc, es, name, inc, ep):
        self.nc, self.es, self.name, self.inc, self.ep = nc, es, name, inc, ep
        self.sems = []
        self.count = 0
        self.ops = []
        self.seen = {}

    def sem_for(self, c):
        i = (c - 1) // self.ep
        while len(self.sems) <= i:
            self.sems.append(self.es.enter_context(self.nc.semaphore(f"{self.name}_{len(self.sems)}")))
        return self.sems[i], (((c - 1) % self.ep) + 1) * self.inc


class Sched:
    def __init__(self, nc, ndma=6):
        self.nc = nc
        self.es = contextlib.ExitStack()
        self.q = {n: Q(nc, self.es, "s" + n, 1, EP_ENG) for n in ("pe", "act", "dve", "pool", "sp")}
        self.dslots = {n: [Q(nc, self.es, f"d{n}{i}", 16, EP_DMA) for i in range(ndma)] for n in ("sp", "pool", "act")}
        self.dnext = {n: 0 for n in self.dslots}

    def _deps(self, reads, writes):
        need = {}
        for r in reads:
            if isinstance(r, MRes):
                for (pq, c) in r.ws:
                    need[pq] = max(need.get(pq, 0), c)
            elif r.w is not None:
                need[r.w[0]] = max(need.get(r.w[0], 0), r.w[1])
        for w in writes:
            if (not isinstance(w, MRes)) and w.w is not None:
                need[w.w[0]] = max(need.get(w.w[0], 0), w.w[1])
            for (pq, c) in w.r:
                need[pq] = max(need.get(pq, 0), c)
        return need

    def _commit(self, tok, reads, writes):
        for r in reads:
            r.r.append(tok)
            if len(r.r) > 48:
                r.r = _compress(r.r)
        for w in writes:
            if isinstance(w, MRes):
                w.ws.append(tok)
                if len(w.ws) > 48:
                    w.ws = _compress(w.ws)
            else:
                w.w = tok
                w.r = []

    def _waits(self, q, need):
        waits = []
        for pq, c in need.items():
            if q.seen.get(pq, 0) >= c:
                continue
            q.seen[pq] = c
            waits.append((pq, c))
        return waits

    def op(self, qn, fn, reads=(), writes=()):
        q = self.q[qn]
        waits = self._waits(q, self._deps(reads, writes))
        q.count += 1
        tok = (q, q.count)
        q.ops.append((waits, fn, tok))
        self._commit(tok, reads, writes)
        return tok

    def dma(self, qn, fn, reads=(), writes=()):
        q = self.q[qn]
        sl = self.dslots[qn]
        dq = sl[self.dnext[qn] % len(sl)]
        self.dnext[qn] += 1
        need = self._deps(reads, writes)
        if dq.count > 0:
            need[dq] = max(need.get(dq, 0), dq.count)
        waits = self._waits(q, need)
        dq.count += 1
        tok = (dq, dq.count)
        q.ops.append((waits, fn, tok))
        self._commit(tok, reads, writes)
        return tok

    def wait_all(self, qn, toks):
        q = self.q[qn]
        need = {}
        for (pq, c) in toks:
            need[pq] = max(need.get(pq, 0), c)
        q.ops.append((self._waits(q, need), None, None))

    def flush(self):
        nc = self.nc
        if not any(q.ops for q in self.q.values()):
            return
        with nc.Block() as block:
            def mk(qn):
                q = self.q[qn]

                def body(e):
                    for waits, fn, tok in q.ops:
                        for (pq, c) in waits:
                            s, v = pq.sem_for(c)
                            e.wait_ge(s, v)
                        if fn is not None:
                            s, _ = tok[0].sem_for(tok[1])
                            fn(e).then_inc(s, tok[0].inc)
                    q.ops = []
                return body
            block.tensor(mk("pe"))
            block.scalar(mk("act"))
            block.vector(mk("dve"))
            block.gpsimd(mk("pool"))
            block.sync(mk("sp"))


class Ring:
    def __init__(self, items):
        self.items = items
        self.i = 0

    def next(self):
        it = self.items[self.i % len(self.items)]
        self.i += 1
        return it


_UID = [0]


class Ctx:
    def __init__(self, S):
        self.S = S
        self.es = contextlib.ExitStack()
        self.n = 0

    def sb(self, shape, dt, name=None):
        _UID[0] += 1
        t = self.es.enter_context(self.S.nc.sbuf_tensor(f"{name or 't'}_{_UID[0]}", list(shape), dt))
        return t, Res()

    def ps(self, shape, dt, name=None):
        _UID[0] += 1
        t = self.es.enter_context(self.S.nc.psum_tensor(f"{name or 'p'}_{_UID[0]}", list(shape), dt))
        return t, Res()

    def ring(self, n, shape, dt, psum=False, name=None):
        return Ring([(self.ps if psum else self.sb)(shape, dt, name) for _ in range(n)])

    def close(self):
        self.S.flush()
        self.es.close()


def host_consts():
    c = {}
    c["ident"] = np.eye(128, dtype=np.float32).astype(ml_dtypes.bfloat16)
    kk = np.arange(128)[:, None]
    qq = np.arange(128)[None, :]
    mm = np.zeros((128, 8, 3, 256), np.float32)
    for h in range(8):
        slope = 2.0 ** (-(h + 1))
        for p, (w, d) in enumerate(PATTERNS):
            steps = w // d
            dist_cur = qq - kk
            dist_nxt = 128 + qq - kk
            for j, dist in enumerate((dist_cur, dist_nxt)):
                valid = (dist >= 0) & (dist <= steps)
                mm[:, h, p, j * 128:(j + 1) * 128] = np.where(valid, -slope * d * dist, -BIG)
    mm2 = np.concatenate([mm[..., 128:256], mm[..., 0:128]], axis=-1)
    c["amask"] = np.exp(mm2.astype(np.float64)).astype(np.float32).reshape(128, 8 * 3 * 256).astype(ml_dtypes.bfloat16)
    c["causal"] = ((kk <= qq) & ((kk // 64) == (qq // 64))).astype(np.uint8)
    c["ustrict"] = (kk < qq).astype(np.float32).astype(ml_dtypes.bfloat16)
    rm = np.ones((128, 2048), np.float32)
    rm[:, ::128] = 0.0
    c["resetm"] = rm
    c["mulrow"] = np.tile((np.arange(64, dtype=np.float32) * RB)[None, :], (128, 1))
    c["brow"] = np.tile(np.arange(NBLK, dtype=np.float32)[None, :], (128, 1))
    c["piota"] = np.arange(128, dtype=np.float32).reshape(128, 1)
    return c


CONST_SPECS = {"ident": ([128, 128], BF16), "amask": ([128, 8 * 3 * 256], BF16), "causal": ([128, 128], mybir.dt.uint8),
               "ustrict": ([128, 128], BF16), "resetm": ([128, 2048], F32), "mulrow": ([128, 64], F32), "brow": ([128, NBLK], F32), "piota": ([128, 1], F32)}

IN_SPECS = {
    "x": ([T, D], F32), "c": ([1, D], F32), "w_ada": ([DEPTH, D, 6 * D], F32), "b_ada": ([DEPTH, 6 * D], F32),
    "norm1_g": ([DEPTH, D], F32), "w_in": ([DEPTH, D, 3584], F32), "attn_norm_g": ([DEPTH, 512], F32),
    "hgrn_lb_logits": ([DEPTH, 512], F32), "hgrn_norm_g": ([DEPTH, 512], F32), "w_out": ([DEPTH, D, D], F32),
    "norm2_g": ([DEPTH, D], F32), "router_w": ([DEPTH, D, 36], F32), "router_b": ([DEPTH, 36], F32),
    "moe_w1_0": ([DEPTH * NE * 128, 2048], F32), "moe_w1_1": ([DEPTH * NE * 128, 2048], F32),
    "moe_w3_0": ([DEPTH * NE * 128, 2048], F32), "moe_w3_1": ([DEPTH * NE * 128, 2048], F32),
    "moe_w2_0": ([DEPTH * NE * 128, 2048], F32), "moe_w2_1": ([DEPTH * NE * 128, 2048], F32),
    "final_g": ([1, D], F32),
}


def build(depth=DEPTH, stop=None, dumps=(), final=True):
    nc = bass.Bass("TRN2", target_bir_lowering=False)
    S = Sched(nc)
    dr = {}
    for k, (shp, dt) in list(IN_SPECS.items()) + list(CONST_SPECS.items()):
        dr[k] = nc.dram_tensor(k, shp, dt, kind="ExternalInput").ap()
    out = nc.dram_tensor("out", [T, D], F32, kind="ExternalOutput").ap()
    rs = {}

    def scratch(name, shp, dt):
        kind = "ExternalOutput" if name in dumps else "Internal"
        dr[name] = nc.dram_tensor(name, shp, dt, kind=kind).ap()
        rs[name] = MRes()
    scratch("Xs", [T, D], F32)
    scratch("QT", [512, T], BF16)
    scratch("KT", [512, T], BF16)
    scratch("VV", [T, 512], BF16)
    scratch("RQT", [512, T], BF16)
    scratch("ZT", [512, T], F32)
    scratch("RI", [T, 512], BF16)
    scratch("RG", [T, 512], BF16)
    for p in range(3):
        scratch(f"OP{p}", [T, 2, 4 * 65], F32)
    scratch("CAT", [T, D], BF16)
    scratch("H2", [T, D], BF16)
    scratch("XS", [NROWS, D], BF16)
    scratch("YS", [NROWS, D], BF16)
    rs["x"] = MRes()
    rs["out"] = MRes()

    G = Ctx(S)
    ident, r_ident = G.sb([128, 128], BF16, "ident")
    causal, r_causal = G.sb([128, 128], mybir.dt.uint8, "causal")
    ustrict, r_ustrict = G.sb([128, 128], BF16, "ustrict")
    onesb, r_onesb = G.sb([128, 128], BF16, "onesb")
    mulrow, r_mulrow = G.sb([128, 64], F32, "mulrow")
    brow, r_brow = G.sb([128, NBLK], F32, "brow")
    piota, r_piota = G.sb([128, 1], F32, "piota")
    widx, r_widx = G.sb([128, NBLK], I32, "widx")
    one11, r_one11 = G.sb([1, 1], F32, "one11")
    onesrow, r_onesrow = G.sb([1, 128], F32, "onesrow")
    colf, r_colf = G.sb([128, 16], F32, "colf")
    G1b, r_G1b = G.sb([128, D], F32, "G1b")
    A2b, r_A2b = G.sb([128, D], F32, "A2b")
    S2b, r_S2b = G.sb([128, D], F32, "S2b")
    G2b, r_G2b = G.sb([128, D], F32, "G2b")
    lbc, r_lbc = G.sb([128, DEPTH, 4], F32, "lbc")
    oml, r_oml = G.sb([128, DEPTH, 4], F32, "oml")
    noml, r_noml = G.sb([128, DEPTH, 4], F32, "noml")
    slot1, r_slot1 = G.sb([128, NT], I32, "slot1")
    slot2, r_slot2 = G.sb([128, NT], I32, "slot2")
    gt1, r_gt1 = G.sb([128, NT], F32, "gt1")
    gt2, r_gt2 = G.sb([128, NT], F32, "gt2")
    lgall, r_lgall = G.sb([128, NT, 36], F32, "lgall")

    dma_rr = [0]

    def ld(out_, in_, reads=(), writes=(), q=None):
        if q is None:
            q = "sp"
        return S.dma(q, lambda e: e.dma_start(out=out_, in_=in_), reads, writes)

    def V(fn, r=(), w=()):
        return S.op("dve", fn, r, w)

    def A(fn, r=(), w=()):
        return S.op("act", fn, r, w)

    def PE(fn, r=(), w=()):
        return S.op("pe", fn, r, w)

    def GP(fn, r=(), w=()):
        return S.op("pool", fn, r, w)

    def setup():
        ld(ident[:], dr["ident"], (), [r_ident], "sp")
        ld(causal[:], dr["causal"], (), [r_causal], "sp")
        ld(ustrict[:], dr["ustrict"], (), [r_ustrict], "sp")
        ld(mulrow[:], dr["mulrow"], (), [r_mulrow], "sp")
        ld(brow[:], dr["brow"], (), [r_brow], "sp")
        ld(piota[:], dr["piota"], (), [r_piota], "sp")
        V(lambda e: e.memset(one11[:], 1.0), (), [r_one11])
        V(lambda e: e.memset(onesrow[:], 1.0), (), [r_onesrow])
        V(lambda e: e.memset(onesb[:], 1.0), (), [r_onesb])
        c = Ctx(S)
        lg, r_lg = c.sb([128, DEPTH, 4], F32)
        ex, r_ex = c.sb([128, DEPTH, 4], F32)
        sm, r_sm = c.sb([128, 4], F32)
        S.dma("sp", lambda e: e.dma_start(out=lg[:], in_=dr["hgrn_lb_logits"].rearrange("l (h k) -> k l h", k=128),
                                          allow_slow_non_contiguous=True), (), [r_lg])
        A(lambda e: e.activation(out=ex[:], in_=lg[:], func=AF.Exp), [r_lg], [r_ex])
        V(lambda e: e.tensor_tensor(out=sm[:], in0=ex[:, 0, :], in1=ex[:, 1, :], op=ALU.add), [r_ex], [r_sm])
        V(lambda e: e.tensor_tensor(out=sm[:], in0=sm[:], in1=ex[:, 2, :], op=ALU.add), [r_ex, r_sm], [r_sm])
        V(lambda e: e.tensor_tensor(out=sm[:], in0=sm[:], in1=ex[:, 3, :], op=ALU.add), [r_ex, r_sm], [r_sm])
        V(lambda e: e.reciprocal(out=sm[:], in_=sm[:]), [r_sm], [r_sm])
        V(lambda e: e.memset(lbc[:, 0, :], 0.0), (), [r_lbc])
        V(lambda e: e.tensor_tensor(out=lbc[:, 1, :], in0=ex[:, 1, :], in1=sm[:], op=ALU.mult), [r_ex, r_sm], [r_lbc])
        for l in (2, 3):
            V(lambda e, l=l: e.tensor_tensor(out=ex[:, l, :], in0=ex[:, l, :], in1=sm[:], op=ALU.mult), [r_ex, r_sm], [r_ex])
            V(lambda e, l=l: e.tensor_tensor(out=lbc[:, l, :], in0=lbc[:, l - 1, :], in1=ex[:, l, :], op=ALU.add), [r_ex, r_lbc], [r_lbc])
        V(lambda e: e.tensor_scalar(out=oml[:], in0=lbc[:], scalar1=-1.0, scalar2=1.0, op0=ALU.mult, op1=ALU.add), [r_lbc], [r_oml])
        V(lambda e: e.tensor_scalar(out=noml[:], in0=lbc[:], scalar1=1.0, scalar2=-1.0, op0=ALU.mult, op1=ALU.add), [r_lbc], [r_noml])
        c.close()

    def p0(l):
        c = Ctx(S)
        crow, r_crow = c.sb([1, D], F32)
        cactc, r_cactc = c.sb([128, 8], F32)
        modrow, r_mod = c.sb([1, 6 * D], F32)
        brow, r_brow = c.sb([1, 6 * D], F32)
        g1row, r_g1 = c.sb([1, D], F32)
        g2row, r_g2 = c.sb([1, D], F32)
        wring = c.ring(2, [128, 8, 512], F32)
        pcol, r_pcol = c.ps([128, 16], F32)
        pacc = c.ring(2, [128, 512], F32, psum=True)
        ld(crow[:], dr["c"], (), [r_crow], "sp")
        ld(brow[:], dr["b_ada"][l:l + 1, :], (), [r_brow], "sp")
        ld(g1row[:], dr["norm1_g"][l:l + 1, :], (), [r_g1], "sp")
        ld(g2row[:], dr["norm2_g"][l:l + 1, :], (), [r_g2], "sp")
        A(lambda e: e.activation(out=crow[:], in_=crow[:], func=AF.Silu), [r_crow], [r_crow])
        for kc in range(8):
            PE(lambda e, kc=kc: e.matmul(pcol[:, kc:kc + 1], lhsT=crow[0:1, kc * 128:(kc + 1) * 128], rhs=one11[0:1, 0:1],
                                         start=True, stop=True), [r_crow, r_one11], [r_pcol])
        V(lambda e: e.tensor_copy(out=cactc[:], in_=pcol[:, 0:8]), [r_pcol], [r_cactc])
        for n in range(12):
            wt, r_wt = wring.next()
            ld(wt[:], dr["w_ada"][l, :, n * 512:(n + 1) * 512].rearrange("(kc p) n -> p kc n", p=128), (), [r_wt])
            acc, r_acc = pacc.next()
            for kc in range(8):
                PE(lambda e, kc=kc, acc=acc, wt=wt: e.matmul(acc[0:1, :], lhsT=cactc[:, kc:kc + 1], rhs=wt[:, kc, :],
                                                            start=(kc == 0), stop=(kc == 7)), [r_cactc, r_wt], [r_acc])
            V(lambda e, n=n, acc=acc: e.tensor_tensor(out=modrow[0:1, n * 512:(n + 1) * 512], in0=acc[0:1, :],
                                                       in1=brow[0:1, n * 512:(n + 1) * 512], op=ALU.add), [r_acc, r_brow], [r_mod])
        V(lambda e: e.scalar_tensor_tensor(out=g1row[:], in0=modrow[0:1, D:2 * D], scalar=1.0, in1=g1row[:], op0=ALU.add, op1=ALU.mult),
          [r_mod, r_g1], [r_g1])
        V(lambda e: e.scalar_tensor_tensor(out=g2row[:], in0=modrow[0:1, 4 * D:5 * D], scalar=1.0, in1=g2row[:], op0=ALU.add, op1=ALU.mult),
          [r_mod, r_g2], [r_g2])
        for kc in range(8):
            PE(lambda e, kc=kc: e.matmul(pcol[:, kc:kc + 1], lhsT=g1row[0:1, kc * 128:(kc + 1) * 128], rhs=one11[0:1, 0:1],
                                         start=True, stop=True), [r_g1, r_one11], [r_pcol])
            PE(lambda e, kc=kc: e.matmul(pcol[:, 8 + kc:9 + kc], lhsT=modrow[0:1, kc * 128:(kc + 1) * 128], rhs=one11[0:1, 0:1],
                                         start=True, stop=True), [r_mod, r_one11], [r_pcol])
        V(lambda e: e.tensor_copy(out=colf[:], in_=pcol[:]), [r_pcol], [r_colf])
        for (src, r_src, off, dst, r_dst) in ((modrow, r_mod, 2 * D, G1b, r_G1b), (g2row, r_g2, 0, A2b, r_A2b),
                                              (modrow, r_mod, 3 * D, S2b, r_S2b), (modrow, r_mod, 5 * D, G2b, r_G2b)):
            for nch in range(2):
                acc, r_acc = pacc.next()
                PE(lambda e, acc=acc, src=src, off=off, nch=nch: e.matmul(acc[:, :], lhsT=onesrow[0:1, :],
                                                                         rhs=src[0:1, off + nch * 512:off + (nch + 1) * 512],
                                                                         start=True, stop=True), [r_src, r_onesrow], [r_acc])
                V(lambda e, acc=acc, dst=dst, nch=nch: e.tensor_copy(out=dst[:, nch * 512:(nch + 1) * 512], in_=acc[:, :]), [r_acc], [r_dst])
        c.close()

    def rstd_from_ss(ss_ap, out_ap, n, r_ss, r_out):
        V(lambda e: e.tensor_scalar(out=out_ap, in0=ss_ap, scalar1=1.0 / n, scalar2=EPS, op0=ALU.mult, op1=ALU.add), [r_ss], [r_out])
        A(lambda e: e.activation(out=out_ap, in_=out_ap, func=AF.Sqrt), [r_out], [r_out])
        V(lambda e: e.reciprocal(out=out_ap, in_=out_ap), [r_out], [r_out])

    def p1(l, xin, r_xin):
        c = Ctx(S)
        winb, r_winb = c.sb([128, 8, 3584], BF16, "winb")
        wst = c.ring(3, [128, 1792], F32, name="wst")
        kk_ = 0
        for kc in range(8):
            for hf in range(2):
                st, r_st = wst.next()
                ld(st[:], dr["w_in"][l, kc * 128:(kc + 1) * 128, hf * 1792:(hf + 1) * 1792], (), [r_st])
                kk_ += 1
                if kk_ % 2 == 0:
                    V(lambda e, st=st, kc=kc, hf=hf: e.tensor_copy(out=winb[:, kc, hf * 1792:(hf + 1) * 1792], in_=st[:]), [r_st], [r_winb])
                else:
                    GP(lambda e, st=st, kc=kc, hf=hf: e.tensor_copy(out=winb[:, kc, hf * 1792:(hf + 1) * 1792], in_=st[:]), [r_st], [r_winb])
        xring = c.ring(6, [128, D], F32, name="xt")
        junk, r_junk = c.sb([128, D], BF16, "junk")
        xnring = c.ring(6, [128, D], BF16, name="xn")
        ss, r_ss = c.sb([128, NT], F32, "ss")
        rstd, r_rstd = c.sb([128, NT], F32, "rstd")
        hTring = c.ring(2, [128, 8, 512], BF16, name="hT")
        ptr = c.ring(2, [128, 8, 128], BF16, psum=True, name="ptr")
        pacc = c.ring(6, [128, 512], F32, psum=True, name="pacc")
        stb = c.ring(6, [128, 512], BF16, name="stb")
        stf = c.ring(4, [128, 512], F32, name="stf")
        V(lambda e: e.memset(ss[:], 0.0), (), [r_ss])
        fm = []
        for m in range(4):
            fm.append((m * 128, "QT", m * 128, "q"))
        for m in range(4):
            fm.append((512 + m * 128, "KT", m * 128, "b"))
        for m in range(4):
            fm.append((1536 + m * 128, "RQT", m * 128, "b"))
        for m in range(4):
            fm.append((2048 + m * 128, "ZT", m * 128, "f"))
        tm = [(1024, "VV", "b"), (2560, "RI", "b"), (3072, "RG", "s")]
        ev = [0]

        def prep(g):
            hT, r_hT = hTring.next()
            tiles = []
            for j in range(4):
                t = g * 4 + j
                xt, r_xt = xring.next()
                ld(xt[:], xin[t * 128:(t + 1) * 128, :], [r_xin], [r_xt])
                A(lambda e, xt=xt, t=t: e.activation(out=junk[:], in_=xt[:], func=AF.Square, accum_out=ss[:, t:t + 1]), [r_xt], [r_junk, r_ss])
                rstd_from_ss(ss[:, t:t + 1], rstd[:, t:t + 1], D, r_ss, r_rstd)
                xn, r_xn = xnring.next()
                A(lambda e, xt=xt, xn=xn, t=t: e.activation(out=xn[:], in_=xt[:], func=AF.Copy, scale=rstd[:, t:t + 1]), [r_xt, r_rstd], [r_xn])
                tiles.append((xn, r_xn))
            for jp in range(2):
                pair = [(tiles[2 * jp + i], ptr.next()) for i in range(2)]
                for kc in range(8):
                    for ((xn, r_xn), (pt, r_pt)) in pair:
                        PE(lambda e, kc=kc, pt=pt, xn=xn: e.transpose(out=pt[:, kc, :], in_=xn[:, kc * 128:(kc + 1) * 128], identity=ident[:]),
                           [r_xn, r_ident], [r_pt])
                for i, ((xn, r_xn), (pt, r_pt)) in enumerate(pair):
                    j = 2 * jp + i
                    V(lambda e, pt=pt, hT=hT, j=j: e.tensor_tensor(out=hT[:, :, j * 128:(j + 1) * 128], in0=pt[:, :, :],
                                                                  in1=colf[:, 0:8].unsqueeze(2).to_broadcast([128, 8, 128]), op=ALU.mult),
                      [r_pt, r_colf], [r_hT])
                    GP(lambda e, hT=hT, j=j: e.tensor_tensor(out=hT[:, :, j * 128:(j + 1) * 128], in0=hT[:, :, j * 128:(j + 1) * 128],
                                                            in1=colf[:, 8:16].unsqueeze(2).to_broadcast([128, 8, 128]), op=ALU.add),
                       [r_hT, r_colf], [r_hT])
            return hT, r_hT

        def evac(kind, acc, r_acc):
            st, r_st = (stf if kind == "f" else stb).next()
            ev[0] += 1
            if kind == "q":
                A(lambda e: e.activation(out=st[:], in_=acc[:], func=AF.Copy, scale=0.125), [r_acc], [r_st])
            elif kind == "s":
                A(lambda e: e.activation(out=st[:], in_=acc[:], func=AF.Silu), [r_acc], [r_st])
            elif ev[0] % 2 == 0:
                A(lambda e: e.activation(out=st[:], in_=acc[:], func=AF.Copy), [r_acc], [r_st])
            else:
                V(lambda e: e.tensor_copy(out=st[:], in_=acc[:]), [r_acc], [r_st])
            return st, r_st

        nxt = prep(0)
        for g in range(NT // 4):
            hT, r_hT = nxt
            if g + 1 < NT // 4:
                nxt = prep(g + 1)
            for f0 in range(0, 16, 4):
                grp = [(fm[f0 + i], pacc.next()) for i in range(4)]
                for kc in range(8):
                    for ((c0, dn, row0, kind), (acc, r_acc)) in grp:
                        PE(lambda e, kc=kc, acc=acc, hT=hT, c0=c0: e.matmul(acc[:, :], lhsT=winb[:, kc, c0:c0 + 128], rhs=hT[:, kc, :],
                                                                           start=(kc == 0), stop=(kc == 7)), [r_winb, r_hT], [r_acc])
                for ((c0, dn, row0, kind), (acc, r_acc)) in grp:
                    st, r_st = evac(kind, acc, r_acc)
                    ld(dr[dn][row0:row0 + 128, g * 512:(g + 1) * 512], st[:], [r_st], [rs[dn]])
            for j in range(4):
                t = g * 4 + j
                grp = [(tm[i], pacc.next()) for i in range(3)]
                for kc in range(8):
                    for ((c0, dn, kind), (acc, r_acc)) in grp:
                        PE(lambda e, kc=kc, acc=acc, hT=hT, c0=c0, j=j: e.matmul(acc[:, :], lhsT=hT[:, kc, j * 128:(j + 1) * 128],
                                                                                 rhs=winb[:, kc, c0:c0 + 512], start=(kc == 0), stop=(kc == 7)),
                           [r_winb, r_hT], [r_acc])
                for ((c0, dn, kind), (acc, r_acc)) in grp:
                    st, r_st = evac(kind, acc, r_acc)
                    ld(dr[dn][t * 128:(t + 1) * 128, :], st[:], [r_st], [rs[dn]])
        c.close()

    def p2(l):
        c = Ctx(S)
        qT2, r_qT2 = c.sb([128, 2, T], BF16, "qT2")
        kT2, r_kT2 = c.sb([128, 2, T], BF16, "kT2")
        amask, r_amask = c.sb([128, 4, 3, 256], BF16, "amask")
        vraw = c.ring(2, [128, 8, 256], BF16, name="vraw")
        vaugr = c.ring(2, [128, 64, 4, 65], BF16, name="vaug")
        pTr = c.ring(8, [128, 256], BF16, name="pT")
        per_ = c.ring(4, [128, 256], BF16, name="pe")
        ostr = c.ring(3, [128, 4, 65], F32, name="ost")
        Sps = c.ring(4, [128, 256], F32, psum=True, name="Sps")
        OpsE = c.ring(2, [128, 2, 65], F32, psum=True, name="OpsE")
        OpsO = c.ring(2, [128, 2, 65], F32, psum=True, name="OpsO")
        ostr = c.ring(4, [128, 4, 65], F32, name="ost2")
        for (vg_, r_vg_) in vaugr.items:
            GP(lambda e, vg_=vg_: e.memset(vg_[:, :, :, 64:65], 1.0), (), [r_vg_])
        for half in range(2):
            ld(amask[:], dr["amask"].rearrange("k (h p q) -> k h p q", h=8, p=3)[:, 4 * half:4 * half + 4], (), [r_amask], "sp")
            for hpi in range(2):
                hp = 2 * half + hpi
                ld(qT2[:, hpi, :], dr["QT"][hp * 128:(hp + 1) * 128, :], [rs["QT"]], [r_qT2])
                ld(kT2[:, hpi, :], dr["KT"][hp * 128:(hp + 1) * 128, :], [rs["KT"]], [r_kT2])
            for p, (w, d) in enumerate(PATTERNS):
                span = 128 * d
                nb = T // span
                vaug, r_vaug = vaugr.next()
                vsrc = dr["VV"].rearrange("(a u r) c -> r u a c", u=128, r=d)
                for r in range(d):
                    for a0 in range(0, nb, 8):
                        na = min(8, nb - a0)
                        vr, r_vr = vraw.next()
                        ld(vr[:, 0:na, :], vsrc[r, :, a0:a0 + na, half * 256:(half + 1) * 256], [rs["VV"]], [r_vr])
                        bi0 = r * nb + a0
                        GP(lambda e, vr=vr, na=na, bi0=bi0, vaug=vaug: e.tensor_copy(out=vaug[:, bi0:bi0 + na, :, 0:64],
                                                                         in_=vr[:, 0:na, :].rearrange("k a (h c) -> k a h c", h=4)),
                           [r_vr], [r_vaug])
                odst = dr[f"OP{p}"].rearrange("(a u r) h c -> r a u h c", u=128, r=d)
                units = [(r, a) for r in range(d) for a in range(nb)]

                def s_stage(r, a, hh, d=d, p=p, span=span, half=half):
                    hpi, pb = hh // 2, 64 * (hh % 2)
                    t0 = span * a + r
                    sp_, r_sp = Sps.next()
                    q_ap = qT2[pb:pb + 64, hpi, t0:t0 + 127 * d + 1:d]
                    c0 = 0 if a > 0 else 128
                    th = []
                    if a > 0:
                        tp = t0 - span
                        th.append(lambda: PE(lambda e: e.matmul(sp_[:, 0:128], lhsT=kT2[pb:pb + 64, hpi, tp:tp + 127 * d + 1:d], rhs=q_ap,
                                                                start=True, stop=True), [r_kT2, r_qT2], [r_sp]))
                    th.append(lambda: PE(lambda e: e.matmul(sp_[:, 128:256], lhsT=kT2[pb:pb + 64, hpi, t0:t0 + 127 * d + 1:d], rhs=q_ap,
                                                            start=True, stop=True), [r_kT2, r_qT2], [r_sp]))
                    pe, r_pe = per_.next()
                    pT, r_pT = pTr.next()

                    def post():
                        A(lambda e: e.activation(out=pe[:, c0:256], in_=sp_[:, c0:256], func=AF.Exp), [r_sp], [r_pe])
                        V(lambda e: e.tensor_tensor(out=pT[:, c0:256], in0=pe[:, c0:256], in1=amask[:, hh, p, c0:256], op=ALU.mult),
                          [r_pe, r_amask], [r_pT])
                    return th, post, pT, r_pT

                def pv_thunks(r, a, hh, pT, r_pT, O, r_O, nb=nb, vaug=vaug, r_vaug=r_vaug):
                    bi = r * nb + a
                    hs = hh // 2
                    th = []
                    if a > 0:
                        th.append(lambda: PE(lambda e: e.matmul(O[:, hs, :], lhsT=pT[:, 0:128], rhs=vaug[:, bi - 1, hh, :], start=True, stop=False),
                                             [r_pT, r_vaug], [r_O]))
                    th.append(lambda: PE(lambda e: e.matmul(O[:, hs, :], lhsT=pT[:, 128:256], rhs=vaug[:, bi, hh, :], start=(a == 0), stop=True),
                                         [r_pT, r_vaug], [r_O]))
                    return th

                def interleave(lists):
                    out_ = []
                    n = max(len(x) for x in lists) if lists else 0
                    for i in range(n):
                        for x in lists:
                            if i < len(x):
                                out_.append(x[i])
                    return out_

                def finish_block(r, a, OE, r_OE, OO, r_OO):
                    ost, r_ost = ostr.next()
                    V(lambda e: e.tensor_copy(out=ost[:, 0:4:2, :], in_=OE[:]), [r_OE], [r_ost])
                    V(lambda e: e.tensor_copy(out=ost[:, 1:4:2, :], in_=OO[:]), [r_OO], [r_ost])
                    ld(odst[r, a, :, half, :], ost[:].rearrange("k h c -> k (h c)"), [r_ost], [rs[f"OP{p}"]])

                flat = [(r, a, hh) for (r, a) in units for hh in range(4)]
                pairs = [flat[i:i + 2] for i in range(0, len(flat), 2)]
                Ocur = {}
                pvq = []

                def merge2(a_, b_):
                    out_ = []
                    ia = ib = 0
                    while ia < len(a_) or ib < len(b_):
                        out_ += a_[ia:ia + 2]
                        ia += 2
                        out_ += b_[ib:ib + 2]
                        ib += 2
                    return out_

                for pr in pairs:
                    st = [s_stage(*u) for u in pr]
                    s_th = interleave([x[0] for x in st])
                    if len(pvq) >= 2:
                        pv_now, fin_now = pvq.pop(0)
                    else:
                        pv_now, fin_now = [], []
                    for f_ in merge2(s_th, pv_now):
                        f_()
                    for fb in fin_now:
                        finish_block(*fb)
                    for x in st:
                        x[1]()
                    fin = []
                    pvl = []
                    for (r, a, hh), x in zip(pr, st):
                        if hh == 0:
                            Ocur[(r, a)] = (OpsE.next(), OpsO.next())
                        (OE, r_OE), (OO, r_OO) = Ocur[(r, a)]
                        O, r_O = (OE, r_OE) if hh % 2 == 0 else (OO, r_OO)
                        pvl.append(pv_thunks(r, a, hh, x[2], x[3], O, r_O))
                        if hh == 3:
                            fin.append((r, a, OE, r_OE, OO, r_OO))
                            del Ocur[(r, a)]
                    pvq.append((interleave(pvl), fin))
                for (pv_now, fin_now) in pvq:
                    for f_ in pv_now:
                        f_()
                    for fb in fin_now:
                        finish_block(*fb)
        c.close()

    def p2b(l):
        c = Ctx(S)
        angb, r_angb = c.sb([128, 512], F32, "angb")
        ld(angb[:], dr["attn_norm_g"][l:l + 1, :].partition_broadcast(128), (), [r_angb], "sp")
        opr = [c.ring(5, [128, 8, 65], F32, name=f"op{p}") for p in range(3)]
        den, r_den = c.sb([128, 8], F32, "den")
        o, r_o = c.sb([128, 8, 64], F32, "o")
        junk, r_junk = c.sb([128, 512], BF16, "junk")
        ss, r_ss = c.sb([128, NT], F32, "ss")
        rstd, r_rstd = c.sb([128, NT], F32, "rstd")
        cst = c.ring(3, [128, 512], BF16, name="cst")
        V(lambda e: e.memset(ss[:], 0.0), (), [r_ss])
        pend_ = {}

        def loads(t):
            tl = []
            for p in range(3):
                tt, r_tt = opr[p].next()
                ld(tt[:].rearrange("k h c -> k (h c)"), dr[f"OP{p}"][t * 128:(t + 1) * 128].rearrange("k a c -> k (a c)"), [rs[f"OP{p}"]], [r_tt])
                tl.append((tt, r_tt))
            pend_[t] = tl
        for t in range(min(3, NT)):
            loads(t)
        for t in range(NT):
            tl = pend_.pop(t)
            if t + 3 < NT:
                loads(t + 3)
            (t0_, r0), (t1_, r1), (t2_, r2) = tl
            V(lambda e, a=t0_, b=t1_: e.tensor_tensor(out=a[:], in0=a[:], in1=b[:], op=ALU.add), [r0, r1], [r0])
            V(lambda e, a=t0_, b=t2_: e.tensor_tensor(out=a[:], in0=a[:], in1=b[:], op=ALU.add), [r0, r2], [r0])
            V(lambda e, a=t0_: e.reciprocal(out=den[:], in_=a[:, :, 64]), [r0], [r_den])
            V(lambda e, a=t0_: e.tensor_tensor(out=o[:], in0=a[:, :, 0:64], in1=den[:].unsqueeze(2).to_broadcast([128, 8, 64]), op=ALU.mult),
              [r0, r_den], [r_o])
            A(lambda e, t=t: e.activation(out=junk[:], in_=o[:].rearrange("k h c -> k (h c)"), func=AF.Square, accum_out=ss[:, t:t + 1]),
              [r_o], [r_junk, r_ss])
            rstd_from_ss(ss[:, t:t + 1], rstd[:, t:t + 1], 512, r_ss, r_rstd)
            cs, r_cs = cst.next()
            V(lambda e, cs=cs, t=t: e.scalar_tensor_tensor(out=cs[:], in0=o[:].rearrange("k h c -> k (h c)"), scalar=rstd[:, t:t + 1],
                                                          in1=angb[:], op0=ALU.mult, op1=ALU.mult), [r_o, r_rstd, r_angb], [r_cs])
            ld(dr["CAT"][t * 128:(t + 1) * 128, 0:512], cs[:], [r_cs], [rs["CAT"]])
        c.close()

    def p3(l):
        c = Ctx(S)
        SEG = 512
        NCK = SEG // 128
        NSEG = T // SEG
        resetm, r_resetm = c.sb([128, SEG], F32, "resetm")
        gnb, r_gnb = c.sb([128, 512], F32, "gnb")
        ld(resetm[:], dr["resetm"][:, 0:SEG], (), [r_resetm], "sp")
        ld(gnb[:], dr["hgrn_norm_g"][l:l + 1, :].partition_broadcast(128), (), [r_gnb], "sp")
        rir = c.ring(2, [128, NCK, 512], BF16, name="ri")
        rgr = c.ring(2, [128, NCK, 512], BF16, name="rgm")
        catr_ = c.ring(2, [128, NCK, 512], BF16, name="catseg")
        zr = c.ring(2, [128, SEG], F32, name="z")
        qr = c.ring(2, [128, SEG], BF16, name="q")
        tset = [[c.sb([128, SEG], F32, f"b{n}{i}") for n in "ABCE"] for i in range(2)]
        o4 = c.ring(8, [128, 5, SEG], BF16, name="o4")
        dcyr = c.ring(8, [128, NCK], F32, name="dcy")
        St = [c.sb([128, 128], F32, f"S{h}") for h in range(4)]
        Sb = [c.sb([128, 128], BF16, f"Sb{h}") for h in range(4)]
        Amr = c.ring(4, [128, 128], BF16, name="Am")
        khTr = c.ring(4, [128, 128], BF16, name="khT")
        junk, r_junk = c.sb([128, 128], BF16, "junk")
        ssr, r_ssr = c.sb([128, 4 * NT], F32, "ssr")
        rsr, r_rsr = c.sb([128, 4 * NT], F32, "rsr")
        psA = c.ring(2, [128, 128], F32, psum=True, name="psA")
        psT = c.ring(1, [128, 128], BF16, psum=True, name="psT")
        psU = c.ring(2, [128, 128], F32, psum=True, name="psU")
        psO = c.ring(2, [128, 128], F32, psum=True, name="psO")
        psX = c.ring(1, [64, 64], F32, psum=True, name="psX")
        V(lambda e: e.memset(ssr[:], 0.0), (), [r_ssr])
        for h in range(4):
            V(lambda e, h=h: e.memset(St[h][0][:], 0.0), (), [St[h][1]])
            V(lambda e, h=h: e.memset(Sb[h][0][:], 0.0), (), [Sb[h][1]])

        def v3(t):
            t = t if isinstance(t, bass.AP) else t[:]
            return t.rearrange("k (c u) -> k c u", u=128)

        def v64(t):
            return t[:].rearrange("k (c u) -> k c u", u=64)

        def ew(sg):
            tk0 = sg * SEG
            ri, r_ri = rir.next()
            rgm, r_rgm = rgr.next()
            ld(ri[:], dr["RI"][tk0:tk0 + SEG, :].rearrange("(c p) f -> p c f", p=128), [rs["RI"]], [r_ri])
            ld(rgm[:], dr["RG"][tk0:tk0 + SEG, :].rearrange("(c p) f -> p c f", p=128), [rs["RG"]], [r_rgm])
            GP(lambda e: e.tensor_tensor(out=rgm[:], in0=rgm[:], in1=gnb[:].unsqueeze(1).to_broadcast([128, NCK, 512]), op=ALU.mult),
               [r_rgm, r_gnb], [r_rgm])
            heads = []

            def head_ops(hd):
                (bA, r_A), (bB, r_B), (bC, r_C), (bE, r_E) = tset[hd % 2]
                ops = []
                AA = lambda *a: ops.append(lambda: A(*a))
                VV = lambda *a: ops.append(lambda: V(*a))
                GG = lambda *a: ops.append(lambda: GP(*a))
                z, r_z = zr.next()
                q, r_q = qr.next()
                ops.append(lambda: ld(z[:], dr["ZT"][hd * 128:(hd + 1) * 128, tk0:tk0 + SEG], [rs["ZT"]], [r_z]))
                ops.append(lambda: ld(q[:], dr["RQT"][hd * 128:(hd + 1) * 128, tk0:tk0 + SEG], [rs["RQT"]], [r_q]))
                o, r_o4 = o4.next()
                dcy, r_dcy = dcyr.next()
                lb_ap, oml_ap = lbc[:, l, hd:hd + 1], oml[:, l, hd:hd + 1]

                def b3(t):
                    return t[:].rearrange("k (c u) -> k c u", u=128)

                def b64(t):
                    return t[:].rearrange("k (c u) -> k c u", u=64)
                VV(lambda e: e.tensor_scalar(out=z[:], in0=z[:], scalar1=-60.0, scalar2=None, op0=ALU.max), [r_z], [r_z])
                AA(lambda e: e.activation(out=bA[:], in_=z[:], func=AF.Exp, scale=-1.0), [r_z], [r_A])
                AA(lambda e: e.activation(out=bB[:], in_=bA[:], func=AF.Ln, bias=1.0), [r_A], [r_B])
                AA(lambda e: e.activation(out=bE[:], in_=bA[:], func=AF.Ln, bias=1.0, scale=lb_ap), [r_A, r_lbc], [r_E])
                VV(lambda e: e.tensor_tensor(out=bE[:], in0=bE[:], in1=bB[:], op=ALU.subtract), [r_E, r_B], [r_E])
                AA(lambda e: e.activation(out=bB[:], in_=bB[:], func=AF.Exp, scale=-1.0), [r_B], [r_B])
                VV(lambda e: e.scalar_tensor_tensor(out=bC[:], in0=bA[:], scalar=oml_ap, in1=bB[:], op0=ALU.mult, op1=ALU.mult),
                   [r_A, r_B, r_oml], [r_C])
                VV(lambda e: e.tensor_tensor_scan(out=bB[:], data0=resetm[:], data1=bE[:], initial=0.0, op0=ALU.mult, op1=ALU.add),
                   [r_resetm, r_E], [r_B])
                VV(lambda e: e.tensor_tensor(out=b64(bE), in0=b64(bB), in1=b64(bB)[:, :, 31:32].to_broadcast([128, 2 * NCK, 64]), op=ALU.subtract),
                   [r_B], [r_E])
                VV(lambda e: e.tensor_scalar(out=bE[:], in0=bE[:], scalar1=-80.0, scalar2=80.0, op0=ALU.max, op1=ALU.min), [r_E], [r_E])
                AA(lambda e: e.activation(out=bA[:], in_=bE[:], func=AF.Exp), [r_E], [r_A])
                VV(lambda e: e.tensor_tensor(out=o[:, 0, :], in0=q[:], in1=bA[:], op=ALU.mult), [r_q, r_A], [r_o4])
                AA(lambda e: e.activation(out=bA[:], in_=bE[:], func=AF.Exp, scale=-1.0), [r_E], [r_A])
                VV(lambda e: e.tensor_tensor(out=o[:, 1, :], in0=bC[:], in1=bA[:], op=ALU.mult), [r_C, r_A], [r_o4])
                AA(lambda e: e.activation(out=bA[:], in_=bB[:], func=AF.Exp), [r_B], [r_A])
                GG(lambda e: e.tensor_tensor(out=o[:, 2, :], in0=q[:], in1=bA[:], op=ALU.mult), [r_q, r_A], [r_o4])
                VV(lambda e: e.tensor_tensor(out=b3(bE), in0=b3(bB), in1=b3(bB)[:, :, 127:128].to_broadcast([128, NCK, 128]), op=ALU.subtract),
                   [r_B], [r_E])
                AA(lambda e: e.activation(out=bA[:], in_=bE[:], func=AF.Exp, scale=-1.0), [r_E], [r_A])
                GG(lambda e: e.tensor_tensor(out=o[:, 3, :], in0=bC[:], in1=bA[:], op=ALU.mult), [r_C, r_A], [r_o4])
                VV(lambda e: e.tensor_tensor(out=b3(bE), in0=b3(bB), in1=b3(bB)[:, :, 63:64].to_broadcast([128, NCK, 128]), op=ALU.subtract),
                   [r_B], [r_E])
                VV(lambda e: e.scalar_tensor_tensor(out=bE[:], in0=bE[:], scalar=-1.0, in1=bE[:], op0=ALU.mult, op1=ALU.min), [r_E], [r_E])
                AA(lambda e: e.activation(out=bA[:], in_=bE[:], func=AF.Exp), [r_E], [r_A])
                VV(lambda e: e.tensor_tensor(out=v3(o[:, 4, :])[:, :, 0:64], in0=b3(bC)[:, :, 0:64], in1=b3(bA)[:, :, 0:64], op=ALU.mult),
                   [r_C, r_A], [r_o4])
                VV(lambda e: e.tensor_tensor(out=v3(o[:, 4, :])[:, :, 64:128], in0=v3(q)[:, :, 64:128], in1=b3(bA)[:, :, 64:128], op=ALU.mult),
                   [r_q, r_A], [r_o4])
                AA(lambda e: e.activation(out=dcy[:], in_=b3(bB)[:, :, 127], func=AF.Exp), [r_B], [r_dcy])
                heads.append((o, r_o4, dcy, r_dcy))
                return ops

            bg = []
            for hp in range(2):
                la = head_ops(2 * hp)
                lb_ = head_ops(2 * hp + 1)
                for i in range(max(len(la), len(lb_))):
                    if i < len(la):
                        bg.append(la[i])
                    if i < len(lb_):
                        bg.append(lb_[i])
            return dict(sg=sg, ri=(ri, r_ri), rgm=(rgm, r_rgm), heads=heads), bg

        def chunks(st, bg):
            sg = st["sg"]
            per_ci = -(-len(bg) // NCK) if bg else 0
            tk0 = sg * SEG
            ri, r_ri = st["ri"]
            rgm, r_rgm = st["rgm"]
            catseg, r_catseg = catr_.next()
            for ci_ in range(NCK):
                one_chunk(st, ci_, sg, ri, r_ri, rgm, r_rgm, catseg, r_catseg)
                for f_ in bg[ci_ * per_ci:(ci_ + 1) * per_ci]:
                    f_()
            ld(dr["CAT"][tk0:tk0 + SEG, 512:1024].rearrange("(c p) f -> p c f", p=128), catseg[:], [r_catseg], [rs["CAT"]])

        def one_chunk(st, ci, sg, ri, r_ri, rgm, r_rgm, catseg, r_catseg):
            if True:
                cs = slice(ci * 128, (ci + 1) * 128)
                g0 = (sg * NCK + ci) * 4
                per = []
                for hd in range(4):
                    o, r_o4, dcy, r_dcy = st["heads"][hd]
                    pa, r_pa = psA.next()
                    PE(lambda e, pa=pa, o=o: e.matmul(pa[:, :], lhsT=o[:, 1, cs], rhs=o[:, 0, cs], start=True, stop=True), [r_o4], [r_pa])
                    px, r_px = psX.next()
                    PE(lambda e, px=px, o=o: e.matmul(px[0:64, :], lhsT=o[:, 4, ci * 128:ci * 128 + 64], rhs=o[:, 4, ci * 128 + 64:(ci + 1) * 128],
                                                      start=True, stop=True), [r_o4], [r_px])
                    pt, r_pt = psT.next()
                    PE(lambda e, pt=pt, o=o: e.transpose(out=pt[:, :], in_=o[:, 3, cs], identity=ident[:]), [r_o4, r_ident], [r_pt])
                    Am, r_Am = Amr.next()
                    GP(lambda e, Am=Am: e.memset(Am[:], 0.0), (), [r_Am])
                    V(lambda e, Am=Am, pa=pa: e.copy_predicated(out=Am[:], mask=causal[:], data=pa[:]), [r_pa, r_causal, r_Am], [r_Am])
                    A(lambda e, Am=Am, px=px: e.activation(out=Am[0:64, 64:128], in_=px[0:64, :], func=AF.Copy), [r_px, r_Am], [r_Am])
                    khT, r_khT = khTr.next()
                    A(lambda e, khT=khT, pt=pt: e.activation(out=khT[:], in_=pt[:], func=AF.Copy), [r_pt], [r_khT])
                    per.append((Am, r_Am, khT, r_khT))
                pos_ = []
                for hd in range(4):
                    o, r_o4, dcy, r_dcy = st["heads"][hd]
                    Am, r_Am, khT, r_khT = per[hd]
                    Sh, r_Sh = St[hd]
                    Sbh, r_Sbh = Sb[hd]
                    v_ap = ri[:, ci, hd * 128:(hd + 1) * 128]
                    pu, r_pu = psU.next()
                    PE(lambda e, pu=pu, khT=khT, v_ap=v_ap: e.matmul(pu[:, :], lhsT=khT[:], rhs=v_ap, start=True, stop=True), [r_khT, r_ri], [r_pu])
                    po, r_po = psO.next()
                    PE(lambda e, po=po, Am=Am, v_ap=v_ap: e.matmul(po[:, :], lhsT=Am[:], rhs=v_ap, start=True, stop=False), [r_Am, r_ri], [r_po])
                    PE(lambda e, po=po, o=o, Sbh=Sbh: e.matmul(po[:, :], lhsT=o[:, 2, cs], rhs=Sbh[:], start=False, stop=True),
                       [r_o4, r_Sbh], [r_po])
                    V(lambda e, Sh=Sh, pu=pu, dcy=dcy: e.scalar_tensor_tensor(out=Sh[:], in0=Sh[:], scalar=dcy[:, ci:ci + 1], in1=pu[:],
                                                                             op0=ALU.mult, op1=ALU.add), [r_Sh, r_pu, r_dcy], [r_Sh])
                    A(lambda e, Sh=Sh, Sbh=Sbh: e.activation(out=Sbh[:], in_=Sh[:], func=AF.Copy), [r_Sh], [r_Sbh])
                    A(lambda e, po=po, gi=g0 + hd: e.activation(out=junk[:], in_=po[:], func=AF.Square, accum_out=ssr[:, gi:gi + 1]),
                      [r_po], [r_junk, r_ssr])
                    rstd_from_ss(ssr[:, g0 + hd:g0 + hd + 1], rsr[:, g0 + hd:g0 + hd + 1], 128, r_ssr, r_rsr)
                    V(lambda e, po=po, gi=g0 + hd, hd=hd: e.scalar_tensor_tensor(out=catseg[:, ci, hd * 128:(hd + 1) * 128], in0=po[:],
                                                                                scalar=rsr[:, gi:gi + 1], in1=rgm[:, ci, hd * 128:(hd + 1) * 128],
                                                                                op0=ALU.mult, op1=ALU.mult), [r_po, r_rsr, r_rgm], [r_catseg])

        nxt, bg0 = ew(0)
        for f_ in bg0:
            f_()
        for sg in range(NSEG):
            cur = nxt
            bgn = []
            if sg + 1 < NSEG:
                nxt, bgn = ew(sg + 1)
            chunks(cur, bgn)
        c.close()

    def p4(l, xin, r_xin):
        c = Ctx(S)
        woutb, r_woutb = c.sb([128, 8, D], BF16, "woutb")
        wst = c.ring(2, [128, 4, D], F32, name="wst")
        for hf in range(2):
            st, r_st = wst.next()
            ld(st[:], dr["w_out"][l, hf * 512:(hf + 1) * 512, :].rearrange("(kc p) n -> p kc n", p=128), (), [r_st])
            (V if hf == 0 else GP)(lambda e, st=st, hf=hf: e.tensor_copy(out=woutb[:, hf * 4:(hf + 1) * 4, :], in_=st[:]), [r_st], [r_woutb])
        wrs, r_wrs = c.sb([128, 8, 36], F32, "wrs")
        wrb, r_wrb = c.sb([128, 8, 36], BF16, "wrb")
        rbb, r_rbb = c.sb([128, 36], F32, "rbb")
        S.dma("sp", lambda e: e.dma_start(out=wrs[:], in_=dr["router_w"][l].rearrange("(kc p) n -> p kc n", p=128),
                                          allow_slow_non_contiguous=True), (), [r_wrs])
        V(lambda e: e.tensor_copy(out=wrb[:], in_=wrs[:]), [r_wrs], [r_wrb])
        ld(rbb[:], dr["router_b"][l:l + 1, :].partition_broadcast(128), (), [r_rbb], "sp")
        catr = c.ring(4, [128, D], BF16, name="cat")
        catTr = c.ring(3, [128, 8, 128], BF16, name="catT")
        xr = c.ring(4, [128, D], F32, name="x")
        x1r = c.ring(3, [128, D], F32, name="x1")
        h2r = c.ring(5, [128, D], BF16, name="h2")
        h2fr = c.ring(2, [128, D], F32, name="h2f")
        h2Tr = c.ring(3, [128, 8, 128], BF16, name="h2T")
        junk, r_junk = c.sb([128, D], BF16, "junk")
        ss, r_ss = c.sb([128, NT], F32, "ss")
        rstd, r_rstd = c.sb([128, NT], F32, "rstd")
        ptr = c.ring(2, [128, 8, 128], BF16, psum=True, name="ptr")
        pacc = c.ring(4, [128, 512], F32, psum=True, name="pacc")
        plg = c.ring(2, [128, 36], F32, psum=True, name="plg")
        V(lambda e: e.memset(ss[:], 0.0), (), [r_ss])
        st_ = {}

        def loads(t):
            ct, r_ct = catr.next()
            ld(ct[:], dr["CAT"][t * 128:(t + 1) * 128, :], [rs["CAT"]], [r_ct])
            xt, r_xt = xr.next()
            ld(xt[:], xin[t * 128:(t + 1) * 128, :], [r_xin], [r_xt])
            st_[t] = dict(ct=(ct, r_ct), xt=(xt, r_xt))

        def transposes(ta, td):
            jobs = []
            if ta is not None:
                pt, r_pt = ptr.next()
                ct, r_ct = st_[ta]["ct"]
                jobs.append([(lambda kc=kc, pt=pt, ct=ct, r_ct=r_ct, r_pt=r_pt: PE(
                    lambda e: e.transpose(out=pt[:, kc, :], in_=ct[:, kc * 128:(kc + 1) * 128], identity=ident[:]), [r_ct, r_ident], [r_pt]))
                    for kc in range(8)])
            if td is not None:
                pt2, r_pt2 = ptr.next()
                h2, r_h2 = st_[td]["h2"]
                jobs.append([(lambda kc=kc, pt2=pt2, h2=h2, r_h2=r_h2, r_pt2=r_pt2: PE(
                    lambda e: e.transpose(out=pt2[:, kc, :], in_=h2[:, kc * 128:(kc + 1) * 128], identity=ident[:]), [r_h2, r_ident], [r_pt2]))
                    for kc in range(8)])
            n = max(len(j) for j in jobs)
            for i in range(n):
                for j in jobs:
                    j[i]()
            if ta is not None:
                cT, r_cT = catTr.next()
                A(lambda e, cT=cT, pt=pt: e.activation(out=cT[:], in_=pt[:], func=AF.Copy), [r_pt], [r_cT])
                st_[ta]["cT"] = (cT, r_cT)
            if td is not None:
                hT, r_hT = h2Tr.next()
                A(lambda e, hT=hT, pt2=pt2: e.activation(out=hT[:], in_=pt2[:], func=AF.Copy), [r_pt2], [r_hT])
                st_[td]["hT"] = (hT, r_hT)

        def matmuls(t, tr):
            accs = None
            if t is not None:
                cT, r_cT = st_[t]["cT"]
                accs = [pacc.next(), pacc.next()]
            if tr is not None:
                hT, r_hT = st_[tr]["hT"]
                pl, r_pl = plg.next()
            for kc in range(8):
                if t is not None:
                    for n in range(2):
                        acc, r_acc = accs[n]
                        PE(lambda e, kc=kc, acc=acc, cT=cT, n=n: e.matmul(acc[:, :], lhsT=cT[:, kc, :], rhs=woutb[:, kc, n * 512:(n + 1) * 512],
                                                                         start=(kc == 0), stop=(kc == 7)), [r_cT, r_woutb], [r_acc])
                if tr is not None:
                    PE(lambda e, kc=kc, pl=pl, hT=hT: e.matmul(pl[:, :], lhsT=hT[:, kc, :], rhs=wrb[:, kc, :], start=(kc == 0), stop=(kc == 7)),
                       [r_hT, r_wrb], [r_pl])
            if tr is not None:
                V(lambda e, pl=pl, tr=tr: e.tensor_tensor(out=lgall[:, tr, :], in0=pl[:, :], in1=rbb[:], op=ALU.add), [r_pl, r_rbb], [r_lgall])
                del st_[tr]
            return accs

        def elementwise(t, accs):
            xt, r_xt = st_[t]["xt"]
            x1, r_x1 = x1r.next()
            for n in range(2):
                acc, r_acc = accs[n]
                V(lambda e, acc=acc, x1=x1, n=n: e.tensor_tensor(out=x1[:, n * 512:(n + 1) * 512], in0=acc[:, :], in1=G1b[:, n * 512:(n + 1) * 512],
                                                                op=ALU.mult), [r_acc, r_G1b], [r_x1])
            GP(lambda e, x1=x1, xt=xt: e.tensor_tensor(out=x1[:], in0=x1[:], in1=xt[:], op=ALU.add), [r_x1, r_xt], [r_x1])
            ld(dr["Xs"][t * 128:(t + 1) * 128, :], x1[:], [r_x1], [rs["Xs"]])
            A(lambda e, x1=x1, t=t: e.activation(out=junk[:], in_=x1[:], func=AF.Square, accum_out=ss[:, t:t + 1]), [r_x1], [r_junk, r_ss])
            rstd_from_ss(ss[:, t:t + 1], rstd[:, t:t + 1], D, r_ss, r_rstd)
            h2f_, r_h2f_ = h2fr.next()
            V(lambda e, x1=x1, t=t, h2f_=h2f_: e.scalar_tensor_tensor(out=h2f_[:], in0=x1[:], scalar=rstd[:, t:t + 1], in1=A2b[:], op0=ALU.mult, op1=ALU.mult),
              [r_x1, r_rstd, r_A2b], [r_h2f_])
            h2, r_h2 = h2r.next()
            GP(lambda e, h2=h2, h2f_=h2f_: e.tensor_tensor(out=h2[:], in0=h2f_[:], in1=S2b[:], op=ALU.add), [r_h2f_, r_S2b], [r_h2])
            ld(dr["H2"][t * 128:(t + 1) * 128, :], h2[:], [r_h2], [rs["H2"]])
            st_[t]["h2"] = (h2, r_h2)

        loads(0)
        loads(1)
        transposes(0, None)
        for t in range(NT + 3):
            if t + 2 < NT:
                loads(t + 2)
            ta = t + 1 if t + 1 < NT else None
            td = t - 2 if 0 <= t - 2 < NT else None
            if ta is not None or td is not None:
                transposes(ta, td)
            tm_ = t if t < NT else None
            trr = t - 3 if 0 <= t - 3 < NT else None
            if tm_ is not None or trr is not None:
                accs = matmuls(tm_, trr)
            if tm_ is not None:
                elementwise(tm_, accs)
        c.close()

    def p4b(l):
        c = Ctx(S)
        N3 = [128, NT, NE]
        gmax, r_gmax = c.sb([128, NT], F32)
        g4, r_g4 = c.sb([128, NT, 4], F32)
        goh, r_goh = c.sb([128, NT, 4], F32)
        gsum, r_gsum = c.sb([128, NT], F32)
        em, r_em = c.sb(N3, F32)
        oh1, r_oh1 = c.sb(N3, F32)
        oh2, r_oh2 = c.sb(N3, F32)
        m1, r_m1 = c.sb([128, NT], F32)
        m2, r_m2 = c.sb([128, NT], F32)
        p1, r_p1 = c.sb([128, NT], F32)
        abf, r_abf = c.sb([128, NT * NE], BF16)
        csb, r_csb = c.sb(N3, F32)
        offs, r_offs = c.sb(N3, F32)
        pos, r_pos = c.sb(N3, F32)
        sf, r_sf = c.sb([128, NT], F32)
        cnt, r_cnt = c.sb([128, NE], F32)
        nblk, r_nblk = c.sb([128, NE], F32)
        pendb, r_pendb = c.sb([128, NE], F32)
        pst, r_pst = c.sb([128, NE], F32)
        onesne, r_onesne = c.sb([128, NE], F32)
        bex, r_bex = c.sb([128, NBLK], F32)
        cmp, r_cmp = c.sb([128, NBLK * NE], F32)
        pp = c.ring(1, [128, NT * NE], F32, psum=True)
        pc = c.ring(1, [128, NT * NE], F32, psum=True)
        lgG = lgall[:, :, 0:4]
        lgE = lgall[:, :, 4:36]

        def bc(ap2, n):
            return ap2.unsqueeze(2).to_broadcast([128, NT, n])
        V(lambda e: e.tensor_reduce(out=gmax[:], in_=lgG, axis=AX.X, op=ALU.max), [r_lgall], [r_gmax])
        V(lambda e: e.tensor_tensor(out=goh[:], in0=lgG, in1=bc(gmax[:], 4), op=ALU.is_equal), [r_lgall, r_gmax], [r_goh])
        V(lambda e: e.tensor_tensor(out=g4[:], in0=lgG, in1=bc(gmax[:], 4), op=ALU.subtract), [r_lgall, r_gmax], [r_g4])
        A(lambda e: e.activation(out=g4[:], in_=g4[:], func=AF.Exp), [r_g4], [r_g4])
        V(lambda e: e.tensor_reduce(out=gsum[:], in_=g4[:], axis=AX.X, op=ALU.add), [r_g4], [r_gsum])
        V(lambda e: e.reciprocal(out=gsum[:], in_=gsum[:]), [r_gsum], [r_gsum])
        V(lambda e: e.tensor_scalar(out=goh[:], in0=goh[:], scalar1=BIG, scalar2=-BIG, op0=ALU.mult, op1=ALU.add), [r_goh], [r_goh])
        V(lambda e: e.tensor_tensor(out=em[:].rearrange("k t (g x) -> k t g x", g=4), in0=lgE.rearrange("k t (g x) -> k t g x", g=4),
                                    in1=goh[:].unsqueeze(3).to_broadcast([128, NT, 4, 8]), op=ALU.add), [r_lgall, r_goh], [r_em])
        V(lambda e: e.tensor_reduce(out=m1[:], in_=em[:], axis=AX.X, op=ALU.max), [r_em], [r_m1])
        V(lambda e: e.tensor_tensor(out=oh1[:], in0=em[:], in1=bc(m1[:], NE), op=ALU.is_equal), [r_em, r_m1], [r_oh1])
        V(lambda e: e.scalar_tensor_tensor(out=em[:], in0=oh1[:], scalar=-BIG, in1=em[:], op0=ALU.mult, op1=ALU.add), [r_oh1, r_em], [r_em])
        V(lambda e: e.tensor_reduce(out=m2[:], in_=em[:], axis=AX.X, op=ALU.max), [r_em], [r_m2])
        V(lambda e: e.tensor_tensor(out=oh2[:], in0=em[:], in1=bc(m2[:], NE), op=ALU.is_equal), [r_em, r_m2], [r_oh2])
        V(lambda e: e.tensor_tensor(out=m2[:], in0=m2[:], in1=m1[:], op=ALU.subtract), [r_m1, r_m2], [r_m2])
        A(lambda e: e.activation(out=m2[:], in_=m2[:], func=AF.Exp), [r_m2], [r_m2])
        V(lambda e: e.tensor_scalar(out=p1[:], in0=m2[:], scalar1=1.0, scalar2=None, op0=ALU.add), [r_m2], [r_p1])
        V(lambda e: e.reciprocal(out=p1[:], in_=p1[:]), [r_p1], [r_p1])
        V(lambda e: e.tensor_tensor(out=gt1[:], in0=p1[:], in1=gsum[:], op=ALU.mult), [r_p1, r_gsum], [r_gt1])
        V(lambda e: e.tensor_tensor(out=m2[:], in0=m2[:], in1=gt1[:], op=ALU.mult), [r_m2, r_gt1], [r_m2])
        V(lambda e: e.tensor_copy(out=gt2[:], in_=m2[:]), [r_m2], [r_gt2])
        V(lambda e: e.tensor_tensor(out=abf[:].rearrange("k (t x) -> k t x", x=NE), in0=oh1[:], in1=oh2[:], op=ALU.add), [r_oh1, r_oh2], [r_abf])
        ppt, r_pp = pp.next()
        pct, r_pc = pc.next()
        for ch in range(4):
            PE(lambda e, ch=ch: e.matmul(ppt[:, ch * 512:(ch + 1) * 512], lhsT=ustrict[:], rhs=abf[:, ch * 512:(ch + 1) * 512], start=True, stop=True),
               [r_ustrict, r_abf], [r_pp])
            PE(lambda e, ch=ch: e.matmul(pct[:, ch * 512:(ch + 1) * 512], lhsT=onesb[:], rhs=abf[:, ch * 512:(ch + 1) * 512], start=True, stop=True),
               [r_onesb, r_abf], [r_pc])
        V(lambda e: e.tensor_copy(out=csb[:].rearrange("k t x -> k (t x)"), in_=pct[:]), [r_pc], [r_csb])
        V(lambda e: e.memset(offs[:, 0, :], 0.0), (), [r_offs])
        for j in range(1, NT):
            V(lambda e, j=j: e.tensor_tensor(out=offs[:, j, :], in0=offs[:, j - 1, :], in1=csb[:, j - 1, :], op=ALU.add), [r_offs, r_csb], [r_offs])
        V(lambda e: e.tensor_tensor(out=cnt[:], in0=offs[:, NT - 1, :], in1=csb[:, NT - 1, :], op=ALU.add), [r_offs, r_csb], [r_cnt])
        V(lambda e: e.tensor_tensor(out=cmp[:, 0:NE * 64].rearrange("k (x m) -> k x m", m=64), in0=cnt[:].unsqueeze(2).to_broadcast([128, NE, 64]),
                                    in1=mulrow[:].unsqueeze(1).to_broadcast([128, NE, 64]), op=ALU.is_gt), [r_cnt, r_mulrow], [r_cmp])
        V(lambda e: e.tensor_reduce(out=nblk[:], in_=cmp[:, 0:NE * 64].rearrange("k (x m) -> k x m", m=64), axis=AX.X, op=ALU.add), [r_cmp], [r_nblk])
        V(lambda e: e.memset(onesne[:], 1.0), (), [r_onesne])
        V(lambda e: e.tensor_tensor_scan(out=pendb[:], data0=onesne[:], data1=nblk[:], initial=0.0, op0=ALU.mult, op1=ALU.add),
          [r_onesne, r_nblk], [r_pendb])
        V(lambda e: e.tensor_tensor(out=pst[:], in0=pendb[:], in1=nblk[:], op=ALU.subtract), [r_pendb, r_nblk], [r_pst])
        V(lambda e: e.tensor_scalar(out=pst[:], in0=pst[:], scalar1=float(RB), scalar2=None, op0=ALU.mult), [r_pst], [r_pst])
        V(lambda e: e.tensor_tensor(out=cmp[:, 0:NBLK * NE].rearrange("k (b x) -> k b x", x=NE), in0=pendb[:].unsqueeze(1).to_broadcast([128, NBLK, NE]),
                                    in1=brow[:].unsqueeze(2).to_broadcast([128, NBLK, NE]), op=ALU.is_le), [r_pendb, r_brow], [r_cmp])
        V(lambda e: e.tensor_reduce(out=bex[:], in_=cmp[:, 0:NBLK * NE].rearrange("k (b x) -> k b x", x=NE), axis=AX.X, op=ALU.add), [r_cmp], [r_bex])
        V(lambda e: e.tensor_scalar(out=bex[:], in0=bex[:], scalar1=float(NE - 1), scalar2=float(128), op0=ALU.min, op1=ALU.mult), [r_bex], [r_bex])
        V(lambda e: e.tensor_scalar(out=bex[:], in0=bex[:], scalar1=piota[:, 0:1], scalar2=float(l * NE * 128), op0=ALU.add, op1=ALU.add),
          [r_bex, r_piota], [r_bex])
        V(lambda e: e.tensor_copy(out=widx[:], in_=bex[:]), [r_bex], [r_widx])
        V(lambda e: e.tensor_tensor(out=offs[:], in0=offs[:], in1=pst[:].unsqueeze(1).to_broadcast([128, NT, NE]), op=ALU.add), [r_offs, r_pst], [r_offs])
        V(lambda e: e.tensor_tensor(out=pos[:].rearrange("k t x -> k (t x)"), in0=ppt[:], in1=offs[:].rearrange("k t x -> k (t x)"), op=ALU.add),
          [r_pp, r_offs], [r_pos])
        for (oh, r_oh, sl, r_sl) in ((oh1, r_oh1, slot1, r_slot1), (oh2, r_oh2, slot2, r_slot2)):
            V(lambda e, oh=oh: e.tensor_tensor(out=oh[:], in0=oh[:], in1=pos[:], op=ALU.mult), [r_oh, r_pos], [r_oh])
            V(lambda e, oh=oh: e.tensor_reduce(out=sf[:], in_=oh[:], axis=AX.X, op=ALU.add), [r_oh], [r_sf])
            V(lambda e: e.tensor_scalar(out=sf[:], in0=sf[:], scalar1=float(NROWS - 1), scalar2=0.0, op0=ALU.min, op1=ALU.max), [r_sf], [r_sf])
            V(lambda e, sl=sl: e.tensor_copy(out=sl[:], in_=sf[:]), [r_sf], [r_sl])
        h2r = c.ring(3, [128, D], BF16, name="h2d")
        for t in range(NT):
            h2, r_h2 = h2r.next()
            ld(h2[:], dr["H2"][t * 128:(t + 1) * 128, :], [rs["H2"]], [r_h2])
            for (sl, r_sl) in ((slot1, r_slot1), (slot2, r_slot2)):
                S.dma("pool", lambda e, h2=h2, sl=sl, t=t: e.indirect_dma_start(
                    out=dr["XS"], out_offset=bass.IndirectOffsetOnAxis(ap=sl[:, t:t + 1], axis=0), in_=h2[:], in_offset=None),
                    [r_h2, r_sl], [rs["XS"]])
        c.close()

    def p5(l):
        c = Ctx(S)
        wsr = c.ring(6, [128, 2048], F32, name="wsr")
        wbr = [c.ring(2, [128, 4096], BF16, name=f"wb{i}") for i in range(3)]
        xrr = c.ring(4, [128, D], BF16, name="xr")
        xTr = c.ring(2, [128, 8, RB], BF16, name="xT")
        actr = c.ring(2, [128, 4, RB], BF16, name="actT")
        silr = c.ring(2, [128, RB], F32, name="sil")
        ysr = c.ring(3, [128, D], BF16, name="ys")
        ptr = c.ring(2, [128, 8, 128], BF16, psum=True, name="ptr")
        pacc = c.ring(6, [128, 512], F32, psum=True, name="pacc")
        wsrc = ((dr["moe_w1_0"], dr["moe_w1_1"]), (dr["moe_w3_0"], dr["moe_w3_1"]), (dr["moe_w2_0"], dr["moe_w2_1"]))
        k = [0]

        def wload(b):
            wb = []
            for i in range(3):
                wt, r_wt = wbr[i].next()
                for hf in range(2):
                    st, r_st = wsr.next()
                    S.dma("pool", lambda e, st=st, i=i, hf=hf: e.indirect_dma_start(
                        out=st[:], out_offset=None, in_=wsrc[i][hf],
                        in_offset=bass.IndirectOffsetOnAxis(ap=widx[:, b:b + 1], axis=0)), [r_widx], [r_st])
                    k[0] += 1
                    dst = wt[:, hf * 2048:(hf + 1) * 2048]
                    if k[0] % 2 == 0:
                        V(lambda e, st=st, dst=dst: e.tensor_copy(out=dst, in_=st[:]), [r_st], [r_wt])
                    else:
                        A(lambda e, st=st, dst=dst: e.activation(out=dst, in_=st[:], func=AF.Copy), [r_st], [r_wt])
                wb.append((wt, r_wt))
            return wb

        def xprep(b):
            xT, r_xT = xTr.next()
            tl = []
            for rt in range(RB // 128):
                xr_, r_xr = xrr.next()
                r0 = b * RB + rt * 128
                ld(xr_[:], dr["XS"][r0:r0 + 128, :], [rs["XS"]], [r_xr])
                tl.append((xr_, r_xr, ptr.next()))
            for kc in range(8):
                for (xr_, r_xr, (pt, r_pt)) in tl:
                    PE(lambda e, kc=kc, pt=pt, xr_=xr_: e.transpose(out=pt[:, kc, :], in_=xr_[:, kc * 128:(kc + 1) * 128], identity=ident[:]),
                       [r_xr, r_ident], [r_pt])
            for rt, (xr_, r_xr, (pt, r_pt)) in enumerate(tl):
                if rt == 0:
                    V(lambda e, xT=xT, pt=pt, rt=rt: e.tensor_copy(out=xT[:, :, rt * 128:(rt + 1) * 128], in_=pt[:]), [r_pt], [r_xT])
                else:
                    A(lambda e, xT=xT, pt=pt, rt=rt: e.activation(out=xT[:, :, rt * 128:(rt + 1) * 128], in_=pt[:], func=AF.Copy), [r_pt], [r_xT])
            return xT, r_xT

        Wn = wload(0)
        Xn = xprep(0)
        for b in range(NBLK):
            (w1b, r_w1b), (w3b, r_w3b), (w2b, r_w2b) = Wn
            xT, r_xT = Xn
            if b + 1 < NBLK:
                Wn = wload(b + 1)
            aT, r_aT = actr.next()
            for mp in range(2):
                accs = []
                for mi in range(2):
                    accs.append((pacc.next(), pacc.next()))
                for kc in range(8):
                    for mi in range(2):
                        mc = 2 * mp + mi
                        (a1, r_a1), (a3, r_a3) = accs[mi]
                        PE(lambda e, kc=kc, mc=mc, a1=a1, w1b=w1b, xT=xT: e.matmul(a1[:, 0:RB], lhsT=w1b[:, kc * 512 + mc * 128:kc * 512 + (mc + 1) * 128],
                                                                                  rhs=xT[:, kc, :], start=(kc == 0), stop=(kc == 7)), [r_w1b, r_xT], [r_a1])
                        PE(lambda e, kc=kc, mc=mc, a3=a3, w3b=w3b, xT=xT: e.matmul(a3[:, 0:RB], lhsT=w3b[:, kc * 512 + mc * 128:kc * 512 + (mc + 1) * 128],
                                                                                  rhs=xT[:, kc, :], start=(kc == 0), stop=(kc == 7)), [r_w3b, r_xT], [r_a3])
                for mi in range(2):
                    mc = 2 * mp + mi
                    (a1, r_a1), (a3, r_a3) = accs[mi]
                    sl, r_sl = silr.next()
                    A(lambda e, sl=sl, a1=a1: e.activation(out=sl[:], in_=a1[:, 0:RB], func=AF.Silu), [r_a1], [r_sl])
                    V(lambda e, sl=sl, a3=a3, mc=mc, aT=aT: e.tensor_tensor(out=aT[:, mc, :], in0=sl[:], in1=a3[:, 0:RB], op=ALU.mult), [r_sl, r_a3], [r_aT])
            if b + 1 < NBLK:
                Xn = xprep(b + 1)
            yaccs = [[pacc.next() for nch in range(2)] for rt in range(RB // 128)]
            for mc in range(4):
                for rt in range(RB // 128):
                    for nch in range(2):
                        acc, r_acc = yaccs[rt][nch]
                        PE(lambda e, mc=mc, acc=acc, rt=rt, nch=nch, aT=aT, w2b=w2b: e.matmul(
                            acc[:, :], lhsT=aT[:, mc, rt * 128:(rt + 1) * 128], rhs=w2b[:, mc * 1024 + nch * 512:mc * 1024 + (nch + 1) * 512],
                            start=(mc == 0), stop=(mc == 3)), [r_aT, r_w2b], [r_acc])
            for rt in range(RB // 128):
                ys, r_ys = ysr.next()
                for nch in range(2):
                    acc, r_acc = yaccs[rt][nch]
                    if nch == 0:
                        A(lambda e, ys=ys, acc=acc: e.activation(out=ys[:, 0:512], in_=acc[:, :], func=AF.Copy), [r_acc], [r_ys])
                    else:
                        V(lambda e, ys=ys, acc=acc: e.tensor_copy(out=ys[:, 512:1024], in_=acc[:, :]), [r_acc], [r_ys])
                r0 = b * RB + rt * 128
                ld(dr["YS"][r0:r0 + 128, :], ys[:], [r_ys], [rs["YS"]])
        c.close()

    def p6(l):
        c = Ctx(S)
        xr = c.ring(4, [128, D], F32, name="x1")
        y1r = c.ring(4, [128, D], BF16, name="y1")
        y2r = c.ring(4, [128, D], BF16, name="y2")
        yfr = c.ring(2, [128, D], F32, name="yf")
        ygr = c.ring(2, [128, D], F32, name="yg")
        st_ = {}

        def loads(t):
            xt, r_xt = xr.next()
            ld(xt[:], dr["Xs"][t * 128:(t + 1) * 128, :], [rs["Xs"]], [r_xt])
            y1, r_y1 = y1r.next()
            y2, r_y2 = y2r.next()
            for (yy, r_yy, sl, r_sl) in ((y1, r_y1, slot1, r_slot1), (y2, r_y2, slot2, r_slot2)):
                S.dma("pool", lambda e, yy=yy, sl=sl, t=t: e.indirect_dma_start(
                    out=yy[:], out_offset=None, in_=dr["YS"], in_offset=bass.IndirectOffsetOnAxis(ap=sl[:, t:t + 1], axis=0)),
                    [rs["YS"], r_sl], [r_yy])
            st_[t] = (xt, r_xt, y1, r_y1, y2, r_y2)
        PF = 3
        for t in range(min(PF, NT)):
            loads(t)
        for t in range(NT):
            xt, r_xt, y1, r_y1, y2, r_y2 = st_.pop(t)
            yf, r_yf = yfr.next()
            yg, r_yg = ygr.next()
            A(lambda e, y1=y1, yf=yf, t=t: e.activation(out=yf[:], in_=y1[:], func=AF.Copy, scale=gt1[:, t:t + 1]), [r_y1, r_gt1], [r_yf])
            V(lambda e, yf=yf, yg=yg, y2=y2, t=t: e.scalar_tensor_tensor(out=yg[:], in0=y2[:], scalar=gt2[:, t:t + 1], in1=yf[:], op0=ALU.mult, op1=ALU.add),
              [r_yf, r_y2, r_gt2], [r_yg])
            V(lambda e, yg=yg: e.tensor_tensor(out=yg[:], in0=yg[:], in1=G2b[:], op=ALU.mult), [r_yg, r_G2b], [r_yg])
            V(lambda e, yg=yg, xt=xt: e.tensor_tensor(out=xt[:], in0=yg[:], in1=xt[:], op=ALU.add), [r_yg, r_xt], [r_xt])
            ld(dr["Xs"][t * 128:(t + 1) * 128, :], xt[:], [r_xt], [rs["Xs"]])
            if t + PF < NT:
                loads(t + PF)
        c.close()

    def pfinal():
        c = Ctx(S)
        fgb, r_fgb = c.sb([128, D], F32, "fgb")
        ld(fgb[:], dr["final_g"].partition_broadcast(128), (), [r_fgb], "sp")
        xr = c.ring(3, [128, D], F32, name="xf")
        junk, r_junk = c.sb([128, D], BF16, "junk")
        ss, r_ss = c.sb([128, NT], F32, "ss")
        rstd, r_rstd = c.sb([128, NT], F32, "rstd")
        V(lambda e: e.memset(ss[:], 0.0), (), [r_ss])
        for t in range(NT):
            xt, r_xt = xr.next()
            ld(xt[:], dr["Xs"][t * 128:(t + 1) * 128, :], [rs["Xs"]], [r_xt])
            A(lambda e, xt=xt, t=t: e.activation(out=junk[:], in_=xt[:], func=AF.Square, accum_out=ss[:, t:t + 1]), [r_xt], [r_junk, r_ss])
            rstd_from_ss(ss[:, t:t + 1], rstd[:, t:t + 1], D, r_ss, r_rstd)
            V(lambda e, xt=xt, t=t: e.scalar_tensor_tensor(out=xt[:], in0=xt[:], scalar=rstd[:, t:t + 1], in1=fgb[:], op0=ALU.mult, op1=ALU.mult),
              [r_xt, r_rstd, r_fgb], [r_xt])
            ld(out[t * 128:(t + 1) * 128, :], xt[:], [r_xt], [rs["out"]])
        c.close()


    def finish():
        toks = []
        for r in rs.values():
            toks += r.ws
        S.wait_all("sp", toks)
        S.flush()
        G.close()
        S.es.close()

    setup()
    xin, r_xin = dr["x"], rs["x"]
    done = False
    for l in range(depth):
        for (nm, fn) in (("p0", lambda: p0(l)), ("p1", lambda: p1(l, xin, r_xin)), ("p2", lambda: p2(l)), ("p2b", lambda: p2b(l)),
                         ("p3", lambda: p3(l)), ("p4", lambda: p4(l, xin, r_xin)), ("p4b", lambda: p4b(l)), ("p5", lambda: p5(l)),
                         ("p6", lambda: p6(l))):
            fn()
            if stop == (nm, l):
                done = True
                break
        if done:
            break
        xin, r_xin = dr["Xs"], rs["Xs"]
    if not done and final:
        pfinal()
    finish()
    return nc


def make_inputs(inputs, b):
    f = lambda a: np.ascontiguousarray(np.asarray(a, dtype=np.float32))
    m = {
        "x": f(inputs["x"][b]), "c": f(inputs["c"][b:b + 1]), "w_ada": f(inputs["w_ada"]), "b_ada": f(inputs["b_ada"]),
        "norm1_g": f(inputs["norm1_g"]), "w_in": f(inputs["w_in"]), "attn_norm_g": f(inputs["attn_norm_g"]),
        "hgrn_lb_logits": f(inputs["hgrn_lb_logits"]), "hgrn_norm_g": f(inputs["hgrn_norm_g"]), "w_out": f(inputs["w_out"]),
        "norm2_g": f(inputs["norm2_g"]),
        "router_w": f(np.concatenate([np.asarray(inputs["router_group_w"]), np.asarray(inputs["router_expert_w"])], axis=-1)),
        "router_b": f(np.concatenate([np.asarray(inputs["router_group_b"]), np.asarray(inputs["router_expert_b"])], axis=-1)),
        "final_g": f(np.asarray(inputs["final_g"]).reshape(1, D)),
    }
    for nm, kc, n in (("moe_w1", 8, 512), ("moe_w3", 8, 512), ("moe_w2", 4, D)):
        w = np.asarray(inputs[nm], dtype=np.float32).reshape(DEPTH, NE, kc, 128, n).transpose(0, 1, 3, 2, 4).reshape(DEPTH * NE * 128, kc * n)
        m[nm + "_0"] = np.ascontiguousarray(w[:, :2048])
        m[nm + "_1"] = np.ascontiguousarray(w[:, 2048:])
    m.update(host_consts())
    return m


def kernel(**inputs):
    nc = build()
    in_maps = [make_inputs(inputs, b) for b in range(2)]
    res = run_bass_kernel_spmd(nc, in_maps, core_ids=[0, 1])
    return np.stack([np.asarray(res.results[b]["out"], dtype=np.float32) for b in range(2)], axis=0)
```

```python
import contextlib
import numpy as np
import ml_dtypes
import concourse.bass as bass
import concourse.mybir as mybir
from concourse.bass_utils import run_bass_kernel_spmd

F32 = mybir.dt.float32
BF16 = mybir.dt.bfloat16
I32 = mybir.dt.int32
ALU = mybir.AluOpType
AF = mybir.ActivationFunctionType
AX = mybir.AxisListType

T = 8192
D = 1024
NT = T // 128
DEPTH = 4
NE = 32
RB = 256
NBLK = (2 * T) // RB + NE
NROWS = NBLK * RB
EPS = 1e-6
BIG = 30000.0
PATTERNS = ((128, 1), (512, 4), (2048, 16))
EP_ENG = 30000
EP_DMA = 3000


class Res:
    __slots__ = ("w", "r")

    def __init__(self):
        self.w = None
        self.r = []


class MRes(Res):
    __slots__ = ("ws",)

    def __init__(self):
        super().__init__()
        self.ws = []


def _compress(lst):
    mx = {}
    for (pq, c) in lst:
        mx[pq] = max(mx.get(pq, 0), c)
    return list(mx.items())


class Q:
    def __init__(self, nc, es, name, inc, ep):
        self.nc, self.es, self.name, self.inc, self.ep = nc, es, name, inc, ep
        self.sems = []
        self.count = 0
        self.ops = []
        self.seen = {}

    def sem_for(self, c):
        i = (c - 1) // self.ep
        while len(self.sems) <= i:
            self.sems.append(self.es.enter_context(self.nc.semaphore(f"{self.name}_{len(self.sems)}")))
        return self.sems[i], (((c - 1) % self.ep) + 1) * self.inc


class Sched:
    def __init__(self, nc, ndma=6):
        self.nc = nc
        self.es = contextlib.ExitStack()
        self.q = {n: Q(nc, self.es, "s" + n, 1, EP_ENG) for n in ("pe", "act", "dve", "pool", "sp")}
        self.dslots = {n: [Q(nc, self.es, f"d{n}{i}", 16, EP_DMA) for i in range(ndma)] for n in ("sp", "pool", "act")}
        self.dnext = {n: 0 for n in self.dslots}

    def _deps(self, reads, writes):
        need = {}
        for r in reads:
            if isinstance(r, MRes):
                for (pq, c) in r.ws:
                    need[pq] = max(need.get(pq, 0), c)
            elif r.w is not None:
                need[r.w[0]] = max(need.get(r.w[0], 0), r.w[1])
        for w in writes:
            if (not isinstance(w, MRes)) and w.w is not None:
                need[w.w[0]] = max(need.get(w.w[0], 0), w.w[1])
            for (pq, c) in w.r:
                need[pq] = max(need.get(pq, 0), c)
        return need

    def _commit(self, tok, reads, writes):
        for r in reads:
            r.r.append(tok)
            if len(r.r) > 48:
                r.r = _compress(r.r)
        for w in writes:
            if isinstance(w, MRes):
                w.ws.append(tok)
                if len(w.ws) > 48:
                    w.ws = _compress(w.ws)
            else:
                w.w = tok
                w.r = []

    def _waits(self, q, need):
        waits = []
        for pq, c in need.items():
            if q.seen.get(pq, 0) >= c:
                continue
            q.seen[pq] = c
            waits.append((pq, c))
        return waits

    def op(self, qn, fn, reads=(), writes=()):
        q = self.q[qn]
        waits = self._waits(q, self._deps(reads, writes))
        q.count += 1
        tok = (q, q.count)
        q.ops.append((waits, fn, tok))
        self._commit(tok, reads, writes)
        return tok

    def dma(self, qn, fn, reads=(), writes=()):
        q = self.q[qn]
        sl = self.dslots[qn]
        dq = sl[self.dnext[qn] % len(sl)]
        self.dnext[qn] += 1
        need = self._deps(reads, writes)
        if dq.count > 0:
            need[dq] = max(need.get(dq, 0), dq.count)
        waits = self._waits(q, need)
        dq.count += 1
        tok = (dq, dq.count)
        q.ops.append((waits, fn, tok))
        self._commit(tok, reads, writes)
        return tok

    def wait_all(self, qn, toks):
        q = self.q[qn]
        need = {}
        for (pq, c) in toks:
            need[pq] = max(need.get(pq, 0), c)
        q.ops.append((self._waits(q, need), None, None))

    def barrier(self):
        toks = []
        for q in self.q.values():
            if q.count > 0:
                toks.append((q, q.count))
        for sl in self.dslots.values():
            for dq in sl:
                if dq.count > 0:
                    toks.append((dq, dq.count))
        for qn in self.q:
            self.wait_all(qn, [t for t in toks if t[0] is not self.q[qn]])

    def flush(self):
        nc = self.nc
        if not any(q.ops for q in self.q.values()):
            return
        with nc.Block() as block:
            def mk(qn):
                q = self.q[qn]

                def body(e):
                    for waits, fn, tok in q.ops:
                        for (pq, c) in waits:
                            s, v = pq.sem_for(c)
                            e.wait_ge(s, v)
                        if fn is not None:
                            s, _ = tok[0].sem_for(tok[1])
                            fn(e).then_inc(s, tok[0].inc)
                    q.ops = []
                return body
            block.tensor(mk("pe"))
            block.scalar(mk("act"))
            block.vector(mk("dve"))
            block.gpsimd(mk("pool"))
            block.sync(mk("sp"))


class Ring:
    def __init__(self, items):
        self.items = items
        self.i = 0

    def next(self):
        it = self.items[self.i % len(self.items)]
        self.i += 1
        return it


_UID = [0]


class Ctx:
    def __init__(self, S):
        self.S = S
        self.es = contextlib.ExitStack()
        self.n = 0

    def sb(self, shape, dt, name=None):
        _UID[0] += 1
        t = self.es.enter_context(self.S.nc.sbuf_tensor(f"{name or 't'}_{_UID[0]}", list(shape), dt))
        return t, Res()

    def ps(self, shape, dt, name=None):
        _UID[0] += 1
        t = self.es.enter_context(self.S.nc.psum_tensor(f"{name or 'p'}_{_UID[0]}", list(shape), dt))
        return t, Res()

    def ring(self, n, shape, dt, psum=False, name=None):
        return Ring([(self.ps if psum else self.sb)(shape, dt, name) for _ in range(n)])

    def close(self):
        self.S.barrier()
        self.S.flush()
        self.es.close()


def host_consts():
    c = {}
    c["ident"] = np.eye(128, dtype=np.float32).astype(ml_dtypes.bfloat16)
    kk = np.arange(128)[:, None]
    qq = np.arange(128)[None, :]
    mm = np.zeros((128, 8, 3, 256), np.float32)
    for h in range(8):
        slope = 2.0 ** (-(h + 1))
        for p, (w, d) in enumerate(PATTERNS):
            steps = w // d
            dist_cur = qq - kk
            dist_nxt = 128 + qq - kk
            for j, dist in enumerate((dist_cur, dist_nxt)):
                valid = (dist >= 0) & (dist <= steps)
                mm[:, h, p, j * 128:(j + 1) * 128] = np.where(valid, -slope * d * dist, -BIG)
    mm2 = np.concatenate([mm[..., 128:256], mm[..., 0:128]], axis=-1)
    c["amask"] = np.exp(mm2.astype(np.float64)).astype(np.float32).reshape(128, 8 * 3 * 256).astype(ml_dtypes.bfloat16)
    c["causal"] = ((kk <= qq) & ((kk // 64) == (qq // 64))).astype(np.uint8)
    c["ustrict"] = (kk < qq).astype(np.float32).astype(ml_dtypes.bfloat16)
    rm = np.ones((128, 2048), np.float32)
    rm[:, ::128] = 0.0
    c["resetm"] = rm
    c["mulrow"] = np.tile((np.arange(64, dtype=np.float32) * RB)[None, :], (128, 1))
    c["brow"] = np.tile(np.arange(NBLK, dtype=np.float32)[None, :], (128, 1))
    c["piota"] = np.arange(128, dtype=np.float32).reshape(128, 1)
    return c


CONST_SPECS = {"ident": ([128, 128], BF16), "amask": ([128, 8 * 3 * 256], BF16), "causal": ([128, 128], mybir.dt.uint8),
               "ustrict": ([128, 128], BF16), "resetm": ([128, 2048], F32), "mulrow": ([128, 64], F32), "brow": ([128, NBLK], F32), "piota": ([128, 1], F32)}

IN_SPECS = {
    "x": ([T, D], F32), "c": ([1, D], F32), "w_ada": ([DEPTH, D, 6 * D], F32), "b_ada": ([DEPTH, 6 * D], F32),
    "norm1_g": ([DEPTH, D], F32), "w_in": ([DEPTH, D, 3584], F32), "attn_norm_g": ([DEPTH, 512], F32),
    "hgrn_lb_logits": ([DEPTH, 512], F32), "hgrn_norm_g": ([DEPTH, 512], F32), "w_out": ([DEPTH, D, D], F32),
    "norm2_g": ([DEPTH, D], F32), "router_w": ([DEPTH, D, 36], F32), "router_b": ([DEPTH, 36], F32),
    "moe_w1_0": ([DEPTH * NE * 128, 2048], F32), "moe_w1_1": ([DEPTH * NE * 128, 2048], F32),
    "moe_w3_0": ([DEPTH * NE * 128, 2048], F32), "moe_w3_1": ([DEPTH * NE * 128, 2048], F32),
    "moe_w2_0": ([DEPTH * NE * 128, 2048], F32), "moe_w2_1": ([DEPTH * NE * 128, 2048], F32),
    "final_g": ([1, D], F32),
}


def build(depth=DEPTH, stop=None, dumps=(), final=True):
    nc = bass.Bass("TRN2", target_bir_lowering=False)
    S = Sched(nc)
    dr = {}
    for k, (shp, dt) in list(IN_SPECS.items()) + list(CONST_SPECS.items()):
        dr[k] = nc.dram_tensor(k, shp, dt, kind="ExternalInput").ap()
    out = nc.dram_tensor("out", [T, D], F32, kind="ExternalOutput").ap()
    rs = {}

    def scratch(name, shp, dt):
        kind = "ExternalOutput" if name in dumps else "Internal"
        dr[name] = nc.dram_tensor(name, shp, dt, kind=kind).ap()
        rs[name] = MRes()
    scratch("Xs", [T, D], F32)
    scratch("QT", [512, T], BF16)
    scratch("KT", [512, T], BF16)
    scratch("VV", [T, 512], BF16)
    scratch("RQT", [512, T], BF16)
    scratch("ZT", [512, T], F32)
    scratch("RI", [T, 512], BF16)
    scratch("RG", [T, 512], BF16)
    for p in range(3):
        scratch(f"OP{p}", [T, 2, 4 * 65], F32)
    scratch("CAT", [T, D], BF16)
    scratch("H2", [T, D], BF16)
    scratch("XS", [NROWS, D], BF16)
    scratch("YS", [NROWS, D], BF16)
    rs["x"] = MRes()
    rs["out"] = MRes()

    G = Ctx(S)
    ident, r_ident = G.sb([128, 128], BF16, "ident")
    causal, r_causal = G.sb([128, 128], mybir.dt.uint8, "causal")
    ustrict, r_ustrict = G.sb([128, 128], BF16, "ustrict")
    onesb, r_onesb = G.sb([128, 128], BF16, "onesb")
    mulrow, r_mulrow = G.sb([128, 64], F32, "mulrow")
    brow, r_brow = G.sb([128, NBLK], F32, "brow")
    piota, r_piota = G.sb([128, 1], F32, "piota")
    widx, r_widx = G.sb([128, NBLK], I32, "widx")
    one11, r_one11 = G.sb([1, 1], F32, "one11")
    onesrow, r_onesrow = G.sb([1, 128], F32, "onesrow")
    colf, r_colf = G.sb([128, 16], F32, "colf")
    G1b, r_G1b = G.sb([128, D], F32, "G1b")
    A2b, r_A2b = G.sb([128, D], F32, "A2b")
    S2b, r_S2b = G.sb([128, D], F32, "S2b")
    G2b, r_G2b = G.sb([128, D], F32, "G2b")
    lbc, r_lbc = G.sb([128, DEPTH, 4], F32, "lbc")
    oml, r_oml = G.sb([128, DEPTH, 4], F32, "oml")
    noml, r_noml = G.sb([128, DEPTH, 4], F32, "noml")
    slot1, r_slot1 = G.sb([128, NT], I32, "slot1")
    slot2, r_slot2 = G.sb([128, NT], I32, "slot2")
    gt1, r_gt1 = G.sb([128, NT], F32, "gt1")
    gt2, r_gt2 = G.sb([128, NT], F32, "gt2")
    lgall, r_lgall = G.sb([128, NT, 36], F32, "lgall")

    dma_rr = [0]

    def ld(out_, in_, reads=(), writes=(), q=None):
        if q is None:
            q = "sp"
        return S.dma(q, lambda e: e.dma_start(out=out_, in_=in_), reads, writes)

    def V(fn, r=(), w=()):
        return S.op("dve", fn, r, w)

    def A(fn, r=(), w=()):
        return S.op("act", fn, r, w)

    def PE(fn, r=(), w=()):
        return S.op("pe", fn, r, w)

    def GP(fn, r=(), w=()):
        return S.op("pool", fn, r, w)

    def setup():
        ld(ident[:], dr["ident"], (), [r_ident], "sp")
        ld(causal[:], dr["causal"], (), [r_causal], "sp")
        ld(ustrict[:], dr["ustrict"], (), [r_ustrict], "sp")
        ld(mulrow[:], dr["mulrow"], (), [r_mulrow], "sp")
        ld(brow[:], dr["brow"], (), [r_brow], "sp")
        ld(piota[:], dr["piota"], (), [r_piota], "sp")
        V(lambda e: e.memset(one11[:], 1.0), (), [r_one11])
        V(lambda e: e.memset(onesrow[:], 1.0), (), [r_onesrow])
        V(lambda e: e.memset(onesb[:], 1.0), (), [r_onesb])
        c = Ctx(S)
        lg, r_lg = c.sb([128, DEPTH, 4], F32)
        ex, r_ex = c.sb([128, DEPTH, 4], F32)
        sm, r_sm = c.sb([128, 4], F32)
        S.dma("sp", lambda e: e.dma_start(out=lg[:], in_=dr["hgrn_lb_logits"].rearrange("l (h k) -> k l h", k=128),
                                          allow_slow_non_contiguous=True), (), [r_lg])
        A(lambda e: e.activation(out=ex[:], in_=lg[:], func=AF.Exp), [r_lg], [r_ex])
        V(lambda e: e.tensor_tensor(out=sm[:], in0=ex[:, 0, :], in1=ex[:, 1, :], op=ALU.add), [r_ex], [r_sm])
        V(lambda e: e.tensor_tensor(out=sm[:], in0=sm[:], in1=ex[:, 2, :], op=ALU.add), [r_ex, r_sm], [r_sm])
        V(lambda e: e.tensor_tensor(out=sm[:], in0=sm[:], in1=ex[:, 3, :], op=ALU.add), [r_ex, r_sm], [r_sm])
        V(lambda e: e.reciprocal(out=sm[:], in_=sm[:]), [r_sm], [r_sm])
        V(lambda e: e.memset(lbc[:, 0, :], 0.0), (), [r_lbc])
        V(lambda e: e.tensor_tensor(out=lbc[:, 1, :], in0=ex[:, 1, :], in1=sm[:], op=ALU.mult), [r_ex, r_sm], [r_lbc])
        for l in (2, 3):
            V(lambda e, l=l: e.tensor_tensor(out=ex[:, l, :], in0=ex[:, l, :], in1=sm[:], op=ALU.mult), [r_ex, r_sm], [r_ex])
            V(lambda e, l=l: e.tensor_tensor(out=lbc[:, l, :], in0=lbc[:, l - 1, :], in1=ex[:, l, :], op=ALU.add), [r_ex, r_lbc], [r_lbc])
        V(lambda e: e.tensor_scalar(out=oml[:], in0=lbc[:], scalar1=-1.0, scalar2=1.0, op0=ALU.mult, op1=ALU.add), [r_lbc], [r_oml])
        V(lambda e: e.tensor_scalar(out=noml[:], in0=lbc[:], scalar1=1.0, scalar2=-1.0, op0=ALU.mult, op1=ALU.add), [r_lbc], [r_noml])
        c.close()

    def p0(l):
        c = Ctx(S)
        crow, r_crow = c.sb([1, D], F32)
        cactc, r_cactc = c.sb([128, 8], F32)
        modrow, r_mod = c.sb([1, 6 * D], F32)
        brow, r_brow = c.sb([1, 6 * D], F32)
        g1row, r_g1 = c.sb([1, D], F32)
        g2row, r_g2 = c.sb([1, D], F32)
        wring = c.ring(2, [128, 8, 512], F32)
        pcol, r_pcol = c.ps([128, 16], F32)
        pacc = c.ring(2, [128, 512], F32, psum=True)
        ld(crow[:], dr["c"], (), [r_crow], "sp")
        ld(brow[:], dr["b_ada"][l:l + 1, :], (), [r_brow], "sp")
        ld(g1row[:], dr["norm1_g"][l:l + 1, :], (), [r_g1], "sp")
        ld(g2row[:], dr["norm2_g"][l:l + 1, :], (), [r_g2], "sp")
        A(lambda e: e.activation(out=crow[:], in_=crow[:], func=AF.Silu), [r_crow], [r_crow])
        for kc in range(8):
            PE(lambda e, kc=kc: e.matmul(pcol[:, kc:kc + 1], lhsT=crow[0:1, kc * 128:(kc + 1) * 128], rhs=one11[0:1, 0:1],
                                         start=True, stop=True), [r_crow, r_one11], [r_pcol])
        V(lambda e: e.tensor_copy(out=cactc[:], in_=pcol[:, 0:8]), [r_pcol], [r_cactc])
        for n in range(12):
            wt, r_wt = wring.next()
            ld(wt[:], dr["w_ada"][l, :, n * 512:(n + 1) * 512].rearrange("(kc p) n -> p kc n", p=128), (), [r_wt])
            acc, r_acc = pacc.next()
            for kc in range(8):
                PE(lambda e, kc=kc, acc=acc, wt=wt: e.matmul(acc[0:1, :], lhsT=cactc[:, kc:kc + 1], rhs=wt[:, kc, :],
                                                            start=(kc == 0), stop=(kc == 7)), [r_cactc, r_wt], [r_acc])
            V(lambda e, n=n, acc=acc: e.tensor_tensor(out=modrow[0:1, n * 512:(n + 1) * 512], in0=acc[0:1, :],
                                                       in1=brow[0:1, n * 512:(n + 1) * 512], op=ALU.add), [r_acc, r_brow], [r_mod])
        V(lambda e: e.scalar_tensor_tensor(out=g1row[:], in0=modrow[0:1, D:2 * D], scalar=1.0, in1=g1row[:], op0=ALU.add, op1=ALU.mult),
          [r_mod, r_g1], [r_g1])
        V(lambda e: e.scalar_tensor_tensor(out=g2row[:], in0=modrow[0:1, 4 * D:5 * D], scalar=1.0, in1=g2row[:], op0=ALU.add, op1=ALU.mult),
          [r_mod, r_g2], [r_g2])
        for kc in range(8):
            PE(lambda e, kc=kc: e.matmul(pcol[:, kc:kc + 1], lhsT=g1row[0:1, kc * 128:(kc + 1) * 128], rhs=one11[0:1, 0:1],
                                         start=True, stop=True), [r_g1, r_one11], [r_pcol])
            PE(lambda e, kc=kc: e.matmul(pcol[:, 8 + kc:9 + kc], lhsT=modrow[0:1, kc * 128:(kc + 1) * 128], rhs=one11[0:1, 0:1],
                                         start=True, stop=True), [r_mod, r_one11], [r_pcol])
        V(lambda e: e.tensor_copy(out=colf[:], in_=pcol[:]), [r_pcol], [r_colf])
        for (src, r_src, off, dst, r_dst) in ((modrow, r_mod, 2 * D, G1b, r_G1b), (g2row, r_g2, 0, A2b, r_A2b),
                                              (modrow, r_mod, 3 * D, S2b, r_S2b), (modrow, r_mod, 5 * D, G2b, r_G2b)):
            for nch in range(2):
                acc, r_acc = pacc.next()
                PE(lambda e, acc=acc, src=src, off=off, nch=nch: e.matmul(acc[:, :], lhsT=onesrow[0:1, :],
                                                                         rhs=src[0:1, off + nch * 512:off + (nch + 1) * 512],
                                                                         start=True, stop=True), [r_src, r_onesrow], [r_acc])
                V(lambda e, acc=acc, dst=dst, nch=nch: e.tensor_copy(out=dst[:, nch * 512:(nch + 1) * 512], in_=acc[:, :]), [r_acc], [r_dst])
        c.close()

    def rstd_from_ss(ss_ap, out_ap, n, r_ss, r_out):
        V(lambda e: e.tensor_scalar(out=out_ap, in0=ss_ap, scalar1=1.0 / n, scalar2=EPS, op0=ALU.mult, op1=ALU.add), [r_ss], [r_out])
        A(lambda e: e.activation(out=out_ap, in_=out_ap, func=AF.Sqrt), [r_out], [r_out])
        V(lambda e: e.reciprocal(out=out_ap, in_=out_ap), [r_out], [r_out])

    def p1(l, xin, r_xin):
        c = Ctx(S)
        winb, r_winb = c.sb([128, 8, 3584], BF16, "winb")
        wst = c.ring(3, [128, 1792], F32, name="wst")
        kk_ = 0
        for kc in range(8):
            for hf in range(2):
                st, r_st = wst.next()
                ld(st[:], dr["w_in"][l, kc * 128:(kc + 1) * 128, hf * 1792:(hf + 1) * 1792], (), [r_st])
                kk_ += 1
                if kk_ % 2 == 0:
                    V(lambda e, st=st, kc=kc, hf=hf: e.tensor_copy(out=winb[:, kc, hf * 1792:(hf + 1) * 1792], in_=st[:]), [r_st], [r_winb])
                else:
                    GP(lambda e, st=st, kc=kc, hf=hf: e.tensor_copy(out=winb[:, kc, hf * 1792:(hf + 1) * 1792], in_=st[:]), [r_st], [r_winb])
        xring = c.ring(6, [128, D], F32, name="xt")
        junk, r_junk = c.sb([128, D], BF16, "junk")
        xnring = c.ring(6, [128, D], BF16, name="xn")
        ss, r_ss = c.sb([128, NT], F32, "ss")
        rstd, r_rstd = c.sb([128, NT], F32, "rstd")
        hTring = c.ring(2, [128, 8, 512], BF16, name="hT")
        ptr = c.ring(2, [128, 8, 128], BF16, psum=True, name="ptr")
        pacc = c.ring(6, [128, 512], F32, psum=True, name="pacc")
        stb = c.ring(6, [128, 512], BF16, name="stb")
        stf = c.ring(4, [128, 512], F32, name="stf")
        V(lambda e: e.memset(ss[:], 0.0), (), [r_ss])
        fm = []
        for m in range(4):
            fm.append((m * 128, "QT", m * 128, "q"))
        for m in range(4):
            fm.append((512 + m * 128, "KT", m * 128, "b"))
        for m in range(4):
            fm.append((1536 + m * 128, "RQT", m * 128, "b"))
        for m in range(4):
            fm.append((2048 + m * 128, "ZT", m * 128, "f"))
        tm = [(1024, "VV", "b"), (2560, "RI", "b"), (3072, "RG", "s")]
        ev = [0]

        def prep(g):
            hT, r_hT = hTring.next()
            tiles = []
            for j in range(4):
                t = g * 4 + j
                xt, r_xt = xring.next()
                ld(xt[:], xin[t * 128:(t + 1) * 128, :], [r_xin], [r_xt])
                A(lambda e, xt=xt, t=t: e.activation(out=junk[:], in_=xt[:], func=AF.Square, accum_out=ss[:, t:t + 1]), [r_xt], [r_junk, r_ss])
                rstd_from_ss(ss[:, t:t + 1], rstd[:, t:t + 1], D, r_ss, r_rstd)
                xn, r_xn = xnring.next()
                A(lambda e, xt=xt, xn=xn, t=t: e.activation(out=xn[:], in_=xt[:], func=AF.Copy, scale=rstd[:, t:t + 1]), [r_xt, r_rstd], [r_xn])
                tiles.append((xn, r_xn))
            for jp in range(2):
                pair = [(tiles[2 * jp + i], ptr.next()) for i in range(2)]
                for kc in range(8):
                    for ((xn, r_xn), (pt, r_pt)) in pair:
                        PE(lambda e, kc=kc, pt=pt, xn=xn: e.transpose(out=pt[:, kc, :], in_=xn[:, kc * 128:(kc + 1) * 128], identity=ident[:]),
                           [r_xn, r_ident], [r_pt])
                for i, ((xn, r_xn), (pt, r_pt)) in enumerate(pair):
                    j = 2 * jp + i
                    V(lambda e, pt=pt, hT=hT, j=j: e.tensor_tensor(out=hT[:, :, j * 128:(j + 1) * 128], in0=pt[:, :, :],
                                                                  in1=colf[:, 0:8].unsqueeze(2).to_broadcast([128, 8, 128]), op=ALU.mult),
                      [r_pt, r_colf], [r_hT])
                    GP(lambda e, hT=hT, j=j: e.tensor_tensor(out=hT[:, :, j * 128:(j + 1) * 128], in0=hT[:, :, j * 128:(j + 1) * 128],
                                                            in1=colf[:, 8:16].unsqueeze(2).to_broadcast([128, 8, 128]), op=ALU.add),
                       [r_hT, r_colf], [r_hT])
            return hT, r_hT

        def evac(kind, acc, r_acc):
            st, r_st = (stf if kind == "f" else stb).next()
            ev[0] += 1
            if kind == "q":
                A(lambda e: e.activation(out=st[:], in_=acc[:], func=AF.Copy, scale=0.125), [r_acc], [r_st])
            elif kind == "s":
                A(lambda e: e.activation(out=st[:], in_=acc[:], func=AF.Silu), [r_acc], [r_st])
            elif ev[0] % 2 == 0:
                A(lambda e: e.activation(out=st[:], in_=acc[:], func=AF.Copy), [r_acc], [r_st])
            else:
                V(lambda e: e.tensor_copy(out=st[:], in_=acc[:]), [r_acc], [r_st])
            return st, r_st

        nxt = prep(0)
        for g in range(NT // 4):
            hT, r_hT = nxt
            if g + 1 < NT // 4:
                nxt = prep(g + 1)
            for f0 in range(0, 16, 4):
                grp = [(fm[f0 + i], pacc.next()) for i in range(4)]
                for kc in range(8):
                    for ((c0, dn, row0, kind), (acc, r_acc)) in grp:
                        PE(lambda e, kc=kc, acc=acc, hT=hT, c0=c0: e.matmul(acc[:, :], lhsT=winb[:, kc, c0:c0 + 128], rhs=hT[:, kc, :],
                                                                           start=(kc == 0), stop=(kc == 7)), [r_winb, r_hT], [r_acc])
                for ((c0, dn, row0, kind), (acc, r_acc)) in grp:
                    st, r_st = evac(kind, acc, r_acc)
                    ld(dr[dn][row0:row0 + 128, g * 512:(g + 1) * 512], st[:], [r_st], [rs[dn]])
            for j in range(4):
                t = g * 4 + j
                grp = [(tm[i], pacc.next()) for i in range(3)]
                for kc in range(8):
                    for ((c0, dn, kind), (acc, r_acc)) in grp:
                        PE(lambda e, kc=kc, acc=acc, hT=hT, c0=c0, j=j: e.matmul(acc[:, :], lhsT=hT[:, kc, j * 128:(j + 1) * 128],
                                                                                 rhs=winb[:, kc, c0:c0 + 512], start=(kc == 0), stop=(kc == 7)),
                           [r_winb, r_hT], [r_acc])
                for ((c0, dn, kind), (acc, r_acc)) in grp:
                    st, r_st = evac(kind, acc, r_acc)
                    ld(dr[dn][t * 128:(t + 1) * 128, :], st[:], [r_st], [rs[dn]])
        c.close()

    def p2(l):
        c = Ctx(S)
        qT2, r_qT2 = c.sb([128, 2, T], BF16, "qT2")
        kT2, r_kT2 = c.sb([128, 2, T], BF16, "kT2")
        amask, r_amask = c.sb([128, 4, 3, 256], BF16, "amask")
        vraw = c.ring(2, [128, 8, 256], BF16, name="vraw")
        vaugr = c.ring(2, [128, 64, 4, 65], BF16, name="vaug")
        pTr = c.ring(8, [128, 256], BF16, name="pT")
        per_ = c.ring(4, [128, 256], BF16, name="pe")
        ostr = c.ring(3, [128, 4, 65], F32, name="ost")
        Sps = c.ring(4, [128, 256], F32, psum=True, name="Sps")
        OpsE = c.ring(2, [128, 2, 65], F32, psum=True, name="OpsE")
        OpsO = c.ring(2, [128, 2, 65], F32, psum=True, name="OpsO")
        ostr = c.ring(4, [128, 4, 65], F32, name="ost2")
        for (vg_, r_vg_) in vaugr.items:
            GP(lambda e, vg_=vg_: e.memset(vg_[:, :, :, 64:65], 1.0), (), [r_vg_])
        for half in range(2):
            ld(amask[:], dr["amask"].rearrange("k (h p q) -> k h p q", h=8, p=3)[:, 4 * half:4 * half + 4], (), [r_amask], "sp")
            for hpi in range(2):
                hp = 2 * half + hpi
                ld(qT2[:, hpi, :], dr["QT"][hp * 128:(hp + 1) * 128, :], [rs["QT"]], [r_qT2])
                ld(kT2[:, hpi, :], dr["KT"][hp * 128:(hp + 1) * 128, :], [rs["KT"]], [r_kT2])
            for p, (w, d) in enumerate(PATTERNS):
                span = 128 * d
                nb = T // span
                vaug, r_vaug = vaugr.next()
                vsrc = dr["VV"].rearrange("(a u r) c -> r u a c", u=128, r=d)
                for r in range(d):
                    for a0 in range(0, nb, 8):
                        na = min(8, nb - a0)
                        vr, r_vr = vraw.next()
                        ld(vr[:, 0:na, :], vsrc[r, :, a0:a0 + na, half * 256:(half + 1) * 256], [rs["VV"]], [r_vr])
                        bi0 = r * nb + a0
                        GP(lambda e, vr=vr, na=na, bi0=bi0, vaug=vaug: e.tensor_copy(out=vaug[:, bi0:bi0 + na, :, 0:64],
                                                                         in_=vr[:, 0:na, :].rearrange("k a (h c) -> k a h c", h=4)),
                           [r_vr], [r_vaug])
                odst = dr[f"OP{p}"].rearrange("(a u r) h c -> r a u h c", u=128, r=d)
                units = [(r, a) for r in range(d) for a in range(nb)]

                def s_stage(r, a, hh, d=d, p=p, span=span, half=half):
                    hpi, pb = hh // 2, 64 * (hh % 2)
                    t0 = span * a + r
                    sp_, r_sp = Sps.next()
                    q_ap = qT2[pb:pb + 64, hpi, t0:t0 + 127 * d + 1:d]
                    c0 = 0 if a > 0 else 128
                    th = []
                    if a > 0:
                        tp = t0 - span
                        th.append(lambda: PE(lambda e: e.matmul(sp_[:, 0:128], lhsT=kT2[pb:pb + 64, hpi, tp:tp + 127 * d + 1:d], rhs=q_ap,
                                                                start=True, stop=True), [r_kT2, r_qT2], [r_sp]))
                    th.append(lambda: PE(lambda e: e.matmul(sp_[:, 128:256], lhsT=kT2[pb:pb + 64, hpi, t0:t0 + 127 * d + 1:d], rhs=q_ap,
                                                            start=True, stop=True), [r_kT2, r_qT2], [r_sp]))
                    pe, r_pe = per_.next()
                    pT, r_pT = pTr.next()

                    def post():
                        A(lambda e: e.activation(out=pe[:, c0:256], in_=sp_[:, c0:256], func=AF.Exp), [r_sp], [r_pe])
                        V(lambda e: e.tensor_tensor(out=pT[:, c0:256], in0=pe[:, c0:256], in1=amask[:, hh, p, c0:256], op=ALU.mult),
                          [r_pe, r_amask], [r_pT])
                    return th, post, pT, r_pT

                def pv_thunks(r, a, hh, pT, r_pT, O, r_O, nb=nb, vaug=vaug, r_vaug=r_vaug):
                    bi = r * nb + a
                    hs = hh // 2
                    th = []
                    if a > 0:
                        th.append(lambda: PE(lambda e: e.matmul(O[:, hs, :], lhsT=pT[:, 0:128], rhs=vaug[:, bi - 1, hh, :], start=True, stop=False),
                                             [r_pT, r_vaug], [r_O]))
                    th.append(lambda: PE(lambda e: e.matmul(O[:, hs, :], lhsT=pT[:, 128:256], rhs=vaug[:, bi, hh, :], start=(a == 0), stop=True),
                                         [r_pT, r_vaug], [r_O]))
                    return th

                def interleave(lists):
                    out_ = []
                    n = max(len(x) for x in lists) if lists else 0
                    for i in range(n):
                        for x in lists:
                            if i < len(x):
                                out_.append(x[i])
                    return out_

                def finish_block(r, a, OE, r_OE, OO, r_OO):
                    ost, r_ost = ostr.next()
                    V(lambda e: e.tensor_copy(out=ost[:, 0:4:2, :], in_=OE[:]), [r_OE], [r_ost])
                    V(lambda e: e.tensor_copy(out=ost[:, 1:4:2, :], in_=OO[:]), [r_OO], [r_ost])
                    ld(odst[r, a, :, half, :], ost[:].rearrange("k h c -> k (h c)"), [r_ost], [rs[f"OP{p}"]])

                flat = [(r, a, hh) for (r, a) in units for hh in range(4)]
                pairs = [flat[i:i + 2] for i in range(0, len(flat), 2)]
                Ocur = {}
                pvq = []

                def merge2(a_, b_):
                    out_ = []
                    ia = ib = 0
                    while ia < len(a_) or ib < len(b_):
                        out_ += a_[ia:ia + 2]
                        ia += 2
                        out_ += b_[ib:ib + 2]
                        ib += 2
                    return out_

                for pr in pairs:
                    st = [s_stage(*u) for u in pr]
                    s_th = interleave([x[0] for x in st])
                    if len(pvq) >= 2:
                        pv_now, fin_now = pvq.pop(0)
                    else:
                        pv_now, fin_now = [], []
                    for f_ in merge2(s_th, pv_now):
                        f_()
                    for fb in fin_now:
                        finish_block(*fb)
                    for x in st:
                        x[1]()
                    fin = []
                    pvl = []
                    for (r, a, hh), x in zip(pr, st):
                        if hh == 0:
                            Ocur[(r, a)] = (OpsE.next(), OpsO.next())
                        (OE, r_OE), (OO, r_OO) = Ocur[(r, a)]
                        O, r_O = (OE, r_OE) if hh % 2 == 0 else (OO, r_OO)
                        pvl.append(pv_thunks(r, a, hh, x[2], x[3], O, r_O))
                        if hh == 3:
                            fin.append((r, a, OE, r_OE, OO, r_OO))
                            del Ocur[(r, a)]
                    pvq.append((interleave(pvl), fin))
                for (pv_now, fin_now) in pvq:
                    for f_ in pv_now:
                        f_()
                    for fb in fin_now:
                        finish_block(*fb)
        c.close()

    def p2b(l):
        c = Ctx(S)
        angb, r_angb = c.sb([128, 512], F32, "angb")
        ld(angb[:], dr["attn_norm_g"][l:l + 1, :].partition_broadcast(128), (), [r_angb], "sp")
        opr = [c.ring(5, [128, 8, 65], F32, name=f"op{p}") for p in range(3)]
        den, r_den = c.sb([128, 8], F32, "den")
        o, r_o = c.sb([128, 8, 64], F32, "o")
        junk, r_junk = c.sb([128, 512], BF16, "junk")
        ss, r_ss = c.sb([128, NT], F32, "ss")
        rstd, r_rstd = c.sb([128, NT], F32, "rstd")
        cst = c.ring(3, [128, 512], BF16, name="cst")
        V(lambda e: e.memset(ss[:], 0.0), (), [r_ss])
        pend_ = {}

        def loads(t):
            tl = []
            for p in range(3):
                tt, r_tt = opr[p].next()
                ld(tt[:].rearrange("k h c -> k (h c)"), dr[f"OP{p}"][t * 128:(t + 1) * 128].rearrange("k a c -> k (a c)"), [rs[f"OP{p}"]], [r_tt])
                tl.append((tt, r_tt))
            pend_[t] = tl
        for t in range(min(3, NT)):
            loads(t)
        for t in range(NT):
            tl = pend_.pop(t)
            if t + 3 < NT:
                loads(t + 3)
            (t0_, r0), (t1_, r1), (t2_, r2) = tl
            V(lambda e, a=t0_, b=t1_: e.tensor_tensor(out=a[:], in0=a[:], in1=b[:], op=ALU.add), [r0, r1], [r0])
            V(lambda e, a=t0_, b=t2_: e.tensor_tensor(out=a[:], in0=a[:], in1=b[:], op=ALU.add), [r0, r2], [r0])
            V(lambda e, a=t0_: e.reciprocal(out=den[:], in_=a[:, :, 64]), [r0], [r_den])
            V(lambda e, a=t0_: e.tensor_tensor(out=o[:], in0=a[:, :, 0:64], in1=den[:].unsqueeze(2).to_broadcast([128, 8, 64]), op=ALU.mult),
              [r0, r_den], [r_o])
            A(lambda e, t=t: e.activation(out=junk[:], in_=o[:].rearrange("k h c -> k (h c)"), func=AF.Square, accum_out=ss[:, t:t + 1]),
              [r_o], [r_junk, r_ss])
            rstd_from_ss(ss[:, t:t + 1], rstd[:, t:t + 1], 512, r_ss, r_rstd)
            cs, r_cs = cst.next()
            V(lambda e, cs=cs, t=t: e.scalar_tensor_tensor(out=cs[:], in0=o[:].rearrange("k h c -> k (h c)"), scalar=rstd[:, t:t + 1],
                                                          in1=angb[:], op0=ALU.mult, op1=ALU.mult), [r_o, r_rstd, r_angb], [r_cs])
            ld(dr["CAT"][t * 128:(t + 1) * 128, 0:512], cs[:], [r_cs], [rs["CAT"]])
        c.close()

    def p3(l):
        c = Ctx(S)
        SEG = 512
        NCK = SEG // 128
        NSEG = T // SEG
        resetm, r_resetm = c.sb([128, SEG], F32, "resetm")
        gnb, r_gnb = c.sb([128, 512], F32, "gnb")
        ld(resetm[:], dr["resetm"][:, 0:SEG], (), [r_resetm], "sp")
        ld(gnb[:], dr["hgrn_norm_g"][l:l + 1, :].partition_broadcast(128), (), [r_gnb], "sp")
        rir = c.ring(2, [128, NCK, 512], BF16, name="ri")
        rgr = c.ring(2, [128, NCK, 512], BF16, name="rgm")
        catr_ = c.ring(2, [128, NCK, 512], BF16, name="catseg")
        zr = c.ring(2, [128, SEG], F32, name="z")
        qr = c.ring(2, [128, SEG], BF16, name="q")
        tset = [[c.sb([128, SEG], F32, f"b{n}{i}") for n in "ABCE"] for i in range(2)]
        o4 = c.ring(8, [128, 5, SEG], BF16, name="o4")
        dcyr = c.ring(8, [128, NCK], F32, name="dcy")
        St = [c.sb([128, 128], F32, f"S{h}") for h in range(4)]
        Sb = [c.sb([128, 128], BF16, f"Sb{h}") for h in range(4)]
        Amr = c.ring(4, [128, 128], BF16, name="Am")
        khTr = c.ring(4, [128, 128], BF16, name="khT")
        junk, r_junk = c.sb([128, 128], BF16, "junk")
        ssr, r_ssr = c.sb([128, 4 * NT], F32, "ssr")
        rsr, r_rsr = c.sb([128, 4 * NT], F32, "rsr")
        psA = c.ring(2, [128, 128], F32, psum=True, name="psA")
        psT = c.ring(1, [128, 128], BF16, psum=True, name="psT")
        psU = c.ring(2, [128, 128], F32, psum=True, name="psU")
        psO = c.ring(2, [128, 128], F32, psum=True, name="psO")
        psX = c.ring(1, [64, 64], F32, psum=True, name="psX")
        V(lambda e: e.memset(ssr[:], 0.0), (), [r_ssr])
        for h in range(4):
            V(lambda e, h=h: e.memset(St[h][0][:], 0.0), (), [St[h][1]])
            V(lambda e, h=h: e.memset(Sb[h][0][:], 0.0), (), [Sb[h][1]])

        def v3(t):
            t = t if isinstance(t, bass.AP) else t[:]
            return t.rearrange("k (c u) -> k c u", u=128)

        def v64(t):
            return t[:].rearrange("k (c u) -> k c u", u=64)

        def ew(sg):
            tk0 = sg * SEG
            ri, r_ri = rir.next()
            rgm, r_rgm = rgr.next()
            ld(ri[:], dr["RI"][tk0:tk0 + SEG, :].rearrange("(c p) f -> p c f", p=128), [rs["RI"]], [r_ri])
            ld(rgm[:], dr["RG"][tk0:tk0 + SEG, :].rearrange("(c p) f -> p c f", p=128), [rs["RG"]], [r_rgm])
            GP(lambda e: e.tensor_tensor(out=rgm[:], in0=rgm[:], in1=gnb[:].unsqueeze(1).to_broadcast([128, NCK, 512]), op=ALU.mult),
               [r_rgm, r_gnb], [r_rgm])
            heads = []

            def head_ops(hd):
                (bA, r_A), (bB, r_B), (bC, r_C), (bE, r_E) = tset[hd % 2]
                ops = []
                AA = lambda *a: ops.append(lambda: A(*a))
                VV = lambda *a: ops.append(lambda: V(*a))
                GG = lambda *a: ops.append(lambda: GP(*a))
                z, r_z = zr.next()
                q, r_q = qr.next()
                ops.append(lambda: ld(z[:], dr["ZT"][hd * 128:(hd + 1) * 128, tk0:tk0 + SEG], [rs["ZT"]], [r_z]))
                ops.append(lambda: ld(q[:], dr["RQT"][hd * 128:(hd + 1) * 128, tk0:tk0 + SEG], [rs["RQT"]], [r_q]))
                o, r_o4 = o4.next()
                dcy, r_dcy = dcyr.next()
                lb_ap, oml_ap = lbc[:, l, hd:hd + 1], oml[:, l, hd:hd + 1]

                def b3(t):
                    return t[:].rearrange("k (c u) -> k c u", u=128)

                def b64(t):
                    return t[:].rearrange("k (c u) -> k c u", u=64)
                VV(lambda e: e.tensor_scalar(out=z[:], in0=z[:], scalar1=-60.0, scalar2=None, op0=ALU.max), [r_z], [r_z])
                AA(lambda e: e.activation(out=bA[:], in_=z[:], func=AF.Exp, scale=-1.0), [r_z], [r_A])
                AA(lambda e: e.activation(out=bB[:], in_=bA[:], func=AF.Ln, bias=1.0), [r_A], [r_B])
                AA(lambda e: e.activation(out=bE[:], in_=bA[:], func=AF.Ln, bias=1.0, scale=lb_ap), [r_A, r_lbc], [r_E])
                VV(lambda e: e.tensor_tensor(out=bE[:], in0=bE[:], in1=bB[:], op=ALU.subtract), [r_E, r_B], [r_E])
                AA(lambda e: e.activation(out=bB[:], in_=bB[:], func=AF.Exp, scale=-1.0), [r_B], [r_B])
                VV(lambda e: e.scalar_tensor_tensor(out=bC[:], in0=bA[:], scalar=oml_ap, in1=bB[:], op0=ALU.mult, op1=ALU.mult),
                   [r_A, r_B, r_oml], [r_C])
                VV(lambda e: e.tensor_tensor_scan(out=bB[:], data0=resetm[:], data1=bE[:], initial=0.0, op0=ALU.mult, op1=ALU.add),
                   [r_resetm, r_E], [r_B])
                VV(lambda e: e.tensor_tensor(out=b64(bE), in0=b64(bB), in1=b64(bB)[:, :, 31:32].to_broadcast([128, 2 * NCK, 64]), op=ALU.subtract),
                   [r_B], [r_E])
                VV(lambda e: e.tensor_scalar(out=bE[:], in0=bE[:], scalar1=-80.0, scalar2=80.0, op0=ALU.max, op1=ALU.min), [r_E], [r_E])
                AA(lambda e: e.activation(out=bA[:], in_=bE[:], func=AF.Exp), [r_E], [r_A])
                VV(lambda e: e.tensor_tensor(out=o[:, 0, :], in0=q[:], in1=bA[:], op=ALU.mult), [r_q, r_A], [r_o4])
                AA(lambda e: e.activation(out=bA[:], in_=bE[:], func=AF.Exp, scale=-1.0), [r_E], [r_A])
                VV(lambda e: e.tensor_tensor(out=o[:, 1, :], in0=bC[:], in1=bA[:], op=ALU.mult), [r_C, r_A], [r_o4])
                AA(lambda e: e.activation(out=bA[:], in_=bB[:], func=AF.Exp), [r_B], [r_A])
                GG(lambda e: e.tensor_tensor(out=o[:, 2, :], in0=q[:], in1=bA[:], op=ALU.mult), [r_q, r_A], [r_o4])
                VV(lambda e: e.tensor_tensor(out=b3(bE), in0=b3(bB), in1=b3(bB)[:, :, 127:128].to_broadcast([128, NCK, 128]), op=ALU.subtract),
                   [r_B], [r_E])
                AA(lambda e: e.activation(out=bA[:], in_=bE[:], func=AF.Exp, scale=-1.0), [r_E], [r_A])
                GG(lambda e: e.tensor_tensor(out=o[:, 3, :], in0=bC[:], in1=bA[:], op=ALU.mult), [r_C, r_A], [r_o4])
                VV(lambda e: e.tensor_tensor(out=b3(bE), in0=b3(bB), in1=b3(bB)[:, :, 63:64].to_broadcast([128, NCK, 128]), op=ALU.subtract),
                   [r_B], [r_E])
                VV(lambda e: e.scalar_tensor_tensor(out=bE[:], in0=bE[:], scalar=-1.0, in1=bE[:], op0=ALU.mult, op1=ALU.min), [r_E], [r_E])
                AA(lambda e: e.activation(out=bA[:], in_=bE[:], func=AF.Exp), [r_E], [r_A])
                VV(lambda e: e.tensor_tensor(out=v3(o[:, 4, :])[:, :, 0:64], in0=b3(bC)[:, :, 0:64], in1=b3(bA)[:, :, 0:64], op=ALU.mult),
                   [r_C, r_A], [r_o4])
                VV(lambda e: e.tensor_tensor(out=v3(o[:, 4, :])[:, :, 64:128], in0=v3(q)[:, :, 64:128], in1=b3(bA)[:, :, 64:128], op=ALU.mult),
                   [r_q, r_A], [r_o4])
                AA(lambda e: e.activation(out=dcy[:], in_=b3(bB)[:, :, 127], func=AF.Exp), [r_B], [r_dcy])
                heads.append((o, r_o4, dcy, r_dcy))
                return ops

            bg = []
            for hp in range(2):
                la = head_ops(2 * hp)
                lb_ = head_ops(2 * hp + 1)
                for i in range(max(len(la), len(lb_))):
                    if i < len(la):
                        bg.append(la[i])
                    if i < len(lb_):
                        bg.append(lb_[i])
            return dict(sg=sg, ri=(ri, r_ri), rgm=(rgm, r_rgm), heads=heads), bg

        def chunks(st, bg):
            sg = st["sg"]
            per_ci = -(-len(bg) // NCK) if bg else 0
            tk0 = sg * SEG
            ri, r_ri = st["ri"]
            rgm, r_rgm = st["rgm"]
            catseg, r_catseg = catr_.next()
            for ci_ in range(NCK):
                one_chunk(st, ci_, sg, ri, r_ri, rgm, r_rgm, catseg, r_catseg)
                for f_ in bg[ci_ * per_ci:(ci_ + 1) * per_ci]:
                    f_()
            ld(dr["CAT"][tk0:tk0 + SEG, 512:1024].rearrange("(c p) f -> p c f", p=128), catseg[:], [r_catseg], [rs["CAT"]])

        def one_chunk(st, ci, sg, ri, r_ri, rgm, r_rgm, catseg, r_catseg):
            if True:
                cs = slice(ci * 128, (ci + 1) * 128)
                g0 = (sg * NCK + ci) * 4
                per = []
                for hd in range(4):
                    o, r_o4, dcy, r_dcy = st["heads"][hd]
                    pa, r_pa = psA.next()
                    PE(lambda e, pa=pa, o=o: e.matmul(pa[:, :], lhsT=o[:, 1, cs], rhs=o[:, 0, cs], start=True, stop=True), [r_o4], [r_pa])
                    px, r_px = psX.next()
                    PE(lambda e, px=px, o=o: e.matmul(px[0:64, :], lhsT=o[:, 4, ci * 128:ci * 128 + 64], rhs=o[:, 4, ci * 128 + 64:(ci + 1) * 128],
                                                      start=True, stop=True), [r_o4], [r_px])
                    pt, r_pt = psT.next()
                    PE(lambda e, pt=pt, o=o: e.transpose(out=pt[:, :], in_=o[:, 3, cs], identity=ident[:]), [r_o4, r_ident], [r_pt])
                    Am, r_Am = Amr.next()
                    GP(lambda e, Am=Am: e.memset(Am[:], 0.0), (), [r_Am])
                    V(lambda e, Am=Am, pa=pa: e.copy_predicated(out=Am[:], mask=causal[:], data=pa[:]), [r_pa, r_causal, r_Am], [r_Am])
                    A(lambda e, Am=Am, px=px: e.activation(out=Am[0:64, 64:128], in_=px[0:64, :], func=AF.Copy), [r_px, r_Am], [r_Am])
                    khT, r_khT = khTr.next()
                    A(lambda e, khT=khT, pt=pt: e.activation(out=khT[:], in_=pt[:], func=AF.Copy), [r_pt], [r_khT])
                    per.append((Am, r_Am, khT, r_khT))
                pos_ = []
                for hd in range(4):
                    o, r_o4, dcy, r_dcy = st["heads"][hd]
                    Am, r_Am, khT, r_khT = per[hd]
                    Sh, r_Sh = St[hd]
                    Sbh, r_Sbh = Sb[hd]
                    v_ap = ri[:, ci, hd * 128:(hd + 1) * 128]
                    pu, r_pu = psU.next()
                    PE(lambda e, pu=pu, khT=khT, v_ap=v_ap: e.matmul(pu[:, :], lhsT=khT[:], rhs=v_ap, start=True, stop=True), [r_khT, r_ri], [r_pu])
                    po, r_po = psO.next()
                    PE(lambda e, po=po, Am=Am, v_ap=v_ap: e.matmul(po[:, :], lhsT=Am[:], rhs=v_ap, start=True, stop=False), [r_Am, r_ri], [r_po])
                    PE(lambda e, po=po, o=o, Sbh=Sbh: e.matmul(po[:, :], lhsT=o[:, 2, cs], rhs=Sbh[:], start=False, stop=True),
                       [r_o4, r_Sbh], [r_po])
                    V(lambda e, Sh=Sh, pu=pu, dcy=dcy: e.scalar_tensor_tensor(out=Sh[:], in0=Sh[:], scalar=dcy[:, ci:ci + 1], in1=pu[:],
                                                                             op0=ALU.mult, op1=ALU.add), [r_Sh, r_pu, r_dcy], [r_Sh])
                    A(lambda e, Sh=Sh, Sbh=Sbh: e.activation(out=Sbh[:], in_=Sh[:], func=AF.Copy), [r_Sh], [r_Sbh])
                    A(lambda e, po=po, gi=g0 + hd: e.activation(out=junk[:], in_=po[:], func=AF.Square, accum_out=ssr[:, gi:gi + 1]),
                      [r_po], [r_junk, r_ssr])
                    rstd_from_ss(ssr[:, g0 + hd:g0 + hd + 1], rsr[:, g0 + hd:g0 + hd + 1], 128, r_ssr, r_rsr)
                    V(lambda e, po=po, gi=g0 + hd, hd=hd: e.scalar_tensor_tensor(out=catseg[:, ci, hd * 128:(hd + 1) * 128], in0=po[:],
                                                                                scalar=rsr[:, gi:gi + 1], in1=rgm[:, ci, hd * 128:(hd + 1) * 128],
                                                                                op0=ALU.mult, op1=ALU.mult), [r_po, r_rsr, r_rgm], [r_catseg])

        nxt, bg0 = ew(0)
        for f_ in bg0:
            f_()
        for sg in range(NSEG):
            cur = nxt
            bgn = []
            if sg + 1 < NSEG:
                nxt, bgn = ew(sg + 1)
            chunks(cur, bgn)
        c.close()

    def p4(l, xin, r_xin):
        c = Ctx(S)
        woutb, r_woutb = c.sb([128, 8, D], BF16, "woutb")
        wst = c.ring(2, [128, 4, D], F32, name="wst")
        for hf in range(2):
            st, r_st = wst.next()
            ld(st[:], dr["w_out"][l, hf * 512:(hf + 1) * 512, :].rearrange("(kc p) n -> p kc n", p=128), (), [r_st])
            (V if hf == 0 else GP)(lambda e, st=st, hf=hf: e.tensor_copy(out=woutb[:, hf * 4:(hf + 1) * 4, :], in_=st[:]), [r_st], [r_woutb])
        wrs, r_wrs = c.sb([128, 8, 36], F32, "wrs")
        wrb, r_wrb = c.sb([128, 8, 36], BF16, "wrb")
        rbb, r_rbb = c.sb([128, 36], F32, "rbb")
        S.dma("sp", lambda e: e.dma_start(out=wrs[:], in_=dr["router_w"][l].rearrange("(kc p) n -> p kc n", p=128),
                                          allow_slow_non_contiguous=True), (), [r_wrs])
        V(lambda e: e.tensor_copy(out=wrb[:], in_=wrs[:]), [r_wrs], [r_wrb])
        ld(rbb[:], dr["router_b"][l:l + 1, :].partition_broadcast(128), (), [r_rbb], "sp")
        catr = c.ring(4, [128, D], BF16, name="cat")
        catTr = c.ring(3, [128, 8, 128], BF16, name="catT")
        xr = c.ring(4, [128, D], F32, name="x")
        x1r = c.ring(3, [128, D], F32, name="x1")
        h2r = c.ring(5, [128, D], BF16, name="h2")
        h2fr = c.ring(2, [128, D], F32, name="h2f")
        h2Tr = c.ring(3, [128, 8, 128], BF16, name="h2T")
        junk, r_junk = c.sb([128, D], BF16, "junk")
        ss, r_ss = c.sb([128, NT], F32, "ss")
        rstd, r_rstd = c.sb([128, NT], F32, "rstd")
        ptr = c.ring(2, [128, 8, 128], BF16, psum=True, name="ptr")
        pacc = c.ring(4, [128, 512], F32, psum=True, name="pacc")
        plg = c.ring(2, [128, 36], F32, psum=True, name="plg")
        V(lambda e: e.memset(ss[:], 0.0), (), [r_ss])
        st_ = {}

        def loads(t):
            ct, r_ct = catr.next()
            ld(ct[:], dr["CAT"][t * 128:(t + 1) * 128, :], [rs["CAT"]], [r_ct])
            xt, r_xt = xr.next()
            ld(xt[:], xin[t * 128:(t + 1) * 128, :], [r_xin], [r_xt])
            st_[t] = dict(ct=(ct, r_ct), xt=(xt, r_xt))

        def transposes(ta, td):
            jobs = []
            if ta is not None:
                pt, r_pt = ptr.next()
                ct, r_ct = st_[ta]["ct"]
                jobs.append([(lambda kc=kc, pt=pt, ct=ct, r_ct=r_ct, r_pt=r_pt: PE(
                    lambda e: e.transpose(out=pt[:, kc, :], in_=ct[:, kc * 128:(kc + 1) * 128], identity=ident[:]), [r_ct, r_ident], [r_pt]))
                    for kc in range(8)])
            if td is not None:
                pt2, r_pt2 = ptr.next()
                h2, r_h2 = st_[td]["h2"]
                jobs.append([(lambda kc=kc, pt2=pt2, h2=h2, r_h2=r_h2, r_pt2=r_pt2: PE(
                    lambda e: e.transpose(out=pt2[:, kc, :], in_=h2[:, kc * 128:(kc + 1) * 128], identity=ident[:]), [r_h2, r_ident], [r_pt2]))
                    for kc in range(8)])
            n = max(len(j) for j in jobs)
            for i in range(n):
                for j in jobs:
                    j[i]()
            if ta is not None:
                cT, r_cT = catTr.next()
                A(lambda e, cT=cT, pt=pt: e.activation(out=cT[:], in_=pt[:], func=AF.Copy), [r_pt], [r_cT])
                st_[ta]["cT"] = (cT, r_cT)
            if td is not None:
                hT, r_hT = h2Tr.next()
                A(lambda e, hT=hT, pt2=pt2: e.activation(out=hT[:], in_=pt2[:], func=AF.Copy), [r_pt2], [r_hT])
                st_[td]["hT"] = (hT, r_hT)

        def matmuls(t, tr):
            accs = None
            if t is not None:
                cT, r_cT = st_[t]["cT"]
                accs = [pacc.next(), pacc.next()]
            if tr is not None:
                hT, r_hT = st_[tr]["hT"]
                pl, r_pl = plg.next()
            for kc in range(8):
                if t is not None:
                    for n in range(2):
                        acc, r_acc = accs[n]
                        PE(lambda e, kc=kc, acc=acc, cT=cT, n=n: e.matmul(acc[:, :], lhsT=cT[:, kc, :], rhs=woutb[:, kc, n * 512:(n + 1) * 512],
                                                                         start=(kc == 0), stop=(kc == 7)), [r_cT, r_woutb], [r_acc])
                if tr is not None:
                    PE(lambda e, kc=kc, pl=pl, hT=hT: e.matmul(pl[:, :], lhsT=hT[:, kc, :], rhs=wrb[:, kc, :], start=(kc == 0), stop=(kc == 7)),
                       [r_hT, r_wrb], [r_pl])
            if tr is not None:
                V(lambda e, pl=pl, tr=tr: e.tensor_tensor(out=lgall[:, tr, :], in0=pl[:, :], in1=rbb[:], op=ALU.add), [r_pl, r_rbb], [r_lgall])
                del st_[tr]
            return accs

        def elementwise(t, accs):
            xt, r_xt = st_[t]["xt"]
            x1, r_x1 = x1r.next()
            for n in range(2):
                acc, r_acc = accs[n]
                V(lambda e, acc=acc, x1=x1, n=n: e.tensor_tensor(out=x1[:, n * 512:(n + 1) * 512], in0=acc[:, :], in1=G1b[:, n * 512:(n + 1) * 512],
                                                                op=ALU.mult), [r_acc, r_G1b], [r_x1])
            GP(lambda e, x1=x1, xt=xt: e.tensor_tensor(out=x1[:], in0=x1[:], in1=xt[:], op=ALU.add), [r_x1, r_xt], [r_x1])
            ld(dr["Xs"][t * 128:(t + 1) * 128, :], x1[:], [r_x1], [rs["Xs"]])
            A(lambda e, x1=x1, t=t: e.activation(out=junk[:], in_=x1[:], func=AF.Square, accum_out=ss[:, t:t + 1]), [r_x1], [r_junk, r_ss])
            rstd_from_ss(ss[:, t:t + 1], rstd[:, t:t + 1], D, r_ss, r_rstd)
            h2f_, r_h2f_ = h2fr.next()
            V(lambda e, x1=x1, t=t, h2f_=h2f_: e.scalar_tensor_tensor(out=h2f_[:], in0=x1[:], scalar=rstd[:, t:t + 1], in1=A2b[:], op0=ALU.mult, op1=ALU.mult),
              [r_x1, r_rstd, r_A2b], [r_h2f_])
            h2, r_h2 = h2r.next()
            GP(lambda e, h2=h2, h2f_=h2f_: e.tensor_tensor(out=h2[:], in0=h2f_[:], in1=S2b[:], op=ALU.add), [r_h2f_, r_S2b], [r_h2])
            ld(dr["H2"][t * 128:(t + 1) * 128, :], h2[:], [r_h2], [rs["H2"]])
            st_[t]["h2"] = (h2, r_h2)

        loads(0)
        loads(1)
        transposes(0, None)
        for t in range(NT + 3):
            if t + 2 < NT:
                loads(t + 2)
            ta = t + 1 if t + 1 < NT else None
            td = t - 2 if 0 <= t - 2 < NT else None
            if ta is not None or td is not None:
                transposes(ta, td)
            tm_ = t if t < NT else None
            trr = t - 3 if 0 <= t - 3 < NT else None
            if tm_ is not None or trr is not None:
                accs = matmuls(tm_, trr)
            if tm_ is not None:
                elementwise(tm_, accs)
        c.close()

    def p4b(l):
        c = Ctx(S)
        N3 = [128, NT, NE]
        gmax, r_gmax = c.sb([128, NT], F32)
        g4, r_g4 = c.sb([128, NT, 4], F32)
        goh, r_goh = c.sb([128, NT, 4], F32)
        gsum, r_gsum = c.sb([128, NT], F32)
        em, r_em = c.sb(N3, F32)
        oh1, r_oh1 = c.sb(N3, F32)
        oh2, r_oh2 = c.sb(N3, F32)
        m1, r_m1 = c.sb([128, NT], F32)
        m2, r_m2 = c.sb([128, NT], F32)
        p1, r_p1 = c.sb([128, NT], F32)
        abf, r_abf = c.sb([128, NT * NE], BF16)
        csb, r_csb = c.sb(N3, F32)
        offs, r_offs = c.sb(N3, F32)
        pos, r_pos = c.sb(N3, F32)
        sf, r_sf = c.sb([128, NT], F32)
        cnt, r_cnt = c.sb([128, NE], F32)
        nblk, r_nblk = c.sb([128, NE], F32)
        pendb, r_pendb = c.sb([128, NE], F32)
        pst, r_pst = c.sb([128, NE], F32)
        onesne, r_onesne = c.sb([128, NE], F32)
        bex, r_bex = c.sb([128, NBLK], F32)
        cmp, r_cmp = c.sb([128, NBLK * NE], F32)
        pp = c.ring(1, [128, NT * NE], F32, psum=True)
        pc = c.ring(1, [128, NT * NE], F32, psum=True)
        lgG = lgall[:, :, 0:4]
        lgE = lgall[:, :, 4:36]

        def bc(ap2, n):
            return ap2.unsqueeze(2).to_broadcast([128, NT, n])
        V(lambda e: e.tensor_reduce(out=gmax[:], in_=lgG, axis=AX.X, op=ALU.max), [r_lgall], [r_gmax])
        V(lambda e: e.tensor_tensor(out=goh[:], in0=lgG, in1=bc(gmax[:], 4), op=ALU.is_equal), [r_lgall, r_gmax], [r_goh])
        V(lambda e: e.tensor_tensor(out=g4[:], in0=lgG, in1=bc(gmax[:], 4), op=ALU.subtract), [r_lgall, r_gmax], [r_g4])
        A(lambda e: e.activation(out=g4[:], in_=g4[:], func=AF.Exp), [r_g4], [r_g4])
        V(lambda e: e.tensor_reduce(out=gsum[:], in_=g4[:], axis=AX.X, op=ALU.add), [r_g4], [r_gsum])
        V(lambda e: e.reciprocal(out=gsum[:], in_=gsum[:]), [r_gsum], [r_gsum])
        V(lambda e: e.tensor_scalar(out=goh[:], in0=goh[:], scalar1=BIG, scalar2=-BIG, op0=ALU.mult, op1=ALU.add), [r_goh], [r_goh])
        V(lambda e: e.tensor_tensor(out=em[:].rearrange("k t (g x) -> k t g x", g=4), in0=lgE.rearrange("k t (g x) -> k t g x", g=4),
                                    in1=goh[:].unsqueeze(3).to_broadcast([128, NT, 4, 8]), op=ALU.add), [r_lgall, r_goh], [r_em])
        V(lambda e: e.tensor_reduce(out=m1[:], in_=em[:], axis=AX.X, op=ALU.max), [r_em], [r_m1])
        V(lambda e: e.tensor_tensor(out=oh1[:], in0=em[:], in1=bc(m1[:], NE), op=ALU.is_equal), [r_em, r_m1], [r_oh1])
        V(lambda e: e.scalar_tensor_tensor(out=em[:], in0=oh1[:], scalar=-BIG, in1=em[:], op0=ALU.mult, op1=ALU.add), [r_oh1, r_em], [r_em])
        V(lambda e: e.tensor_reduce(out=m2[:], in_=em[:], axis=AX.X, op=ALU.max), [r_em], [r_m2])
        V(lambda e: e.tensor_tensor(out=oh2[:], in0=em[:], in1=bc(m2[:], NE), op=ALU.is_equal), [r_em, r_m2], [r_oh2])
        V(lambda e: e.tensor_tensor(out=m2[:], in0=m2[:], in1=m1[:], op=ALU.subtract), [r_m1, r_m2], [r_m2])
        A(lambda e: e.activation(out=m2[:], in_=m2[:], func=AF.Exp), [r_m2], [r_m2])
        V(lambda e: e.tensor_scalar(out=p1[:], in0=m2[:], scalar1=1.0, scalar2=None, op0=ALU.add), [r_m2], [r_p1])
        V(lambda e: e.reciprocal(out=p1[:], in_=p1[:]), [r_p1], [r_p1])
        V(lambda e: e.tensor_tensor(out=gt1[:], in0=p1[:], in1=gsum[:], op=ALU.mult), [r_p1, r_gsum], [r_gt1])
        V(lambda e: e.tensor_tensor(out=m2[:], in0=m2[:], in1=gt1[:], op=ALU.mult), [r_m2, r_gt1], [r_m2])
        V(lambda e: e.tensor_copy(out=gt2[:], in_=m2[:]), [r_m2], [r_gt2])
        V(lambda e: e.tensor_tensor(out=abf[:].rearrange("k (t x) -> k t x", x=NE), in0=oh1[:], in1=oh2[:], op=ALU.add), [r_oh1, r_oh2], [r_abf])
        ppt, r_pp = pp.next()
        pct, r_pc = pc.next()
        for ch in range(4):
            PE(lambda e, ch=ch: e.matmul(ppt[:, ch * 512:(ch + 1) * 512], lhsT=ustrict[:], rhs=abf[:, ch * 512:(ch + 1) * 512], start=True, stop=True),
               [r_ustrict, r_abf], [r_pp])
            PE(lambda e, ch=ch: e.matmul(pct[:, ch * 512:(ch + 1) * 512], lhsT=onesb[:], rhs=abf[:, ch * 512:(ch + 1) * 512], start=True, stop=True),
               [r_onesb, r_abf], [r_pc])
        V(lambda e: e.tensor_copy(out=csb[:].rearrange("k t x -> k (t x)"), in_=pct[:]), [r_pc], [r_csb])
        V(lambda e: e.memset(offs[:, 0, :], 0.0), (), [r_offs])
        for j in range(1, NT):
            V(lambda e, j=j: e.tensor_tensor(out=offs[:, j, :], in0=offs[:, j - 1, :], in1=csb[:, j - 1, :], op=ALU.add), [r_offs, r_csb], [r_offs])
        V(lambda e: e.tensor_tensor(out=cnt[:], in0=offs[:, NT - 1, :], in1=csb[:, NT - 1, :], op=ALU.add), [r_offs, r_csb], [r_cnt])
        V(lambda e: e.tensor_tensor(out=cmp[:, 0:NE * 64].rearrange("k (x m) -> k x m", m=64), in0=cnt[:].unsqueeze(2).to_broadcast([128, NE, 64]),
                                    in1=mulrow[:].unsqueeze(1).to_broadcast([128, NE, 64]), op=ALU.is_gt), [r_cnt, r_mulrow], [r_cmp])
        V(lambda e: e.tensor_reduce(out=nblk[:], in_=cmp[:, 0:NE * 64].rearrange("k (x m) -> k x m", m=64), axis=AX.X, op=ALU.add), [r_cmp], [r_nblk])
        V(lambda e: e.memset(onesne[:], 1.0), (), [r_onesne])
        V(lambda e: e.tensor_tensor_scan(out=pendb[:], data0=onesne[:], data1=nblk[:], initial=0.0, op0=ALU.mult, op1=ALU.add),
          [r_onesne, r_nblk], [r_pendb])
        V(lambda e: e.tensor_tensor(out=pst[:], in0=pendb[:], in1=nblk[:], op=ALU.subtract), [r_pendb, r_nblk], [r_pst])
        V(lambda e: e.tensor_scalar(out=pst[:], in0=pst[:], scalar1=float(RB), scalar2=None, op0=ALU.mult), [r_pst], [r_pst])
        V(lambda e: e.tensor_tensor(out=cmp[:, 0:NBLK * NE].rearrange("k (b x) -> k b x", x=NE), in0=pendb[:].unsqueeze(1).to_broadcast([128, NBLK, NE]),
                                    in1=brow[:].unsqueeze(2).to_broadcast([128, NBLK, NE]), op=ALU.is_le), [r_pendb, r_brow], [r_cmp])
        V(lambda e: e.tensor_reduce(out=bex[:], in_=cmp[:, 0:NBLK * NE].rearrange("k (b x) -> k b x", x=NE), axis=AX.X, op=ALU.add), [r_cmp], [r_bex])
        V(lambda e: e.tensor_scalar(out=bex[:], in0=bex[:], scalar1=float(NE - 1), scalar2=float(128), op0=ALU.min, op1=ALU.mult), [r_bex], [r_bex])
        V(lambda e: e.tensor_scalar(out=bex[:], in0=bex[:], scalar1=piota[:, 0:1], scalar2=float(l * NE * 128), op0=ALU.add, op1=ALU.add),
          [r_bex, r_piota], [r_bex])
        V(lambda e: e.tensor_copy(out=widx[:], in_=bex[:]), [r_bex], [r_widx])
        V(lambda e: e.tensor_tensor(out=offs[:], in0=offs[:], in1=pst[:].unsqueeze(1).to_broadcast([128, NT, NE]), op=ALU.add), [r_offs, r_pst], [r_offs])
        V(lambda e: e.tensor_tensor(out=pos[:].rearrange("k t x -> k (t x)"), in0=ppt[:], in1=offs[:].rearrange("k t x -> k (t x)"), op=ALU.add),
          [r_pp, r_offs], [r_pos])
        for (oh, r_oh, sl, r_sl) in ((oh1, r_oh1, slot1, r_slot1), (oh2, r_oh2, slot2, r_slot2)):
            V(lambda e, oh=oh: e.tensor_tensor(out=oh[:], in0=oh[:], in1=pos[:], op=ALU.mult), [r_oh, r_pos], [r_oh])
            V(lambda e, oh=oh: e.tensor_reduce(out=sf[:], in_=oh[:], axis=AX.X, op=ALU.add), [r_oh], [r_sf])
            V(lambda e: e.tensor_scalar(out=sf[:], in0=sf[:], scalar1=float(NROWS - 1), scalar2=0.0, op0=ALU.min, op1=ALU.max), [r_sf], [r_sf])
            V(lambda e, sl=sl: e.tensor_copy(out=sl[:], in_=sf[:]), [r_sf], [r_sl])
        h2r = c.ring(3, [128, D], BF16, name="h2d")
        for t in range(NT):
            h2, r_h2 = h2r.next()
            ld(h2[:], dr["H2"][t * 128:(t + 1) * 128, :], [rs["H2"]], [r_h2])
            for (sl, r_sl) in ((slot1, r_slot1), (slot2, r_slot2)):
                S.dma("pool", lambda e, h2=h2, sl=sl, t=t: e.indirect_dma_start(
                    out=dr["XS"], out_offset=bass.IndirectOffsetOnAxis(ap=sl[:, t:t + 1], axis=0), in_=h2[:], in_offset=None),
                    [r_h2, r_sl], [rs["XS"]])
        c.close()

    def p5(l):
        c = Ctx(S)
        wsr = c.ring(6, [128, 2048], F32, name="wsr")
        wbr = [c.ring(2, [128, 4096], BF16, name=f"wb{i}") for i in range(3)]
        xrr = c.ring(4, [128, D], BF16, name="xr")
        xTr = c.ring(2, [128, 8, RB], BF16, name="xT")
        actr = c.ring(2, [128, 4, RB], BF16, name="actT")
        silr = c.ring(2, [128, RB], F32, name="sil")
        ysr = c.ring(3, [128, D], BF16, name="ys")
        ptr = c.ring(2, [128, 8, 128], BF16, psum=True, name="ptr")
        pacc = c.ring(6, [128, 512], F32, psum=True, name="pacc")
        wsrc = ((dr["moe_w1_0"], dr["moe_w1_1"]), (dr["moe_w3_0"], dr["moe_w3_1"]), (dr["moe_w2_0"], dr["moe_w2_1"]))
        k = [0]

        def wload(b):
            wb = []
            for i in range(3):
                wt, r_wt = wbr[i].next()
                for hf in range(2):
                    st, r_st = wsr.next()
                    S.dma("pool", lambda e, st=st, i=i, hf=hf: e.indirect_dma_start(
                        out=st[:], out_offset=None, in_=wsrc[i][hf],
                        in_offset=bass.IndirectOffsetOnAxis(ap=widx[:, b:b + 1], axis=0)), [r_widx], [r_st])
                    k[0] += 1
                    dst = wt[:, hf * 2048:(hf + 1) * 2048]
                    if k[0] % 2 == 0:
                        V(lambda e, st=st, dst=dst: e.tensor_copy(out=dst, in_=st[:]), [r_st], [r_wt])
                    else:
                        A(lambda e, st=st, dst=dst: e.activation(out=dst, in_=st[:], func=AF.Copy), [r_st], [r_wt])
                wb.append((wt, r_wt))
            return wb

        def xprep(b):
            xT, r_xT = xTr.next()
            tl = []
            for rt in range(RB // 128):
                xr_, r_xr = xrr.next()
                r0 = b * RB + rt * 128
                ld(xr_[:], dr["XS"][r0:r0 + 128, :], [rs["XS"]], [r_xr])
                tl.append((xr_, r_xr, ptr.next()))
            for kc in range(8):
                for (xr_, r_xr, (pt, r_pt)) in tl:
                    PE(lambda e, kc=kc, pt=pt, xr_=xr_: e.transpose(out=pt[:, kc, :], in_=xr_[:, kc * 128:(kc + 1) * 128], identity=ident[:]),
                       [r_xr, r_ident], [r_pt])
            for rt, (xr_, r_xr, (pt, r_pt)) in enumerate(tl):
                if rt == 0:
                    V(lambda e, xT=xT, pt=pt, rt=rt: e.tensor_copy(out=xT[:, :, rt * 128:(rt + 1) * 128], in_=pt[:]), [r_pt], [r_xT])
                else:
                    A(lambda e, xT=xT, pt=pt, rt=rt: e.activation(out=xT[:, :, rt * 128:(rt + 1) * 128], in_=pt[:], func=AF.Copy), [r_pt], [r_xT])
            return xT, r_xT

        Wn = wload(0)
        Xn = xprep(0)
        for b in range(NBLK):
            (w1b, r_w1b), (w3b, r_w3b), (w2b, r_w2b) = Wn
            xT, r_xT = Xn
            if b + 1 < NBLK:
                Wn = wload(b + 1)
            aT, r_aT = actr.next()
            for mp in range(2):
                accs = []
                for mi in range(2):
                    accs.append((pacc.next(), pacc.next()))
                for kc in range(8):
                    for mi in range(2):
                        mc = 2 * mp + mi
                        (a1, r_a1), (a3, r_a3) = accs[mi]
                        PE(lambda e, kc=kc, mc=mc, a1=a1, w1b=w1b, xT=xT: e.matmul(a1[:, 0:RB], lhsT=w1b[:, kc * 512 + mc * 128:kc * 512 + (mc + 1) * 128],
                                                                                  rhs=xT[:, kc, :], start=(kc == 0), stop=(kc == 7)), [r_w1b, r_xT], [r_a1])
                        PE(lambda e, kc=kc, mc=mc, a3=a3, w3b=w3b, xT=xT: e.matmul(a3[:, 0:RB], lhsT=w3b[:, kc * 512 + mc * 128:kc * 512 + (mc + 1) * 128],
                                                                                  rhs=xT[:, kc, :], start=(kc == 0), stop=(kc == 7)), [r_w3b, r_xT], [r_a3])
                for mi in range(2):
                    mc = 2 * mp + mi
                    (a1, r_a1), (a3, r_a3) = accs[mi]
                    sl, r_sl = silr.next()
                    A(lambda e, sl=sl, a1=a1: e.activation(out=sl[:], in_=a1[:, 0:RB], func=AF.Silu), [r_a1], [r_sl])
                    V(lambda e, sl=sl, a3=a3, mc=mc, aT=aT: e.tensor_tensor(out=aT[:, mc, :], in0=sl[:], in1=a3[:, 0:RB], op=ALU.mult), [r_sl, r_a3], [r_aT])
            if b + 1 < NBLK:
                Xn = xprep(b + 1)
            yaccs = [[pacc.next() for nch in range(2)] for rt in range(RB // 128)]
            for mc in range(4):
                for rt in range(RB // 128):
                    for nch in range(2):
                        acc, r_acc = yaccs[rt][nch]
                        PE(lambda e, mc=mc, acc=acc, rt=rt, nch=nch, aT=aT, w2b=w2b: e.matmul(
                            acc[:, :], lhsT=aT[:, mc, rt * 128:(rt + 1) * 128], rhs=w2b[:, mc * 1024 + nch * 512:mc * 1024 + (nch + 1) * 512],
                            start=(mc == 0), stop=(mc == 3)), [r_aT, r_w2b], [r_acc])
            for rt in range(RB // 128):
                ys, r_ys = ysr.next()
                for nch in range(2):
                    acc, r_acc = yaccs[rt][nch]
                    if nch == 0:
                        A(lambda e, ys=ys, acc=acc: e.activation(out=ys[:, 0:512], in_=acc[:, :], func=AF.Copy), [r_acc], [r_ys])
                    else:
                        V(lambda e, ys=ys, acc=acc: e.tensor_copy(out=ys[:, 512:1024], in_=acc[:, :]), [r_acc], [r_ys])
                r0 = b * RB + rt * 128
                ld(dr["YS"][r0:r0 + 128, :], ys[:], [r_ys], [rs["YS"]])
        c.close()

    def p6(l):
        c = Ctx(S)
        xr = c.ring(4, [128, D], F32, name="x1")
        y1r = c.ring(4, [128, D], BF16, name="y1")
        y2r = c.ring(4, [128, D], BF16, name="y2")
        yfr = c.ring(2, [128, D], F32, name="yf")
        ygr = c.ring(2, [128, D], F32, name="yg")
        st_ = {}

        def loads(t):
            xt, r_xt = xr.next()
            ld(xt[:], dr["Xs"][t * 128:(t + 1) * 128, :], [rs["Xs"]], [r_xt])
            y1, r_y1 = y1r.next()
            y2, r_y2 = y2r.next()
            for (yy, r_yy, sl, r_sl) in ((y1, r_y1, slot1, r_slot1), (y2, r_y2, slot2, r_slot2)):
                S.dma("pool", lambda e, yy=yy, sl=sl, t=t: e.indirect_dma_start(
                    out=yy[:], out_offset=None, in_=dr["YS"], in_offset=bass.IndirectOffsetOnAxis(ap=sl[:, t:t + 1], axis=0)),
                    [rs["YS"], r_sl], [r_yy])
            st_[t] = (xt, r_xt, y1, r_y1, y2, r_y2)
        PF = 3
        for t in range(min(PF, NT)):
            loads(t)
        for t in range(NT):
            xt, r_xt, y1, r_y1, y2, r_y2 = st_.pop(t)
            yf, r_yf = yfr.next()
            yg, r_yg = ygr.next()
            A(lambda e, y1=y1, yf=yf, t=t: e.activation(out=yf[:], in_=y1[:], func=AF.Copy, scale=gt1[:, t:t + 1]), [r_y1, r_gt1], [r_yf])
            V(lambda e, yf=yf, yg=yg, y2=y2, t=t: e.scalar_tensor_tensor(out=yg[:], in0=y2[:], scalar=gt2[:, t:t + 1], in1=yf[:], op0=ALU.mult, op1=ALU.add),
              [r_yf, r_y2, r_gt2], [r_yg])
            V(lambda e, yg=yg: e.tensor_tensor(out=yg[:], in0=yg[:], in1=G2b[:], op=ALU.mult), [r_yg, r_G2b], [r_yg])
            V(lambda e, yg=yg, xt=xt: e.tensor_tensor(out=xt[:], in0=yg[:], in1=xt[:], op=ALU.add), [r_yg, r_xt], [r_xt])
            ld(dr["Xs"][t * 128:(t + 1) * 128, :], xt[:], [r_xt], [rs["Xs"]])
            if t + PF < NT:
                loads(t + PF)
        c.close()

    def pfinal():
        c = Ctx(S)
        fgb, r_fgb = c.sb([128, D], F32, "fgb")
        ld(fgb[:], dr["final_g"].partition_broadcast(128), (), [r_fgb], "sp")
        xr = c.ring(3, [128, D], F32, name="xf")
        junk, r_junk = c.sb([128, D], BF16, "junk")
        ss, r_ss = c.sb([128, NT], F32, "ss")
        rstd, r_rstd = c.sb([128, NT], F32, "rstd")
        V(lambda e: e.memset(ss[:], 0.0), (), [r_ss])
        for t in range(NT):
            xt, r_xt = xr.next()
            ld(xt[:], dr["Xs"][t * 128:(t + 1) * 128, :], [rs["Xs"]], [r_xt])
            A(lambda e, xt=xt, t=t: e.activation(out=junk[:], in_=xt[:], func=AF.Square, accum_out=ss[:, t:t + 1]), [r_xt], [r_junk, r_ss])
            rstd_from_ss(ss[:, t:t + 1], rstd[:, t:t + 1], D, r_ss, r_rstd)
            V(lambda e, xt=xt, t=t: e.scalar_tensor_tensor(out=xt[:], in0=xt[:], scalar=rstd[:, t:t + 1], in1=fgb[:], op0=ALU.mult, op1=ALU.mult),
              [r_xt, r_rstd, r_fgb], [r_xt])
            ld(out[t * 128:(t + 1) * 128, :], xt[:], [r_xt], [rs["out"]])
        c.close()


    def finish():
        toks = []
        for r in rs.values():
            toks += r.ws
        S.wait_all("sp", toks)
        S.flush()
        G.close()
        S.es.close()

    setup()
    xin, r_xin = dr["x"], rs["x"]
    done = False
    for l in range(depth):
        for (nm, fn) in (("p0", lambda: p0(l)), ("p1", lambda: p1(l, xin, r_xin)), ("p2", lambda: p2(l)), ("p2b", lambda: p2b(l)),
                         ("p3", lambda: p3(l)), ("p4", lambda: p4(l, xin, r_xin)), ("p4b", lambda: p4b(l)), ("p5", lambda: p5(l)),
                         ("p6", lambda: p6(l))):
            fn()
            if stop == (nm, l):
                done = True
                break
        if done:
            break
        xin, r_xin = dr["Xs"], rs["Xs"]
    if not done and final:
        pfinal()
    finish()
    return nc


def make_inputs(inputs, b):
    f = lambda a: np.ascontiguousarray(np.asarray(a, dtype=np.float32))
    m = {
        "x": f(inputs["x"][b]), "c": f(inputs["c"][b:b + 1]), "w_ada": f(inputs["w_ada"]), "b_ada": f(inputs["b_ada"]),
        "norm1_g": f(inputs["norm1_g"]), "w_in": f(inputs["w_in"]), "attn_norm_g": f(inputs["attn_norm_g"]),
        "hgrn_lb_logits": f(inputs["hgrn_lb_logits"]), "hgrn_norm_g": f(inputs["hgrn_norm_g"]), "w_out": f(inputs["w_out"]),
        "norm2_g": f(inputs["norm2_g"]),
        "router_w": f(np.concatenate([np.asarray(inputs["router_group_w"]), np.asarray(inputs["router_expert_w"])], axis=-1)),
        "router_b": f(np.concatenate([np.asarray(inputs["router_group_b"]), np.asarray(inputs["router_expert_b"])], axis=-1)),
        "final_g": f(np.asarray(inputs["final_g"]).reshape(1, D)),
    }
    for nm, kc, n in (("moe_w1", 8, 512), ("moe_w3", 8, 512), ("moe_w2", 4, D)):
        w = np.asarray(inputs[nm], dtype=np.float32).reshape(DEPTH, NE, kc, 128, n).transpose(0, 1, 3, 2, 4).reshape(DEPTH * NE * 128, kc * n)
        m[nm + "_0"] = np.ascontiguousarray(w[:, :2048])
        m[nm + "_1"] = np.ascontiguousarray(w[:, 2048:])
    m.update(host_consts())
    return m


def kernel(**inputs):
    nc = build()
    in_maps = [make_inputs(inputs, b) for b in range(2)]
    res = run_bass_kernel_spmd(nc, in_maps, core_ids=[0, 1])
    return np.stack([np.asarray(res.results[b]["out"], dtype=np.float32) for b in range(2)], axis=0)
```
